# Optimizing a Trainium2 kernel written in Bass

```python
import math
import jax, jax.numpy as jnp
from jax import lax
import numpy as np

D_MODEL = 1024
BATCH = 8
SEQ = 2048
DEPTH = 2

POOL_WINDOWS = (2, 4, 8, 16)
POOL_WIDTH = D_MODEL // 2
POOL_GROUP = POOL_WIDTH // len(POOL_WINDOWS)
ATTN_HEADS = 16
ATTN_HEAD_DIM = D_MODEL // ATTN_HEADS
ATTN_WIDTH = ATTN_HEADS * ATTN_HEAD_DIM
IDX_HEADS = 8
IDX_HEAD_DIM = 64
TOPK_MAX = 256
TOPK_DIVISOR = 4
Q_BLOCK = 128
REL_BUCKETS = 32
REL_MAX_DISTANCE = 128
S5_WIDTH = D_MODEL // 2
S5_GROUP = 16
S5_GROUPS = S5_WIDTH // S5_GROUP
S5_STATE = 64
DT_MIN = 1e-3
DT_MAX = 1e-1
N_BRANCHES = 3
FFN_HIDDEN = ((8 * D_MODEL // 3 + 255) // 256) * 256
NORM_EPS = 1e-6

IN_SIZES = (POOL_WIDTH, ATTN_WIDTH, ATTN_HEAD_DIM, ATTN_HEAD_DIM, IDX_HEADS * IDX_HEAD_DIM, IDX_HEAD_DIM, IDX_HEADS, S5_WIDTH, N_BRANCHES * D_MODEL)
IN_WIDTH = POOL_WIDTH + ATTN_WIDTH + 2 * ATTN_HEAD_DIM + IDX_HEADS * IDX_HEAD_DIM + IDX_HEAD_DIM + IDX_HEADS + S5_WIDTH + N_BRANCHES * D_MODEL

kernel_name = 'hybrid_gated_pool_dsa_s5_block'


def rms_norm(x, g):
    xf = x.astype(jnp.float32)
    y = xf * lax.rsqrt(jnp.mean(xf * xf, axis=-1, keepdims=True) + NORM_EPS)
    return (y * g.astype(jnp.float32)).astype(x.dtype)


def split_columns(z):
    parts = []
    start = 0
    for size in IN_SIZES:
        parts.append(z[..., start:start + size])
        start += size
    return parts


def pool_mixer(u, mix_w, scale):
    b, s, _ = u.shape
    uf = u.astype(jnp.float32)
    csum = jnp.cumsum(uf, axis=1)
    pos = jnp.arange(s, dtype=jnp.float32)[:, None]
    outs = []
    for gi, w in enumerate(POOL_WINDOWS):
        sl = slice(gi * POOL_GROUP, (gi + 1) * POOL_GROUP)
        c = csum[..., sl]
        c_prev = jnp.pad(c, ((0, 0), (w, 0), (0, 0)))[:, :s]
        count = jnp.minimum(pos + 1.0, float(w))
        outs.append((c - c_prev) / count - uf[..., sl])
    d = jnp.stack(outs, axis=2).astype(u.dtype)
    y = jnp.einsum('bsgc,gcd->bsgd', d, mix_w).reshape(b, s, POOL_WIDTH)
    return y * scale


def rel_bucket(dist):
    max_exact = REL_BUCKETS // 2
    d_f = jnp.maximum(dist, 1).astype(jnp.float32)
    large = max_exact + (jnp.log(d_f / max_exact) / math.log(REL_MAX_DISTANCE / max_exact) * (REL_BUCKETS - max_exact)).astype(jnp.int32)
    large = jnp.minimum(large, REL_BUCKETS - 1)
    return jnp.where(dist < max_exact, dist, large)


def sparse_attention(q, k, v, qi, ki, wi, rel_bias):
    b, s = q.shape[0], q.shape[1]
    topk = min(TOPK_MAX, s // TOPK_DIVISOR)
    nb = s // Q_BLOCK
    idx_scale = (IDX_HEADS ** -0.5) * (IDX_HEAD_DIM ** -0.5)
    attn_scale = ATTN_HEAD_DIM ** -0.5
    key_pos = jnp.arange(s, dtype=jnp.int32)
    gather = jax.vmap(lambda table, idx: table[idx])

    def to_blocks(a):
        return jnp.moveaxis(a.reshape((b, nb, Q_BLOCK) + a.shape[2:]), 1, 0)

    def block(args):
        qb, qib, wib, start = args
        q_pos = start + jnp.arange(Q_BLOCK, dtype=jnp.int32)
        causal = key_pos[None, :] <= q_pos[:, None]
        rel = jax.nn.relu(jnp.einsum('bqhd,bsd->bqhs', qib, ki))
        score = jnp.einsum('bqhs,bqh->bqs', rel, wib) * idx_scale
        score = jnp.where(causal[None], score, -jnp.inf)
        _, sel = lax.top_k(score, topk)
        k_sel = gather(k, sel)
        v_sel = gather(v, sel)
        dist = q_pos[None, :, None] - sel
        bias = rel_bias[rel_bucket(jnp.maximum(dist, 0))]
        logits = jnp.einsum('bqhd,bqkd->bqhk', qb, k_sel).astype(jnp.float32) * attn_scale
        logits = logits + jnp.moveaxis(bias, -1, 2).astype(jnp.float32)
        logits = jnp.where((dist >= 0)[:, :, None, :], logits, jnp.finfo(jnp.float32).min)
        p = jax.nn.softmax(logits, axis=-1).astype(v.dtype)
        return jnp.einsum('bqhk,bqkd->bqhd', p, v_sel)

    starts = jnp.arange(nb, dtype=jnp.int32) * Q_BLOCK
    out = lax.map(block, (to_blocks(q), to_blocks(qi), to_blocks(wi), starts))
    return jnp.moveaxis(out, 0, 1).reshape(b, s, ATTN_WIDTH)


def _ssm_combine(earlier, later):
    a1r, a1i, b1r, b1i = earlier
    a2r, a2i, b2r, b2i = later
    ar = a2r * a1r - a2i * a1i
    ai = a2r * a1i + a2i * a1r
    br = a2r * b1r - a2i * b1i + b2r
    bi = a2r * b1i + a2i * b1r + b2i
    return (ar, ai, br, bi)


def s5_mixer(u, lam_re, lam_im, log_dt, b_re, b_im, c_re, c_im, d_skip, glu_w):
    b, s, _ = u.shape
    uf = u.astype(jnp.float32)
    ug = uf.reshape(b, s, S5_GROUPS, S5_GROUP)
    dt = jnp.exp(log_dt.astype(jnp.float32))[:, None]
    lr = lam_re.astype(jnp.float32)
    li = lam_im.astype(jnp.float32)
    mag = jnp.exp(lr * dt)
    a_re = mag * jnp.cos(li * dt)
    a_im = mag * jnp.sin(li * dt)
    den = lr * lr + li * li
    coef_re = ((a_re - 1.0) * lr + a_im * li) / den
    coef_im = (a_im * lr - (a_re - 1.0) * li) / den
    br = b_re.astype(jnp.float32)
    bi = b_im.astype(jnp.float32)
    bbar_re = coef_re[..., None] * br - coef_im[..., None] * bi
    bbar_im = coef_re[..., None] * bi + coef_im[..., None] * br
    bu_re = jnp.einsum('bsgi,gni->bsgn', ug, bbar_re)
    bu_im = jnp.einsum('bsgi,gni->bsgn', ug, bbar_im)
    a_re_t = jnp.broadcast_to(a_re[None, None], (1, s, S5_GROUPS, S5_STATE))
    a_im_t = jnp.broadcast_to(a_im[None, None], (1, s, S5_GROUPS, S5_STATE))
    _, _, x_re, x_im = lax.associative_scan(_ssm_combine, (a_re_t, a_im_t, bu_re, bu_im), axis=1)
    y = jnp.einsum('bsgn,gin->bsgi', x_re, c_re.astype(jnp.float32)) - jnp.einsum('bsgn,gin->bsgi', x_im, c_im.astype(jnp.float32))
    y = y.reshape(b, s, S5_WIDTH) + d_skip.astype(jnp.float32) * uf
    z = jax.nn.gelu(y).astype(u.dtype)
    a, g = jnp.split(z @ glu_w, 2, axis=-1)
    return a * jax.nn.sigmoid(g)


def setup_inputs(seed: int = 0) -> dict:
    key = jax.random.key(seed)
    ks = jax.random.split(key, 24)
    f32 = jnp.float32

    def nrm(k, shape, scale):
        return jax.random.normal(k, shape, f32) * scale

    def gain(k, width):
        return 1.0 + 0.02 * jax.random.normal(k, (DEPTH, width), f32)

    lam_im0 = jnp.pi * jnp.arange(S5_STATE, dtype=f32)
    return {
        'x': nrm(ks[0], (BATCH, SEQ, D_MODEL), 1.0),
        'norm_mix_pre': gain(ks[1], D_MODEL),
        'norm_mix_post': gain(ks[2], D_MODEL),
        'norm_ffn_pre': gain(ks[3], D_MODEL),
        'norm_ffn_post': gain(ks[4], D_MODEL),
        'w_in': nrm(ks[5], (DEPTH, D_MODEL, IN_WIDTH), D_MODEL ** -0.5),
        'pool_mix_w': nrm(ks[6], (DEPTH, len(POOL_WINDOWS), POOL_GROUP, POOL_GROUP), POOL_GROUP ** -0.5),
        'pool_scale': gain(ks[7], POOL_WIDTH),
        'pool_out_w': nrm(ks[8], (DEPTH, POOL_WIDTH, D_MODEL), POOL_WIDTH ** -0.5),
        'attn_out_w': nrm(ks[9], (DEPTH, ATTN_WIDTH, D_MODEL), ATTN_WIDTH ** -0.5),
        'rel_bias': nrm(ks[10], (REL_BUCKETS, ATTN_HEADS), 0.5),
        's5_lambda_re': -0.5 + 0.01 * jax.random.normal(ks[11], (DEPTH, S5_GROUPS, S5_STATE), f32),
        's5_lambda_im': lam_im0[None, None, :] + 0.01 * jax.random.normal(ks[12], (DEPTH, S5_GROUPS, S5_STATE), f32),
        's5_log_dt': jax.random.uniform(ks[13], (DEPTH, S5_GROUPS), f32, math.log(DT_MIN), math.log(DT_MAX)),
        's5_b_re': nrm(ks[14], (DEPTH, S5_GROUPS, S5_STATE, S5_GROUP), (2 * S5_GROUP) ** -0.5),
        's5_b_im': nrm(ks[15], (DEPTH, S5_GROUPS, S5_STATE, S5_GROUP), (2 * S5_GROUP) ** -0.5),
        's5_c_re': nrm(ks[16], (DEPTH, S5_GROUPS, S5_GROUP, S5_STATE), S5_STATE ** -0.5),
        's5_c_im': nrm(ks[17], (DEPTH, S5_GROUPS, S5_GROUP, S5_STATE), S5_STATE ** -0.5),
        's5_d': nrm(ks[18], (DEPTH, S5_WIDTH), 1.0),
        's5_glu_w': nrm(ks[19], (DEPTH, S5_WIDTH, 2 * D_MODEL), S5_WIDTH ** -0.5),
        'w_out': nrm(ks[20], (DEPTH, D_MODEL, D_MODEL), D_MODEL ** -0.5),
        'ffn_w_in': nrm(ks[21], (DEPTH, D_MODEL, 2 * FFN_HIDDEN), D_MODEL ** -0.5),
        'ffn_w_out': nrm(ks[22], (DEPTH, FFN_HIDDEN, D_MODEL), FFN_HIDDEN ** -0.5),
    }


def reference(x, norm_mix_pre, norm_mix_post, norm_ffn_pre, norm_ffn_post, w_in, pool_mix_w, pool_scale, pool_out_w, attn_out_w, rel_bias, s5_lambda_re, s5_lambda_im, s5_log_dt, s5_b_re, s5_b_im, s5_c_re, s5_c_im, s5_d, s5_glu_w, w_out, ffn_w_in, ffn_w_out):
    b, s, _ = x.shape
    for l in range(DEPTH):
        h = rms_norm(x, norm_mix_pre[l])
        z = h @ w_in[l]
        pool_u, q, k, v, qi, ki, wi, s5_u, gates = split_columns(z)
        y_pool = pool_mixer(pool_u, pool_mix_w[l], pool_scale[l]) @ pool_out_w[l]
        y_attn = sparse_attention(q.reshape(b, s, ATTN_HEADS, ATTN_HEAD_DIM), k, v, qi.reshape(b, s, IDX_HEADS, IDX_HEAD_DIM), ki, wi, rel_bias) @ attn_out_w[l]
        y_s5 = s5_mixer(s5_u, s5_lambda_re[l], s5_lambda_im[l], s5_log_dt[l], s5_b_re[l], s5_b_im[l], s5_c_re[l], s5_c_im[l], s5_d[l], s5_glu_w[l])
        g = jax.nn.sigmoid(gates.reshape(b, s, N_BRANCHES, D_MODEL))
        merged = g[:, :, 0] * y_pool + g[:, :, 1] * y_attn + g[:, :, 2] * y_s5
        x = x + rms_norm(merged @ w_out[l], norm_mix_post[l])
        h = rms_norm(x, norm_ffn_pre[l])
        gate, up = jnp.split(h @ ffn_w_in[l], 2, axis=-1)
        f = (jax.nn.silu(gate) * up) @ ffn_w_out[l]
        x = x + rms_norm(f, norm_ffn_post[l])
    return x
```

```python
import contextlib
import math
import numpy as np
import concourse.bass as bass
import concourse.mybir as mybir
from concourse.bass_utils import run_bass_kernel_spmd

F32 = mybir.dt.float32
BF16 = mybir.dt.bfloat16
I32 = mybir.dt.int32
ALU = mybir.AluOpType
AF = mybir.ActivationFunctionType

ENGS = ("pe", "act", "dve", "pool", "sp")


class Op:
    __slots__ = ("eng", "fn", "waits", "signal", "slot", "seq", "dcount")

    def __init__(self, eng, fn):
        self.eng = eng
        self.fn = fn
        self.waits = {}
        self.signal = False
        self.slot = None
        self.seq = 0
        self.dcount = 0


class Prog:
    def __init__(self):
        self.ops = {e: [] for e in ENGS}
        self.last_w = {}
        self.readers = {}
        self.slot_count = {}
        self.pending = {e: {} for e in ENGS}

    def _add_wait(self, op, tok, raw):
        kind, who, val = tok
        if kind == "E":
            if who == op.eng and not raw:
                return
            if who == "pe" and op.eng == "pe":
                return
            self.ops[who][val].signal = True
        k = (kind, who)
        if op.waits.get(k, -1) < val:
            op.waits[k] = val

    def op(self, eng, fn, r=(), w=(), slot=None):
        o = Op(eng, fn)
        o.seq = len(self.ops[eng])
        if slot is not None:
            o.slot = slot
            self.slot_count[slot] = self.slot_count.get(slot, 0) + 1
            o.dcount = self.slot_count[slot]
            tok = ("D", slot, o.dcount)
        else:
            tok = ("E", eng, o.seq)
        for k, v in self.pending[eng].items():
            if o.waits.get(k, -1) < v:
                o.waits[k] = v
        self.pending[eng] = {}
        for k in r:
            lw = self.last_w.get(k)
            if lw is not None:
                self._add_wait(o, lw, True)
        for k in w:
            lw = self.last_w.get(k)
            if lw is not None:
                self._add_wait(o, lw, False)
            for t in self.readers.get(k, ()):
                self._add_wait(o, t, False)
        self.ops[eng].append(o)
        for k in r:
            self.readers.setdefault(k, []).append(tok)
        for k in w:
            self.last_w[k] = tok
            self.readers[k] = []
        return o

    def barrier(self):
        toks = {}
        for e in ENGS:
            for o in reversed(self.ops[e]):
                if o.slot is None:
                    o.signal = True
                    toks[("E", e)] = o.seq
                    break
        for s, c in self.slot_count.items():
            toks[("D", s)] = c
        for e in ENGS:
            for k, v in toks.items():
                if k == ("E", e):
                    continue
                if self.pending[e].get(k, -1) < v:
                    self.pending[e][k] = v
        self.last_w = {}
        self.readers = {}

    def emit(self, nc, final_slots=()):
        with contextlib.ExitStack() as st:
            esem = {e: st.enter_context(nc.semaphore("s_" + e)) for e in ENGS}
            dsem = {s: st.enter_context(nc.semaphore("d_%d" % i)) for i, s in enumerate(self.slot_count)}
            sigcount = {}
            for e in ENGS:
                c = 0
                arr = []
                for o in self.ops[e]:
                    if o.slot is None and o.signal:
                        c += 1
                    arr.append(c)
                sigcount[e] = arr
            block = st.enter_context(nc.Block())
            prog = self

            def make(e):
                def body(eng):
                    waited = {}
                    for o in prog.ops[e]:
                        for (kind, who), val in o.waits.items():
                            if kind == "E":
                                sem = esem[who]
                                v = sigcount[who][val]
                            else:
                                sem = dsem[who]
                                v = 16 * val
                            if waited.get((kind, who), -1) >= v:
                                continue
                            waited[(kind, who)] = v
                            eng.wait_ge(sem, v)
                        ins = o.fn(eng)
                        if o.slot is not None:
                            ins.then_inc(dsem[o.slot], 16)
                        elif o.signal:
                            ins.then_inc(esem[e], 1)
                    if e == "sp":
                        for s in final_slots:
                            eng.wait_ge(dsem[s], 16 * prog.slot_count[s])
                return body

            block.tensor(make("pe"))
            block.scalar(make("act"))
            block.vector(make("dve"))
            block.gpsimd(make("pool"))
            block.sync(make("sp"))


S = 2048
D = 1024
TB = 512
NTB = 4
KC = 8
NL = 2
FH = 2816
NJ = 22
EPS = 1e-6
IDX_SCALE = (8 ** -0.5) * (64 ** -0.5)
NEG = -30000.0
NBIS = 15

C_POOL, C_Q, C_K, C_V, C_QI, C_KI, C_WI, C_S5, C_G = 0, 512, 1536, 1600, 1664, 2176, 2240, 2248, 2760

PF_GMP, PF_GMO, PF_GFP, PF_GFO, PF_PSC = 0, 8, 16, 24, 32
PF_LRS, PF_LIS, PF_LDS = 36, 52, 68
PF_LRX, PF_LIX, PF_LDX, PF_BRX, PF_BIX = 84, 596, 1108, 1620, 2132
NPF = 2644
CF_CMASK, CF_INVC, CF_ONES, CF_PAR = 0, 128, 144, 208
NCF = 212
CB_ID, CB_SEL, CB_BIAS = 0, 128, 640
NCB = 640 + 4096

ARENA = 86 * 1024
RSLOT = 4096
NRING = 3
KV0 = 75 * 1024


def rel_bucket_np(dist):
    dist = np.asarray(dist, np.int32)
    d_f = np.maximum(dist, 1).astype(np.float32)
    large = 16 + (np.log(d_f / np.float32(16)) / np.float32(math.log(128 / 16)) * np.float32(16)).astype(np.int32)
    large = np.minimum(large, 31)
    return np.where(dist < 16, dist, large)


class Builder:
    def __init__(self, nlayers=NL, stop=None, taps=()):
        self.nlayers = nlayers
        self.stop = stop
        self.taps = taps
        self.seg_off = {}
        self.seg_total = 0
        self.ring_i = 0
        self.bank_i = 0
        self.bankset = list(range(8))
        self.tapouts = {}

    def carve(self, off, parts, shape, dt):
        esz = 2 if dt == BF16 else 4
        n = int(np.prod(shape))
        assert off % 4 == 0 and off + n * esz <= ARENA, (off, n, esz)
        ap = self.arena[0:parts, off // 2: off // 2 + n * esz // 2]
        if dt != BF16:
            ap = ap.bitcast(dt)
        if len(shape) == 2:
            return ap.rearrange("p (a b) -> p a b", a=shape[0])
        if len(shape) == 3:
            return ap.rearrange("p (a b c) -> p a b c", a=shape[0], b=shape[1])
        return ap

    def bank(self):
        b = self.bankset[self.bank_i % len(self.bankset)]
        self.bank_i += 1
        return b

    def psb(self, b, parts=128, n=512):
        return self.ps[0:parts, b * 512: b * 512 + n]

    def wload(self, l, name, n):
        assert n <= RSLOT
        if name not in self.seg_off:
            self.seg_off[name] = (self.seg_total, n)
            self.seg_total += n
        off, n0 = self.seg_off[name]
        assert n0 == n
        slot = self.ring_i % NRING
        self.ring_i += 1
        dst = self.ring[:, slot * RSLOT: slot * RSLOT + n]
        src = self.wstream[l, :, off:off + n]
        key = ("ring", slot)
        self.P.op("pool", lambda e: e.dma_start(out=dst, in_=src, max_dma_last_dim=8192), w=[key], slot="ring%d" % slot)
        return dst, key

    def mm(self, out, lhsT, rhs, start, stop, r, w):
        self.P.op("pe", lambda e: e.matmul(out, lhsT, rhs, start=start, stop=stop), r=r, w=w)

    def act(self, out, in_, func, r, w, scale=1.0, bias=0.0):
        self.P.op("act", lambda e: e.activation(out=out, in_=in_, func=func, bias=bias, scale=scale), r=r, w=w)

    def tt(self, out, in0, in1, op, r, w):
        self.P.op("dve", lambda e: e.tensor_tensor(out=out, in0=in0, in1=in1, op=op), r=r, w=w)

    def ts(self, out, in0, s1, s2, op0, op1, r, w, accum_out=None):
        if accum_out is None:
            self.P.op("dve", lambda e: e.tensor_scalar(out=out, in0=in0, scalar1=s1, scalar2=s2, op0=op0, op1=op1), r=r, w=w)
        else:
            self.P.op("dve", lambda e: e.tensor_scalar(out=out, in0=in0, scalar1=s1, scalar2=s2, op0=op0, op1=op1, accum_out=accum_out), r=r, w=w)

    def stt(self, out, in0, scalar, in1, op0, op1, r, w):
        self.P.op("dve", lambda e: e.scalar_tensor_tensor(out=out, in0=in0, scalar=scalar, in1=in1, op0=op0, op1=op1), r=r, w=w)

    def dma(self, eng, out, in_, r, w, slot):
        self.P.op(eng, lambda e: e.dma_start(out=out, in_=in_), r=r, w=w, slot=slot)

    def tap(self, name, ap, keys, shape, parts=128):
        if name not in self.taps:
            return
        t = self.nc.dram_tensor("tap_" + name, [parts] + list(shape), ap.dtype, kind="ExternalOutput").ap()
        self.tapouts[name] = t
        self.P.op("sp", lambda e: e.dma_start(out=t, in_=ap), r=keys, slot="tap_" + name)
        self.final_slots.append("tap_" + name)

    def rms_rstd(self, src_f32, src_keys, sq, tag):
        P = self.P
        for c in range(KC):
            self.act(sq[:, c, :], src_f32[:, c, :], AF.Square, r=[src_keys[c]], w=[("sq", c)])
        b = self.bank()
        for c in range(KC):
            self.mm(self.psb(b), self.ones_bf[:, :], sq[:, c, :], c == 0, c == KC - 1, r=[("sq", c), "ones_bf"], w=[("ps", b)])
        self.act(self.rstd[:, :], self.psb(b), AF.Sqrt, r=[("ps", b), "epsc"], w=["rstd"], scale=1.0 / D, bias=self.epsc[:, 0:1])
        P.op("dve", lambda e: e.reciprocal(out=self.rstd[:, :], in_=self.rstd[:, :]), r=["rstd"], w=["rstd"])

    def build(self):
        nc = bass.Bass("TRN2", target_bir_lowering=False)
        self.nc = nc
        self.final_slots = []
        P = self.P = Prog()
        xin = nc.dram_tensor("x", [128, KC, S], F32, kind="ExternalInput").ap()
        out = nc.dram_tensor("out", [128, KC, S], F32, kind="ExternalOutput").ap()
        xres = nc.dram_tensor("xres", [128, KC, S], F32, kind="Internal").ap()
        self.wstream = nc.dram_tensor("wstream", [NL, 128, self.wtotal], F32, kind="ExternalInput").ap()
        pfd = nc.dram_tensor("pf", [NL, 128, NPF], F32, kind="ExternalInput").ap()
        cfd = nc.dram_tensor("cf", [128, NCF], F32, kind="ExternalInput").ap()
        cbd = nc.dram_tensor("cb", [128, NCB], F32, kind="ExternalInput").ap()
        b31d = nc.dram_tensor("b31", [1, 16 * TB], F32, kind="ExternalInput").ap()
        with contextlib.ExitStack() as st:
            E = st.enter_context
            self.arena = E(nc.sbuf_tensor("arena", [128, ARENA // 2], BF16))
            hT = E(nc.sbuf_tensor("hT", [128, KC, S], BF16))
            merged = E(nc.sbuf_tensor("merged", [128, KC, S], BF16))
            self.ring = E(nc.sbuf_tensor("ring", [128, NRING * RSLOT], BF16))
            pf = E(nc.sbuf_tensor("pfs", [128, NPF], F32))
            self.pf_t = pf
            cf = E(nc.sbuf_tensor("cfs", [128, NCF], F32))
            cb = E(nc.sbuf_tensor("cbs", [128, NCB], BF16))
            self.ones_bf = E(nc.sbuf_tensor("ones_bf", [128, 128], BF16))
            self.rstd = E(nc.sbuf_tensor("rstd", [128, TB], F32))
            self.epsc = E(nc.sbuf_tensor("epsc", [128, 8], F32))
            self.hpi = E(nc.sbuf_tensor("hpi", [128, 8], F32))
            self.ps = E(nc.psum_tensor("ps", [128, 4096], F32))
            ps = self.ps
            ident = cb[:, CB_ID:CB_ID + 128]
            sel4 = cb[:, CB_SEL:CB_SEL + 512].rearrange("p (h q) -> p h q", h=4)
            biasT = cb[:, CB_BIAS:CB_BIAS + 4096].rearrange("p (k h q) -> p k h q", k=2, h=16)
            cmask = cf[:, CF_CMASK:CF_CMASK + 128]
            invc = cf[:, CF_INVC:CF_INVC + 16]
            ones64 = cf[:, CF_ONES:CF_ONES + 64]
            self.cf = cf

            P.op("dve", lambda e: e.memset(self.ones_bf[:, :], 1.0), w=["ones_bf"])
            P.op("dve", lambda e: e.memset(self.epsc[:, :], EPS), w=["epsc"])
            P.op("dve", lambda e: e.memset(self.hpi[:, :], math.pi / 2), w=["hpi"])
            self.dma("sp", cf[:, :], cfd, r=[], w=["cf"], slot="cf")
            P.op("pool", lambda e: e.dma_start(out=cb[:, :], in_=cbd, max_dma_last_dim=8192), w=["cb"], slot="cb")

            for l in range(self.nlayers):
                xsrc = xin if l == 0 else xres
                xdst = out if l == self.nlayers - 1 else xres
                self.layer(l, xsrc, xdst, hT, merged, pf, pfd, ident, sel4, biasT, cmask, invc, ones64, b31d)
                if self.stop is not None:
                    break
            P.barrier()
            P.emit(nc, final_slots=self.final_slots)
        return nc

    def layer(self, l, xsrc, xdst, hT, merged, pf, pfd, ident, sel4, biasT, cmask, invc, ones64, b31d):
        P = self.P
        nc = self.nc
        ps = self.ps
        stop = self.stop
        P.barrier()
        self.bankset = list(range(8))
        self.dma("sp", pf[:, :], pfd[l], r=[], w=["pf"], slot="pf")
        gmp = pf[:, PF_GMP:PF_GMP + 8]
        gmo = pf[:, PF_GMO:PF_GMO + 8]
        gfp = pf[:, PF_GFP:PF_GFP + 8]
        gfo = pf[:, PF_GFO:PF_GFO + 8]
        psc = pf[:, PF_PSC:PF_PSC + 4]

        xblk = self.carve(0, 128, [KC, TB], F32)
        sq = self.carve(16 * 1024, 128, [KC, TB], BF16)
        for n in range(NTB):
            tsl = slice(n * TB, (n + 1) * TB)
            self.dma("sp", xblk[:, :, :], xsrc[:, :, tsl], r=[], w=[("xblk", c) for c in range(KC)], slot="xblk")
            self.rms_rstd(xblk, [("xblk", c) for c in range(KC)], sq, "p1")
            for c in range(KC):
                self.stt(hT[:, c, tsl], xblk[:, c, :], gmp[:, c:c + 1], self.rstd[:, :], ALU.mult, ALU.mult,
                         r=[("xblk", c), "pf", "rstd"], w=[("hT", c, n)])
        self.tap("hT", hT[:, :, :], [("hT", c, n) for c in range(KC) for n in range(NTB)], [KC, S])
        if stop == 1:
            return
        P.barrier()

        kaugT = self.arena[0:65, KV0 // 2: KV0 // 2 + S]
        kiT = self.arena[0:64, KV0 // 2 + S: KV0 // 2 + 2 * S]
        vaug = self.arena[:, KV0 // 2 + 2 * S: KV0 // 2 + 2 * S + 16 * 66].rearrange("p (t d) -> p t d", t=16)
        wi_off = KV0 + 2 * (2 * S + 16 * 66)
        wi_s = self.arena[:, wi_off // 2: wi_off // 2 + 256].bitcast(F32).rearrange("p (t h) -> p t h", t=16)
        assert wi_off + 512 <= ARENA
        uz = self.carve(0, 128, [4, S], BF16)
        upp = [self.carve(16 * 1024, 128, [1, S], F32)[:, 0, :], self.carve(24 * 1024, 128, [1, S], F32)[:, 0, :]]
        dT = self.carve(32 * 1024, 128, [4, S], BF16)
        yp = self.carve(48 * 1024, 128, [4, S], BF16)
        sgt = self.carve(64 * 1024, 128, [1, TB], BF16)[:, 0, :]

        def hkeys(n):
            return [("hT", c, n) for c in range(KC)]

        seg, sk = self.wload(l, "kk", 1024)
        seg = seg.rearrange("p (k m) -> p k m", k=KC)
        P.op("dve", lambda e: e.memset(kaugT[64:65, :], 1.0), w=["kaug1"])
        P.op("dve", lambda e: e.memset(vaug[:, :, 64:65], 1.0), w=["vaug1"])
        for n in range(NTB):
            tsl = slice(n * TB, (n + 1) * TB)
            for half, dst, key in ((0, kaugT, "kT"), (1, kiT, "kiT")):
                b = self.bank()
                for kc in range(KC):
                    self.mm(self.psb(b, 64), seg[:, kc, half * 64:(half + 1) * 64], hT[:, kc, tsl], kc == 0, kc == KC - 1,
                            r=[sk, ("hT", kc, n)], w=[("ps", b)])
                self.act(dst[0:64, tsl], self.psb(b, 64), AF.Copy, r=[("ps", b)], w=[(key, n)])
        seg, sk = self.wload(l, "vw", KC * 72)
        seg = seg.rearrange("p (k m) -> p k m", k=KC)
        for tt_ in range(16):
            n = tt_ // 4
            b = self.bank()
            for kc in range(KC):
                self.mm(ps[:, b * 512: b * 512 + 72], hT[:, kc, tt_ * 128:(tt_ + 1) * 128], seg[:, kc, :], kc == 0, kc == KC - 1,
                        r=[sk, ("hT", kc, n)], w=[("ps", b)])
            self.act(vaug[:, tt_, 0:64], ps[:, b * 512: b * 512 + 64], AF.Copy, r=[("ps", b)], w=[("vaug", tt_)])
            self.act(wi_s[:, tt_, :], ps[:, b * 512 + 64: b * 512 + 72], AF.Copy, r=[("ps", b)], w=[("wi", tt_)], scale=IDX_SCALE)
        for cc in range(4):
            seg, sk = self.wload(l, "su%d" % cc, 1024)
            seg = seg.rearrange("p (k m) -> p k m", k=KC)
            for n in range(NTB):
                tsl = slice(n * TB, (n + 1) * TB)
                b = self.bank()
                for kc in range(KC):
                    self.mm(self.psb(b), seg[:, kc, :], hT[:, kc, tsl], kc == 0, kc == KC - 1, r=[sk, ("hT", kc, n)], w=[("ps", b)])
                self.act(uz[:, cc, tsl], self.psb(b), AF.Copy, r=[("ps", b)], w=[("uz", cc, n)])
        for cc in range(4):
            seg, sk = self.wload(l, "pu%d" % cc, 1024)
            seg = seg.rearrange("p (k m) -> p k m", k=KC)
            u0 = upp[0]
            for n in range(NTB):
                tsl = slice(n * TB, (n + 1) * TB)
                b = self.bank()
                for kc in range(KC):
                    self.mm(self.psb(b), seg[:, kc, :], hT[:, kc, tsl], kc == 0, kc == KC - 1, r=[sk, ("hT", kc, n)], w=[("ps", b)])
                self.act(u0[:, tsl], self.psb(b), AF.Copy, r=[("ps", b)], w=["up0"])
            wlen = 2 ** (cc + 1)
            sA = upp[1]
            sB = self.carve(66 * 1024, 128, [1, S], F32)[:, 0, :]
            bufs = [(sA, "upA"), (sB, "upB")]
            k = 1
            bi = 0
            src, srck = u0, "up0"
            while k < wlen:
                dstb, dstk = bufs[bi % 2]
                self.tt(dstb[:, k:], src[:, k:], src[:, :S - k], ALU.add, r=[srck], w=[dstk])
                P.op("dve", lambda e, d=dstb, s_=src, k=k: e.tensor_copy(out=d[:, 0:k], in_=s_[:, 0:k]), r=[srck], w=[dstk])
                src, srck = dstb, dstk
                bi += 1
                k *= 2
            self.stt(dT[:, cc, :], src[:, :], 1.0 / wlen, u0[:, :], ALU.mult, ALU.subtract, r=[srck, "up0"], w=[("dT", cc)])
            tmpc = self.carve(64 * 1024 + 1024, 128, [1, 16], F32)[:, 0, :]
            self.tt(tmpc[:, 0:wlen - 1], src[:, 0:wlen - 1], invc[:, 0:wlen - 1], ALU.mult, r=[srck, "cf"], w=["tmpc"])
            self.tt(dT[:, cc, 0:wlen - 1], tmpc[:, 0:wlen - 1], u0[:, 0:wlen - 1], ALU.subtract, r=["tmpc", "up0", ("dT", cc)], w=[("dT", cc)])
        self.tap("dT", dT[:, :, :], [("dT", cc) for cc in range(4)], [4, S])
        self.tap("kT", kaugT[:, :], [("kT", n) for n in range(NTB)] + ["kaug1"], [S], parts=65)
        self.tap("vaug", vaug[:, :, :], [("vaug", t) for t in range(16)] + ["vaug1"], [16, 66])
        self.tap("wi", wi_s[:, :, :], [("wi", t) for t in range(16)], [16, 8])
        if stop == 2:
            return

        seg, sk = self.wload(l, "mix", 512)
        seg = seg.rearrange("p (g m) -> p g m", g=4)
        for cc in range(4):
            for n in range(NTB):
                tsl = slice(n * TB, (n + 1) * TB)
                b = self.bank()
                self.mm(self.psb(b), seg[:, cc, :], dT[:, cc, tsl], True, True, r=[sk, ("dT", cc)], w=[("ps", b)])
                self.act(yp[:, cc, tsl], self.psb(b), AF.Copy, r=[("ps", b), "pf"], w=[("yp", cc, n)], scale=psc[:, cc:cc + 1])
        for c in range(KC):
            segp, skp = self.wload(l, "po%d" % c, 512)
            segp = segp.rearrange("p (k m) -> p k m", k=4)
            segg, skg = self.wload(l, "g0_%d" % c, 1024)
            segg = segg.rearrange("p (k m) -> p k m", k=KC)
            for n in range(NTB):
                tsl = slice(n * TB, (n + 1) * TB)
                by = self.bank()
                for cc in range(4):
                    self.mm(self.psb(by), segp[:, cc, :], yp[:, cc, tsl], cc == 0, cc == 3, r=[skp, ("yp", cc, n)], w=[("ps", by)])
                bg = self.bank()
                for kc in range(KC):
                    self.mm(self.psb(bg), segg[:, kc, :], hT[:, kc, tsl], kc == 0, kc == KC - 1, r=[skg, ("hT", kc, n)], w=[("ps", bg)])
                self.act(sgt[:, :], self.psb(bg), AF.Sigmoid, r=[("ps", bg)], w=["sgt"])
                self.tt(merged[:, c, tsl], sgt[:, :], self.psb(by), ALU.mult, r=["sgt", ("ps", by)], w=[("mg", c, n)])
        self.tap("mg3", merged[:, :, S - 256:S], [("mg", c, n) for c in range(KC) for n in range(NTB)], [KC, 256])
        if stop == 3:
            return
        P.barrier()

        self.phase_s5(l, hT, merged, pf, uz)
        self.tap("mg4", merged[:, :, S - 256:S], [("mg", c, n) for c in range(KC) for n in range(NTB)], [KC, 256])
        if stop == 4:
            return
        P.barrier()

        self.phase_attn(l, hT, merged, kaugT, kiT, vaug, wi_s, ident, sel4, biasT, cmask, ones64, b31d)
        self.tap("mg5", merged[:, :, S - 256:S], [("mg", c, n) for c in range(KC) for n in range(NTB)], [KC, 256])
        if stop == 5:
            return
        P.barrier()

        self.bankset = list(range(8))
        xT = self.carve(0, 128, [KC, S], F32)
        sq = hT[:, 4:6, :].rearrange("p a b -> p (a b)").rearrange("p (c t) -> p c t", c=KC)
        xblk = hT[:, 0:4, :].rearrange("p a b -> p (a b)").bitcast(F32).rearrange("p (c t) -> p c t", c=KC)
        tmpf = self.carve(64 * 1024, 128, [1, TB], F32)[:, 0, :]
        for c in range(KC):
            seg, sk = self.wload(l, "wo%d" % c, 1024)
            seg = seg.rearrange("p (k m) -> p k m", k=KC)
            for n in range(NTB):
                tsl = slice(n * TB, (n + 1) * TB)
                b = self.bank()
                for kc in range(KC):
                    self.mm(self.psb(b), seg[:, kc, :], merged[:, kc, tsl], kc == 0, kc == KC - 1, r=[sk, ("mg", kc, n)], w=[("ps", b)])
                self.act(xT[:, c, tsl], self.psb(b), AF.Copy, r=[("ps", b)], w=[("xT", c, n)])
        for n in range(NTB):
            tsl = slice(n * TB, (n + 1) * TB)
            self.dma("sp", xblk[:, :, :], xsrc[:, :, tsl], r=[], w=[("xblk", c) for c in range(KC)], slot="xblk")
            self.rms_rstd(xT[:, :, tsl], [("xT", c, n) for c in range(KC)], sq, "p6")
            for c in range(KC):
                self.stt(tmpf[:, :], xT[:, c, tsl], gmo[:, c:c + 1], self.rstd[:, :], ALU.mult, ALU.mult,
                         r=[("xT", c, n), "pf", "rstd"], w=["tmpf"])
                self.tt(xT[:, c, tsl], tmpf[:, :], xblk[:, c, :], ALU.add, r=["tmpf", ("xblk", c)], w=[("xT", c, n)])
        self.tap("x6", xT[:, :, S - 256:S], [("xT", c, n) for c in range(KC) for n in range(NTB)], [KC, 256])
        if stop == 6:
            return
        P.barrier()

        fT = self.carve(64 * 1024, 128, [NJ, TB], BF16)
        sq = merged[:, 4:6, :].rearrange("p a b -> p (a b)").rearrange("p (c t) -> p c t", c=KC)
        mbuf = merged[:, 0:4, :].rearrange("p a b -> p (a b)").bitcast(F32).rearrange("p (c t) -> p c t", c=KC)
        sgf = merged[:, 6, 0:TB]
        tmpf = merged[:, 7, 0:2 * TB].bitcast(F32)
        for n in range(NTB):
            tsl = slice(n * TB, (n + 1) * TB)
            self.rms_rstd(xT[:, :, tsl], [("xT", c, n) for c in range(KC)], sq, "p7a")
            for c in range(KC):
                self.stt(hT[:, c, tsl], xT[:, c, tsl], gfp[:, c:c + 1], self.rstd[:, :], ALU.mult, ALU.mult,
                         r=[("xT", c, n), "pf", "rstd"], w=[("hT", c, n)])
            for j in range(NJ):
                seg, sk = self.wload(l, "f%d" % j, 2048)
                seg = seg.rearrange("p (k m) -> p k m", k=KC)
                bg = self.bank()
                for kc in range(KC):
                    self.mm(self.psb(bg), seg[:, kc, 0:128], hT[:, kc, tsl], kc == 0, kc == KC - 1, r=[sk, ("hT", kc, n)], w=[("ps", bg)])
                bu = self.bank()
                for kc in range(KC):
                    self.mm(self.psb(bu), seg[:, kc, 128:256], hT[:, kc, tsl], kc == 0, kc == KC - 1, r=[sk, ("hT", kc, n)], w=[("ps", bu)])
                self.act(sgf, self.psb(bg), AF.Silu, r=[("ps", bg)], w=["sgf"])
                self.tt(fT[:, j, :], sgf, self.psb(bu), ALU.mult, r=["sgf", ("ps", bu)], w=[("fT", j)])
            for c in range(KC):
                seg, sk = self.wload(l, "fo%d" % c, NJ * 128)
                seg = seg.rearrange("p (k m) -> p k m", k=NJ)
                b = self.bank()
                for j in range(NJ):
                    self.mm(self.psb(b), seg[:, j, :], fT[:, j, :], j == 0, j == NJ - 1, r=[sk, ("fT", j)], w=[("ps", b)])
                self.act(mbuf[:, c, :], self.psb(b), AF.Copy, r=[("ps", b)], w=[("mbuf", c)])
            self.rms_rstd(mbuf, [("mbuf", c) for c in range(KC)], sq, "p7b")
            for c in range(KC):
                self.stt(tmpf, mbuf[:, c, :], gfo[:, c:c + 1], self.rstd[:, :], ALU.mult, ALU.mult,
                         r=[("mbuf", c), "pf", "rstd"], w=["tmpf"])
                self.tt(mbuf[:, c, :], tmpf, xT[:, c, tsl], ALU.add, r=["tmpf", ("xT", c, n)], w=[("mbuf", c)])
            self.dma("sp", xdst[:, :, tsl], mbuf[:, :, :], r=[("mbuf", c) for c in range(KC)], w=[], slot="xout")
        if "xout" not in self.final_slots:
            self.final_slots.append("xout")

    def phase_s5(self, l, hT, merged, pf, uz):
        P = self.P
        K = 1024
        Ec = self.carve(16 * K, 128, [1, S], F32)[:, 0, :]
        Es = self.carve(24 * K, 128, [1, S], F32)[:, 0, :]
        vre = self.carve(32 * K, 128, [1, S], F32)[:, 0, :]
        vim = self.carve(40 * K, 128, [1, S], F32)[:, 0, :]
        xre = self.carve(48 * K, 128, [1, S], BF16)[:, 0, :]
        xim = self.carve(52 * K, 128, [1, S], BF16)[:, 0, :]
        tmp1 = self.carve(56 * K, 128, [1, TB], F32)[:, 0, :]
        tmp2 = self.carve(58 * K, 128, [1, TB], F32)[:, 0, :]
        Bre = self.carve(60 * K, 128, [2, 4, 128], BF16)
        Bim = self.carve(62 * K, 128, [2, 4, 128], BF16)
        gt = self.carve(71 * K, 128, [1, TB], F32)[:, 0, :]
        sm = self.carve(64 * K, 128, [16, 16], F32)
        s1 = self.carve(66 * K, 128, [1, TB], BF16)[:, 0, :]
        s2 = self.carve(67 * K, 128, [1, TB], BF16)[:, 0, :]
        t3 = self.carve(68 * K, 128, [1, TB], F32)[:, 0, :]
        xw = self.carve(32 * K, 128, [8, 512], F32)
        smi = self.carve(70 * K, 128, [1, 16], I32)[:, 0, :]
        TWO_PI = 2.0 * math.pi

        def pfx(o):
            return pf[:, o:o + 512]

        def dve(fn, r, w):
            P.op("dve", fn, r=r, w=w)

        def zoh(lr, li, ld, wk, n, tag, itile):
            dt_, lrdt, th, kf, q, sn, cs, t0 = wk[:8]
            kk = ["z%s%d" % (tag, i) for i in range(8)]
            kdt, klrdt, kth, kkf, kq, ksn, kcs, kt0 = kk
            ki = "z%si" % tag
            self.act(dt_, ld, AF.Exp, r=["pf"], w=[kdt])
            self.tt(lrdt, lr, dt_, ALU.mult, r=["pf", kdt], w=[klrdt])
            self.tt(th, li, dt_, ALU.mult, r=["pf", kdt], w=[kth])
            self.ts(kf, th, 1.0 / TWO_PI, None, ALU.mult, ALU.bypass, r=[kth], w=[kkf])
            dve(lambda e: e.tensor_copy(out=itile, in_=kf), r=[kkf], w=[ki])
            dve(lambda e: e.tensor_copy(out=kf, in_=itile), r=[ki], w=[kkf])
            self.stt(q, kf, -TWO_PI, th, ALU.mult, ALU.add, r=[kkf, kth], w=[kq])
            self.act(sn, q, AF.Sin, r=[kq], w=[ksn], scale=0.25)
            self.act(cs, q, AF.Sin, r=[kq, "hpi"], w=[kcs], scale=0.25, bias=self.hpi[:, 0:1])
            for it in range(2):
                self.tt(t0, sn, sn, ALU.mult, r=[ksn], w=[kt0])
                self.stt(sn, sn, 2.0, cs, ALU.mult, ALU.mult, r=[ksn, kcs], w=[ksn])
                self.ts(cs, t0, -2.0, 1.0, ALU.mult, ALU.add, r=[kt0], w=[kcs])
            self.act(dt_, lrdt, AF.Exp, r=[klrdt], w=[kdt])
            return dict(mag=dt_, cos=cs, sin=sn, keys=[kdt, kcs, ksn], k=kk)

        wkx = [xw[:, i, :] for i in range(8)]
        smx = self.carve(56 * K, 128, [1, 512], I32)[:, 0, :]
        zx = zoh(pfx(PF_LRX), pfx(PF_LIX), pfx(PF_LDX), wkx, 512, "x", smx)
        K_ = zx["k"]
        mg_, cs_x, sn_x = wkx[0], wkx[6], wkx[5]
        are, aim, den, cre, cim = wkx[1], wkx[2], wkx[3], wkx[4], wkx[7]
        kare, kaim, kden, kcre, kcim = K_[1], K_[2], K_[3], K_[4], K_[7]
        kmg, kcs, ksn = K_[0], K_[6], K_[5]
        lr, li = pfx(PF_LRX), pfx(PF_LIX)
        self.tt(are, mg_, cs_x, ALU.mult, r=[kmg, kcs], w=[kare])
        self.tt(aim, mg_, sn_x, ALU.mult, r=[kmg, ksn], w=[kaim])
        self.ts(are, are, -1.0, None, ALU.add, ALU.bypass, r=[kare], w=[kare])
        t0, kt0 = wkx[0], K_[0]
        t1, kt1, t2, kt2 = wkx[5], K_[5], wkx[6], K_[6]
        self.tt(den, lr, lr, ALU.mult, r=["pf"], w=[kden])
        self.tt(cre, li, li, ALU.mult, r=["pf"], w=[kcre])
        self.tt(den, den, cre, ALU.add, r=[kden, kcre], w=[kden])
        dve(lambda e: e.reciprocal(out=den, in_=den), r=[kden], w=[kden])
        self.tt(cre, are, lr, ALU.mult, r=[kare, "pf"], w=[kcre])
        self.tt(t0, aim, li, ALU.mult, r=[kaim, "pf"], w=[kt0])
        self.tt(cre, cre, t0, ALU.add, r=[kcre, kt0], w=[kcre])
        self.tt(cre, cre, den, ALU.mult, r=[kcre, kden], w=[kcre])
        self.tt(cim, aim, lr, ALU.mult, r=[kaim, "pf"], w=[kcim])
        self.tt(t0, are, li, ALU.mult, r=[kare, "pf"], w=[kt0])
        self.tt(cim, cim, t0, ALU.subtract, r=[kcim, kt0], w=[kcim])
        self.tt(cim, cim, den, ALU.mult, r=[kcim, kden], w=[kcim])
        br, bi = pfx(PF_BRX), pfx(PF_BIX)
        self.tt(t1, cre, br, ALU.mult, r=[kcre, "pf"], w=[kt1])
        self.tt(t2, cim, bi, ALU.mult, r=[kcim, "pf"], w=[kt2])
        self.tt(t1, t1, t2, ALU.subtract, r=[kt1, kt2], w=[kt1])
        for v in range(2):
            self.ts(Bre[:, v, :, :].rearrange("p a b -> p (a b)"), t1, self.cf[:, CF_PAR + v:CF_PAR + v + 1], None, ALU.mult, ALU.bypass,
                    r=[kt1, "cf"], w=["Bre"])
        self.tt(t1, cre, bi, ALU.mult, r=[kcre, "pf"], w=[kt1])
        self.tt(t2, cim, br, ALU.mult, r=[kcim, "pf"], w=[kt2])
        self.tt(t1, t1, t2, ALU.add, r=[kt1, kt2], w=[kt1])
        for v in range(2):
            self.ts(Bim[:, v, :, :].rearrange("p a b -> p (a b)"), t1, self.cf[:, CF_PAR + v:CF_PAR + v + 1], None, ALU.mult, ALU.bypass,
                    r=[kt1, "cf"], w=["Bim"])

        wks = [sm[:, i, :] for i in range(8)]
        zs = zoh(pf[:, PF_LRS:PF_LRS + 16], pf[:, PF_LIS:PF_LIS + 16], pf[:, PF_LDS:PF_LDS + 16], wks, 16, "s", smi)
        mag, cth, sth = zs["mag"], zs["cos"], zs["sin"]
        nsc = sm[:, 8, :]

        segC, skC = self.wload(l, "sC", 4096)
        Cre = segC[:, 0:2048].rearrange("p (j m) -> p j m", j=16)
        Cim = segC[:, 2048:4096].rearrange("p (j m) -> p j m", j=16)
        segD, skD = self.wload(l, "sD", 512)
        Dg = segD.rearrange("p (c m) -> p c m", c=4)

        self.bankset = [0, 1, 2, 3]
        Ec2 = [self.carve((16 + 4 * i) * K, 128, [1, TB], F32)[:, 0, :] for i in range(2)]
        Es2 = [self.carve((18 + 4 * i) * K, 128, [1, TB], F32)[:, 0, :] for i in range(2)]
        vslot_re = [self.carve((32 + 4 * i) * K, 128, [1, TB], F32)[:, 0, :] for i in range(4)]
        vslot_im = [self.carve((34 + 4 * i) * K, 128, [1, TB], F32)[:, 0, :] for i in range(4)]
        ptmp1 = self.carve(73 * K, 128, [1, TB], F32)[:, 0, :]
        ptmp2 = self.carve(24 * K, 128, [1, TB], F32)[:, 0, :]
        car = self.carve(26 * K, 128, [1, 16], F32)[:, 0, :]
        vi = 0
        for cc in range(4):
            ybanks = [4, 5, 6, 7]
            for jj in range(4):
                j = cc * 4 + jj
                rows = slice(64 * (jj // 2), 64 * (jj // 2) + 64)
                pv = jj % 2
                Ec, Es = Ec2[j % 2], Es2[j % 2]
                ke, ks = ("Ec", j % 2), ("Es", j % 2)
                dve(lambda e, j=j, Ec=Ec: e.tensor_copy(out=Ec[:, 0:1], in_=cth[:, j:j + 1]), r=zs["keys"], w=[ke])
                dve(lambda e, j=j, Es=Es: e.tensor_copy(out=Es[:, 0:1], in_=sth[:, j:j + 1]), r=zs["keys"], w=[ks])
                nn = 1
                lev = 0
                while nn < TB:
                    cs_ = Ec[:, nn - 1:nn]
                    ss_ = Es[:, nn - 1:nn]
                    ns_ = nsc[:, lev:lev + 1]
                    self.ts(ns_, ss_, -1.0, None, ALU.mult, ALU.bypass, r=[ks], w=["nsc"])
                    self.ts(Ec[:, nn:2 * nn], Ec[:, 0:nn], cs_, None, ALU.mult, ALU.bypass, r=[ke], w=[ke])
                    self.stt(Ec[:, nn:2 * nn], Es[:, 0:nn], ns_, Ec[:, nn:2 * nn], ALU.mult, ALU.add, r=[ks, "nsc", ke], w=[ke])
                    self.ts(Es[:, nn:2 * nn], Es[:, 0:nn], cs_, None, ALU.mult, ALU.bypass, r=[ks, ke], w=[ks])
                    self.stt(Es[:, nn:2 * nn], Ec[:, 0:nn], ss_, Es[:, nn:2 * nn], ALU.mult, ALU.add, r=[ke, ks], w=[ks])
                    nn *= 2
                    lev += 1
                cl, sl_, nsl = Ec[:, TB - 1:TB], Es[:, TB - 1:TB], nsc[:, 12:13]
                self.ts(nsl, sl_, -1.0, None, ALU.mult, ALU.bypass, r=[ks], w=["nsl"])
                for n in range(NTB):
                    tsl = slice(n * TB, (n + 1) * TB)
                    vre, vim = vslot_re[vi % 4], vslot_im[vi % 4]
                    kvr, kvi = ("vre", vi % 4), ("vim", vi % 4)
                    vi += 1
                    b1 = self.bank()
                    self.mm(self.psb(b1), Bre[rows, pv, cc, :], uz[rows, cc, tsl], True, True, r=["Bre", ("uz", cc, n)], w=[("ps", b1)])
                    b2 = self.bank()
                    self.mm(self.psb(b2), Bim[rows, pv, cc, :], uz[rows, cc, tsl], True, True, r=["Bim", ("uz", cc, n)], w=[("ps", b2)])
                    self.tt(vre, Ec, self.psb(b1), ALU.mult, r=[ke, ks, ("ps", b1)], w=[kvr])
                    self.tt(tmp1, Es, self.psb(b2), ALU.mult, r=[ks, ("ps", b2)], w=["tmp1"])
                    self.tt(vre, vre, tmp1, ALU.add, r=[kvr, "tmp1"], w=[kvr])
                    self.tt(vim, Ec, self.psb(b2), ALU.mult, r=[ke, ("ps", b2)], w=[kvi])
                    self.tt(tmp2, Es, self.psb(b1), ALU.mult, r=[ks, ("ps", b1)], w=["tmp2"])
                    self.tt(vim, vim, tmp2, ALU.subtract, r=[kvi, "tmp2"], w=[kvi])
                    if n == 0:
                        dve(lambda e, j=j, vre=vre: e.tensor_tensor_scan(out=vre, data0=mag[:, j:j + 1].to_broadcast([128, TB]), data1=vre,
                                                                        initial=0.0, op0=ALU.mult, op1=ALU.add), r=[kvr] + zs["keys"], w=[kvr])
                        dve(lambda e, j=j, vim=vim: e.tensor_tensor_scan(out=vim, data0=mag[:, j:j + 1].to_broadcast([128, TB]), data1=vim,
                                                                        initial=0.0, op0=ALU.mult, op1=ALU.add), r=[kvi] + zs["keys"], w=[kvi])
                    else:
                        dve(lambda e, j=j, vre=vre: e.tensor_tensor_scan(out=vre, data0=mag[:, j:j + 1].to_broadcast([128, TB]), data1=vre,
                                                                        initial=car[:, 0:1], op0=ALU.mult, op1=ALU.add), r=[kvr, "car"] + zs["keys"], w=[kvr])
                        dve(lambda e, j=j, vim=vim: e.tensor_tensor_scan(out=vim, data0=mag[:, j:j + 1].to_broadcast([128, TB]), data1=vim,
                                                                        initial=car[:, 1:2], op0=ALU.mult, op1=ALU.add), r=[kvi, "car"] + zs["keys"], w=[kvi])
                    if n < NTB - 1:
                        wr_l, wi_l = vre[:, TB - 1:TB], vim[:, TB - 1:TB]
                        self.ts(car[:, 2:3], wr_l, cl, None, ALU.mult, ALU.bypass, r=[kvr, ke], w=["car2"])
                        self.ts(car[:, 3:4], wr_l, sl_, None, ALU.mult, ALU.bypass, r=[kvr, ks], w=["car3"])
                        self.stt(car[:, 0:1], wi_l, nsl, car[:, 2:3], ALU.mult, ALU.add, r=[kvi, "nsl", "car2"], w=["car"])
                        self.stt(car[:, 1:2], wi_l, cl, car[:, 3:4], ALU.mult, ALU.add, r=[kvi, ke, "car3", "car"], w=["car"])
                    def ptt(out, in0, in1, op, r, w):
                        P.op("pool", lambda e: e.tensor_tensor(out=out, in0=in0, in1=in1, op=op), r=r, w=w)
                    ptt(ptmp1, Ec, vre, ALU.mult, [ke, kvr], ["ptmp1"])
                    ptt(ptmp2, Es, vim, ALU.mult, [ks, kvi], ["ptmp2"])
                    ptt(xre[:, tsl], ptmp1, ptmp2, ALU.subtract, ["ptmp1", "ptmp2"], [("xre", n)])
                    ptt(ptmp1, Es, vre, ALU.mult, [ks, kvr, ("xre", n)], ["ptmp1"])
                    ptt(ptmp2, Ec, vim, ALU.mult, [ke, kvi, ("xre", n)], ["ptmp2"])
                    ptt(ptmp1, ptmp1, ptmp2, ALU.add, ["ptmp1", "ptmp2"], ["ptmp1"])
                    xo = xim[:, tsl]
                    P.op("pool", lambda e, xo=xo: e.tensor_scalar(out=xo, in0=ptmp1, scalar1=-1.0, scalar2=0.0, op0=ALU.mult, op1=ALU.add),
                         r=["ptmp1"], w=[("xim", n)])
                    yb = ybanks[n]
                    self.mm(self.psb(yb), Cre[:, j, :], xre[:, tsl], jj == 0, False, r=[skC, ("xre", n)], w=[("ps", yb)])
                    self.mm(self.psb(yb), Cim[:, j, :], xim[:, tsl], False, False, r=[skC, ("xim", n)], w=[("ps", yb)])
                    if jj == 3:
                        self.mm(self.psb(yb), Dg[:, cc, :], uz[:, cc, tsl], False, True, r=[skD, ("uz", cc, n)], w=[("ps", yb)])
            for n in range(NTB):
                tsl = slice(n * TB, (n + 1) * TB)
                yb = ybanks[n]
                self.act(gt, self.psb(yb), AF.Square, r=[("ps", yb)], w=["gt"])
                self.ts(gt, gt, 0.044715, 1.0, ALU.mult, ALU.add, r=["gt"], w=["gt"])
                self.tt(gt, gt, self.psb(yb), ALU.mult, r=["gt", ("ps", yb)], w=["gt"])
                self.act(t3, gt, AF.Sigmoid, r=["gt"], w=["t3"], scale=1.5957691216057308)
                self.tt(uz[:, cc, tsl], t3, self.psb(yb), ALU.mult, r=["t3", ("ps", yb)], w=[("uz", cc, n)])
        self.tap("zT", uz[:, :, :], [("uz", cc, n) for cc in range(4) for n in range(NTB)], [4, S])
        self.bankset = list(range(8))
        for c in range(KC):
            segl, skl = self.wload(l, "glu%d" % c, 1024)
            segl = segl.rearrange("p (a k m) -> p a k m", a=2, k=4)
            segg, skg = self.wload(l, "g2_%d" % c, 1024)
            segg = segg.rearrange("p (k m) -> p k m", k=KC)
            for n in range(NTB):
                tsl = slice(n * TB, (n + 1) * TB)
                ba = self.bank()
                for cc in range(4):
                    self.mm(self.psb(ba), segl[:, 0, cc, :], uz[:, cc, tsl], cc == 0, cc == 3, r=[skl, ("uz", cc, n)], w=[("ps", ba)])
                bb = self.bank()
                for cc in range(4):
                    self.mm(self.psb(bb), segl[:, 1, cc, :], uz[:, cc, tsl], cc == 0, cc == 3, r=[skl, ("uz", cc, n)], w=[("ps", bb)])
                bg = self.bank()
                for kc in range(KC):
                    self.mm(self.psb(bg), segg[:, kc, :], hT[:, kc, tsl], kc == 0, kc == KC - 1, r=[skg, ("hT", kc, n)], w=[("ps", bg)])
                self.act(s1, self.psb(bb), AF.Sigmoid, r=[("ps", bb)], w=["s1"])
                self.act(s2, self.psb(bg), AF.Sigmoid, r=[("ps", bg)], w=["s2"])
                self.tt(t3, s1, self.psb(ba), ALU.mult, r=["s1", ("ps", ba)], w=["t3"])
                self.tt(t3, t3, s2, ALU.mult, r=["t3", "s2"], w=["t3"])
                self.tt(merged[:, c, tsl], merged[:, c, tsl], t3, ALU.add, r=[("mg", c, n), "t3"], w=[("mg", c, n)])

    def phase_attn(self, l, hT, merged, kaugT, kiT, vaug, wi_s, ident, sel4, biasT, cmask, ones64, b31d):
        P = self.P
        K = 1024
        ps = self.ps
        qT = self.arena[0:65, 0: 16 * TB].rearrange("p (h q) -> p h q", h=16)
        qiT = self.arena[0:64, 8 * K: 8 * K + 8 * TB].rearrange("p (h q) -> p h q", h=8)
        OTn = self.arena[0:64, 12 * K: 12 * K + 16 * TB].rearrange("p (h q) -> p h q", h=16)
        score2 = [self.carve(40 * K, 128, [1, S], F32)[:, 0, :], self.pf_t[:, PF_LRX:PF_LRX + S]]
        rl = [self.carve(48 * K, 128, [1, TB], F32)[:, 0, :], self.carve(50 * K, 128, [1, TB], F32)[:, 0, :]]
        nm3 = [self.carve((52 + 4 * i) * K, 128, [1, S], BF16)[:, 0, :] for i in range(3)]
        self.idx_i = 0
        PT = [self.carve(64 * K, 128, [1, 1024], BF16)[:, 0, :], self.carve(66 * K, 128, [1, 1024], BF16)[:, 0, :]]
        ot = self.carve(68 * K, 64, [1, 1024], F32)[:, 0, :]
        bis = self.carve(72 * K, 128, [1, 16], F32)[:, 0, :]
        sgt = self.carve(73 * K, 128, [1, TB], BF16)[:, 0, :]
        lnr = self.arena[64:65, 12 * K: 12 * K + 2048].bitcast(F32)
        rrow = self.arena[64:65, 14 * K: 14 * K + 1024]
        P.op("pool", lambda e: e.dma_start(out=self.arena[64:65, 0:16 * TB], in_=b31d, max_dma_last_dim=8192), w=["qT64"], slot="b31")

        def emit_ao(nn):
            tsl = slice(nn * TB, (nn + 1) * TB)
            self.bankset = [0, 1, 2, 3, 4, 5]
            for c in range(KC):
                sego, sko = self.wload(l, "ao%d" % c, 2048)
                sego = sego.rearrange("p (h m) -> p h m", h=16)
                segg, skg = self.wload(l, "g1_%d" % c, 1024)
                segg = segg.rearrange("p (k m) -> p k m", k=KC)
                by = self.bank()
                for h in range(16):
                    self.mm(self.psb(by), sego[0:64, h, :], OTn[:, h, :], h == 0, h == 15, r=[sko] + [("OTn", i) for i in range(4)], w=[("ps", by)])
                bg = self.bank()
                for kc in range(KC):
                    self.mm(self.psb(bg), segg[:, kc, :], hT[:, kc, tsl], kc == 0, kc == KC - 1, r=[skg, ("hT", kc, nn)], w=[("ps", bg)])
                self.act(sgt, self.psb(bg), AF.Sigmoid, r=[("ps", bg)], w=["sgt"])
                t3 = rl[0]
                self.tt(t3, sgt, self.psb(by), ALU.mult, r=["sgt", ("ps", by)], w=[("rl", 0)])
                self.tt(merged[:, c, tsl], merged[:, c, tsl], t3, ALU.add, r=[("mg", c, nn), ("rl", 0)], w=[("mg", c, nn)])

        for n in range(NTB):
            tsl = slice(n * TB, (n + 1) * TB)
            self.bankset = list(range(8))
            for hp in range(8):
                seg, sk = self.wload(l, "q%d" % hp, 1024)
                seg = seg.rearrange("p (k m) -> p k m", k=KC)
                for half in range(2):
                    h = 2 * hp + half
                    b = self.bank()
                    for kc in range(KC):
                        self.mm(self.psb(b, 64), seg[:, kc, half * 64:(half + 1) * 64], hT[:, kc, tsl], kc == 0, kc == KC - 1,
                                r=[sk, ("hT", kc, n)], w=[("ps", b)])
                    self.act(qT[0:64, h, :], self.psb(b, 64), AF.Copy, r=[("ps", b)], w=[("qT", h)], scale=0.125)
            for hp in range(4):
                seg, sk = self.wload(l, "qi%d" % hp, 1024)
                seg = seg.rearrange("p (k m) -> p k m", k=KC)
                for half in range(2):
                    h = 2 * hp + half
                    b = self.bank()
                    for kc in range(KC):
                        self.mm(self.psb(b, 64), seg[:, kc, half * 64:(half + 1) * 64], hT[:, kc, tsl], kc == 0, kc == KC - 1,
                                r=[sk, ("hT", kc, n)], w=[("ps", b)])
                    self.act(qiT[0:64, h, :], self.psb(b, 64), AF.Copy, r=[("ps", b)], w=[("qiT", h)])
            if n == 0:
                self.tap("qT", qT[:, :, :], [("qT", h) for h in range(16)] + ["qT64"], [16, TB], parts=65)

            def genA(qq):
                qb = 4 * n + qq
                qsl = slice(qq * 128, (qq + 1) * 128)
                L = (qb + 1) * 128
                sc = score2[qb % 2]
                ngr = (L + 511) // 512
                for kg in range(ngr):
                    k0 = kg * 512
                    nk = min(512, L - k0)
                    sk_ = ("sc", qb % 2, kg)
                    for h in range(8):
                        b = 6 + (self.idx_i % 2)
                        r_ = rl[self.idx_i % 2]
                        rk = ("rl", self.idx_i % 2)
                        self.idx_i += 1
                        self.mm(self.psb(b, 128, nk), qiT[0:64, h, qsl], kiT[0:64, k0:k0 + nk], True, True,
                                r=[("qiT", h)] + [("kiT", i) for i in range(NTB)], w=[("ps", b)])
                        self.act(r_[:, 0:nk], self.psb(b, 128, nk), AF.Relu, r=[("ps", b)], w=[rk])
                        wcol = wi_s[:, qb, h:h + 1]
                        wk = [("wi", qb)]
                        if h == 0:
                            ndiag = nk - 128 if (k0 + nk == L) else nk
                            if ndiag > 0:
                                self.ts(sc[:, k0:k0 + ndiag], r_[:, 0:ndiag], wcol, None, ALU.mult, ALU.bypass, r=[rk] + wk, w=[sk_])
                            if k0 + nk == L:
                                self.stt(sc[:, L - 128:L], r_[:, nk - 128:nk], wcol, cmask, ALU.mult, ALU.add, r=[rk, "cf"] + wk, w=[sk_])
                        else:
                            self.stt(sc[:, k0:k0 + nk], r_[:, 0:nk], wcol, sc[:, k0:k0 + nk], ALU.mult, ALU.add,
                                     r=[rk, sk_] + wk, w=[sk_])
                        yield

            def genB(qq):
                qb = 4 * n + qq
                L = (qb + 1) * 128
                sc = score2[qb % 2]
                nmb = nm3[qb % 3]
                nmk = ("nm", qb % 3)
                ngr = (L + 511) // 512
                sck = [("sc", qb % 2, kg) for kg in range(ngr)]
                if qb >= 2:
                    o = 8 * (qb % 2)
                    cA, cB, cnt, tmpb, thr = bis[:, o:o + 1], bis[:, o + 1:o + 2], bis[:, o + 2:o + 3], bis[:, o + 3:o + 4], bis[:, o + 4:o + 5]
                    kp = "b%d" % (qb % 2)
                    P.op("dve", lambda e, cA=cA: e.memset(cA, 0.0), w=[kp + "c0"])
                    cur, nxt = cA, cB
                    curk, nxtk = kp + "c0", kp + "c1"
                    step = 4.0
                    for it in range(NBIS):
                        self.ts(nmb[:, 0:L], sc[:, 0:L], cur, None, ALU.is_ge, ALU.add, r=sck + [curk], w=[nmk, kp + "cnt"], accum_out=cnt)
                        self.ts(tmpb, cnt, 256.0, 2.0 * step, ALU.is_ge, ALU.mult, r=[kp + "cnt"], w=[kp + "tmpb"])
                        self.ts(nxt, tmpb, -step, cur, ALU.add, ALU.add, r=[kp + "tmpb", curk], w=[nxtk])
                        cur, nxt = nxt, cur
                        curk, nxtk = nxtk, curk
                        step *= 0.5
                        yield
                    self.ts(thr, cur, -2.0 * step - 1e-5, None, ALU.add, ALU.bypass, r=[curk], w=[kp + "thr"])
                    self.ts(nmb[:, 0:L], sc[:, 0:L], thr, NEG, ALU.is_lt, ALU.mult, r=sck + [kp + "thr"], w=[nmk])
                else:
                    self.ts(nmb[:, 0:L], sc[:, 0:L], -16.0, NEG, ALU.is_lt, ALU.mult, r=sck, w=[nmk])
                if qb == 5:
                    self.tap("score5", sc[:, 0:L], sck, [L])
                    self.tap("nm5", nmb[:, 0:L], [nmk], [L])
                yield

            def n_units_A(qq):
                qb = 4 * n + qq
                return 8 * (((qb + 1) * 128 + 511) // 512)

            def step_gen(g):
                if g is None:
                    return None
                try:
                    next(g)
                    return g
                except StopIteration:
                    return None

            def drain(g):
                while g is not None:
                    g = step_gen(g)

            def emit_attn(qq, gB, nB, gA, nA):
                qb = 4 * n + qq
                qsl = slice(qq * 128, (qq + 1) * 128)
                nmb = nm3[qb % 3]
                nmk = ("nm", qb % 3)
                niter = 2 * (qb + 1)
                perB = -(-nB // niter)
                perA = -(-nA // niter)
                for hh in range(2):
                    def emit_S(kb):
                        sb = kb % 2
                        near = (qb - kb) < 2
                        kk = 64 if near else 65
                        ksl = slice(kb * 128, (kb + 1) * 128)
                        for bk in range(2):
                            bnk = 2 * sb + bk
                            hs = slice(hh * 8 + bk * 4, hh * 8 + bk * 4 + 4)
                            outp = self.psb(bnk).rearrange("p (h q) -> p h q", h=4)
                            qk = [("qT", h) for h in range(hh * 8 + bk * 4, hh * 8 + bk * 4 + 4)] + ["qT64"]
                            self.mm(outp, kaugT[0:kk, ksl], qT[0:kk, hs, qsl], True, False,
                                    r=qk + [("kT", kb // 4), "kaug1"], w=[("ps", bnk)])
                            self.mm(outp, nmb[:, ksl], sel4, False, not near, r=[nmk, "cb"], w=[("ps", bnk)])
                            if near:
                                self.mm(outp, ident, biasT[:, qb - kb, hs, :], False, True, r=["cb"], w=[("ps", bnk)])
                        self.act(PT[sb][:, :], ps[:, 2 * sb * 512: 2 * sb * 512 + 1024], AF.Exp,
                                 r=[("ps", 2 * sb), ("ps", 2 * sb + 1)], w=[("PT", sb)])

                    def emit_PV(kb):
                        sb = kb % 2
                        for bk in range(2):
                            self.mm(self.psb(4 + bk, 65), vaug[:, kb, 0:65], PT[sb][:, bk * 512:(bk + 1) * 512], kb == 0, kb == qb,
                                    r=[("PT", sb), ("vaug", kb), "vaug1"], w=[("ps", 4 + bk)])

                    for kb in range(qb + 1):
                        emit_S(kb)
                        if kb > 0:
                            emit_PV(kb - 1)
                        for _ in range(perB):
                            gB = step_gen(gB)
                        for _ in range(perA):
                            gA = step_gen(gA)
                    emit_PV(qb)
                    if hh == 1:
                        drain(gB)
                        drain(gA)
                        gB = gA = None
                    self.act(lnr, ps[64:65, 2048:3072], AF.Ln, r=[("ps", 4), ("ps", 5)], w=["lnr"])
                    self.act(rrow, lnr, AF.Exp, r=["lnr"], w=["rrow"], scale=-1.0)
                    for bk in range(2):
                        self.mm(self.psb(6 + bk, 64), self.ones_bf[64:65, 0:64], rrow[:, bk * 512:(bk + 1) * 512], True, True,
                                r=["ones_bf", "rrow"], w=[("ps", 6 + bk)])
                    self.act(ot[:, :], ps[0:64, 2048:3072], AF.Copy, r=[("ps", 4), ("ps", 5)], w=["ot"])
                    for bk in range(2):
                        hs = slice(hh * 8 + bk * 4, hh * 8 + bk * 4 + 4)
                        self.tt(OTn[:, hs, qsl], ot[:, bk * 512:(bk + 1) * 512].rearrange("p (h q) -> p h q", h=4),
                                self.psb(6 + bk, 64).rearrange("p (h q) -> p h q", h=4), ALU.mult,
                                r=["ot", ("ps", 6 + bk)], w=[("OTn", hh * 2 + bk)])

            drain(genA(0))
            drain(genA(1))
            if n > 0:
                emit_ao(n - 1)
            drain(genB(0))
            for qq in range(4):
                gB = genB(qq + 1) if qq + 1 < 4 else None
                gA = genA(qq + 2) if qq + 2 < 4 else None
                emit_attn(qq, gB, NBIS + 1, gA, n_units_A(qq + 2) if qq + 2 < 4 else 0)
            if n == 0:
                self.tap("OTn", OTn[:, :, :], [("OTn", i) for i in range(4)], [16, TB], parts=64)
        emit_ao(NTB - 1)


def km(w):
    k, m = w.shape
    return np.ascontiguousarray(w.reshape(k // 128, 128, m).transpose(1, 0, 2)).reshape(128, (k // 128) * m)


def host_segments(inp, l):
    w = inp["w_in"][l]
    segs = {}
    segs["kk"] = km(np.concatenate([w[:, C_K:C_K + 64], w[:, C_KI:C_KI + 64]], axis=1))
    segs["vw"] = km(np.concatenate([w[:, C_V:C_V + 64], w[:, C_WI:C_WI + 8]], axis=1))
    for cc in range(4):
        segs["pu%d" % cc] = km(w[:, C_POOL + cc * 128: C_POOL + (cc + 1) * 128])
        segs["su%d" % cc] = km(w[:, C_S5 + cc * 128: C_S5 + (cc + 1) * 128])
    segs["mix"] = np.ascontiguousarray(inp["pool_mix_w"][l].transpose(1, 0, 2)).reshape(128, 512)
    for c in range(8):
        cs = slice(c * 128, (c + 1) * 128)
        segs["po%d" % c] = km(inp["pool_out_w"][l][:, cs])
        for b in range(3):
            segs["g%d_%d" % (b, c)] = km(w[:, C_G + b * 1024 + c * 128: C_G + b * 1024 + (c + 1) * 128])
        glu = inp["s5_glu_w"][l]
        segs["glu%d" % c] = np.concatenate([km(glu[:, cs]), km(glu[:, 1024 + c * 128: 1024 + (c + 1) * 128])], axis=1)
        segs["q%d" % c] = km(w[:, C_Q + c * 128: C_Q + (c + 1) * 128])
        ao = inp["attn_out_w"][l][:, cs].reshape(16, 64, 128).transpose(1, 0, 2).reshape(64, 2048)
        segs["ao%d" % c] = np.concatenate([ao, np.zeros((64, 2048), np.float32)], axis=0)
        segs["wo%d" % c] = km(inp["w_out"][l][:, cs])
        segs["fo%d" % c] = km(inp["ffn_w_out"][l][:, cs])
    for hp in range(4):
        segs["qi%d" % hp] = km(w[:, C_QI + hp * 128: C_QI + (hp + 1) * 128])
    fw = inp["ffn_w_in"][l]
    for j in range(NJ):
        segs["f%d" % j] = km(np.concatenate([fw[:, j * 128:(j + 1) * 128], fw[:, FH + j * 128: FH + (j + 1) * 128]], axis=1))
    cre = inp["s5_c_re"][l]
    cim = inp["s5_c_im"][l]
    Cre = np.zeros((128, 16, 128), np.float32)
    Cim = np.zeros((128, 16, 128), np.float32)
    for g in range(32):
        j, g2 = g // 2, g % 2
        jj = j % 4
        m0 = 32 * jj + 16 * g2
        Cre[g2 * 64:(g2 + 1) * 64, j, m0:m0 + 16] = cre[g].T
        Cim[g2 * 64:(g2 + 1) * 64, j, m0:m0 + 16] = cim[g].T
    segs["sC"] = np.concatenate([Cre.reshape(128, 2048), Cim.reshape(128, 2048)], axis=1)
    Dg = np.zeros((128, 4, 128), np.float32)
    d = inp["s5_d"][l]
    for cc in range(4):
        Dg[np.arange(128), cc, np.arange(128)] = d[cc * 128:(cc + 1) * 128]
    segs["sD"] = Dg.reshape(128, 512)
    return segs


def host_pf(inp, l):
    pf = np.zeros((128, NPF), np.float32)
    for off, name in ((PF_GMP, "norm_mix_pre"), (PF_GMO, "norm_mix_post"), (PF_GFP, "norm_ffn_pre"), (PF_GFO, "norm_ffn_post")):
        pf[:, off:off + 8] = inp[name][l].reshape(8, 128).T
    pf[:, PF_PSC:PF_PSC + 4] = inp["pool_scale"][l].reshape(4, 128).T
    lr, li, ld = inp["s5_lambda_re"][l], inp["s5_lambda_im"][l], inp["s5_log_dt"][l]
    for j in range(16):
        for g2 in range(2):
            g = 2 * j + g2
            pf[g2 * 64:(g2 + 1) * 64, PF_LRS + j] = lr[g]
            pf[g2 * 64:(g2 + 1) * 64, PF_LIS + j] = li[g]
            pf[g2 * 64:(g2 + 1) * 64, PF_LDS + j] = ld[g]
    br, bi = inp["s5_b_re"][l], inp["s5_b_im"][l]
    X = np.zeros((5, 128, 4, 128), np.float32)
    for cc in range(4):
        for p in range(128):
            g = cc * 8 + p // 16
            i = p % 16
            X[0, p, cc, :] = np.tile(lr[g], 2)
            X[1, p, cc, :] = np.tile(li[g], 2)
            X[2, p, cc, :] = ld[g]
            g2 = g % 2
            X[3, p, cc, g2 * 64:(g2 + 1) * 64] = br[g, :, i]
            X[4, p, cc, g2 * 64:(g2 + 1) * 64] = bi[g, :, i]
    for k, off in enumerate((PF_LRX, PF_LIX, PF_LDX, PF_BRX, PF_BIX)):
        pf[:, off:off + 512] = X[k].reshape(128, 512)
    return pf


def host_consts(inp):
    cf = np.zeros((128, NCF), np.float32)
    q = np.arange(128)[:, None]
    s = np.arange(128)[None, :]
    cf[:, CF_CMASK:CF_CMASK + 128] = np.where(s > q, np.float32(-1e4), np.float32(0.0))
    cf[:, CF_INVC:CF_INVC + 16] = (1.0 / np.arange(1, 17, dtype=np.float32))[None, :]
    cf[:, CF_ONES:CF_ONES + 64] = 1.0
    par = ((np.arange(128) // 32) % 2).astype(np.float32)
    cf[:, CF_PAR] = 1.0 - par
    cf[:, CF_PAR + 1] = par
    cb = np.zeros((128, NCB), np.float32)
    cb[:, CB_ID:CB_ID + 128] = np.eye(128, dtype=np.float32)
    cb[:, CB_SEL:CB_SEL + 512] = np.tile(np.eye(128, dtype=np.float32), (1, 4))
    rb = inp["rel_bias"]
    sl = np.arange(128)[:, None]
    ql = np.arange(128)[None, :]
    bt = np.zeros((128, 2, 16, 128), np.float32)
    for kind in range(2):
        dist = ql - sl + 128 * kind
        idx = rel_bucket_np(np.maximum(dist, 0))
        bt[:, kind, :, :] = rb[idx].transpose(0, 2, 1)
    cb[:, CB_BIAS:CB_BIAS + 4096] = bt.reshape(128, 4096)
    b31 = np.ascontiguousarray(np.repeat(rb[31][:, None], TB, axis=1)).reshape(1, 16 * TB).astype(np.float32)
    return cf, cb, b31


_CACHE = {}


def get_program(nlayers=NL, stop=None, taps=()):
    key = (nlayers, stop, tuple(taps))
    if key not in _CACHE:
        b0 = Builder(nlayers, stop, taps)
        b0.wtotal = 1 << 20
        b0.build()
        b = Builder(nlayers, stop, taps)
        b.wtotal = max(b0.seg_total, 16)
        nc = b.build()
        _CACHE[key] = (nc, b)
    return _CACHE[key]


def run(inputs, nlayers=NL, stop=None, taps=()):
    nc, b = get_program(nlayers, stop, taps)
    inp = {k: np.asarray(v, dtype=np.float32) for k, v in inputs.items()}
    ws = np.zeros((NL, 128, b.wtotal), np.float32)
    pfs = np.zeros((NL, 128, NPF), np.float32)
    for l in range(NL):
        segs = host_segments(inp, l)
        for name, (off, n) in b.seg_off.items():
            a = segs[name]
            assert a.shape == (128, n), (name, a.shape, n)
            ws[l, :, off:off + n] = a
        pfs[l] = host_pf(inp, l)
    cf, cb, b31 = host_consts(inp)
    x = inp["x"]
    in_maps = []
    for c in range(8):
        xt = np.ascontiguousarray(x[c].T.reshape(KC, 128, S).transpose(1, 0, 2))
        in_maps.append({"x": xt, "wstream": ws, "pf": pfs, "cf": cf, "cb": cb, "b31": b31})
    res = run_bass_kernel_spmd(nc, in_maps, core_ids=list(range(8)))
    return res, b


def kernel(**inputs):
    res, b = run(inputs)
    outs = []
    for c in range(8):
        o = res.results[c]["out"]
        outs.append(np.ascontiguousarray(o.transpose(1, 0, 2).reshape(D, S).T))
    return np.stack(outs, axis=0).astype(np.float32)
```

```python
import contextlib
import math
import numpy as np
import concourse.bass as bass
import concourse.mybir as mybir
from concourse.bass_utils import run_bass_kernel_spmd

F32 = mybir.dt.float32
BF16 = mybir.dt.bfloat16
I32 = mybir.dt.int32
ALU = mybir.AluOpType
AF = mybir.ActivationFunctionType

ENGS = ("pe", "act", "dve", "pool", "sp")


class Op:
    __slots__ = ("eng", "fn", "waits", "signal", "slot", "seq", "dcount")

    def __init__(self, eng, fn):
        self.eng = eng
        self.fn = fn
        self.waits = {}
        self.signal = False
        self.slot = None
        self.seq = 0
        self.dcount = 0


class Prog:
    def __init__(self):
        self.ops = {e: [] for e in ENGS}
        self.last_w = {}
        self.readers = {}
        self.slot_count = {}
        self.pending = {e: {} for e in ENGS}

    def _add_wait(self, op, tok, raw):
        kind, who, val = tok
        if kind == "E":
            if who == op.eng and not raw:
                return
            if who == "pe" and op.eng == "pe":
                return
            self.ops[who][val].signal = True
        k = (kind, who)
        if op.waits.get(k, -1) < val:
            op.waits[k] = val

    def op(self, eng, fn, r=(), w=(), slot=None):
        o = Op(eng, fn)
        o.seq = len(self.ops[eng])
        if slot is not None:
            o.slot = slot
            self.slot_count[slot] = self.slot_count.get(slot, 0) + 1
            o.dcount = self.slot_count[slot]
            tok = ("D", slot, o.dcount)
        else:
            tok = ("E", eng, o.seq)
        for k, v in self.pending[eng].items():
            if o.waits.get(k, -1) < v:
                o.waits[k] = v
        self.pending[eng] = {}
        for k in r:
            lw = self.last_w.get(k)
            if lw is not None:
                self._add_wait(o, lw, True)
        for k in w:
            lw = self.last_w.get(k)
            if lw is not None:
                self._add_wait(o, lw, False)
            for t in self.readers.get(k, ()):
                self._add_wait(o, t, False)
        self.ops[eng].append(o)
        for k in r:
            self.readers.setdefault(k, []).append(tok)
        for k in w:
            self.last_w[k] = tok
            self.readers[k] = []
        return o

    def barrier(self):
        toks = {}
        for e in ENGS:
            for o in reversed(self.ops[e]):
                if o.slot is None:
                    o.signal = True
                    toks[("E", e)] = o.seq
                    break
        for s, c in self.slot_count.items():
            toks[("D", s)] = c
        for e in ENGS:
            for k, v in toks.items():
                if k == ("E", e):
                    continue
                if self.pending[e].get(k, -1) < v:
                    self.pending[e][k] = v
        self.last_w = {}
        self.readers = {}

    def emit(self, nc, final_slots=()):
        with contextlib.ExitStack() as st:
            esem = {e: st.enter_context(nc.semaphore("s_" + e)) for e in ENGS}
            dsem = {s: st.enter_context(nc.semaphore("d_%d" % i)) for i, s in enumerate(self.slot_count)}
            sigcount = {}
            for e in ENGS:
                c = 0
                arr = []
                for o in self.ops[e]:
                    if o.slot is None and o.signal:
                        c += 1
                    arr.append(c)
                sigcount[e] = arr
            block = st.enter_context(nc.Block())
            prog = self

            def make(e):
                def body(eng):
                    waited = {}
                    for o in prog.ops[e]:
                        for (kind, who), val in o.waits.items():
                            if kind == "E":
                                sem = esem[who]
                                v = sigcount[who][val]
                            else:
                                sem = dsem[who]
                                v = 16 * val
                            if waited.get((kind, who), -1) >= v:
                                continue
                            waited[(kind, who)] = v
                            eng.wait_ge(sem, v)
                        ins = o.fn(eng)
                        if o.slot is not None:
                            ins.then_inc(dsem[o.slot], 16)
                        elif o.signal:
                            ins.then_inc(esem[e], 1)
                    if e == "sp":
                        for s in final_slots:
                            eng.wait_ge(dsem[s], 16 * prog.slot_count[s])
                return body

            block.tensor(make("pe"))
            block.scalar(make("act"))
            block.vector(make("dve"))
            block.gpsimd(make("pool"))
            block.sync(make("sp"))


S = 2048
D = 1024
TB = 512
NTB = 4
KC = 8
NL = 2
FH = 2816
NJ = 22
EPS = 1e-6
IDX_SCALE = (8 ** -0.5) * (64 ** -0.5)
NEG = -30000.0
NBIS = 15

C_POOL, C_Q, C_K, C_V, C_QI, C_KI, C_WI, C_S5, C_G = 0, 512, 1536, 1600, 1664, 2176, 2240, 2248, 2760

PF_GMP, PF_GMO, PF_GFP, PF_GFO, PF_PSC = 0, 8, 16, 24, 32
PF_LRS, PF_LIS, PF_LDS = 36, 52, 68
PF_LRX, PF_LIX, PF_LDX, PF_BRX, PF_BIX = 84, 596, 1108, 1620, 2132
NPF = 2644
CF_CMASK, CF_INVC, CF_ONES, CF_PAR = 0, 128, 144, 208
NCF = 212
CB_ID, CB_SEL, CB_BIAS = 0, 128, 640
NCB = 640 + 4096

ARENA = 86 * 1024
RSLOT = 4096
NRING = 3
KV0 = 75 * 1024


def rel_bucket_np(dist):
    dist = np.asarray(dist, np.int32)
    d_f = np.maximum(dist, 1).astype(np.float32)
    large = 16 + (np.log(d_f / np.float32(16)) / np.float32(math.log(128 / 16)) * np.float32(16)).astype(np.int32)
    large = np.minimum(large, 31)
    return np.where(dist < 16, dist, large)


class Builder:
    def __init__(self, nlayers=NL, stop=None, taps=()):
        self.nlayers = nlayers
        self.stop = stop
        self.taps = taps
        self.seg_off = {}
        self.seg_total = 0
        self.ring_i = 0
        self.bank_i = 0
        self.bankset = list(range(8))
        self.tapouts = {}

    def carve(self, off, parts, shape, dt):
        esz = 2 if dt == BF16 else 4
        n = int(np.prod(shape))
        assert off % 4 == 0 and off + n * esz <= ARENA, (off, n, esz)
        ap = self.arena[0:parts, off // 2: off // 2 + n * esz // 2]
        if dt != BF16:
            ap = ap.bitcast(dt)
        if len(shape) == 2:
            return ap.rearrange("p (a b) -> p a b", a=shape[0])
        if len(shape) == 3:
            return ap.rearrange("p (a b c) -> p a b c", a=shape[0], b=shape[1])
        return ap

    def bank(self):
        b = self.bankset[self.bank_i % len(self.bankset)]
        self.bank_i += 1
        return b

    def psb(self, b, parts=128, n=512):
        return self.ps[0:parts, b * 512: b * 512 + n]

    def wload(self, l, name, n):
        assert n <= RSLOT
        if name not in self.seg_off:
            self.seg_off[name] = (self.seg_total, n)
            self.seg_total += n
        off, n0 = self.seg_off[name]
        assert n0 == n
        slot = self.ring_i % NRING
        self.ring_i += 1
        dst = self.ring[:, slot * RSLOT: slot * RSLOT + n]
        src = self.wstream[l, :, off:off + n]
        key = ("ring", slot)
        self.P.op("pool", lambda e: e.dma_start(out=dst, in_=src, max_dma_last_dim=8192), w=[key], slot="ring%d" % slot)
        return dst, key

    def mm(self, out, lhsT, rhs, start, stop, r, w):
        self.P.op("pe", lambda e: e.matmul(out, lhsT, rhs, start=start, stop=stop), r=r, w=w)

    def act(self, out, in_, func, r, w, scale=1.0, bias=0.0):
        self.P.op("act", lambda e: e.activation(out=out, in_=in_, func=func, bias=bias, scale=scale), r=r, w=w)

    def tt(self, out, in0, in1, op, r, w):
        self.P.op("dve", lambda e: e.tensor_tensor(out=out, in0=in0, in1=in1, op=op), r=r, w=w)

    def ts(self, out, in0, s1, s2, op0, op1, r, w, accum_out=None):
        if accum_out is None:
            self.P.op("dve", lambda e: e.tensor_scalar(out=out, in0=in0, scalar1=s1, scalar2=s2, op0=op0, op1=op1), r=r, w=w)
        else:
            self.P.op("dve", lambda e: e.tensor_scalar(out=out, in0=in0, scalar1=s1, scalar2=s2, op0=op0, op1=op1, accum_out=accum_out), r=r, w=w)

    def stt(self, out, in0, scalar, in1, op0, op1, r, w):
        self.P.op("dve", lambda e: e.scalar_tensor_tensor(out=out, in0=in0, scalar=scalar, in1=in1, op0=op0, op1=op1), r=r, w=w)

    def dma(self, eng, out, in_, r, w, slot):
        self.P.op(eng, lambda e: e.dma_start(out=out, in_=in_), r=r, w=w, slot=slot)

    def tap(self, name, ap, keys, shape, parts=128):
        if name not in self.taps:
            return
        t = self.nc.dram_tensor("tap_" + name, [parts] + list(shape), ap.dtype, kind="ExternalOutput").ap()
        self.tapouts[name] = t
        self.P.op("sp", lambda e: e.dma_start(out=t, in_=ap), r=keys, slot="tap_" + name)
        self.final_slots.append("tap_" + name)

    def rms_rstd(self, src_f32, src_keys, sq, tag):
        P = self.P
        for c in range(KC):
            self.act(sq[:, c, :], src_f32[:, c, :], AF.Square, r=[src_keys[c]], w=[("sq", c)])
        b = self.bank()
        for c in range(KC):
            self.mm(self.psb(b), self.ones_bf[:, :], sq[:, c, :], c == 0, c == KC - 1, r=[("sq", c), "ones_bf"], w=[("ps", b)])
        self.act(self.rstd[:, :], self.psb(b), AF.Sqrt, r=[("ps", b), "epsc"], w=["rstd"], scale=1.0 / D, bias=self.epsc[:, 0:1])
        P.op("dve", lambda e: e.reciprocal(out=self.rstd[:, :], in_=self.rstd[:, :]), r=["rstd"], w=["rstd"])

    def build(self):
        nc = bass.Bass("TRN2", target_bir_lowering=False)
        self.nc = nc
        self.final_slots = []
        P = self.P = Prog()
        xin = nc.dram_tensor("x", [128, KC, S], F32, kind="ExternalInput").ap()
        out = nc.dram_tensor("out", [128, KC, S], F32, kind="ExternalOutput").ap()
        xres = nc.dram_tensor("xres", [128, KC, S], F32, kind="Internal").ap()
        self.wstream = nc.dram_tensor("wstream", [NL, 128, self.wtotal], F32, kind="ExternalInput").ap()
        pfd = nc.dram_tensor("pf", [NL, 128, NPF], F32, kind="ExternalInput").ap()
        cfd = nc.dram_tensor("cf", [128, NCF], F32, kind="ExternalInput").ap()
        cbd = nc.dram_tensor("cb", [128, NCB], F32, kind="ExternalInput").ap()
        b31d = nc.dram_tensor("b31", [1, 16 * TB], F32, kind="ExternalInput").ap()
        with contextlib.ExitStack() as st:
            E = st.enter_context
            self.arena = E(nc.sbuf_tensor("arena", [128, ARENA // 2], BF16))
            hT = E(nc.sbuf_tensor("hT", [128, KC, S], BF16))
            merged = E(nc.sbuf_tensor("merged", [128, KC, S], BF16))
            self.ring = E(nc.sbuf_tensor("ring", [128, NRING * RSLOT], BF16))
            pf = E(nc.sbuf_tensor("pfs", [128, NPF], F32))
            self.pf_t = pf
            cf = E(nc.sbuf_tensor("cfs", [128, NCF], F32))
            cb = E(nc.sbuf_tensor("cbs", [128, NCB], BF16))
            self.ones_bf = E(nc.sbuf_tensor("ones_bf", [128, 128], BF16))
            self.rstd = E(nc.sbuf_tensor("rstd", [128, TB], F32))
            self.epsc = E(nc.sbuf_tensor("epsc", [128, 8], F32))
            self.hpi = E(nc.sbuf_tensor("hpi", [128, 8], F32))
            self.ps = E(nc.psum_tensor("ps", [128, 4096], F32))
            ps = self.ps
            ident = cb[:, CB_ID:CB_ID + 128]
            sel4 = cb[:, CB_SEL:CB_SEL + 512].rearrange("p (h q) -> p h q", h=4)
            biasT = cb[:, CB_BIAS:CB_BIAS + 4096].rearrange("p (k h q) -> p k h q", k=2, h=16)
            cmask = cf[:, CF_CMASK:CF_CMASK + 128]
            invc = cf[:, CF_INVC:CF_INVC + 16]
            ones64 = cf[:, CF_ONES:CF_ONES + 64]
            self.cf = cf

            P.op("dve", lambda e: e.memset(self.ones_bf[:, :], 1.0), w=["ones_bf"])
            P.op("dve", lambda e: e.memset(self.epsc[:, :], EPS), w=["epsc"])
            P.op("dve", lambda e: e.memset(self.hpi[:, :], math.pi / 2), w=["hpi"])
            self.dma("sp", cf[:, :], cfd, r=[], w=["cf"], slot="cf")
            P.op("pool", lambda e: e.dma_start(out=cb[:, :], in_=cbd, max_dma_last_dim=8192), w=["cb"], slot="cb")

            for l in range(self.nlayers):
                xsrc = xin if l == 0 else xres
                xdst = out if l == self.nlayers - 1 else xres
                self.layer(l, xsrc, xdst, hT, merged, pf, pfd, ident, sel4, biasT, cmask, invc, ones64, b31d)
                if self.stop is not None:
                    break
            P.barrier()
            P.emit(nc, final_slots=self.final_slots)
        return nc

    def layer(self, l, xsrc, xdst, hT, merged, pf, pfd, ident, sel4, biasT, cmask, invc, ones64, b31d):
        P = self.P
        nc = self.nc
        ps = self.ps
        stop = self.stop
        P.barrier()
        self.bankset = list(range(8))
        self.dma("sp", pf[:, :], pfd[l], r=[], w=["pf"], slot="pf")
        gmp = pf[:, PF_GMP:PF_GMP + 8]
        gmo = pf[:, PF_GMO:PF_GMO + 8]
        gfp = pf[:, PF_GFP:PF_GFP + 8]
        gfo = pf[:, PF_GFO:PF_GFO + 8]
        psc = pf[:, PF_PSC:PF_PSC + 4]

        xblk = self.carve(0, 128, [KC, TB], F32)
        sq = self.carve(16 * 1024, 128, [KC, TB], BF16)
        for n in range(NTB):
            tsl = slice(n * TB, (n + 1) * TB)
            self.dma("sp", xblk[:, :, :], xsrc[:, :, tsl], r=[], w=[("xblk", c) for c in range(KC)], slot="xblk")
            self.rms_rstd(xblk, [("xblk", c) for c in range(KC)], sq, "p1")
            for c in range(KC):
                self.stt(hT[:, c, tsl], xblk[:, c, :], gmp[:, c:c + 1], self.rstd[:, :], ALU.mult, ALU.mult,
                         r=[("xblk", c), "pf", "rstd"], w=[("hT", c, n)])
        self.tap("hT", hT[:, :, :], [("hT", c, n) for c in range(KC) for n in range(NTB)], [KC, S])
        if stop == 1:
            return
        P.barrier()

        kaugT = self.arena[0:65, KV0 // 2: KV0 // 2 + S]
        kiT = self.arena[0:64, KV0 // 2 + S: KV0 // 2 + 2 * S]
        vaug = self.arena[:, KV0 // 2 + 2 * S: KV0 // 2 + 2 * S + 16 * 66].rearrange("p (t d) -> p t d", t=16)
        wi_off = KV0 + 2 * (2 * S + 16 * 66)
        wi_s = self.arena[:, wi_off // 2: wi_off // 2 + 256].bitcast(F32).rearrange("p (t h) -> p t h", t=16)
        assert wi_off + 512 <= ARENA
        uz = self.carve(0, 128, [4, S], BF16)
        upp = [self.carve(16 * 1024, 128, [1, S], F32)[:, 0, :], self.carve(24 * 1024, 128, [1, S], F32)[:, 0, :]]
        dT = self.carve(32 * 1024, 128, [4, S], BF16)
        yp = self.carve(48 * 1024, 128, [4, S], BF16)
        sgt = self.carve(64 * 1024, 128, [1, TB], BF16)[:, 0, :]

        def hkeys(n):
            return [("hT", c, n) for c in range(KC)]

        seg, sk = self.wload(l, "kk", 1024)
        seg = seg.rearrange("p (k m) -> p k m", k=KC)
        P.op("dve", lambda e: e.memset(kaugT[64:65, :], 1.0), w=["kaug1"])
        P.op("dve", lambda e: e.memset(vaug[:, :, 64:65], 1.0), w=["vaug1"])
        for n in range(NTB):
            tsl = slice(n * TB, (n + 1) * TB)
            for half, dst, key in ((0, kaugT, "kT"), (1, kiT, "kiT")):
                b = self.bank()
                for kc in range(KC):
                    self.mm(self.psb(b, 64), seg[:, kc, half * 64:(half + 1) * 64], hT[:, kc, tsl], kc == 0, kc == KC - 1,
                            r=[sk, ("hT", kc, n)], w=[("ps", b)])
                self.act(dst[0:64, tsl], self.psb(b, 64), AF.Copy, r=[("ps", b)], w=[(key, n)])
        seg, sk = self.wload(l, "vw", KC * 72)
        seg = seg.rearrange("p (k m) -> p k m", k=KC)
        for tt_ in range(16):
            n = tt_ // 4
            b = self.bank()
            for kc in range(KC):
                self.mm(ps[:, b * 512: b * 512 + 72], hT[:, kc, tt_ * 128:(tt_ + 1) * 128], seg[:, kc, :], kc == 0, kc == KC - 1,
                        r=[sk, ("hT", kc, n)], w=[("ps", b)])
            self.act(vaug[:, tt_, 0:64], ps[:, b * 512: b * 512 + 64], AF.Copy, r=[("ps", b)], w=[("vaug", tt_)])
            self.act(wi_s[:, tt_, :], ps[:, b * 512 + 64: b * 512 + 72], AF.Copy, r=[("ps", b)], w=[("wi", tt_)], scale=IDX_SCALE)
        for cc in range(4):
            seg, sk = self.wload(l, "su%d" % cc, 1024)
            seg = seg.rearrange("p (k m) -> p k m", k=KC)
            for n in range(NTB):
                tsl = slice(n * TB, (n + 1) * TB)
                b = self.bank()
                for kc in range(KC):
                    self.mm(self.psb(b), seg[:, kc, :], hT[:, kc, tsl], kc == 0, kc == KC - 1, r=[sk, ("hT", kc, n)], w=[("ps", b)])
                self.act(uz[:, cc, tsl], self.psb(b), AF.Copy, r=[("ps", b)], w=[("uz", cc, n)])
        for cc in range(4):
            seg, sk = self.wload(l, "pu%d" % cc, 1024)
            seg = seg.rearrange("p (k m) -> p k m", k=KC)
            u0 = upp[0]
            for n in range(NTB):
                tsl = slice(n * TB, (n + 1) * TB)
                b = self.bank()
                for kc in range(KC):
                    self.mm(self.psb(b), seg[:, kc, :], hT[:, kc, tsl], kc == 0, kc == KC - 1, r=[sk, ("hT", kc, n)], w=[("ps", b)])
                self.act(u0[:, tsl], self.psb(b), AF.Copy, r=[("ps", b)], w=["up0"])
            wlen = 2 ** (cc + 1)
            sA = upp[1]
            sB = self.carve(66 * 1024, 128, [1, S], F32)[:, 0, :]
            bufs = [(sA, "upA"), (sB, "upB")]
            k = 1
            bi = 0
            src, srck = u0, "up0"
            while k < wlen:
                dstb, dstk = bufs[bi % 2]
                self.tt(dstb[:, k:], src[:, k:], src[:, :S - k], ALU.add, r=[srck], w=[dstk])
                P.op("dve", lambda e, d=dstb, s_=src, k=k: e.tensor_copy(out=d[:, 0:k], in_=s_[:, 0:k]), r=[srck], w=[dstk])
                src, srck = dstb, dstk
                bi += 1
                k *= 2
            self.stt(dT[:, cc, :], src[:, :], 1.0 / wlen, u0[:, :], ALU.mult, ALU.subtract, r=[srck, "up0"], w=[("dT", cc)])
            tmpc = self.carve(64 * 1024 + 1024, 128, [1, 16], F32)[:, 0, :]
            self.tt(tmpc[:, 0:wlen - 1], src[:, 0:wlen - 1], invc[:, 0:wlen - 1], ALU.mult, r=[srck, "cf"], w=["tmpc"])
            self.tt(dT[:, cc, 0:wlen - 1], tmpc[:, 0:wlen - 1], u0[:, 0:wlen - 1], ALU.subtract, r=["tmpc", "up0", ("dT", cc)], w=[("dT", cc)])
        self.tap("dT", dT[:, :, :], [("dT", cc) for cc in range(4)], [4, S])
        self.tap("kT", kaugT[:, :], [("kT", n) for n in range(NTB)] + ["kaug1"], [S], parts=65)
        self.tap("vaug", vaug[:, :, :], [("vaug", t) for t in range(16)] + ["vaug1"], [16, 66])
        self.tap("wi", wi_s[:, :, :], [("wi", t) for t in range(16)], [16, 8])
        if stop == 2:
            return

        seg, sk = self.wload(l, "mix", 512)
        seg = seg.rearrange("p (g m) -> p g m", g=4)
        for cc in range(4):
            for n in range(NTB):
                tsl = slice(n * TB, (n + 1) * TB)
                b = self.bank()
                self.mm(self.psb(b), seg[:, cc, :], dT[:, cc, tsl], True, True, r=[sk, ("dT", cc)], w=[("ps", b)])
                self.act(yp[:, cc, tsl], self.psb(b), AF.Copy, r=[("ps", b), "pf"], w=[("yp", cc, n)], scale=psc[:, cc:cc + 1])
        for c in range(KC):
            segp, skp = self.wload(l, "po%d" % c, 512)
            segp = segp.rearrange("p (k m) -> p k m", k=4)
            segg, skg = self.wload(l, "g0_%d" % c, 1024)
            segg = segg.rearrange("p (k m) -> p k m", k=KC)
            for n in range(NTB):
                tsl = slice(n * TB, (n + 1) * TB)
                by = self.bank()
                for cc in range(4):
                    self.mm(self.psb(by), segp[:, cc, :], yp[:, cc, tsl], cc == 0, cc == 3, r=[skp, ("yp", cc, n)], w=[("ps", by)])
                bg = self.bank()
                for kc in range(KC):
                    self.mm(self.psb(bg), segg[:, kc, :], hT[:, kc, tsl], kc == 0, kc == KC - 1, r=[skg, ("hT", kc, n)], w=[("ps", bg)])
                self.act(sgt[:, :], self.psb(bg), AF.Sigmoid, r=[("ps", bg)], w=["sgt"])
                self.tt(merged[:, c, tsl], sgt[:, :], self.psb(by), ALU.mult, r=["sgt", ("ps", by)], w=[("mg", c, n)])
        self.tap("mg3", merged[:, :, S - 256:S], [("mg", c, n) for c in range(KC) for n in range(NTB)], [KC, 256])
        if stop == 3:
            return
        P.barrier()

        self.phase_s5(l, hT, merged, pf, uz)
        self.tap("mg4", merged[:, :, S - 256:S], [("mg", c, n) for c in range(KC) for n in range(NTB)], [KC, 256])
        if stop == 4:
            return
        P.barrier()

        self.phase_attn(l, hT, merged, kaugT, kiT, vaug, wi_s, ident, sel4, biasT, cmask, ones64, b31d)
        self.tap("mg5", merged[:, :, S - 256:S], [("mg", c, n) for c in range(KC) for n in range(NTB)], [KC, 256])
        if stop == 5:
            return
        P.barrier()

        self.bankset = list(range(8))
        xT = self.carve(0, 128, [KC, S], F32)
        sq = hT[:, 4:6, :].rearrange("p a b -> p (a b)").rearrange("p (c t) -> p c t", c=KC)
        xblk = hT[:, 0:4, :].rearrange("p a b -> p (a b)").bitcast(F32).rearrange("p (c t) -> p c t", c=KC)
        tmpf = self.carve(64 * 1024, 128, [1, TB], F32)[:, 0, :]
        for c in range(KC):
            seg, sk = self.wload(l, "wo%d" % c, 1024)
            seg = seg.rearrange("p (k m) -> p k m", k=KC)
            for n in range(NTB):
                tsl = slice(n * TB, (n + 1) * TB)
                b = self.bank()
                for kc in range(KC):
                    self.mm(self.psb(b), seg[:, kc, :], merged[:, kc, tsl], kc == 0, kc == KC - 1, r=[sk, ("mg", kc, n)], w=[("ps", b)])
                self.act(xT[:, c, tsl], self.psb(b), AF.Copy, r=[("ps", b)], w=[("xT", c, n)])
        for n in range(NTB):
            tsl = slice(n * TB, (n + 1) * TB)
            self.dma("sp", xblk[:, :, :], xsrc[:, :, tsl], r=[], w=[("xblk", c) for c in range(KC)], slot="xblk")
            self.rms_rstd(xT[:, :, tsl], [("xT", c, n) for c in range(KC)], sq, "p6")
            for c in range(KC):
                self.stt(tmpf[:, :], xT[:, c, tsl], gmo[:, c:c + 1], self.rstd[:, :], ALU.mult, ALU.mult,
                         r=[("xT", c, n), "pf", "rstd"], w=["tmpf"])
                self.tt(xT[:, c, tsl], tmpf[:, :], xblk[:, c, :], ALU.add, r=["tmpf", ("xblk", c)], w=[("xT", c, n)])
        self.tap("x6", xT[:, :, S - 256:S], [("xT", c, n) for c in range(KC) for n in range(NTB)], [KC, 256])
        if stop == 6:
            return
        P.barrier()

        fT = self.carve(64 * 1024, 128, [NJ, TB], BF16)
        sq = merged[:, 4:6, :].rearrange("p a b -> p (a b)").rearrange("p (c t) -> p c t", c=KC)
        mbuf = merged[:, 0:4, :].rearrange("p a b -> p (a b)").bitcast(F32).rearrange("p (c t) -> p c t", c=KC)
        sgf = merged[:, 6, 0:TB]
        tmpf = merged[:, 7, 0:2 * TB].bitcast(F32)
        for n in range(NTB):
            tsl = slice(n * TB, (n + 1) * TB)
            self.rms_rstd(xT[:, :, tsl], [("xT", c, n) for c in range(KC)], sq, "p7a")
            for c in range(KC):
                self.stt(hT[:, c, tsl], xT[:, c, tsl], gfp[:, c:c + 1], self.rstd[:, :], ALU.mult, ALU.mult,
                         r=[("xT", c, n), "pf", "rstd"], w=[("hT", c, n)])
            for j in range(NJ):
                seg, sk = self.wload(l, "f%d" % j, 2048)
                seg = seg.rearrange("p (k m) -> p k m", k=KC)
                bg = self.bank()
                for kc in range(KC):
                    self.mm(self.psb(bg), seg[:, kc, 0:128], hT[:, kc, tsl], kc == 0, kc == KC - 1, r=[sk, ("hT", kc, n)], w=[("ps", bg)])
                bu = self.bank()
                for kc in range(KC):
                    self.mm(self.psb(bu), seg[:, kc, 128:256], hT[:, kc, tsl], kc == 0, kc == KC - 1, r=[sk, ("hT", kc, n)], w=[("ps", bu)])
                self.act(sgf, self.psb(bg), AF.Silu, r=[("ps", bg)], w=["sgf"])
                self.tt(fT[:, j, :], sgf, self.psb(bu), ALU.mult, r=["sgf", ("ps", bu)], w=[("fT", j)])
            for c in range(KC):
                seg, sk = self.wload(l, "fo%d" % c, NJ * 128)
                seg = seg.rearrange("p (k m) -> p k m", k=NJ)
                b = self.bank()
                for j in range(NJ):
                    self.mm(self.psb(b), seg[:, j, :], fT[:, j, :], j == 0, j == NJ - 1, r=[sk, ("fT", j)], w=[("ps", b)])
                self.act(mbuf[:, c, :], self.psb(b), AF.Copy, r=[("ps", b)], w=[("mbuf", c)])
            self.rms_rstd(mbuf, [("mbuf", c) for c in range(KC)], sq, "p7b")
            for c in range(KC):
                self.stt(tmpf, mbuf[:, c, :], gfo[:, c:c + 1], self.rstd[:, :], ALU.mult, ALU.mult,
                         r=[("mbuf", c), "pf", "rstd"], w=["tmpf"])
                self.tt(mbuf[:, c, :], tmpf, xT[:, c, tsl], ALU.add, r=["tmpf", ("xT", c, n)], w=[("mbuf", c)])
            self.dma("sp", xdst[:, :, tsl], mbuf[:, :, :], r=[("mbuf", c) for c in range(KC)], w=[], slot="xout")
        if "xout" not in self.final_slots:
            self.final_slots.append("xout")

    def phase_s5(self, l, hT, merged, pf, uz):
        P = self.P
        K = 1024
        Ec = self.carve(16 * K, 128, [1, S], F32)[:, 0, :]
        Es = self.carve(24 * K, 128, [1, S], F32)[:, 0, :]
        vre = self.carve(32 * K, 128, [1, S], F32)[:, 0, :]
        vim = self.carve(40 * K, 128, [1, S], F32)[:, 0, :]
        xre = self.carve(48 * K, 128, [1, S], BF16)[:, 0, :]
        xim = self.carve(52 * K, 128, [1, S], BF16)[:, 0, :]
        tmp1 = self.carve(56 * K, 128, [1, TB], F32)[:, 0, :]
        tmp2 = self.carve(58 * K, 128, [1, TB], F32)[:, 0, :]
        Bre = self.carve(60 * K, 128, [2, 4, 128], BF16)
        Bim = self.carve(62 * K, 128, [2, 4, 128], BF16)
        gt = self.carve(71 * K, 128, [1, TB], F32)[:, 0, :]
        sm = self.carve(64 * K, 128, [16, 16], F32)
        s1 = self.carve(66 * K, 128, [1, TB], BF16)[:, 0, :]
        s2 = self.carve(67 * K, 128, [1, TB], BF16)[:, 0, :]
        t3 = self.carve(68 * K, 128, [1, TB], F32)[:, 0, :]
        xw = self.carve(32 * K, 128, [8, 512], F32)
        smi = self.carve(70 * K, 128, [1, 16], I32)[:, 0, :]
        TWO_PI = 2.0 * math.pi

        def pfx(o):
            return pf[:, o:o + 512]

        def dve(fn, r, w):
            P.op("dve", fn, r=r, w=w)

        def zoh(lr, li, ld, wk, n, tag, itile):
            dt_, lrdt, th, kf, q, sn, cs, t0 = wk[:8]
            kk = ["z%s%d" % (tag, i) for i in range(8)]
            kdt, klrdt, kth, kkf, kq, ksn, kcs, kt0 = kk
            ki = "z%si" % tag
            self.act(dt_, ld, AF.Exp, r=["pf"], w=[kdt])
            self.tt(lrdt, lr, dt_, ALU.mult, r=["pf", kdt], w=[klrdt])
            self.tt(th, li, dt_, ALU.mult, r=["pf", kdt], w=[kth])
            self.ts(kf, th, 1.0 / TWO_PI, None, ALU.mult, ALU.bypass, r=[kth], w=[kkf])
            dve(lambda e: e.tensor_copy(out=itile, in_=kf), r=[kkf], w=[ki])
            dve(lambda e: e.tensor_copy(out=kf, in_=itile), r=[ki], w=[kkf])
            self.stt(q, kf, -TWO_PI, th, ALU.mult, ALU.add, r=[kkf, kth], w=[kq])
            self.act(sn, q, AF.Sin, r=[kq], w=[ksn], scale=0.25)
            self.act(cs, q, AF.Sin, r=[kq, "hpi"], w=[kcs], scale=0.25, bias=self.hpi[:, 0:1])
            for it in range(2):
                self.tt(t0, sn, sn, ALU.mult, r=[ksn], w=[kt0])
                self.stt(sn, sn, 2.0, cs, ALU.mult, ALU.mult, r=[ksn, kcs], w=[ksn])
                self.ts(cs, t0, -2.0, 1.0, ALU.mult, ALU.add, r=[kt0], w=[kcs])
            self.act(dt_, lrdt, AF.Exp, r=[klrdt], w=[kdt])
            return dict(mag=dt_, cos=cs, sin=sn, keys=[kdt, kcs, ksn], k=kk)

        wkx = [xw[:, i, :] for i in range(8)]
        smx = self.carve(56 * K, 128, [1, 512], I32)[:, 0, :]
        zx = zoh(pfx(PF_LRX), pfx(PF_LIX), pfx(PF_LDX), wkx, 512, "x", smx)
        K_ = zx["k"]
        mg_, cs_x, sn_x = wkx[0], wkx[6], wkx[5]
        are, aim, den, cre, cim = wkx[1], wkx[2], wkx[3], wkx[4], wkx[7]
        kare, kaim, kden, kcre, kcim = K_[1], K_[2], K_[3], K_[4], K_[7]
        kmg, kcs, ksn = K_[0], K_[6], K_[5]
        lr, li = pfx(PF_LRX), pfx(PF_LIX)
        self.tt(are, mg_, cs_x, ALU.mult, r=[kmg, kcs], w=[kare])
        self.tt(aim, mg_, sn_x, ALU.mult, r=[kmg, ksn], w=[kaim])
        self.ts(are, are, -1.0, None, ALU.add, ALU.bypass, r=[kare], w=[kare])
        t0, kt0 = wkx[0], K_[0]
        t1, kt1, t2, kt2 = wkx[5], K_[5], wkx[6], K_[6]
        self.tt(den, lr, lr, ALU.mult, r=["pf"], w=[kden])
        self.tt(cre, li, li, ALU.mult, r=["pf"], w=[kcre])
        self.tt(den, den, cre, ALU.add, r=[kden, kcre], w=[kden])
        dve(lambda e: e.reciprocal(out=den, in_=den), r=[kden], w=[kden])
        self.tt(cre, are, lr, ALU.mult, r=[kare, "pf"], w=[kcre])
        self.tt(t0, aim, li, ALU.mult, r=[kaim, "pf"], w=[kt0])
        self.tt(cre, cre, t0, ALU.add, r=[kcre, kt0], w=[kcre])
        self.tt(cre, cre, den, ALU.mult, r=[kcre, kden], w=[kcre])
        self.tt(cim, aim, lr, ALU.mult, r=[kaim, "pf"], w=[kcim])
        self.tt(t0, are, li, ALU.mult, r=[kare, "pf"], w=[kt0])
        self.tt(cim, cim, t0, ALU.subtract, r=[kcim, kt0], w=[kcim])
        self.tt(cim, cim, den, ALU.mult, r=[kcim, kden], w=[kcim])
        br, bi = pfx(PF_BRX), pfx(PF_BIX)
        self.tt(t1, cre, br, ALU.mult, r=[kcre, "pf"], w=[kt1])
        self.tt(t2, cim, bi, ALU.mult, r=[kcim, "pf"], w=[kt2])
        self.tt(t1, t1, t2, ALU.subtract, r=[kt1, kt2], w=[kt1])
        for v in range(2):
            self.ts(Bre[:, v, :, :].rearrange("p a b -> p (a b)"), t1, self.cf[:, CF_PAR + v:CF_PAR + v + 1], None, ALU.mult, ALU.bypass,
                    r=[kt1, "cf"], w=["Bre"])
        self.tt(t1, cre, bi, ALU.mult, r=[kcre, "pf"], w=[kt1])
        self.tt(t2, cim, br, ALU.mult, r=[kcim, "pf"], w=[kt2])
        self.tt(t1, t1, t2, ALU.add, r=[kt1, kt2], w=[kt1])
        for v in range(2):
            self.ts(Bim[:, v, :, :].rearrange("p a b -> p (a b)"), t1, self.cf[:, CF_PAR + v:CF_PAR + v + 1], None, ALU.mult, ALU.bypass,
                    r=[kt1, "cf"], w=["Bim"])

        wks = [sm[:, i, :] for i in range(8)]
        zs = zoh(pf[:, PF_LRS:PF_LRS + 16], pf[:, PF_LIS:PF_LIS + 16], pf[:, PF_LDS:PF_LDS + 16], wks, 16, "s", smi)
        mag, cth, sth = zs["mag"], zs["cos"], zs["sin"]
        nsc = sm[:, 8, :]

        segC, skC = self.wload(l, "sC", 4096)
        Cre = segC[:, 0:2048].rearrange("p (j m) -> p j m", j=16)
        Cim = segC[:, 2048:4096].rearrange("p (j m) -> p j m", j=16)
        segD, skD = self.wload(l, "sD", 512)
        Dg = segD.rearrange("p (c m) -> p c m", c=4)

        self.bankset = [0, 1, 2, 3]
        Ec2 = [self.carve((16 + 4 * i) * K, 128, [1, TB], F32)[:, 0, :] for i in range(2)]
        Es2 = [self.carve((18 + 4 * i) * K, 128, [1, TB], F32)[:, 0, :] for i in range(2)]
        vslot_re = [self.carve((32 + 4 * i) * K, 128, [1, TB], F32)[:, 0, :] for i in range(4)]
        vslot_im = [self.carve((34 + 4 * i) * K, 128, [1, TB], F32)[:, 0, :] for i in range(4)]
        pt4 = [self.carve(o * K, 128, [1, TB], F32)[:, 0, :] for o in (73, 24, 27, 29)]
        car = self.carve(26 * K, 128, [1, 16], F32)[:, 0, :]
        vi = 0
        pending = None
        for cc in range(4):
            ybanks = [4, 5, 6, 7]
            for jj in range(4):
                j = cc * 4 + jj
                rows = slice(64 * (jj // 2), 64 * (jj // 2) + 64)
                pv = jj % 2
                Ec, Es = Ec2[j % 2], Es2[j % 2]
                ke, ks = ("Ec", j % 2), ("Es", j % 2)
                dve(lambda e, j=j, Ec=Ec: e.tensor_copy(out=Ec[:, 0:1], in_=cth[:, j:j + 1]), r=zs["keys"], w=[ke])
                dve(lambda e, j=j, Es=Es: e.tensor_copy(out=Es[:, 0:1], in_=sth[:, j:j + 1]), r=zs["keys"], w=[ks])
                nn = 1
                lev = 0
                while nn < TB:
                    cs_ = Ec[:, nn - 1:nn]
                    ss_ = Es[:, nn - 1:nn]
                    ns_ = nsc[:, lev:lev + 1]
                    self.ts(ns_, ss_, -1.0, None, ALU.mult, ALU.bypass, r=[ks], w=["nsc"])
                    self.ts(Ec[:, nn:2 * nn], Ec[:, 0:nn], cs_, None, ALU.mult, ALU.bypass, r=[ke], w=[ke])
                    self.stt(Ec[:, nn:2 * nn], Es[:, 0:nn], ns_, Ec[:, nn:2 * nn], ALU.mult, ALU.add, r=[ks, "nsc", ke], w=[ke])
                    self.ts(Es[:, nn:2 * nn], Es[:, 0:nn], cs_, None, ALU.mult, ALU.bypass, r=[ks, ke], w=[ks])
                    self.stt(Es[:, nn:2 * nn], Ec[:, 0:nn], ss_, Es[:, nn:2 * nn], ALU.mult, ALU.add, r=[ke, ks], w=[ks])
                    nn *= 2
                    lev += 1
                cl, sl_, nsl = Ec[:, TB - 1:TB], Es[:, TB - 1:TB], nsc[:, 12:13]
                self.ts(nsl, sl_, -1.0, None, ALU.mult, ALU.bypass, r=[ks], w=["nsl"])
                for n in range(NTB):
                    tsl = slice(n * TB, (n + 1) * TB)
                    vre, vim = vslot_re[vi % 4], vslot_im[vi % 4]
                    kvr, kvi = ("vre", vi % 4), ("vim", vi % 4)
                    vi += 1
                    b1 = self.bank()
                    self.mm(self.psb(b1), Bre[rows, pv, cc, :], uz[rows, cc, tsl], True, True, r=["Bre", ("uz", cc, n)], w=[("ps", b1)])
                    b2 = self.bank()
                    self.mm(self.psb(b2), Bim[rows, pv, cc, :], uz[rows, cc, tsl], True, True, r=["Bim", ("uz", cc, n)], w=[("ps", b2)])
                    self.tt(vre, Ec, self.psb(b1), ALU.mult, r=[ke, ks, ("ps", b1)], w=[kvr])
                    self.tt(tmp1, Es, self.psb(b2), ALU.mult, r=[ks, ("ps", b2)], w=["tmp1"])
                    self.tt(vre, vre, tmp1, ALU.add, r=[kvr, "tmp1"], w=[kvr])
                    self.tt(vim, Ec, self.psb(b2), ALU.mult, r=[ke, ("ps", b2)], w=[kvi])
                    self.tt(tmp2, Es, self.psb(b1), ALU.mult, r=[ks, ("ps", b1)], w=["tmp2"])
                    self.tt(vim, vim, tmp2, ALU.subtract, r=[kvi, "tmp2"], w=[kvi])
                    if n == 0:
                        dve(lambda e, j=j, vre=vre: e.tensor_tensor_scan(out=vre, data0=mag[:, j:j + 1].to_broadcast([128, TB]), data1=vre,
                                                                        initial=0.0, op0=ALU.mult, op1=ALU.add), r=[kvr] + zs["keys"], w=[kvr])
                        dve(lambda e, j=j, vim=vim: e.tensor_tensor_scan(out=vim, data0=mag[:, j:j + 1].to_broadcast([128, TB]), data1=vim,
                                                                        initial=0.0, op0=ALU.mult, op1=ALU.add), r=[kvi] + zs["keys"], w=[kvi])
                    else:
                        dve(lambda e, j=j, vre=vre: e.tensor_tensor_scan(out=vre, data0=mag[:, j:j + 1].to_broadcast([128, TB]), data1=vre,
                                                                        initial=car[:, 0:1], op0=ALU.mult, op1=ALU.add), r=[kvr, "car"] + zs["keys"], w=[kvr])
                        dve(lambda e, j=j, vim=vim: e.tensor_tensor_scan(out=vim, data0=mag[:, j:j + 1].to_broadcast([128, TB]), data1=vim,
                                                                        initial=car[:, 1:2], op0=ALU.mult, op1=ALU.add), r=[kvi, "car"] + zs["keys"], w=[kvi])
                    if n < NTB - 1:
                        wr_l, wi_l = vre[:, TB - 1:TB], vim[:, TB - 1:TB]
                        self.ts(car[:, 2:3], wr_l, cl, None, ALU.mult, ALU.bypass, r=[kvr, ke], w=["car2"])
                        self.ts(car[:, 3:4], wr_l, sl_, None, ALU.mult, ALU.bypass, r=[kvr, ks], w=["car3"])
                        self.stt(car[:, 0:1], wi_l, nsl, car[:, 2:3], ALU.mult, ALU.add, r=[kvi, "nsl", "car2"], w=["car"])
                        self.stt(car[:, 1:2], wi_l, cl, car[:, 3:4], ALU.mult, ALU.add, r=[kvi, ke, "car3", "car"], w=["car"])
                    def ptt(out, in0, in1, op, r, w):
                        P.op("pool", lambda e: e.tensor_tensor(out=out, in0=in0, in1=in1, op=op), r=r, w=w)
                    yb = ybanks[n]
                    ptt(pt4[0], Ec, vre, ALU.mult, [ke, kvr], ["pt0"])
                    ptt(pt4[1], Es, vim, ALU.mult, [ks, kvi], ["pt1"])
                    if pending is not None:
                        pending()
                        pending = None
                    ptt(pt4[2], Es, vre, ALU.mult, [ks, kvr], ["pt2"])
                    ptt(pt4[3], Ec, vim, ALU.mult, [ke, kvi], ["pt3"])
                    ptt(xre[:, tsl], pt4[0], pt4[1], ALU.subtract, ["pt0", "pt1"], [("xre", n)])
                    ptt(pt4[2], pt4[2], pt4[3], ALU.add, ["pt2", "pt3"], ["pt2"])
                    self.mm(self.psb(yb), Cre[:, j, :], xre[:, tsl], jj == 0, False, r=[skC, ("xre", n)], w=[("ps", yb)])

                    def fin(j=j, jj=jj, cc=cc, n=n, tsl=tsl, yb=yb):
                        xo = xim[:, tsl]
                        P.op("pool", lambda e: e.tensor_scalar(out=xo, in0=pt4[2], scalar1=-1.0, scalar2=0.0, op0=ALU.mult, op1=ALU.add),
                             r=["pt2"], w=[("xim", n)])
                        self.mm(self.psb(yb), Cim[:, j, :], xim[:, tsl], False, False, r=[skC, ("xim", n)], w=[("ps", yb)])
                        if jj == 3:
                            self.mm(self.psb(yb), Dg[:, cc, :], uz[:, cc, tsl], False, True, r=[skD, ("uz", cc, n)], w=[("ps", yb)])
                    pending = fin
                pending()
                pending = None
            for n in range(NTB):
                tsl = slice(n * TB, (n + 1) * TB)
                yb = ybanks[n]
                self.act(gt, self.psb(yb), AF.Square, r=[("ps", yb)], w=["gt"])
                self.ts(gt, gt, 0.044715, 1.0, ALU.mult, ALU.add, r=["gt"], w=["gt"])
                self.tt(gt, gt, self.psb(yb), ALU.mult, r=["gt", ("ps", yb)], w=["gt"])
                self.act(t3, gt, AF.Sigmoid, r=["gt"], w=["t3"], scale=1.5957691216057308)
                self.tt(uz[:, cc, tsl], t3, self.psb(yb), ALU.mult, r=["t3", ("ps", yb)], w=[("uz", cc, n)])
        self.tap("zT", uz[:, :, :], [("uz", cc, n) for cc in range(4) for n in range(NTB)], [4, S])
        self.bankset = list(range(8))
        for c in range(KC):
            segl, skl = self.wload(l, "glu%d" % c, 1024)
            segl = segl.rearrange("p (a k m) -> p a k m", a=2, k=4)
            segg, skg = self.wload(l, "g2_%d" % c, 1024)
            segg = segg.rearrange("p (k m) -> p k m", k=KC)
            for n in range(NTB):
                tsl = slice(n * TB, (n + 1) * TB)
                ba = self.bank()
                for cc in range(4):
                    self.mm(self.psb(ba), segl[:, 0, cc, :], uz[:, cc, tsl], cc == 0, cc == 3, r=[skl, ("uz", cc, n)], w=[("ps", ba)])
                bb = self.bank()
                for cc in range(4):
                    self.mm(self.psb(bb), segl[:, 1, cc, :], uz[:, cc, tsl], cc == 0, cc == 3, r=[skl, ("uz", cc, n)], w=[("ps", bb)])
                bg = self.bank()
                for kc in range(KC):
                    self.mm(self.psb(bg), segg[:, kc, :], hT[:, kc, tsl], kc == 0, kc == KC - 1, r=[skg, ("hT", kc, n)], w=[("ps", bg)])
                self.act(s1, self.psb(bb), AF.Sigmoid, r=[("ps", bb)], w=["s1"])
                self.act(s2, self.psb(bg), AF.Sigmoid, r=[("ps", bg)], w=["s2"])
                self.tt(t3, s1, self.psb(ba), ALU.mult, r=["s1", ("ps", ba)], w=["t3"])
                self.tt(t3, t3, s2, ALU.mult, r=["t3", "s2"], w=["t3"])
                self.tt(merged[:, c, tsl], merged[:, c, tsl], t3, ALU.add, r=[("mg", c, n), "t3"], w=[("mg", c, n)])

    def phase_attn(self, l, hT, merged, kaugT, kiT, vaug, wi_s, ident, sel4, biasT, cmask, ones64, b31d):
        P = self.P
        K = 1024
        ps = self.ps
        qT = self.arena[0:65, 0: 16 * TB].rearrange("p (h q) -> p h q", h=16)
        qiT = self.arena[0:64, 8 * K: 8 * K + 8 * TB].rearrange("p (h q) -> p h q", h=8)
        OTn = self.arena[0:64, 12 * K: 12 * K + 16 * TB].rearrange("p (h q) -> p h q", h=16)
        score2 = [self.carve(40 * K, 128, [1, S], F32)[:, 0, :], self.pf_t[:, PF_LRX:PF_LRX + S]]
        rl = [self.carve(48 * K, 128, [1, TB], F32)[:, 0, :], self.carve(50 * K, 128, [1, TB], F32)[:, 0, :]]
        nm3 = [self.carve((52 + 4 * i) * K, 128, [1, S], BF16)[:, 0, :] for i in range(3)]
        self.idx_i = 0
        PT = [self.carve(64 * K, 128, [1, 1024], BF16)[:, 0, :], self.carve(66 * K, 128, [1, 1024], BF16)[:, 0, :]]
        ot = self.carve(68 * K, 64, [1, 1024], F32)[:, 0, :]
        bis = self.carve(72 * K, 128, [1, 16], F32)[:, 0, :]
        sgt = self.carve(73 * K, 128, [1, TB], BF16)[:, 0, :]
        lnr = self.arena[64:65, 12 * K: 12 * K + 2048].bitcast(F32)
        rrow = self.arena[64:65, 14 * K: 14 * K + 1024]
        P.op("pool", lambda e: e.dma_start(out=self.arena[64:65, 0:16 * TB], in_=b31d, max_dma_last_dim=8192), w=["qT64"], slot="b31")

        def emit_ao(nn):
            tsl = slice(nn * TB, (nn + 1) * TB)
            self.bankset = [0, 1, 2, 3, 4, 5]
            for c in range(KC):
                sego, sko = self.wload(l, "ao%d" % c, 2048)
                sego = sego.rearrange("p (h m) -> p h m", h=16)
                segg, skg = self.wload(l, "g1_%d" % c, 1024)
                segg = segg.rearrange("p (k m) -> p k m", k=KC)
                by = self.bank()
                for h in range(16):
                    self.mm(self.psb(by), sego[0:64, h, :], OTn[:, h, :], h == 0, h == 15, r=[sko] + [("OTn", i) for i in range(4)], w=[("ps", by)])
                bg = self.bank()
                for kc in range(KC):
                    self.mm(self.psb(bg), segg[:, kc, :], hT[:, kc, tsl], kc == 0, kc == KC - 1, r=[skg, ("hT", kc, nn)], w=[("ps", bg)])
                self.act(sgt, self.psb(bg), AF.Sigmoid, r=[("ps", bg)], w=["sgt"])
                t3 = rl[0]
                self.tt(t3, sgt, self.psb(by), ALU.mult, r=["sgt", ("ps", by)], w=[("rl", 0)])
                self.tt(merged[:, c, tsl], merged[:, c, tsl], t3, ALU.add, r=[("mg", c, nn), ("rl", 0)], w=[("mg", c, nn)])

        for n in range(NTB):
            tsl = slice(n * TB, (n + 1) * TB)
            self.bankset = list(range(8))
            for hp in range(8):
                seg, sk = self.wload(l, "q%d" % hp, 1024)
                seg = seg.rearrange("p (k m) -> p k m", k=KC)
                for half in range(2):
                    h = 2 * hp + half
                    b = self.bank()
                    for kc in range(KC):
                        self.mm(self.psb(b, 64), seg[:, kc, half * 64:(half + 1) * 64], hT[:, kc, tsl], kc == 0, kc == KC - 1,
                                r=[sk, ("hT", kc, n)], w=[("ps", b)])
                    self.act(qT[0:64, h, :], self.psb(b, 64), AF.Copy, r=[("ps", b)], w=[("qT", h)], scale=0.125)
            for hp in range(4):
                seg, sk = self.wload(l, "qi%d" % hp, 1024)
                seg = seg.rearrange("p (k m) -> p k m", k=KC)
                for half in range(2):
                    h = 2 * hp + half
                    b = self.bank()
                    for kc in range(KC):
                        self.mm(self.psb(b, 64), seg[:, kc, half * 64:(half + 1) * 64], hT[:, kc, tsl], kc == 0, kc == KC - 1,
                                r=[sk, ("hT", kc, n)], w=[("ps", b)])
                    self.act(qiT[0:64, h, :], self.psb(b, 64), AF.Copy, r=[("ps", b)], w=[("qiT", h)])
            if n == 0:
                self.tap("qT", qT[:, :, :], [("qT", h) for h in range(16)] + ["qT64"], [16, TB], parts=65)

            def genA(qq):
                qb = 4 * n + qq
                qsl = slice(qq * 128, (qq + 1) * 128)
                L = (qb + 1) * 128
                sc = score2[qb % 2]
                ngr = (L + 511) // 512
                for kg in range(ngr):
                    k0 = kg * 512
                    nk = min(512, L - k0)
                    sk_ = ("sc", qb % 2, kg)
                    for h in range(8):
                        b = 6 + (self.idx_i % 2)
                        r_ = rl[self.idx_i % 2]
                        rk = ("rl", self.idx_i % 2)
                        self.idx_i += 1
                        self.mm(self.psb(b, 128, nk), qiT[0:64, h, qsl], kiT[0:64, k0:k0 + nk], True, True,
                                r=[("qiT", h)] + [("kiT", i) for i in range(NTB)], w=[("ps", b)])
                        self.act(r_[:, 0:nk], self.psb(b, 128, nk), AF.Relu, r=[("ps", b)], w=[rk])
                        wcol = wi_s[:, qb, h:h + 1]
                        wk = [("wi", qb)]
                        if h == 0:
                            ndiag = nk - 128 if (k0 + nk == L) else nk
                            if ndiag > 0:
                                self.ts(sc[:, k0:k0 + ndiag], r_[:, 0:ndiag], wcol, None, ALU.mult, ALU.bypass, r=[rk] + wk, w=[sk_])
                            if k0 + nk == L:
                                self.stt(sc[:, L - 128:L], r_[:, nk - 128:nk], wcol, cmask, ALU.mult, ALU.add, r=[rk, "cf"] + wk, w=[sk_])
                        else:
                            self.stt(sc[:, k0:k0 + nk], r_[:, 0:nk], wcol, sc[:, k0:k0 + nk], ALU.mult, ALU.add,
                                     r=[rk, sk_] + wk, w=[sk_])
                        yield

            def genB(qq):
                qb = 4 * n + qq
                L = (qb + 1) * 128
                sc = score2[qb % 2]
                nmb = nm3[qb % 3]
                nmk = ("nm", qb % 3)
                ngr = (L + 511) // 512
                sck = [("sc", qb % 2, kg) for kg in range(ngr)]
                if qb >= 2:
                    o = 8 * (qb % 2)
                    cA, cB, cnt, tmpb, thr = bis[:, o:o + 1], bis[:, o + 1:o + 2], bis[:, o + 2:o + 3], bis[:, o + 3:o + 4], bis[:, o + 4:o + 5]
                    kp = "b%d" % (qb % 2)
                    P.op("dve", lambda e, cA=cA: e.memset(cA, 0.0), w=[kp + "c0"])
                    cur, nxt = cA, cB
                    curk, nxtk = kp + "c0", kp + "c1"
                    step = 4.0
                    for it in range(NBIS):
                        self.ts(nmb[:, 0:L], sc[:, 0:L], cur, None, ALU.is_ge, ALU.add, r=sck + [curk], w=[nmk, kp + "cnt"], accum_out=cnt)
                        self.ts(tmpb, cnt, 256.0, 2.0 * step, ALU.is_ge, ALU.mult, r=[kp + "cnt"], w=[kp + "tmpb"])
                        self.ts(nxt, tmpb, -step, cur, ALU.add, ALU.add, r=[kp + "tmpb", curk], w=[nxtk])
                        cur, nxt = nxt, cur
                        curk, nxtk = nxtk, curk
                        step *= 0.5
                        yield
                    self.ts(thr, cur, -2.0 * step - 1e-5, None, ALU.add, ALU.bypass, r=[curk], w=[kp + "thr"])
                    self.ts(nmb[:, 0:L], sc[:, 0:L], thr, NEG, ALU.is_lt, ALU.mult, r=sck + [kp + "thr"], w=[nmk])
                else:
                    self.ts(nmb[:, 0:L], sc[:, 0:L], -16.0, NEG, ALU.is_lt, ALU.mult, r=sck, w=[nmk])
                if qb == 5:
                    self.tap("score5", sc[:, 0:L], sck, [L])
                    self.tap("nm5", nmb[:, 0:L], [nmk], [L])
                yield

            def n_units_A(qq):
                qb = 4 * n + qq
                return 8 * (((qb + 1) * 128 + 511) // 512)

            def step_gen(g):
                if g is None:
                    return None
                try:
                    next(g)
                    return g
                except StopIteration:
                    return None

            def drain(g):
                while g is not None:
                    g = step_gen(g)

            def emit_attn(qq, gB, nB, gA, nA):
                qb = 4 * n + qq
                qsl = slice(qq * 128, (qq + 1) * 128)
                nmb = nm3[qb % 3]
                nmk = ("nm", qb % 3)
                niter = 2 * (qb + 1)
                perB = -(-nB // niter)
                perA = -(-nA // niter)
                for hh in range(2):
                    def emit_S(kb):
                        sb = kb % 2
                        near = (qb - kb) < 2
                        kk = 64 if near else 65
                        ksl = slice(kb * 128, (kb + 1) * 128)
                        for bk in range(2):
                            bnk = 2 * sb + bk
                            hs = slice(hh * 8 + bk * 4, hh * 8 + bk * 4 + 4)
                            outp = self.psb(bnk).rearrange("p (h q) -> p h q", h=4)
                            qk = [("qT", h) for h in range(hh * 8 + bk * 4, hh * 8 + bk * 4 + 4)] + ["qT64"]
                            self.mm(outp, kaugT[0:kk, ksl], qT[0:kk, hs, qsl], True, False,
                                    r=qk + [("kT", kb // 4), "kaug1"], w=[("ps", bnk)])
                            self.mm(outp, nmb[:, ksl], sel4, False, not near, r=[nmk, "cb"], w=[("ps", bnk)])
                            if near:
                                self.mm(outp, ident, biasT[:, qb - kb, hs, :], False, True, r=["cb"], w=[("ps", bnk)])
                        self.act(PT[sb][:, :], ps[:, 2 * sb * 512: 2 * sb * 512 + 1024], AF.Exp,
                                 r=[("ps", 2 * sb), ("ps", 2 * sb + 1)], w=[("PT", sb)])

                    def emit_PV(kb):
                        sb = kb % 2
                        for bk in range(2):
                            self.mm(self.psb(4 + bk, 65), vaug[:, kb, 0:65], PT[sb][:, bk * 512:(bk + 1) * 512], kb == 0, kb == qb,
                                    r=[("PT", sb), ("vaug", kb), "vaug1"], w=[("ps", 4 + bk)])

                    for kb in range(qb + 1):
                        emit_S(kb)
                        if kb > 0:
                            emit_PV(kb - 1)
                        for _ in range(perB):
                            gB = step_gen(gB)
                        for _ in range(perA):
                            gA = step_gen(gA)
                    emit_PV(qb)
                    if hh == 1:
                        drain(gB)
                        drain(gA)
                        gB = gA = None
                    self.act(lnr, ps[64:65, 2048:3072], AF.Ln, r=[("ps", 4), ("ps", 5)], w=["lnr"])
                    self.act(rrow, lnr, AF.Exp, r=["lnr"], w=["rrow"], scale=-1.0)
                    for bk in range(2):
                        self.mm(self.psb(6 + bk, 64), self.ones_bf[64:65, 0:64], rrow[:, bk * 512:(bk + 1) * 512], True, True,
                                r=["ones_bf", "rrow"], w=[("ps", 6 + bk)])
                    self.act(ot[:, :], ps[0:64, 2048:3072], AF.Copy, r=[("ps", 4), ("ps", 5)], w=["ot"])
                    for bk in range(2):
                        hs = slice(hh * 8 + bk * 4, hh * 8 + bk * 4 + 4)
                        self.tt(OTn[:, hs, qsl], ot[:, bk * 512:(bk + 1) * 512].rearrange("p (h q) -> p h q", h=4),
                                self.psb(6 + bk, 64).rearrange("p (h q) -> p h q", h=4), ALU.mult,
                                r=["ot", ("ps", 6 + bk)], w=[("OTn", hh * 2 + bk)])

            drain(genA(0))
            drain(genA(1))
            if n > 0:
                emit_ao(n - 1)
            drain(genB(0))
            for qq in range(4):
                gB = genB(qq + 1) if qq + 1 < 4 else None
                gA = genA(qq + 2) if qq + 2 < 4 else None
                emit_attn(qq, gB, NBIS + 1, gA, n_units_A(qq + 2) if qq + 2 < 4 else 0)
            if n == 0:
                self.tap("OTn", OTn[:, :, :], [("OTn", i) for i in range(4)], [16, TB], parts=64)
        emit_ao(NTB - 1)


def km(w):
    k, m = w.shape
    return np.ascontiguousarray(w.reshape(k // 128, 128, m).transpose(1, 0, 2)).reshape(128, (k // 128) * m)


def host_segments(inp, l):
    w = inp["w_in"][l]
    segs = {}
    segs["kk"] = km(np.concatenate([w[:, C_K:C_K + 64], w[:, C_KI:C_KI + 64]], axis=1))
    segs["vw"] = km(np.concatenate([w[:, C_V:C_V + 64], w[:, C_WI:C_WI + 8]], axis=1))
    for cc in range(4):
        segs["pu%d" % cc] = km(w[:, C_POOL + cc * 128: C_POOL + (cc + 1) * 128])
        segs["su%d" % cc] = km(w[:, C_S5 + cc * 128: C_S5 + (cc + 1) * 128])
    segs["mix"] = np.ascontiguousarray(inp["pool_mix_w"][l].transpose(1, 0, 2)).reshape(128, 512)
    for c in range(8):
        cs = slice(c * 128, (c + 1) * 128)
        segs["po%d" % c] = km(inp["pool_out_w"][l][:, cs])
        for b in range(3):
            segs["g%d_%d" % (b, c)] = km(w[:, C_G + b * 1024 + c * 128: C_G + b * 1024 + (c + 1) * 128])
        glu = inp["s5_glu_w"][l]
        segs["glu%d" % c] = np.concatenate([km(glu[:, cs]), km(glu[:, 1024 + c * 128: 1024 + (c + 1) * 128])], axis=1)
        segs["q%d" % c] = km(w[:, C_Q + c * 128: C_Q + (c + 1) * 128])
        ao = inp["attn_out_w"][l][:, cs].reshape(16, 64, 128).transpose(1, 0, 2).reshape(64, 2048)
        segs["ao%d" % c] = np.concatenate([ao, np.zeros((64, 2048), np.float32)], axis=0)
        segs["wo%d" % c] = km(inp["w_out"][l][:, cs])
        segs["fo%d" % c] = km(inp["ffn_w_out"][l][:, cs])
    for hp in range(4):
        segs["qi%d" % hp] = km(w[:, C_QI + hp * 128: C_QI + (hp + 1) * 128])
    fw = inp["ffn_w_in"][l]
    for j in range(NJ):
        segs["f%d" % j] = km(np.concatenate([fw[:, j * 128:(j + 1) * 128], fw[:, FH + j * 128: FH + (j + 1) * 128]], axis=1))
    cre = inp["s5_c_re"][l]
    cim = inp["s5_c_im"][l]
    Cre = np.zeros((128, 16, 128), np.float32)
    Cim = np.zeros((128, 16, 128), np.float32)
    for g in range(32):
        j, g2 = g // 2, g % 2
        jj = j % 4
        m0 = 32 * jj + 16 * g2
        Cre[g2 * 64:(g2 + 1) * 64, j, m0:m0 + 16] = cre[g].T
        Cim[g2 * 64:(g2 + 1) * 64, j, m0:m0 + 16] = cim[g].T
    segs["sC"] = np.concatenate([Cre.reshape(128, 2048), Cim.reshape(128, 2048)], axis=1)
    Dg = np.zeros((128, 4, 128), np.float32)
    d = inp["s5_d"][l]
    for cc in range(4):
        Dg[np.arange(128), cc, np.arange(128)] = d[cc * 128:(cc + 1) * 128]
    segs["sD"] = Dg.reshape(128, 512)
    return segs


def host_pf(inp, l):
    pf = np.zeros((128, NPF), np.float32)
    for off, name in ((PF_GMP, "norm_mix_pre"), (PF_GMO, "norm_mix_post"), (PF_GFP, "norm_ffn_pre"), (PF_GFO, "norm_ffn_post")):
        pf[:, off:off + 8] = inp[name][l].reshape(8, 128).T
    pf[:, PF_PSC:PF_PSC + 4] = inp["pool_scale"][l].reshape(4, 128).T
    lr, li, ld = inp["s5_lambda_re"][l], inp["s5_lambda_im"][l], inp["s5_log_dt"][l]
    for j in range(16):
        for g2 in range(2):
            g = 2 * j + g2
            pf[g2 * 64:(g2 + 1) * 64, PF_LRS + j] = lr[g]
            pf[g2 * 64:(g2 + 1) * 64, PF_LIS + j] = li[g]
            pf[g2 * 64:(g2 + 1) * 64, PF_LDS + j] = ld[g]
    br, bi = inp["s5_b_re"][l], inp["s5_b_im"][l]
    X = np.zeros((5, 128, 4, 128), np.float32)
    for cc in range(4):
        for p in range(128):
            g = cc * 8 + p // 16
            i = p % 16
            X[0, p, cc, :] = np.tile(lr[g], 2)
            X[1, p, cc, :] = np.tile(li[g], 2)
            X[2, p, cc, :] = ld[g]
            g2 = g % 2
            X[3, p, cc, g2 * 64:(g2 + 1) * 64] = br[g, :, i]
            X[4, p, cc, g2 * 64:(g2 + 1) * 64] = bi[g, :, i]
    for k, off in enumerate((PF_LRX, PF_LIX, PF_LDX, PF_BRX, PF_BIX)):
        pf[:, off:off + 512] = X[k].reshape(128, 512)
    return pf


def host_consts(inp):
    cf = np.zeros((128, NCF), np.float32)
    q = np.arange(128)[:, None]
    s = np.arange(128)[None, :]
    cf[:, CF_CMASK:CF_CMASK + 128] = np.where(s > q, np.float32(-1e4), np.float32(0.0))
    cf[:, CF_INVC:CF_INVC + 16] = (1.0 / np.arange(1, 17, dtype=np.float32))[None, :]
    cf[:, CF_ONES:CF_ONES + 64] = 1.0
    par = ((np.arange(128) // 32) % 2).astype(np.float32)
    cf[:, CF_PAR] = 1.0 - par
    cf[:, CF_PAR + 1] = par
    cb = np.zeros((128, NCB), np.float32)
    cb[:, CB_ID:CB_ID + 128] = np.eye(128, dtype=np.float32)
    cb[:, CB_SEL:CB_SEL + 512] = np.tile(np.eye(128, dtype=np.float32), (1, 4))
    rb = inp["rel_bias"]
    sl = np.arange(128)[:, None]
    ql = np.arange(128)[None, :]
    bt = np.zeros((128, 2, 16, 128), np.float32)
    for kind in range(2):
        dist = ql - sl + 128 * kind
        idx = rel_bucket_np(np.maximum(dist, 0))
        bt[:, kind, :, :] = rb[idx].transpose(0, 2, 1)
    cb[:, CB_BIAS:CB_BIAS + 4096] = bt.reshape(128, 4096)
    b31 = np.ascontiguousarray(np.repeat(rb[31][:, None], TB, axis=1)).reshape(1, 16 * TB).astype(np.float32)
    return cf, cb, b31


_CACHE = {}


def get_program(nlayers=NL, stop=None, taps=()):
    key = (nlayers, stop, tuple(taps))
    if key not in _CACHE:
        b0 = Builder(nlayers, stop, taps)
        b0.wtotal = 1 << 20
        b0.build()
        b = Builder(nlayers, stop, taps)
        b.wtotal = max(b0.seg_total, 16)
        nc = b.build()
        _CACHE[key] = (nc, b)
    return _CACHE[key]


def run(inputs, nlayers=NL, stop=None, taps=()):
    nc, b = get_program(nlayers, stop, taps)
    inp = {k: np.asarray(v, dtype=np.float32) for k, v in inputs.items()}
    ws = np.zeros((NL, 128, b.wtotal), np.float32)
    pfs = np.zeros((NL, 128, NPF), np.float32)
    for l in range(NL):
        segs = host_segments(inp, l)
        for name, (off, n) in b.seg_off.items():
            a = segs[name]
            assert a.shape == (128, n), (name, a.shape, n)
            ws[l, :, off:off + n] = a
        pfs[l] = host_pf(inp, l)
    cf, cb, b31 = host_consts(inp)
    x = inp["x"]
    in_maps = []
    for c in range(8):
        xt = np.ascontiguousarray(x[c].T.reshape(KC, 128, S).transpose(1, 0, 2))
        in_maps.append({"x": xt, "wstream": ws, "pf": pfs, "cf": cf, "cb": cb, "b31": b31})
    res = run_bass_kernel_spmd(nc, in_maps, core_ids=list(range(8)))
    return res, b


def kernel(**inputs):
    res, b = run(inputs)
    outs = []
    for c in range(8):
        o = res.results[c]["out"]
        outs.append(np.ascontiguousarray(o.transpose(1, 0, 2).reshape(D, S).T))
    return np.stack(outs, axis=0).astype(np.float32)
```

```python
import contextlib
import math
import numpy as np
import concourse.bass as bass
import concourse.mybir as mybir
from concourse.bass_utils import run_bass_kernel_spmd

F32 = mybir.dt.float32
BF16 = mybir.dt.bfloat16
I32 = mybir.dt.int32
ALU = mybir.AluOpType
AF = mybir.ActivationFunctionType

ENGS = ("pe", "act", "dve", "pool", "sp")


class Op:
    __slots__ = ("eng", "fn", "waits", "signal", "slot", "seq", "dcount")

    def __init__(self, eng, fn):
        self.eng = eng
        self.fn = fn
        self.waits = {}
        self.signal = False
        self.slot = None
        self.seq = 0
        self.dcount = 0


class Prog:
    def __init__(self):
        self.ops = {e: [] for e in ENGS}
        self.last_w = {}
        self.readers = {}
        self.slot_count = {}
        self.pending = {e: {} for e in ENGS}

    def _add_wait(self, op, tok, raw):
        kind, who, val = tok
        if kind == "E":
            if who == op.eng and not raw:
                return
            if who == "pe" and op.eng == "pe":
                return
            self.ops[who][val].signal = True
        k = (kind, who)
        if op.waits.get(k, -1) < val:
            op.waits[k] = val

    def op(self, eng, fn, r=(), w=(), slot=None):
        o = Op(eng, fn)
        o.seq = len(self.ops[eng])
        if slot is not None:
            o.slot = slot
            self.slot_count[slot] = self.slot_count.get(slot, 0) + 1
            o.dcount = self.slot_count[slot]
            tok = ("D", slot, o.dcount)
        else:
            tok = ("E", eng, o.seq)
        for k, v in self.pending[eng].items():
            if o.waits.get(k, -1) < v:
                o.waits[k] = v
        self.pending[eng] = {}
        for k in r:
            lw = self.last_w.get(k)
            if lw is not None:
                self._add_wait(o, lw, True)
        for k in w:
            lw = self.last_w.get(k)
            if lw is not None:
                self._add_wait(o, lw, False)
            for t in self.readers.get(k, ()):
                self._add_wait(o, t, False)
        self.ops[eng].append(o)
        for k in r:
            self.readers.setdefault(k, []).append(tok)
        for k in w:
            self.last_w[k] = tok
            self.readers[k] = []
        return o

    def barrier(self):
        toks = {}
        for e in ENGS:
            for o in reversed(self.ops[e]):
                if o.slot is None:
                    o.signal = True
                    toks[("E", e)] = o.seq
                    break
        for s, c in self.slot_count.items():
            toks[("D", s)] = c
        for e in ENGS:
            for k, v in toks.items():
                if k == ("E", e):
                    continue
                if self.pending[e].get(k, -1) < v:
                    self.pending[e][k] = v
        self.last_w = {}
        self.readers = {}

    def emit(self, nc, final_slots=()):
        with contextlib.ExitStack() as st:
            esem = {e: st.enter_context(nc.semaphore("s_" + e)) for e in ENGS}
            dsem = {s: st.enter_context(nc.semaphore("d_%d" % i)) for i, s in enumerate(self.slot_count)}
            sigcount = {}
            for e in ENGS:
                c = 0
                arr = []
                for o in self.ops[e]:
                    if o.slot is None and o.signal:
                        c += 1
                    arr.append(c)
                sigcount[e] = arr
            block = st.enter_context(nc.Block())
            prog = self

            def make(e):
                def body(eng):
                    waited = {}
                    for o in prog.ops[e]:
                        for (kind, who), val in o.waits.items():
                            if kind == "E":
                                sem = esem[who]
                                v = sigcount[who][val]
                            else:
                                sem = dsem[who]
                                v = 16 * val
                            if waited.get((kind, who), -1) >= v:
                                continue
                            waited[(kind, who)] = v
                            eng.wait_ge(sem, v)
                        ins = o.fn(eng)
                        if o.slot is not None:
                            ins.then_inc(dsem[o.slot], 16)
                        elif o.signal:
                            ins.then_inc(esem[e], 1)
                    if e == "sp":
                        for s in final_slots:
                            eng.wait_ge(dsem[s], 16 * prog.slot_count[s])
                return body

            block.tensor(make("pe"))
            block.scalar(make("act"))
            block.vector(make("dve"))
            block.gpsimd(make("pool"))
            block.sync(make("sp"))


S = 2048
D = 1024
TB = 512
NTB = 4
KC = 8
NL = 2
FH = 2816
NJ = 22
EPS = 1e-6
IDX_SCALE = (8 ** -0.5) * (64 ** -0.5)
NEG = -30000.0
NBIS = 15

C_POOL, C_Q, C_K, C_V, C_QI, C_KI, C_WI, C_S5, C_G = 0, 512, 1536, 1600, 1664, 2176, 2240, 2248, 2760

PF_GMP, PF_GMO, PF_GFP, PF_GFO, PF_PSC = 0, 8, 16, 24, 32
PF_LRS, PF_LIS, PF_LDS = 36, 52, 68
PF_LRX, PF_LIX, PF_LDX, PF_BRX, PF_BIX = 84, 596, 1108, 1620, 2132
NPF = 2644
CF_CMASK, CF_INVC, CF_ONES, CF_PAR = 0, 128, 144, 208
NCF = 212
CB_ID, CB_SEL, CB_BIAS = 0, 128, 640
NCB = 640 + 4096

ARENA = 86 * 1024
RSLOT = 4096
NRING = 4
KV0 = 75 * 1024


def rel_bucket_np(dist):
    dist = np.asarray(dist, np.int32)
    d_f = np.maximum(dist, 1).astype(np.float32)
    large = 16 + (np.log(d_f / np.float32(16)) / np.float32(math.log(128 / 16)) * np.float32(16)).astype(np.int32)
    large = np.minimum(large, 31)
    return np.where(dist < 16, dist, large)


class Builder:
    def __init__(self, nlayers=NL, stop=None, taps=()):
        self.nlayers = nlayers
        self.stop = stop
        self.taps = taps
        self.seg_off = {}
        self.seg_total = 0
        self.ring_i = 0
        self.bank_i = 0
        self.bankset = list(range(8))
        self.tapouts = {}

    def carve(self, off, parts, shape, dt):
        esz = 2 if dt == BF16 else 4
        n = int(np.prod(shape))
        assert off % 4 == 0 and off + n * esz <= ARENA, (off, n, esz)
        ap = self.arena[0:parts, off // 2: off // 2 + n * esz // 2]
        if dt != BF16:
            ap = ap.bitcast(dt)
        if len(shape) == 2:
            return ap.rearrange("p (a b) -> p a b", a=shape[0])
        if len(shape) == 3:
            return ap.rearrange("p (a b c) -> p a b c", a=shape[0], b=shape[1])
        return ap

    def bank(self):
        b = self.bankset[self.bank_i % len(self.bankset)]
        self.bank_i += 1
        return b

    def psb(self, b, parts=128, n=512):
        return self.ps[0:parts, b * 512: b * 512 + n]

    def wload(self, l, name, n):
        assert n <= RSLOT
        if name not in self.seg_off:
            self.seg_off[name] = (self.seg_total, n)
            self.seg_total += n
        off, n0 = self.seg_off[name]
        assert n0 == n
        slot = self.ring_i % NRING
        self.ring_i += 1
        dst = self.ring[:, slot * RSLOT: slot * RSLOT + n]
        src = self.wstream[l, :, off:off + n]
        key = ("ring", slot)
        self.P.op("pool", lambda e: e.dma_start(out=dst, in_=src, max_dma_last_dim=8192), w=[key], slot="ring%d" % slot)
        return dst, key

    def mm(self, out, lhsT, rhs, start, stop, r, w):
        self.P.op("pe", lambda e: e.matmul(out, lhsT, rhs, start=start, stop=stop), r=r, w=w)

    def act(self, out, in_, func, r, w, scale=1.0, bias=0.0):
        self.P.op("act", lambda e: e.activation(out=out, in_=in_, func=func, bias=bias, scale=scale), r=r, w=w)

    def tt(self, out, in0, in1, op, r, w):
        self.P.op("dve", lambda e: e.tensor_tensor(out=out, in0=in0, in1=in1, op=op), r=r, w=w)

    def ts(self, out, in0, s1, s2, op0, op1, r, w, accum_out=None):
        if accum_out is None:
            self.P.op("dve", lambda e: e.tensor_scalar(out=out, in0=in0, scalar1=s1, scalar2=s2, op0=op0, op1=op1), r=r, w=w)
        else:
            self.P.op("dve", lambda e: e.tensor_scalar(out=out, in0=in0, scalar1=s1, scalar2=s2, op0=op0, op1=op1, accum_out=accum_out), r=r, w=w)

    def stt(self, out, in0, scalar, in1, op0, op1, r, w):
        self.P.op("dve", lambda e: e.scalar_tensor_tensor(out=out, in0=in0, scalar=scalar, in1=in1, op0=op0, op1=op1), r=r, w=w)

    def dma(self, eng, out, in_, r, w, slot):
        self.P.op(eng, lambda e: e.dma_start(out=out, in_=in_), r=r, w=w, slot=slot)

    def tap(self, name, ap, keys, shape, parts=128):
        if name not in self.taps:
            return
        t = self.nc.dram_tensor("tap_" + name, [parts] + list(shape), ap.dtype, kind="ExternalOutput").ap()
        self.tapouts[name] = t
        self.P.op("sp", lambda e: e.dma_start(out=t, in_=ap), r=keys, slot="tap_" + name)
        self.final_slots.append("tap_" + name)

    def rms_rstd(self, src_f32, src_keys, sq, tag):
        P = self.P
        for c in range(KC):
            self.act(sq[:, c, :], src_f32[:, c, :], AF.Square, r=[src_keys[c]], w=[("sq", c)])
        b = self.bank()
        for c in range(KC):
            self.mm(self.psb(b), self.ones_bf[:, :], sq[:, c, :], c == 0, c == KC - 1, r=[("sq", c), "ones_bf"], w=[("ps", b)])
        self.act(self.rstd[:, :], self.psb(b), AF.Sqrt, r=[("ps", b), "epsc"], w=["rstd"], scale=1.0 / D, bias=self.epsc[:, 0:1])
        P.op("dve", lambda e: e.reciprocal(out=self.rstd[:, :], in_=self.rstd[:, :]), r=["rstd"], w=["rstd"])

    def build(self):
        nc = bass.Bass("TRN2", target_bir_lowering=False)
        self.nc = nc
        self.final_slots = []
        P = self.P = Prog()
        xin = nc.dram_tensor("x", [128, KC, S], F32, kind="ExternalInput").ap()
        out = nc.dram_tensor("out", [128, KC, S], F32, kind="ExternalOutput").ap()
        xres = nc.dram_tensor("xres", [128, KC, S], F32, kind="Internal").ap()
        self.wstream = nc.dram_tensor("wstream", [NL, 128, self.wtotal], F32, kind="ExternalInput").ap()
        pfd = nc.dram_tensor("pf", [NL, 128, NPF], F32, kind="ExternalInput").ap()
        cfd = nc.dram_tensor("cf", [128, NCF], F32, kind="ExternalInput").ap()
        cbd = nc.dram_tensor("cb", [128, NCB], F32, kind="ExternalInput").ap()
        b31d = nc.dram_tensor("b31", [1, 16 * TB], F32, kind="ExternalInput").ap()
        with contextlib.ExitStack() as st:
            E = st.enter_context
            self.arena = E(nc.sbuf_tensor("arena", [128, ARENA // 2], BF16))
            hT = E(nc.sbuf_tensor("hT", [128, KC, S], BF16))
            merged = E(nc.sbuf_tensor("merged", [128, KC, S], BF16))
            self.ring = E(nc.sbuf_tensor("ring", [128, NRING * RSLOT], BF16))
            pf = E(nc.sbuf_tensor("pfs", [128, NPF], F32))
            self.pf_t = pf
            cf = E(nc.sbuf_tensor("cfs", [128, NCF], F32))
            cb = E(nc.sbuf_tensor("cbs", [128, NCB], BF16))
            self.ones_bf = E(nc.sbuf_tensor("ones_bf", [128, 128], BF16))
            self.rstd = E(nc.sbuf_tensor("rstd", [128, TB], F32))
            self.epsc = E(nc.sbuf_tensor("epsc", [128, 8], F32))
            self.hpi = E(nc.sbuf_tensor("hpi", [128, 8], F32))
            self.ps = E(nc.psum_tensor("ps", [128, 4096], F32))
            ps = self.ps
            ident = cb[:, CB_ID:CB_ID + 128]
            sel4 = cb[:, CB_SEL:CB_SEL + 512].rearrange("p (h q) -> p h q", h=4)
            biasT = cb[:, CB_BIAS:CB_BIAS + 4096].rearrange("p (k h q) -> p k h q", k=2, h=16)
            cmask = cf[:, CF_CMASK:CF_CMASK + 128]
            invc = cf[:, CF_INVC:CF_INVC + 16]
            ones64 = cf[:, CF_ONES:CF_ONES + 64]
            self.cf = cf

            P.op("dve", lambda e: e.memset(self.ones_bf[:, :], 1.0), w=["ones_bf"])
            P.op("dve", lambda e: e.memset(self.epsc[:, :], EPS), w=["epsc"])
            P.op("dve", lambda e: e.memset(self.hpi[:, :], math.pi / 2), w=["hpi"])
            self.dma("sp", cf[:, :], cfd, r=[], w=["cf"], slot="cf")
            P.op("pool", lambda e: e.dma_start(out=cb[:, :], in_=cbd, max_dma_last_dim=8192), w=["cb"], slot="cb")

            for l in range(self.nlayers):
                xsrc = xin if l == 0 else xres
                xdst = out if l == self.nlayers - 1 else xres
                self.layer(l, xsrc, xdst, hT, merged, pf, pfd, ident, sel4, biasT, cmask, invc, ones64, b31d)
                if self.stop is not None:
                    break
            P.barrier()
            P.emit(nc, final_slots=self.final_slots)
        return nc

    def layer(self, l, xsrc, xdst, hT, merged, pf, pfd, ident, sel4, biasT, cmask, invc, ones64, b31d):
        P = self.P
        nc = self.nc
        ps = self.ps
        stop = self.stop
        P.barrier()
        self.bankset = list(range(8))
        self.dma("sp", pf[:, :], pfd[l], r=[], w=["pf"], slot="pf")
        gmp = pf[:, PF_GMP:PF_GMP + 8]
        gmo = pf[:, PF_GMO:PF_GMO + 8]
        gfp = pf[:, PF_GFP:PF_GFP + 8]
        gfo = pf[:, PF_GFO:PF_GFO + 8]
        psc = pf[:, PF_PSC:PF_PSC + 4]

        xblk = self.carve(0, 128, [KC, TB], F32)
        sq = self.carve(16 * 1024, 128, [KC, TB], BF16)
        for n in range(NTB):
            tsl = slice(n * TB, (n + 1) * TB)
            self.dma("sp", xblk[:, :, :], xsrc[:, :, tsl], r=[], w=[("xblk", c) for c in range(KC)], slot="xblk")
            self.rms_rstd(xblk, [("xblk", c) for c in range(KC)], sq, "p1")
            for c in range(KC):
                self.stt(hT[:, c, tsl], xblk[:, c, :], gmp[:, c:c + 1], self.rstd[:, :], ALU.mult, ALU.mult,
                         r=[("xblk", c), "pf", "rstd"], w=[("hT", c, n)])
        self.tap("hT", hT[:, :, :], [("hT", c, n) for c in range(KC) for n in range(NTB)], [KC, S])
        if stop == 1:
            return
        P.barrier()

        kaugT = self.arena[0:65, KV0 // 2: KV0 // 2 + S]
        kiT = self.arena[0:64, KV0 // 2 + S: KV0 // 2 + 2 * S]
        vaug = self.arena[:, KV0 // 2 + 2 * S: KV0 // 2 + 2 * S + 16 * 66].rearrange("p (t d) -> p t d", t=16)
        wi_off = KV0 + 2 * (2 * S + 16 * 66)
        wi_s = self.arena[:, wi_off // 2: wi_off // 2 + 256].bitcast(F32).rearrange("p (t h) -> p t h", t=16)
        assert wi_off + 512 <= ARENA
        uz = self.carve(0, 128, [4, S], BF16)
        upp = [self.carve(16 * 1024, 128, [1, S], F32)[:, 0, :], self.carve(24 * 1024, 128, [1, S], F32)[:, 0, :]]
        dT = self.carve(32 * 1024, 128, [4, S], BF16)
        yp = self.carve(48 * 1024, 128, [4, S], BF16)
        sgt = self.carve(64 * 1024, 128, [1, TB], BF16)[:, 0, :]

        def hkeys(n):
            return [("hT", c, n) for c in range(KC)]

        seg, sk = self.wload(l, "kk", 1024)
        seg = seg.rearrange("p (k m) -> p k m", k=KC)
        P.op("dve", lambda e: e.memset(kaugT[64:65, :], 1.0), w=["kaug1"])
        P.op("dve", lambda e: e.memset(vaug[:, :, 64:65], 1.0), w=["vaug1"])
        for n in range(NTB):
            tsl = slice(n * TB, (n + 1) * TB)
            for half, dst, key in ((0, kaugT, "kT"), (1, kiT, "kiT")):
                b = self.bank()
                for kc in range(KC):
                    self.mm(self.psb(b, 64), seg[:, kc, half * 64:(half + 1) * 64], hT[:, kc, tsl], kc == 0, kc == KC - 1,
                            r=[sk, ("hT", kc, n)], w=[("ps", b)])
                self.act(dst[0:64, tsl], self.psb(b, 64), AF.Copy, r=[("ps", b)], w=[(key, n)])
        seg, sk = self.wload(l, "vw", KC * 72)
        seg = seg.rearrange("p (k m) -> p k m", k=KC)
        for tt_ in range(16):
            n = tt_ // 4
            b = self.bank()
            for kc in range(KC):
                self.mm(ps[:, b * 512: b * 512 + 72], hT[:, kc, tt_ * 128:(tt_ + 1) * 128], seg[:, kc, :], kc == 0, kc == KC - 1,
                        r=[sk, ("hT", kc, n)], w=[("ps", b)])
            self.act(vaug[:, tt_, 0:64], ps[:, b * 512: b * 512 + 64], AF.Copy, r=[("ps", b)], w=[("vaug", tt_)])
            self.act(wi_s[:, tt_, :], ps[:, b * 512 + 64: b * 512 + 72], AF.Copy, r=[("ps", b)], w=[("wi", tt_)], scale=IDX_SCALE)
        for cc in range(4):
            seg, sk = self.wload(l, "su%d" % cc, 1024)
            seg = seg.rearrange("p (k m) -> p k m", k=KC)
            for n in range(NTB):
                tsl = slice(n * TB, (n + 1) * TB)
                b = self.bank()
                for kc in range(KC):
                    self.mm(self.psb(b), seg[:, kc, :], hT[:, kc, tsl], kc == 0, kc == KC - 1, r=[sk, ("hT", kc, n)], w=[("ps", b)])
                self.act(uz[:, cc, tsl], self.psb(b), AF.Copy, r=[("ps", b)], w=[("uz", cc, n)])
        for cc in range(4):
            seg, sk = self.wload(l, "pu%d" % cc, 1024)
            seg = seg.rearrange("p (k m) -> p k m", k=KC)
            u0 = upp[0]
            for n in range(NTB):
                tsl = slice(n * TB, (n + 1) * TB)
                b = self.bank()
                for kc in range(KC):
                    self.mm(self.psb(b), seg[:, kc, :], hT[:, kc, tsl], kc == 0, kc == KC - 1, r=[sk, ("hT", kc, n)], w=[("ps", b)])
                self.act(u0[:, tsl], self.psb(b), AF.Copy, r=[("ps", b)], w=["up0"])
            wlen = 2 ** (cc + 1)
            sA = upp[1]
            sB = self.carve(66 * 1024, 128, [1, S], F32)[:, 0, :]
            bufs = [(sA, "upA"), (sB, "upB")]
            k = 1
            bi = 0
            src, srck = u0, "up0"
            while k < wlen:
                dstb, dstk = bufs[bi % 2]
                self.tt(dstb[:, k:], src[:, k:], src[:, :S - k], ALU.add, r=[srck], w=[dstk])
                P.op("dve", lambda e, d=dstb, s_=src, k=k: e.tensor_copy(out=d[:, 0:k], in_=s_[:, 0:k]), r=[srck], w=[dstk])
                src, srck = dstb, dstk
                bi += 1
                k *= 2
            self.stt(dT[:, cc, :], src[:, :], 1.0 / wlen, u0[:, :], ALU.mult, ALU.subtract, r=[srck, "up0"], w=[("dT", cc)])
            tmpc = self.carve(64 * 1024 + 1024, 128, [1, 16], F32)[:, 0, :]
            self.tt(tmpc[:, 0:wlen - 1], src[:, 0:wlen - 1], invc[:, 0:wlen - 1], ALU.mult, r=[srck, "cf"], w=["tmpc"])
            self.tt(dT[:, cc, 0:wlen - 1], tmpc[:, 0:wlen - 1], u0[:, 0:wlen - 1], ALU.subtract, r=["tmpc", "up0", ("dT", cc)], w=[("dT", cc)])
        self.tap("dT", dT[:, :, :], [("dT", cc) for cc in range(4)], [4, S])
        self.tap("kT", kaugT[:, :], [("kT", n) for n in range(NTB)] + ["kaug1"], [S], parts=65)
        self.tap("vaug", vaug[:, :, :], [("vaug", t) for t in range(16)] + ["vaug1"], [16, 66])
        self.tap("wi", wi_s[:, :, :], [("wi", t) for t in range(16)], [16, 8])
        if stop == 2:
            return

        seg, sk = self.wload(l, "mix", 512)
        seg = seg.rearrange("p (g m) -> p g m", g=4)
        for cc in range(4):
            for n in range(NTB):
                tsl = slice(n * TB, (n + 1) * TB)
                b = self.bank()
                self.mm(self.psb(b), seg[:, cc, :], dT[:, cc, tsl], True, True, r=[sk, ("dT", cc)], w=[("ps", b)])
                self.act(yp[:, cc, tsl], self.psb(b), AF.Copy, r=[("ps", b), "pf"], w=[("yp", cc, n)], scale=psc[:, cc:cc + 1])
        for c in range(KC):
            segp, skp = self.wload(l, "po%d" % c, 512)
            segp = segp.rearrange("p (k m) -> p k m", k=4)
            segg, skg = self.wload(l, "g0_%d" % c, 1024)
            segg = segg.rearrange("p (k m) -> p k m", k=KC)
            for n in range(NTB):
                tsl = slice(n * TB, (n + 1) * TB)
                by = self.bank()
                for cc in range(4):
                    self.mm(self.psb(by), segp[:, cc, :], yp[:, cc, tsl], cc == 0, cc == 3, r=[skp, ("yp", cc, n)], w=[("ps", by)])
                bg = self.bank()
                for kc in range(KC):
                    self.mm(self.psb(bg), segg[:, kc, :], hT[:, kc, tsl], kc == 0, kc == KC - 1, r=[skg, ("hT", kc, n)], w=[("ps", bg)])
                self.act(sgt[:, :], self.psb(bg), AF.Sigmoid, r=[("ps", bg)], w=["sgt"])
                self.tt(merged[:, c, tsl], sgt[:, :], self.psb(by), ALU.mult, r=["sgt", ("ps", by)], w=[("mg", c, n)])
        self.tap("mg3", merged[:, :, S - 256:S], [("mg", c, n) for c in range(KC) for n in range(NTB)], [KC, 256])
        if stop == 3:
            return
        P.barrier()

        self.phase_s5(l, hT, merged, pf, uz)
        self.tap("mg4", merged[:, :, S - 256:S], [("mg", c, n) for c in range(KC) for n in range(NTB)], [KC, 256])
        if stop == 4:
            return
        P.barrier()

        self.phase_attn(l, hT, merged, kaugT, kiT, vaug, wi_s, ident, sel4, biasT, cmask, ones64, b31d)
        self.tap("mg5", merged[:, :, S - 256:S], [("mg", c, n) for c in range(KC) for n in range(NTB)], [KC, 256])
        if stop == 5:
            return
        P.barrier()

        self.bankset = list(range(8))
        xT = self.carve(0, 128, [KC, S], F32)
        sq = hT[:, 4:6, :].rearrange("p a b -> p (a b)").rearrange("p (c t) -> p c t", c=KC)
        xblk = hT[:, 0:4, :].rearrange("p a b -> p (a b)").bitcast(F32).rearrange("p (c t) -> p c t", c=KC)
        tmpf = self.carve(64 * 1024, 128, [1, TB], F32)[:, 0, :]
        for c in range(KC):
            seg, sk = self.wload(l, "wo%d" % c, 1024)
            seg = seg.rearrange("p (k m) -> p k m", k=KC)
            for n in range(NTB):
                tsl = slice(n * TB, (n + 1) * TB)
                b = self.bank()
                for kc in range(KC):
                    self.mm(self.psb(b), seg[:, kc, :], merged[:, kc, tsl], kc == 0, kc == KC - 1, r=[sk, ("mg", kc, n)], w=[("ps", b)])
                self.act(xT[:, c, tsl], self.psb(b), AF.Copy, r=[("ps", b)], w=[("xT", c, n)])
        for n in range(NTB):
            tsl = slice(n * TB, (n + 1) * TB)
            self.dma("sp", xblk[:, :, :], xsrc[:, :, tsl], r=[], w=[("xblk", c) for c in range(KC)], slot="xblk")
            self.rms_rstd(xT[:, :, tsl], [("xT", c, n) for c in range(KC)], sq, "p6")
            for c in range(KC):
                self.stt(tmpf[:, :], xT[:, c, tsl], gmo[:, c:c + 1], self.rstd[:, :], ALU.mult, ALU.mult,
                         r=[("xT", c, n), "pf", "rstd"], w=["tmpf"])
                self.tt(xT[:, c, tsl], tmpf[:, :], xblk[:, c, :], ALU.add, r=["tmpf", ("xblk", c)], w=[("xT", c, n)])
        self.tap("x6", xT[:, :, S - 256:S], [("xT", c, n) for c in range(KC) for n in range(NTB)], [KC, 256])
        if stop == 6:
            return
        P.barrier()

        fT = self.carve(64 * 1024, 128, [NJ, TB], BF16)
        sq = merged[:, 4:6, :].rearrange("p a b -> p (a b)").rearrange("p (c t) -> p c t", c=KC)
        mbuf = merged[:, 0:4, :].rearrange("p a b -> p (a b)").bitcast(F32).rearrange("p (c t) -> p c t", c=KC)
        sgf = merged[:, 6, 0:TB]
        tmpf = merged[:, 7, 0:2 * TB].bitcast(F32)
        for n in range(NTB):
            tsl = slice(n * TB, (n + 1) * TB)
            self.rms_rstd(xT[:, :, tsl], [("xT", c, n) for c in range(KC)], sq, "p7a")
            for c in range(KC):
                self.stt(hT[:, c, tsl], xT[:, c, tsl], gfp[:, c:c + 1], self.rstd[:, :], ALU.mult, ALU.mult,
                         r=[("xT", c, n), "pf", "rstd"], w=[("hT", c, n)])
            for j in range(NJ):
                seg, sk = self.wload(l, "f%d" % j, 2048)
                seg = seg.rearrange("p (k m) -> p k m", k=KC)
                bg = self.bank()
                for kc in range(KC):
                    self.mm(self.psb(bg), seg[:, kc, 0:128], hT[:, kc, tsl], kc == 0, kc == KC - 1, r=[sk, ("hT", kc, n)], w=[("ps", bg)])
                bu = self.bank()
                for kc in range(KC):
                    self.mm(self.psb(bu), seg[:, kc, 128:256], hT[:, kc, tsl], kc == 0, kc == KC - 1, r=[sk, ("hT", kc, n)], w=[("ps", bu)])
                self.act(sgf, self.psb(bg), AF.Silu, r=[("ps", bg)], w=["sgf"])
                self.tt(fT[:, j, :], sgf, self.psb(bu), ALU.mult, r=["sgf", ("ps", bu)], w=[("fT", j)])
            for c in range(KC):
                seg, sk = self.wload(l, "fo%d" % c, NJ * 128)
                seg = seg.rearrange("p (k m) -> p k m", k=NJ)
                b = self.bank()
                for j in range(NJ):
                    self.mm(self.psb(b), seg[:, j, :], fT[:, j, :], j == 0, j == NJ - 1, r=[sk, ("fT", j)], w=[("ps", b)])
                self.act(mbuf[:, c, :], self.psb(b), AF.Copy, r=[("ps", b)], w=[("mbuf", c)])
            self.rms_rstd(mbuf, [("mbuf", c) for c in range(KC)], sq, "p7b")
            for c in range(KC):
                self.stt(tmpf, mbuf[:, c, :], gfo[:, c:c + 1], self.rstd[:, :], ALU.mult, ALU.mult,
                         r=[("mbuf", c), "pf", "rstd"], w=["tmpf"])
                self.tt(mbuf[:, c, :], tmpf, xT[:, c, tsl], ALU.add, r=["tmpf", ("xT", c, n)], w=[("mbuf", c)])
            self.dma("sp", xdst[:, :, tsl], mbuf[:, :, :], r=[("mbuf", c) for c in range(KC)], w=[], slot="xout")
        if "xout" not in self.final_slots:
            self.final_slots.append("xout")

    def phase_s5(self, l, hT, merged, pf, uz):
        P = self.P
        K = 1024
        Ec = self.carve(16 * K, 128, [1, S], F32)[:, 0, :]
        Es = self.carve(24 * K, 128, [1, S], F32)[:, 0, :]
        vre = self.carve(32 * K, 128, [1, S], F32)[:, 0, :]
        vim = self.carve(40 * K, 128, [1, S], F32)[:, 0, :]
        xre = self.carve(48 * K, 128, [1, S], BF16)[:, 0, :]
        xim = self.carve(52 * K, 128, [1, S], BF16)[:, 0, :]
        tmp1 = self.carve(56 * K, 128, [1, TB], F32)[:, 0, :]
        tmp2 = self.carve(58 * K, 128, [1, TB], F32)[:, 0, :]
        Bre = self.carve(60 * K, 128, [2, 4, 128], BF16)
        Bim = self.carve(62 * K, 128, [2, 4, 128], BF16)
        gt = self.carve(71 * K, 128, [1, TB], F32)[:, 0, :]
        sm = self.carve(64 * K, 128, [16, 16], F32)
        s1 = self.carve(66 * K, 128, [1, TB], BF16)[:, 0, :]
        s2 = self.carve(67 * K, 128, [1, TB], BF16)[:, 0, :]
        t3 = self.carve(68 * K, 128, [1, TB], F32)[:, 0, :]
        xw = self.carve(32 * K, 128, [8, 512], F32)
        smi = self.carve(70 * K, 128, [1, 16], I32)[:, 0, :]
        TWO_PI = 2.0 * math.pi

        def pfx(o):
            return pf[:, o:o + 512]

        def dve(fn, r, w):
            P.op("dve", fn, r=r, w=w)

        def zoh(lr, li, ld, wk, n, tag, itile):
            dt_, lrdt, th, kf, q, sn, cs, t0 = wk[:8]
            kk = ["z%s%d" % (tag, i) for i in range(8)]
            kdt, klrdt, kth, kkf, kq, ksn, kcs, kt0 = kk
            ki = "z%si" % tag
            self.act(dt_, ld, AF.Exp, r=["pf"], w=[kdt])
            self.tt(lrdt, lr, dt_, ALU.mult, r=["pf", kdt], w=[klrdt])
            self.tt(th, li, dt_, ALU.mult, r=["pf", kdt], w=[kth])
            self.ts(kf, th, 1.0 / TWO_PI, None, ALU.mult, ALU.bypass, r=[kth], w=[kkf])
            dve(lambda e: e.tensor_copy(out=itile, in_=kf), r=[kkf], w=[ki])
            dve(lambda e: e.tensor_copy(out=kf, in_=itile), r=[ki], w=[kkf])
            self.stt(q, kf, -TWO_PI, th, ALU.mult, ALU.add, r=[kkf, kth], w=[kq])
            self.act(sn, q, AF.Sin, r=[kq], w=[ksn], scale=0.25)
            self.act(cs, q, AF.Sin, r=[kq, "hpi"], w=[kcs], scale=0.25, bias=self.hpi[:, 0:1])
            for it in range(2):
                self.tt(t0, sn, sn, ALU.mult, r=[ksn], w=[kt0])
                self.stt(sn, sn, 2.0, cs, ALU.mult, ALU.mult, r=[ksn, kcs], w=[ksn])
                self.ts(cs, t0, -2.0, 1.0, ALU.mult, ALU.add, r=[kt0], w=[kcs])
            self.act(dt_, lrdt, AF.Exp, r=[klrdt], w=[kdt])
            return dict(mag=dt_, cos=cs, sin=sn, keys=[kdt, kcs, ksn], k=kk)

        wkx = [xw[:, i, :] for i in range(8)]
        smx = self.carve(56 * K, 128, [1, 512], I32)[:, 0, :]
        zx = zoh(pfx(PF_LRX), pfx(PF_LIX), pfx(PF_LDX), wkx, 512, "x", smx)
        K_ = zx["k"]
        mg_, cs_x, sn_x = wkx[0], wkx[6], wkx[5]
        are, aim, den, cre, cim = wkx[1], wkx[2], wkx[3], wkx[4], wkx[7]
        kare, kaim, kden, kcre, kcim = K_[1], K_[2], K_[3], K_[4], K_[7]
        kmg, kcs, ksn = K_[0], K_[6], K_[5]
        lr, li = pfx(PF_LRX), pfx(PF_LIX)
        self.tt(are, mg_, cs_x, ALU.mult, r=[kmg, kcs], w=[kare])
        self.tt(aim, mg_, sn_x, ALU.mult, r=[kmg, ksn], w=[kaim])
        self.ts(are, are, -1.0, None, ALU.add, ALU.bypass, r=[kare], w=[kare])
        t0, kt0 = wkx[0], K_[0]
        t1, kt1, t2, kt2 = wkx[5], K_[5], wkx[6], K_[6]
        self.tt(den, lr, lr, ALU.mult, r=["pf"], w=[kden])
        self.tt(cre, li, li, ALU.mult, r=["pf"], w=[kcre])
        self.tt(den, den, cre, ALU.add, r=[kden, kcre], w=[kden])
        dve(lambda e: e.reciprocal(out=den, in_=den), r=[kden], w=[kden])
        self.tt(cre, are, lr, ALU.mult, r=[kare, "pf"], w=[kcre])
        self.tt(t0, aim, li, ALU.mult, r=[kaim, "pf"], w=[kt0])
        self.tt(cre, cre, t0, ALU.add, r=[kcre, kt0], w=[kcre])
        self.tt(cre, cre, den, ALU.mult, r=[kcre, kden], w=[kcre])
        self.tt(cim, aim, lr, ALU.mult, r=[kaim, "pf"], w=[kcim])
        self.tt(t0, are, li, ALU.mult, r=[kare, "pf"], w=[kt0])
        self.tt(cim, cim, t0, ALU.subtract, r=[kcim, kt0], w=[kcim])
        self.tt(cim, cim, den, ALU.mult, r=[kcim, kden], w=[kcim])
        br, bi = pfx(PF_BRX), pfx(PF_BIX)
        self.tt(t1, cre, br, ALU.mult, r=[kcre, "pf"], w=[kt1])
        self.tt(t2, cim, bi, ALU.mult, r=[kcim, "pf"], w=[kt2])
        self.tt(t1, t1, t2, ALU.subtract, r=[kt1, kt2], w=[kt1])
        for v in range(2):
            self.ts(Bre[:, v, :, :].rearrange("p a b -> p (a b)"), t1, self.cf[:, CF_PAR + v:CF_PAR + v + 1], None, ALU.mult, ALU.bypass,
                    r=[kt1, "cf"], w=["Bre"])
        self.tt(t1, cre, bi, ALU.mult, r=[kcre, "pf"], w=[kt1])
        self.tt(t2, cim, br, ALU.mult, r=[kcim, "pf"], w=[kt2])
        self.tt(t1, t1, t2, ALU.add, r=[kt1, kt2], w=[kt1])
        for v in range(2):
            self.ts(Bim[:, v, :, :].rearrange("p a b -> p (a b)"), t1, self.cf[:, CF_PAR + v:CF_PAR + v + 1], None, ALU.mult, ALU.bypass,
                    r=[kt1, "cf"], w=["Bim"])

        wks = [sm[:, i, :] for i in range(8)]
        zs = zoh(pf[:, PF_LRS:PF_LRS + 16], pf[:, PF_LIS:PF_LIS + 16], pf[:, PF_LDS:PF_LDS + 16], wks, 16, "s", smi)
        mag, cth, sth = zs["mag"], zs["cos"], zs["sin"]
        nsc = sm[:, 8, :]

        segC, skC = self.wload(l, "sC", 4096)
        Cre = segC[:, 0:2048].rearrange("p (j m) -> p j m", j=16)
        Cim = segC[:, 2048:4096].rearrange("p (j m) -> p j m", j=16)
        segD, skD = self.wload(l, "sD", 512)
        Dg = segD.rearrange("p (c m) -> p c m", c=4)

        self.bankset = [0, 1, 2, 3]
        for cc in range(4):
            ybanks = [4, 5, 6, 7]
            for jj in range(4):
                j = cc * 4 + jj
                rows = slice(64 * (jj // 2), 64 * (jj // 2) + 64)
                pv = jj % 2
                dve(lambda e, j=j: e.tensor_copy(out=Ec[:, 0:1], in_=cth[:, j:j + 1]), r=zs["keys"], w=["Ec"])
                dve(lambda e, j=j: e.tensor_copy(out=Es[:, 0:1], in_=sth[:, j:j + 1]), r=zs["keys"], w=["Es"])
                nn = 1
                lev = 0
                while nn < S:
                    cs_ = Ec[:, nn - 1:nn]
                    ss_ = Es[:, nn - 1:nn]
                    ns_ = nsc[:, lev:lev + 1]
                    self.ts(ns_, ss_, -1.0, None, ALU.mult, ALU.bypass, r=["Es"], w=["nsc"])
                    self.ts(vre[:, 0:nn], Ec[:, 0:nn], cs_, None, ALU.mult, ALU.bypass, r=["Ec"], w=["vre"])
                    self.ts(vim[:, 0:nn], Es[:, 0:nn], cs_, None, ALU.mult, ALU.bypass, r=["Es", "Ec"], w=["vim"])
                    self.stt(Ec[:, nn:2 * nn], Es[:, 0:nn], ns_, vre[:, 0:nn], ALU.mult, ALU.add, r=["Es", "nsc", "vre"], w=["Ec"])
                    self.stt(Es[:, nn:2 * nn], Ec[:, 0:nn], ss_, vim[:, 0:nn], ALU.mult, ALU.add, r=["Ec", "Es", "vim"], w=["Es"])
                    nn *= 2
                    lev += 1
                for n in range(NTB):
                    tsl = slice(n * TB, (n + 1) * TB)
                    b1 = self.bank()
                    self.mm(self.psb(b1), Bre[rows, pv, cc, :], uz[rows, cc, tsl], True, True, r=["Bre", ("uz", cc, n)], w=[("ps", b1)])
                    b2 = self.bank()
                    self.mm(self.psb(b2), Bim[rows, pv, cc, :], uz[rows, cc, tsl], True, True, r=["Bim", ("uz", cc, n)], w=[("ps", b2)])
                    kv = ("v", n)
                    self.tt(vre[:, tsl], Ec[:, tsl], self.psb(b1), ALU.mult, r=["Ec", "Es", ("ps", b1)], w=[("vre", n)])
                    self.tt(tmp1, Es[:, tsl], self.psb(b2), ALU.mult, r=["Es", ("ps", b2)], w=["tmp1"])
                    self.tt(vre[:, tsl], vre[:, tsl], tmp1, ALU.add, r=[("vre", n), "tmp1"], w=[("vre", n)])
                    self.tt(vim[:, tsl], Ec[:, tsl], self.psb(b2), ALU.mult, r=["Ec", ("ps", b2)], w=[("vim", n)])
                    self.tt(tmp2, Es[:, tsl], self.psb(b1), ALU.mult, r=["Es", ("ps", b1)], w=["tmp2"])
                    self.tt(vim[:, tsl], vim[:, tsl], tmp2, ALU.subtract, r=[("vim", n), "tmp2"], w=[("vim", n)])
                vk = [("vre", n) for n in range(NTB)]
                ik = [("vim", n) for n in range(NTB)]
                dve(lambda e, j=j: e.tensor_tensor_scan(out=vre[:, :], data0=mag[:, j:j + 1].to_broadcast([128, S]), data1=vre[:, :],
                                                       initial=0.0, op0=ALU.mult, op1=ALU.add), r=vk + zs["keys"], w=vk + ["vre"])
                dve(lambda e, j=j: e.tensor_tensor_scan(out=vim[:, :], data0=mag[:, j:j + 1].to_broadcast([128, S]), data1=vim[:, :],
                                                       initial=0.0, op0=ALU.mult, op1=ALU.add), r=ik + zs["keys"], w=ik + ["vim"])
                for n in range(NTB):
                    tsl = slice(n * TB, (n + 1) * TB)
                    self.tt(tmp1, Ec[:, tsl], vre[:, tsl], ALU.mult, r=["Ec", ("vre", n)], w=["tmp1"])
                    self.tt(tmp2, Es[:, tsl], vim[:, tsl], ALU.mult, r=["Es", ("vim", n)], w=["tmp2"])
                    self.tt(xre[:, tsl], tmp1, tmp2, ALU.subtract, r=["tmp1", "tmp2"], w=[("xre", n)])
                    self.tt(tmp1, Es[:, tsl], vre[:, tsl], ALU.mult, r=["Es", ("vre", n), ("xre", n)], w=["tmp1"])
                    self.tt(tmp2, Ec[:, tsl], vim[:, tsl], ALU.mult, r=["Ec", ("vim", n), ("xre", n)], w=["tmp2"])
                    self.stt(xim[:, tsl], tmp1, -1.0, tmp2, ALU.mult, ALU.subtract, r=["tmp1", "tmp2"], w=[("xim", n)])
                    yb = ybanks[n]
                    self.mm(self.psb(yb), Cre[:, j, :], xre[:, tsl], jj == 0, False, r=[skC, ("xre", n)], w=[("ps", yb)])
                    self.mm(self.psb(yb), Cim[:, j, :], xim[:, tsl], False, False, r=[skC, ("xim", n)], w=[("ps", yb)])
                    if jj == 3:
                        self.mm(self.psb(yb), Dg[:, cc, :], uz[:, cc, tsl], False, True, r=[skD, ("uz", cc, n)], w=[("ps", yb)])
            for n in range(NTB):
                tsl = slice(n * TB, (n + 1) * TB)
                yb = ybanks[n]
                self.act(gt, self.psb(yb), AF.Square, r=[("ps", yb)], w=["gt"])
                self.ts(gt, gt, 0.044715, 1.0, ALU.mult, ALU.add, r=["gt"], w=["gt"])
                self.tt(gt, gt, self.psb(yb), ALU.mult, r=["gt", ("ps", yb)], w=["gt"])
                self.act(t3, gt, AF.Sigmoid, r=["gt"], w=["t3"], scale=1.5957691216057308)
                self.tt(uz[:, cc, tsl], t3, self.psb(yb), ALU.mult, r=["t3", ("ps", yb)], w=[("uz", cc, n)])
        self.tap("zT", uz[:, :, :], [("uz", cc, n) for cc in range(4) for n in range(NTB)], [4, S])
        self.bankset = list(range(8))
        for c in range(KC):
            segl, skl = self.wload(l, "glu%d" % c, 1024)
            segl = segl.rearrange("p (a k m) -> p a k m", a=2, k=4)
            segg, skg = self.wload(l, "g2_%d" % c, 1024)
            segg = segg.rearrange("p (k m) -> p k m", k=KC)
            for n in range(NTB):
                tsl = slice(n * TB, (n + 1) * TB)
                ba = self.bank()
                for cc in range(4):
                    self.mm(self.psb(ba), segl[:, 0, cc, :], uz[:, cc, tsl], cc == 0, cc == 3, r=[skl, ("uz", cc, n)], w=[("ps", ba)])
                bb = self.bank()
                for cc in range(4):
                    self.mm(self.psb(bb), segl[:, 1, cc, :], uz[:, cc, tsl], cc == 0, cc == 3, r=[skl, ("uz", cc, n)], w=[("ps", bb)])
                bg = self.bank()
                for kc in range(KC):
                    self.mm(self.psb(bg), segg[:, kc, :], hT[:, kc, tsl], kc == 0, kc == KC - 1, r=[skg, ("hT", kc, n)], w=[("ps", bg)])
                self.act(s1, self.psb(bb), AF.Sigmoid, r=[("ps", bb)], w=["s1"])
                self.act(s2, self.psb(bg), AF.Sigmoid, r=[("ps", bg)], w=["s2"])
                self.tt(t3, s1, self.psb(ba), ALU.mult, r=["s1", ("ps", ba)], w=["t3"])
                self.tt(t3, t3, s2, ALU.mult, r=["t3", "s2"], w=["t3"])
                self.tt(merged[:, c, tsl], merged[:, c, tsl], t3, ALU.add, r=[("mg", c, n), "t3"], w=[("mg", c, n)])

    def phase_attn(self, l, hT, merged, kaugT, kiT, vaug, wi_s, ident, sel4, biasT, cmask, ones64, b31d):
        P = self.P
        K = 1024
        ps = self.ps
        qT = self.arena[0:65, 0: 16 * TB].rearrange("p (h q) -> p h q", h=16)
        qiT = self.arena[0:64, 8 * K: 8 * K + 8 * TB].rearrange("p (h q) -> p h q", h=8)
        OTn = self.arena[0:64, 12 * K: 12 * K + 16 * TB].rearrange("p (h q) -> p h q", h=16)
        score2 = [self.carve(40 * K, 128, [1, S], F32)[:, 0, :], self.pf_t[:, PF_LRX:PF_LRX + S]]
        rl = [self.carve(48 * K, 128, [1, TB], F32)[:, 0, :], self.carve(50 * K, 128, [1, TB], F32)[:, 0, :]]
        nm3 = [self.carve((52 + 4 * i) * K, 128, [1, S], BF16)[:, 0, :] for i in range(3)]
        self.idx_i = 0
        PT = [self.carve(64 * K, 128, [1, 1024], BF16)[:, 0, :], self.carve(66 * K, 128, [1, 1024], BF16)[:, 0, :]]
        ot = self.carve(68 * K, 64, [1, 1024], F32)[:, 0, :]
        aotmp = self.carve(60 * K, 128, [1, TB], F32)[:, 0, :]
        bis = self.carve(72 * K, 128, [1, 16], F32)[:, 0, :]
        sgt = self.carve(73 * K, 128, [1, TB], BF16)[:, 0, :]
        lnr = self.arena[64:65, 12 * K: 12 * K + 2048].bitcast(F32)
        rrow = self.arena[64:65, 14 * K: 14 * K + 1024]
        P.op("pool", lambda e: e.dma_start(out=self.arena[64:65, 0:16 * TB], in_=b31d, max_dma_last_dim=8192), w=["qT64"], slot="b31")

        def emit_ao(nn):
            tsl = slice(nn * TB, (nn + 1) * TB)
            self.bankset = [0, 1, 2, 3, 4, 5]
            for c in range(KC):
                sego, sko = self.wload(l, "ao%d" % c, 2048)
                sego = sego.rearrange("p (h m) -> p h m", h=16)
                segg, skg = self.wload(l, "g1_%d" % c, 1024)
                segg = segg.rearrange("p (k m) -> p k m", k=KC)
                by = self.bank()
                for h in range(16):
                    self.mm(self.psb(by), sego[0:64, h, :], OTn[:, h, :], h == 0, h == 15, r=[sko] + [("OTn", i) for i in range(4)], w=[("ps", by)])
                bg = self.bank()
                for kc in range(KC):
                    self.mm(self.psb(bg), segg[:, kc, :], hT[:, kc, tsl], kc == 0, kc == KC - 1, r=[skg, ("hT", kc, nn)], w=[("ps", bg)])
                self.act(sgt, self.psb(bg), AF.Sigmoid, r=[("ps", bg)], w=["sgt"])
                t3 = aotmp
                self.tt(t3, sgt, self.psb(by), ALU.mult, r=["sgt", ("ps", by)], w=["aotmp"])
                self.tt(merged[:, c, tsl], merged[:, c, tsl], t3, ALU.add, r=[("mg", c, nn), "aotmp"], w=[("mg", c, nn)])
                yield

        for n in range(NTB):
            tsl = slice(n * TB, (n + 1) * TB)
            self.bankset = list(range(8))
            for hp in range(8):
                seg, sk = self.wload(l, "q%d" % hp, 1024)
                seg = seg.rearrange("p (k m) -> p k m", k=KC)
                for half in range(2):
                    h = 2 * hp + half
                    b = self.bank()
                    for kc in range(KC):
                        self.mm(self.psb(b, 64), seg[:, kc, half * 64:(half + 1) * 64], hT[:, kc, tsl], kc == 0, kc == KC - 1,
                                r=[sk, ("hT", kc, n)], w=[("ps", b)])
                    self.act(qT[0:64, h, :], self.psb(b, 64), AF.Copy, r=[("ps", b)], w=[("qT", h)], scale=0.125)
            for hp in range(4):
                seg, sk = self.wload(l, "qi%d" % hp, 1024)
                seg = seg.rearrange("p (k m) -> p k m", k=KC)
                for half in range(2):
                    h = 2 * hp + half
                    b = self.bank()
                    for kc in range(KC):
                        self.mm(self.psb(b, 64), seg[:, kc, half * 64:(half + 1) * 64], hT[:, kc, tsl], kc == 0, kc == KC - 1,
                                r=[sk, ("hT", kc, n)], w=[("ps", b)])
                    self.act(qiT[0:64, h, :], self.psb(b, 64), AF.Copy, r=[("ps", b)], w=[("qiT", h)])
            if n == 0:
                self.tap("qT", qT[:, :, :], [("qT", h) for h in range(16)] + ["qT64"], [16, TB], parts=65)

            def genA(qq):
                qb = 4 * n + qq
                qsl = slice(qq * 128, (qq + 1) * 128)
                L = (qb + 1) * 128
                sc = score2[qb % 2]
                ngr = (L + 511) // 512
                for kg in range(ngr):
                    k0 = kg * 512
                    nk = min(512, L - k0)
                    sk_ = ("sc", qb % 2, kg)
                    for h in range(8):
                        b = 6 + (self.idx_i % 2)
                        r_ = rl[self.idx_i % 2]
                        rk = ("rl", self.idx_i % 2)
                        self.idx_i += 1
                        self.mm(self.psb(b, 128, nk), qiT[0:64, h, qsl], kiT[0:64, k0:k0 + nk], True, True,
                                r=[("qiT", h)] + [("kiT", i) for i in range(NTB)], w=[("ps", b)])
                        self.act(r_[:, 0:nk], self.psb(b, 128, nk), AF.Relu, r=[("ps", b)], w=[rk])
                        wcol = wi_s[:, qb, h:h + 1]
                        wk = [("wi", qb)]
                        if h == 0:
                            ndiag = nk - 128 if (k0 + nk == L) else nk
                            if ndiag > 0:
                                self.ts(sc[:, k0:k0 + ndiag], r_[:, 0:ndiag], wcol, None, ALU.mult, ALU.bypass, r=[rk] + wk, w=[sk_])
                            if k0 + nk == L:
                                self.stt(sc[:, L - 128:L], r_[:, nk - 128:nk], wcol, cmask, ALU.mult, ALU.add, r=[rk, "cf"] + wk, w=[sk_])
                        else:
                            self.stt(sc[:, k0:k0 + nk], r_[:, 0:nk], wcol, sc[:, k0:k0 + nk], ALU.mult, ALU.add,
                                     r=[rk, sk_] + wk, w=[sk_])
                        yield

            def genB(qq):
                qb = 4 * n + qq
                L = (qb + 1) * 128
                sc = score2[qb % 2]
                nmb = nm3[qb % 2]
                nmk = ("nm", qb % 2)
                ngr = (L + 511) // 512
                sck = [("sc", qb % 2, kg) for kg in range(ngr)]
                if qb >= 2:
                    o = 8 * (qb % 2)
                    cA, cB, cnt, tmpb, thr = bis[:, o:o + 1], bis[:, o + 1:o + 2], bis[:, o + 2:o + 3], bis[:, o + 3:o + 4], bis[:, o + 4:o + 5]
                    kp = "b%d" % (qb % 2)
                    P.op("dve", lambda e, cA=cA: e.memset(cA, 0.0), w=[kp + "c0"])
                    cur, nxt = cA, cB
                    curk, nxtk = kp + "c0", kp + "c1"
                    step = 4.0
                    for it in range(NBIS):
                        self.ts(nmb[:, 0:L], sc[:, 0:L], cur, None, ALU.is_ge, ALU.add, r=sck + [curk], w=[nmk, kp + "cnt"], accum_out=cnt)
                        self.ts(tmpb, cnt, 256.0, 2.0 * step, ALU.is_ge, ALU.mult, r=[kp + "cnt"], w=[kp + "tmpb"])
                        self.ts(nxt, tmpb, -step, cur, ALU.add, ALU.add, r=[kp + "tmpb", curk], w=[nxtk])
                        cur, nxt = nxt, cur
                        curk, nxtk = nxtk, curk
                        step *= 0.5
                        yield
                    self.ts(thr, cur, -2.0 * step - 1e-5, None, ALU.add, ALU.bypass, r=[curk], w=[kp + "thr"])
                    self.ts(nmb[:, 0:L], sc[:, 0:L], thr, NEG, ALU.is_lt, ALU.mult, r=sck + [kp + "thr"], w=[nmk])
                else:
                    self.ts(nmb[:, 0:L], sc[:, 0:L], -16.0, NEG, ALU.is_lt, ALU.mult, r=sck, w=[nmk])
                if qb == 5:
                    self.tap("score5", sc[:, 0:L], sck, [L])
                    self.tap("nm5", nmb[:, 0:L], [nmk], [L])
                yield

            def n_units_A(qq):
                qb = 4 * n + qq
                return 8 * (((qb + 1) * 128 + 511) // 512)

            def step_gen(g):
                if g is None:
                    return None
                try:
                    next(g)
                    return g
                except StopIteration:
                    return None

            def drain(g):
                while g is not None:
                    g = step_gen(g)

            def emit_attn(qq, gB, nB, gA, nA):
                qb = 4 * n + qq
                qsl = slice(qq * 128, (qq + 1) * 128)
                nmb = nm3[qb % 2]
                nmk = ("nm", qb % 2)
                niter = 2 * (qb + 1)
                perB = -(-nB // niter)
                perA = -(-nA // niter)
                for hh in range(2):
                    def emit_S(kb):
                        sb = kb % 2
                        near = (qb - kb) < 2
                        kk = 64 if near else 65
                        ksl = slice(kb * 128, (kb + 1) * 128)
                        for bk in range(2):
                            bnk = 2 * sb + bk
                            hs = slice(hh * 8 + bk * 4, hh * 8 + bk * 4 + 4)
                            outp = self.psb(bnk).rearrange("p (h q) -> p h q", h=4)
                            qk = [("qT", h) for h in range(hh * 8 + bk * 4, hh * 8 + bk * 4 + 4)] + ["qT64"]
                            self.mm(outp, kaugT[0:kk, ksl], qT[0:kk, hs, qsl], True, False,
                                    r=qk + [("kT", kb // 4), "kaug1"], w=[("ps", bnk)])
                            self.mm(outp, nmb[:, ksl], sel4, False, not near, r=[nmk, "cb"], w=[("ps", bnk)])
                            if near:
                                self.mm(outp, ident, biasT[:, qb - kb, hs, :], False, True, r=["cb"], w=[("ps", bnk)])
                        self.act(PT[sb][:, :], ps[:, 2 * sb * 512: 2 * sb * 512 + 1024], AF.Exp,
                                 r=[("ps", 2 * sb), ("ps", 2 * sb + 1)], w=[("PT", sb)])

                    def emit_PV(kb):
                        sb = kb % 2
                        for bk in range(2):
                            self.mm(self.psb(4 + bk, 65), vaug[:, kb, 0:65], PT[sb][:, bk * 512:(bk + 1) * 512], kb == 0, kb == qb,
                                    r=[("PT", sb), ("vaug", kb), "vaug1"], w=[("ps", 4 + bk)])

                    for kb in range(qb + 1):
                        emit_S(kb)
                        if kb > 0:
                            emit_PV(kb - 1)
                        for _ in range(perB):
                            gB = step_gen(gB)
                        for _ in range(perA):
                            gA = step_gen(gA)
                    emit_PV(qb)
                    if hh == 1:
                        drain(gB)
                        drain(gA)
                        gB = gA = None
                    self.act(lnr, ps[64:65, 2048:3072], AF.Ln, r=[("ps", 4), ("ps", 5)], w=["lnr"])
                    self.act(rrow, lnr, AF.Exp, r=["lnr"], w=["rrow"], scale=-1.0)
                    for bk in range(2):
                        self.mm(self.psb(6 + bk, 64), self.ones_bf[64:65, 0:64], rrow[:, bk * 512:(bk + 1) * 512], True, True,
                                r=["ones_bf", "rrow"], w=[("ps", 6 + bk)])
                    self.act(ot[:, :], ps[0:64, 2048:3072], AF.Copy, r=[("ps", 4), ("ps", 5)], w=["ot"])
                    for bk in range(2):
                        hs = slice(hh * 8 + bk * 4, hh * 8 + bk * 4 + 4)
                        self.tt(OTn[:, hs, qsl], ot[:, bk * 512:(bk + 1) * 512].rearrange("p (h q) -> p h q", h=4),
                                self.psb(6 + bk, 64).rearrange("p (h q) -> p h q", h=4), ALU.mult,
                                r=["ot", ("ps", 6 + bk)], w=[("OTn", hh * 2 + bk)])

            drain(genA(0))
            gB0, gA1 = genB(0), genA(1)
            gAO = emit_ao(n - 1) if n > 0 else None
            rnd = 0
            while gB0 is not None or gA1 is not None or gAO is not None:
                gB0 = step_gen(gB0)
                gA1 = step_gen(step_gen(gA1))
                if rnd % 2 == 1:
                    gAO = step_gen(gAO)
                rnd += 1
            for qq in range(4):
                gB = genB(qq + 1) if qq + 1 < 4 else None
                gA = genA(qq + 2) if qq + 2 < 4 else None
                emit_attn(qq, gB, NBIS + 1, gA, n_units_A(qq + 2) if qq + 2 < 4 else 0)
            if n == 0:
                self.tap("OTn", OTn[:, :, :], [("OTn", i) for i in range(4)], [16, TB], parts=64)
        for _ in emit_ao(NTB - 1):
            pass


def km(w):
    k, m = w.shape
    return np.ascontiguousarray(w.reshape(k // 128, 128, m).transpose(1, 0, 2)).reshape(128, (k // 128) * m)


def host_segments(inp, l):
    w = inp["w_in"][l]
    segs = {}
    segs["kk"] = km(np.concatenate([w[:, C_K:C_K + 64], w[:, C_KI:C_KI + 64]], axis=1))
    segs["vw"] = km(np.concatenate([w[:, C_V:C_V + 64], w[:, C_WI:C_WI + 8]], axis=1))
    for cc in range(4):
        segs["pu%d" % cc] = km(w[:, C_POOL + cc * 128: C_POOL + (cc + 1) * 128])
        segs["su%d" % cc] = km(w[:, C_S5 + cc * 128: C_S5 + (cc + 1) * 128])
    segs["mix"] = np.ascontiguousarray(inp["pool_mix_w"][l].transpose(1, 0, 2)).reshape(128, 512)
    for c in range(8):
        cs = slice(c * 128, (c + 1) * 128)
        segs["po%d" % c] = km(inp["pool_out_w"][l][:, cs])
        for b in range(3):
            segs["g%d_%d" % (b, c)] = km(w[:, C_G + b * 1024 + c * 128: C_G + b * 1024 + (c + 1) * 128])
        glu = inp["s5_glu_w"][l]
        segs["glu%d" % c] = np.concatenate([km(glu[:, cs]), km(glu[:, 1024 + c * 128: 1024 + (c + 1) * 128])], axis=1)
        segs["q%d" % c] = km(w[:, C_Q + c * 128: C_Q + (c + 1) * 128])
        ao = inp["attn_out_w"][l][:, cs].reshape(16, 64, 128).transpose(1, 0, 2).reshape(64, 2048)
        segs["ao%d" % c] = np.concatenate([ao, np.zeros((64, 2048), np.float32)], axis=0)
        segs["wo%d" % c] = km(inp["w_out"][l][:, cs])
        segs["fo%d" % c] = km(inp["ffn_w_out"][l][:, cs])
    for hp in range(4):
        segs["qi%d" % hp] = km(w[:, C_QI + hp * 128: C_QI + (hp + 1) * 128])
    fw = inp["ffn_w_in"][l]
    for j in range(NJ):
        segs["f%d" % j] = km(np.concatenate([fw[:, j * 128:(j + 1) * 128], fw[:, FH + j * 128: FH + (j + 1) * 128]], axis=1))
    cre = inp["s5_c_re"][l]
    cim = inp["s5_c_im"][l]
    Cre = np.zeros((128, 16, 128), np.float32)
    Cim = np.zeros((128, 16, 128), np.float32)
    for g in range(32):
        j, g2 = g // 2, g % 2
        jj = j % 4
        m0 = 32 * jj + 16 * g2
        Cre[g2 * 64:(g2 + 1) * 64, j, m0:m0 + 16] = cre[g].T
        Cim[g2 * 64:(g2 + 1) * 64, j, m0:m0 + 16] = cim[g].T
    segs["sC"] = np.concatenate([Cre.reshape(128, 2048), Cim.reshape(128, 2048)], axis=1)
    Dg = np.zeros((128, 4, 128), np.float32)
    d = inp["s5_d"][l]
    for cc in range(4):
        Dg[np.arange(128), cc, np.arange(128)] = d[cc * 128:(cc + 1) * 128]
    segs["sD"] = Dg.reshape(128, 512)
    return segs


def host_pf(inp, l):
    pf = np.zeros((128, NPF), np.float32)
    for off, name in ((PF_GMP, "norm_mix_pre"), (PF_GMO, "norm_mix_post"), (PF_GFP, "norm_ffn_pre"), (PF_GFO, "norm_ffn_post")):
        pf[:, off:off + 8] = inp[name][l].reshape(8, 128).T
    pf[:, PF_PSC:PF_PSC + 4] = inp["pool_scale"][l].reshape(4, 128).T
    lr, li, ld = inp["s5_lambda_re"][l], inp["s5_lambda_im"][l], inp["s5_log_dt"][l]
    for j in range(16):
        for g2 in range(2):
            g = 2 * j + g2
            pf[g2 * 64:(g2 + 1) * 64, PF_LRS + j] = lr[g]
            pf[g2 * 64:(g2 + 1) * 64, PF_LIS + j] = li[g]
            pf[g2 * 64:(g2 + 1) * 64, PF_LDS + j] = ld[g]
    br, bi = inp["s5_b_re"][l], inp["s5_b_im"][l]
    X = np.zeros((5, 128, 4, 128), np.float32)
    for cc in range(4):
        for p in range(128):
            g = cc * 8 + p // 16
            i = p % 16
            X[0, p, cc, :] = np.tile(lr[g], 2)
            X[1, p, cc, :] = np.tile(li[g], 2)
            X[2, p, cc, :] = ld[g]
            g2 = g % 2
            X[3, p, cc, g2 * 64:(g2 + 1) * 64] = br[g, :, i]
            X[4, p, cc, g2 * 64:(g2 + 1) * 64] = bi[g, :, i]
    for k, off in enumerate((PF_LRX, PF_LIX, PF_LDX, PF_BRX, PF_BIX)):
        pf[:, off:off + 512] = X[k].reshape(128, 512)
    return pf


def host_consts(inp):
    cf = np.zeros((128, NCF), np.float32)
    q = np.arange(128)[:, None]
    s = np.arange(128)[None, :]
    cf[:, CF_CMASK:CF_CMASK + 128] = np.where(s > q, np.float32(-1e4), np.float32(0.0))
    cf[:, CF_INVC:CF_INVC + 16] = (1.0 / np.arange(1, 17, dtype=np.float32))[None, :]
    cf[:, CF_ONES:CF_ONES + 64] = 1.0
    par = ((np.arange(128) // 32) % 2).astype(np.float32)
    cf[:, CF_PAR] = 1.0 - par
    cf[:, CF_PAR + 1] = par
    cb = np.zeros((128, NCB), np.float32)
    cb[:, CB_ID:CB_ID + 128] = np.eye(128, dtype=np.float32)
    cb[:, CB_SEL:CB_SEL + 512] = np.tile(np.eye(128, dtype=np.float32), (1, 4))
    rb = inp["rel_bias"]
    sl = np.arange(128)[:, None]
    ql = np.arange(128)[None, :]
    bt = np.zeros((128, 2, 16, 128), np.float32)
    for kind in range(2):
        dist = ql - sl + 128 * kind
        idx = rel_bucket_np(np.maximum(dist, 0))
        bt[:, kind, :, :] = rb[idx].transpose(0, 2, 1)
    cb[:, CB_BIAS:CB_BIAS + 4096] = bt.reshape(128, 4096)
    b31 = np.ascontiguousarray(np.repeat(rb[31][:, None], TB, axis=1)).reshape(1, 16 * TB).astype(np.float32)
    return cf, cb, b31


_CACHE = {}


def get_program(nlayers=NL, stop=None, taps=()):
    key = (nlayers, stop, tuple(taps))
    if key not in _CACHE:
        b0 = Builder(nlayers, stop, taps)
        b0.wtotal = 1 << 20
        b0.build()
        b = Builder(nlayers, stop, taps)
        b.wtotal = max(b0.seg_total, 16)
        nc = b.build()
        _CACHE[key] = (nc, b)
    return _CACHE[key]


def run(inputs, nlayers=NL, stop=None, taps=()):
    nc, b = get_program(nlayers, stop, taps)
    inp = {k: np.asarray(v, dtype=np.float32) for k, v in inputs.items()}
    ws = np.zeros((NL, 128, b.wtotal), np.float32)
    pfs = np.zeros((NL, 128, NPF), np.float32)
    for l in range(NL):
        segs = host_segments(inp, l)
        for name, (off, n) in b.seg_off.items():
            a = segs[name]
            assert a.shape == (128, n), (name, a.shape, n)
            ws[l, :, off:off + n] = a
        pfs[l] = host_pf(inp, l)
    cf, cb, b31 = host_consts(inp)
    x = inp["x"]
    in_maps = []
    for c in range(8):
        xt = np.ascontiguousarray(x[c].T.reshape(KC, 128, S).transpose(1, 0, 2))
        in_maps.append({"x": xt, "wstream": ws, "pf": pfs, "cf": cf, "cb": cb, "b31": b31})
    res = run_bass_kernel_spmd(nc, in_maps, core_ids=list(range(8)))
    return res, b


def kernel(**inputs):
    res, b = run(inputs)
    outs = []
    for c in range(8):
        o = res.results[c]["out"]
        outs.append(np.ascontiguousarray(o.transpose(1, 0, 2).reshape(D, S).T))
    return np.stack(outs, axis=0).astype(np.float32)
```

```python
import contextlib
import math
import numpy as np
import concourse.bass as bass
import concourse.mybir as mybir
from concourse.bass_utils import run_bass_kernel_spmd

F32 = mybir.dt.float32
BF16 = mybir.dt.bfloat16
I32 = mybir.dt.int32
ALU = mybir.AluOpType
AF = mybir.ActivationFunctionType

ENGS = ("pe", "act", "dve", "pool", "sp")


class Op:
    __slots__ = ("eng", "fn", "waits", "signal", "slot", "seq", "dcount")

    def __init__(self, eng, fn):
        self.eng = eng
        self.fn = fn
        self.waits = {}
        self.signal = False
        self.slot = None
        self.seq = 0
        self.dcount = 0


class Prog:
    def __init__(self):
        self.ops = {e: [] for e in ENGS}
        self.last_w = {}
        self.readers = {}
        self.slot_count = {}
        self.pending = {e: {} for e in ENGS}

    def _add_wait(self, op, tok, raw):
        kind, who, val = tok
        if kind == "E":
            if who == op.eng and not raw:
                return
            if who == "pe" and op.eng == "pe":
                return
            self.ops[who][val].signal = True
        k = (kind, who)
        if op.waits.get(k, -1) < val:
            op.waits[k] = val

    def op(self, eng, fn, r=(), w=(), slot=None):
        o = Op(eng, fn)
        o.seq = len(self.ops[eng])
        if slot is not None:
            o.slot = slot
            self.slot_count[slot] = self.slot_count.get(slot, 0) + 1
            o.dcount = self.slot_count[slot]
            tok = ("D", slot, o.dcount)
        else:
            tok = ("E", eng, o.seq)
        for k, v in self.pending[eng].items():
            if o.waits.get(k, -1) < v:
                o.waits[k] = v
        self.pending[eng] = {}
        for k in r:
            lw = self.last_w.get(k)
            if lw is not None:
                self._add_wait(o, lw, True)
        for k in w:
            lw = self.last_w.get(k)
            if lw is not None:
                self._add_wait(o, lw, False)
            for t in self.readers.get(k, ()):
                self._add_wait(o, t, False)
        self.ops[eng].append(o)
        for k in r:
            self.readers.setdefault(k, []).append(tok)
        for k in w:
            self.last_w[k] = tok
            self.readers[k] = []
        return o

    def barrier(self):
        toks = {}
        for e in ENGS:
            for o in reversed(self.ops[e]):
                if o.slot is None:
                    o.signal = True
                    toks[("E", e)] = o.seq
                    break
        for s, c in self.slot_count.items():
            toks[("D", s)] = c
        for e in ENGS:
            for k, v in toks.items():
                if k == ("E", e):
                    continue
                if self.pending[e].get(k, -1) < v:
                    self.pending[e][k] = v
        self.last_w = {}
        self.readers = {}

    def emit(self, nc, final_slots=()):
        with contextlib.ExitStack() as st:
            esem = {e: st.enter_context(nc.semaphore("s_" + e)) for e in ENGS}
            dsem = {s: st.enter_context(nc.semaphore("d_%d" % i)) for i, s in enumerate(self.slot_count)}
            sigcount = {}
            for e in ENGS:
                c = 0
                arr = []
                for o in self.ops[e]:
                    if o.slot is None and o.signal:
                        c += 1
                    arr.append(c)
                sigcount[e] = arr
            block = st.enter_context(nc.Block())
            prog = self

            def make(e):
                def body(eng):
                    waited = {}
                    for o in prog.ops[e]:
                        for (kind, who), val in o.waits.items():
                            if kind == "E":
                                sem = esem[who]
                                v = sigcount[who][val]
                            else:
                                sem = dsem[who]
                                v = 16 * val
                            if waited.get((kind, who), -1) >= v:
                                continue
                            waited[(kind, who)] = v
                            eng.wait_ge(sem, v)
                        ins = o.fn(eng)
                        if o.slot is not None:
                            ins.then_inc(dsem[o.slot], 16)
                        elif o.signal:
                            ins.then_inc(esem[e], 1)
                    if e == "sp":
                        for s in final_slots:
                            eng.wait_ge(dsem[s], 16 * prog.slot_count[s])
                return body

            block.tensor(make("pe"))
            block.scalar(make("act"))
            block.vector(make("dve"))
            block.gpsimd(make("pool"))
            block.sync(make("sp"))


S = 2048
D = 1024
TB = 512
NTB = 4
KC = 8
NL = 2
FH = 2816
NJ = 22
EPS = 1e-6
IDX_SCALE = (8 ** -0.5) * (64 ** -0.5)
NEG = -30000.0
NBIS = 15

C_POOL, C_Q, C_K, C_V, C_QI, C_KI, C_WI, C_S5, C_G = 0, 512, 1536, 1600, 1664, 2176, 2240, 2248, 2760

PF_GMP, PF_GMO, PF_GFP, PF_GFO, PF_PSC = 0, 8, 16, 24, 32
PF_LRS, PF_LIS, PF_LDS = 36, 52, 68
PF_LRX, PF_LIX, PF_LDX, PF_BRX, PF_BIX = 84, 596, 1108, 1620, 2132
NPF = 2644
CF_CMASK, CF_INVC, CF_ONES, CF_PAR = 0, 128, 144, 208
NCF = 212
CB_ID, CB_SEL, CB_BIAS = 0, 128, 640
NCB = 640 + 4096

ARENA = 86 * 1024
RSLOT = 4096
NRING = 4
KV0 = 75 * 1024


def rel_bucket_np(dist):
    dist = np.asarray(dist, np.int32)
    d_f = np.maximum(dist, 1).astype(np.float32)
    large = 16 + (np.log(d_f / np.float32(16)) / np.float32(math.log(128 / 16)) * np.float32(16)).astype(np.int32)
    large = np.minimum(large, 31)
    return np.where(dist < 16, dist, large)


class Builder:
    def __init__(self, nlayers=NL, stop=None, taps=()):
        self.nlayers = nlayers
        self.stop = stop
        self.taps = taps
        self.seg_off = {}
        self.seg_total = 0
        self.ring_i = 0
        self.bank_i = 0
        self.bankset = list(range(8))
        self.tapouts = {}

    def carve(self, off, parts, shape, dt):
        esz = 2 if dt == BF16 else 4
        n = int(np.prod(shape))
        assert off % 4 == 0 and off + n * esz <= ARENA, (off, n, esz)
        ap = self.arena[0:parts, off // 2: off // 2 + n * esz // 2]
        if dt != BF16:
            ap = ap.bitcast(dt)
        if len(shape) == 2:
            return ap.rearrange("p (a b) -> p a b", a=shape[0])
        if len(shape) == 3:
            return ap.rearrange("p (a b c) -> p a b c", a=shape[0], b=shape[1])
        return ap

    def bank(self):
        b = self.bankset[self.bank_i % len(self.bankset)]
        self.bank_i += 1
        return b

    def psb(self, b, parts=128, n=512):
        return self.ps[0:parts, b * 512: b * 512 + n]

    def wload(self, l, name, n):
        assert n <= RSLOT
        if name not in self.seg_off:
            self.seg_off[name] = (self.seg_total, n)
            self.seg_total += n
        off, n0 = self.seg_off[name]
        assert n0 == n
        slot = self.ring_i % NRING
        self.ring_i += 1
        dst = self.ring[:, slot * RSLOT: slot * RSLOT + n]
        src = self.wstream[l, :, off:off + n]
        key = ("ring", slot)
        self.P.op("pool", lambda e: e.dma_start(out=dst, in_=src, max_dma_last_dim=8192), w=[key], slot="ring%d" % slot)
        return dst, key

    def mm(self, out, lhsT, rhs, start, stop, r, w):
        self.P.op("pe", lambda e: e.matmul(out, lhsT, rhs, start=start, stop=stop), r=r, w=w)

    def act(self, out, in_, func, r, w, scale=1.0, bias=0.0):
        self.P.op("act", lambda e: e.activation(out=out, in_=in_, func=func, bias=bias, scale=scale), r=r, w=w)

    def tt(self, out, in0, in1, op, r, w):
        self.P.op("dve", lambda e: e.tensor_tensor(out=out, in0=in0, in1=in1, op=op), r=r, w=w)

    def ts(self, out, in0, s1, s2, op0, op1, r, w, accum_out=None):
        if accum_out is None:
            self.P.op("dve", lambda e: e.tensor_scalar(out=out, in0=in0, scalar1=s1, scalar2=s2, op0=op0, op1=op1), r=r, w=w)
        else:
            self.P.op("dve", lambda e: e.tensor_scalar(out=out, in0=in0, scalar1=s1, scalar2=s2, op0=op0, op1=op1, accum_out=accum_out), r=r, w=w)

    def stt(self, out, in0, scalar, in1, op0, op1, r, w):
        self.P.op("dve", lambda e: e.scalar_tensor_tensor(out=out, in0=in0, scalar=scalar, in1=in1, op0=op0, op1=op1), r=r, w=w)

    def dma(self, eng, out, in_, r, w, slot):
        self.P.op(eng, lambda e: e.dma_start(out=out, in_=in_), r=r, w=w, slot=slot)

    def tap(self, name, ap, keys, shape, parts=128):
        if name not in self.taps:
            return
        t = self.nc.dram_tensor("tap_" + name, [parts] + list(shape), ap.dtype, kind="ExternalOutput").ap()
        self.tapouts[name] = t
        self.P.op("sp", lambda e: e.dma_start(out=t, in_=ap), r=keys, slot="tap_" + name)
        self.final_slots.append("tap_" + name)

    def rms_rstd(self, src_f32, src_keys, sq, tag):
        P = self.P
        for c in range(KC):
            self.act(sq[:, c, :], src_f32[:, c, :], AF.Square, r=[src_keys[c]], w=[("sq", c)])
        b = self.bank()
        for c in range(KC):
            self.mm(self.psb(b), self.ones_bf[:, :], sq[:, c, :], c == 0, c == KC - 1, r=[("sq", c), "ones_bf"], w=[("ps", b)])
        self.act(self.rstd[:, :], self.psb(b), AF.Sqrt, r=[("ps", b), "epsc"], w=["rstd"], scale=1.0 / D, bias=self.epsc[:, 0:1])
        P.op("dve", lambda e: e.reciprocal(out=self.rstd[:, :], in_=self.rstd[:, :]), r=["rstd"], w=["rstd"])

    def build(self):
        nc = bass.Bass("TRN2", target_bir_lowering=False)
        self.nc = nc
        self.final_slots = []
        P = self.P = Prog()
        xin = nc.dram_tensor("x", [128, KC, S], F32, kind="ExternalInput").ap()
        out = nc.dram_tensor("out", [128, KC, S], F32, kind="ExternalOutput").ap()
        xres = nc.dram_tensor("xres", [128, KC, S], F32, kind="Internal").ap()
        self.wstream = nc.dram_tensor("wstream", [NL, 128, self.wtotal], F32, kind="ExternalInput").ap()
        pfd = nc.dram_tensor("pf", [NL, 128, NPF], F32, kind="ExternalInput").ap()
        cfd = nc.dram_tensor("cf", [128, NCF], F32, kind="ExternalInput").ap()
        cbd = nc.dram_tensor("cb", [128, NCB], F32, kind="ExternalInput").ap()
        b31d = nc.dram_tensor("b31", [1, 16 * TB], F32, kind="ExternalInput").ap()
        with contextlib.ExitStack() as st:
            E = st.enter_context
            self.arena = E(nc.sbuf_tensor("arena", [128, ARENA // 2], BF16))
            hT = E(nc.sbuf_tensor("hT", [128, KC, S], BF16))
            merged = E(nc.sbuf_tensor("merged", [128, KC, S], BF16))
            self.ring = E(nc.sbuf_tensor("ring", [128, NRING * RSLOT], BF16))
            pf = E(nc.sbuf_tensor("pfs", [128, NPF], F32))
            self.pf_t = pf
            cf = E(nc.sbuf_tensor("cfs", [128, NCF], F32))
            cb = E(nc.sbuf_tensor("cbs", [128, NCB], BF16))
            self.ones_bf = E(nc.sbuf_tensor("ones_bf", [128, 128], BF16))
            self.rstd = E(nc.sbuf_tensor("rstd", [128, TB], F32))
            self.epsc = E(nc.sbuf_tensor("epsc", [128, 8], F32))
            self.hpi = E(nc.sbuf_tensor("hpi", [128, 8], F32))
            self.ps = E(nc.psum_tensor("ps", [128, 4096], F32))
            ps = self.ps
            ident = cb[:, CB_ID:CB_ID + 128]
            sel4 = cb[:, CB_SEL:CB_SEL + 512].rearrange("p (h q) -> p h q", h=4)
            biasT = cb[:, CB_BIAS:CB_BIAS + 4096].rearrange("p (k h q) -> p k h q", k=2, h=16)
            cmask = cf[:, CF_CMASK:CF_CMASK + 128]
            invc = cf[:, CF_INVC:CF_INVC + 16]
            ones64 = cf[:, CF_ONES:CF_ONES + 64]
            self.cf = cf

            P.op("dve", lambda e: e.memset(self.ones_bf[:, :], 1.0), w=["ones_bf"])
            P.op("dve", lambda e: e.memset(self.epsc[:, :], EPS), w=["epsc"])
            P.op("dve", lambda e: e.memset(self.hpi[:, :], math.pi / 2), w=["hpi"])
            self.dma("sp", cf[:, :], cfd, r=[], w=["cf"], slot="cf")
            P.op("pool", lambda e: e.dma_start(out=cb[:, :], in_=cbd, max_dma_last_dim=8192), w=["cb"], slot="cb")

            for l in range(self.nlayers):
                xsrc = xin if l == 0 else xres
                xdst = out if l == self.nlayers - 1 else xres
                self.layer(l, xsrc, xdst, hT, merged, pf, pfd, ident, sel4, biasT, cmask, invc, ones64, b31d)
                if self.stop is not None:
                    break
            P.barrier()
            P.emit(nc, final_slots=self.final_slots)
        return nc

    def layer(self, l, xsrc, xdst, hT, merged, pf, pfd, ident, sel4, biasT, cmask, invc, ones64, b31d):
        P = self.P
        nc = self.nc
        ps = self.ps
        stop = self.stop
        P.barrier()
        self.bankset = list(range(8))
        self.dma("sp", pf[:, :], pfd[l], r=[], w=["pf"], slot="pf")
        gmp = pf[:, PF_GMP:PF_GMP + 8]
        gmo = pf[:, PF_GMO:PF_GMO + 8]
        gfp = pf[:, PF_GFP:PF_GFP + 8]
        gfo = pf[:, PF_GFO:PF_GFO + 8]
        psc = pf[:, PF_PSC:PF_PSC + 4]

        xblk = self.carve(0, 128, [KC, TB], F32)
        sq = self.carve(16 * 1024, 128, [KC, TB], BF16)
        for n in range(NTB):
            tsl = slice(n * TB, (n + 1) * TB)
            self.dma("sp", xblk[:, :, :], xsrc[:, :, tsl], r=[], w=[("xblk", c) for c in range(KC)], slot="xblk")
            self.rms_rstd(xblk, [("xblk", c) for c in range(KC)], sq, "p1")
            for c in range(KC):
                self.stt(hT[:, c, tsl], xblk[:, c, :], gmp[:, c:c + 1], self.rstd[:, :], ALU.mult, ALU.mult,
                         r=[("xblk", c), "pf", "rstd"], w=[("hT", c, n)])
        self.tap("hT", hT[:, :, :], [("hT", c, n) for c in range(KC) for n in range(NTB)], [KC, S])
        if stop == 1:
            return
        P.barrier()

        kaugT = self.arena[0:65, KV0 // 2: KV0 // 2 + S]
        kiT = self.arena[0:64, KV0 // 2 + S: KV0 // 2 + 2 * S]
        vaug = self.arena[:, KV0 // 2 + 2 * S: KV0 // 2 + 2 * S + 16 * 66].rearrange("p (t d) -> p t d", t=16)
        wi_off = KV0 + 2 * (2 * S + 16 * 66)
        wi_s = self.arena[:, wi_off // 2: wi_off // 2 + 256].bitcast(F32).rearrange("p (t h) -> p t h", t=16)
        assert wi_off + 512 <= ARENA
        uz = self.carve(0, 128, [4, S], BF16)
        upp = [self.carve(16 * 1024, 128, [1, S], F32)[:, 0, :], self.carve(24 * 1024, 128, [1, S], F32)[:, 0, :]]
        dT = self.carve(32 * 1024, 128, [4, S], BF16)
        yp = self.carve(48 * 1024, 128, [4, S], BF16)
        sgt = self.carve(64 * 1024, 128, [1, TB], BF16)[:, 0, :]

        def hkeys(n):
            return [("hT", c, n) for c in range(KC)]

        seg, sk = self.wload(l, "kk", 1024)
        seg = seg.rearrange("p (k m) -> p k m", k=KC)
        P.op("dve", lambda e: e.memset(kaugT[64:65, :], 1.0), w=["kaug1"])
        P.op("dve", lambda e: e.memset(vaug[:, :, 64:65], 1.0), w=["vaug1"])
        for n in range(NTB):
            tsl = slice(n * TB, (n + 1) * TB)
            for half, dst, key in ((0, kaugT, "kT"), (1, kiT, "kiT")):
                b = self.bank()
                for kc in range(KC):
                    self.mm(self.psb(b, 64), seg[:, kc, half * 64:(half + 1) * 64], hT[:, kc, tsl], kc == 0, kc == KC - 1,
                            r=[sk, ("hT", kc, n)], w=[("ps", b)])
                self.act(dst[0:64, tsl], self.psb(b, 64), AF.Copy, r=[("ps", b)], w=[(key, n)])
        seg, sk = self.wload(l, "vw", KC * 72)
        seg = seg.rearrange("p (k m) -> p k m", k=KC)
        for tt_ in range(16):
            n = tt_ // 4
            b = self.bank()
            for kc in range(KC):
                self.mm(ps[:, b * 512: b * 512 + 72], hT[:, kc, tt_ * 128:(tt_ + 1) * 128], seg[:, kc, :], kc == 0, kc == KC - 1,
                        r=[sk, ("hT", kc, n)], w=[("ps", b)])
            self.act(vaug[:, tt_, 0:64], ps[:, b * 512: b * 512 + 64], AF.Copy, r=[("ps", b)], w=[("vaug", tt_)])
            self.act(wi_s[:, tt_, :], ps[:, b * 512 + 64: b * 512 + 72], AF.Copy, r=[("ps", b)], w=[("wi", tt_)], scale=IDX_SCALE)
        for cc in range(4):
            seg, sk = self.wload(l, "su%d" % cc, 1024)
            seg = seg.rearrange("p (k m) -> p k m", k=KC)
            for n in range(NTB):
                tsl = slice(n * TB, (n + 1) * TB)
                b = self.bank()
                for kc in range(KC):
                    self.mm(self.psb(b), seg[:, kc, :], hT[:, kc, tsl], kc == 0, kc == KC - 1, r=[sk, ("hT", kc, n)], w=[("ps", b)])
                self.act(uz[:, cc, tsl], self.psb(b), AF.Copy, r=[("ps", b)], w=[("uz", cc, n)])
        for cc in range(4):
            seg, sk = self.wload(l, "pu%d" % cc, 1024)
            seg = seg.rearrange("p (k m) -> p k m", k=KC)
            u0 = upp[0]
            for n in range(NTB):
                tsl = slice(n * TB, (n + 1) * TB)
                b = self.bank()
                for kc in range(KC):
                    self.mm(self.psb(b), seg[:, kc, :], hT[:, kc, tsl], kc == 0, kc == KC - 1, r=[sk, ("hT", kc, n)], w=[("ps", b)])
                self.act(u0[:, tsl], self.psb(b), AF.Copy, r=[("ps", b)], w=["up0"])
            wlen = 2 ** (cc + 1)
            sA = upp[1]
            sB = self.carve(66 * 1024, 128, [1, S], F32)[:, 0, :]
            bufs = [(sA, "upA"), (sB, "upB")]
            k = 1
            bi = 0
            src, srck = u0, "up0"
            while k < wlen:
                dstb, dstk = bufs[bi % 2]
                self.tt(dstb[:, k:], src[:, k:], src[:, :S - k], ALU.add, r=[srck], w=[dstk])
                P.op("dve", lambda e, d=dstb, s_=src, k=k: e.tensor_copy(out=d[:, 0:k], in_=s_[:, 0:k]), r=[srck], w=[dstk])
                src, srck = dstb, dstk
                bi += 1
                k *= 2
            self.stt(dT[:, cc, :], src[:, :], 1.0 / wlen, u0[:, :], ALU.mult, ALU.subtract, r=[srck, "up0"], w=[("dT", cc)])
            tmpc = self.carve(64 * 1024 + 1024, 128, [1, 16], F32)[:, 0, :]
            self.tt(tmpc[:, 0:wlen - 1], src[:, 0:wlen - 1], invc[:, 0:wlen - 1], ALU.mult, r=[srck, "cf"], w=["tmpc"])
            self.tt(dT[:, cc, 0:wlen - 1], tmpc[:, 0:wlen - 1], u0[:, 0:wlen - 1], ALU.subtract, r=["tmpc", "up0", ("dT", cc)], w=[("dT", cc)])
        self.tap("dT", dT[:, :, :], [("dT", cc) for cc in range(4)], [4, S])
        self.tap("kT", kaugT[:, :], [("kT", n) for n in range(NTB)] + ["kaug1"], [S], parts=65)
        self.tap("vaug", vaug[:, :, :], [("vaug", t) for t in range(16)] + ["vaug1"], [16, 66])
        self.tap("wi", wi_s[:, :, :], [("wi", t) for t in range(16)], [16, 8])
        if stop == 2:
            return

        seg, sk = self.wload(l, "mix", 512)
        seg = seg.rearrange("p (g m) -> p g m", g=4)
        for cc in range(4):
            for n in range(NTB):
                tsl = slice(n * TB, (n + 1) * TB)
                b = self.bank()
                self.mm(self.psb(b), seg[:, cc, :], dT[:, cc, tsl], True, True, r=[sk, ("dT", cc)], w=[("ps", b)])
                self.act(yp[:, cc, tsl], self.psb(b), AF.Copy, r=[("ps", b), "pf"], w=[("yp", cc, n)], scale=psc[:, cc:cc + 1])
        for c in range(KC):
            segp, skp = self.wload(l, "po%d" % c, 512)
            segp = segp.rearrange("p (k m) -> p k m", k=4)
            segg, skg = self.wload(l, "g0_%d" % c, 1024)
            segg = segg.rearrange("p (k m) -> p k m", k=KC)
            for n in range(NTB):
                tsl = slice(n * TB, (n + 1) * TB)
                by = self.bank()
                for cc in range(4):
                    self.mm(self.psb(by), segp[:, cc, :], yp[:, cc, tsl], cc == 0, cc == 3, r=[skp, ("yp", cc, n)], w=[("ps", by)])
                bg = self.bank()
                for kc in range(KC):
                    self.mm(self.psb(bg), segg[:, kc, :], hT[:, kc, tsl], kc == 0, kc == KC - 1, r=[skg, ("hT", kc, n)], w=[("ps", bg)])
                self.act(sgt[:, :], self.psb(bg), AF.Sigmoid, r=[("ps", bg)], w=["sgt"])
                self.tt(merged[:, c, tsl], sgt[:, :], self.psb(by), ALU.mult, r=["sgt", ("ps", by)], w=[("mg", c, n)])
        self.tap("mg3", merged[:, :, S - 256:S], [("mg", c, n) for c in range(KC) for n in range(NTB)], [KC, 256])
        if stop == 3:
            return
        P.barrier()

        self.phase_s5(l, hT, merged, pf, uz)
        self.tap("mg4", merged[:, :, S - 256:S], [("mg", c, n) for c in range(KC) for n in range(NTB)], [KC, 256])
        if stop == 4:
            return
        P.barrier()

        self.phase_attn(l, hT, merged, kaugT, kiT, vaug, wi_s, ident, sel4, biasT, cmask, ones64, b31d)
        self.tap("mg5", merged[:, :, S - 256:S], [("mg", c, n) for c in range(KC) for n in range(NTB)], [KC, 256])
        if stop == 5:
            return
        P.barrier()

        self.bankset = list(range(8))
        xT = self.carve(0, 128, [KC, S], F32)
        sq = hT[:, 4:6, :].rearrange("p a b -> p (a b)").rearrange("p (c t) -> p c t", c=KC)
        xblk = hT[:, 0:4, :].rearrange("p a b -> p (a b)").bitcast(F32).rearrange("p (c t) -> p c t", c=KC)
        tmpf = self.carve(64 * 1024, 128, [1, TB], F32)[:, 0, :]
        for c in range(KC):
            seg, sk = self.wload(l, "wo%d" % c, 1024)
            seg = seg.rearrange("p (k m) -> p k m", k=KC)
            for n in range(NTB):
                tsl = slice(n * TB, (n + 1) * TB)
                b = self.bank()
                for kc in range(KC):
                    self.mm(self.psb(b), seg[:, kc, :], merged[:, kc, tsl], kc == 0, kc == KC - 1, r=[sk, ("mg", kc, n)], w=[("ps", b)])
                self.act(xT[:, c, tsl], self.psb(b), AF.Copy, r=[("ps", b)], w=[("xT", c, n)])
        for n in range(NTB):
            tsl = slice(n * TB, (n + 1) * TB)
            self.dma("sp", xblk[:, :, :], xsrc[:, :, tsl], r=[], w=[("xblk", c) for c in range(KC)], slot="xblk")
            self.rms_rstd(xT[:, :, tsl], [("xT", c, n) for c in range(KC)], sq, "p6")
            for c in range(KC):
                self.stt(tmpf[:, :], xT[:, c, tsl], gmo[:, c:c + 1], self.rstd[:, :], ALU.mult, ALU.mult,
                         r=[("xT", c, n), "pf", "rstd"], w=["tmpf"])
                self.tt(xT[:, c, tsl], tmpf[:, :], xblk[:, c, :], ALU.add, r=["tmpf", ("xblk", c)], w=[("xT", c, n)])
        self.tap("x6", xT[:, :, S - 256:S], [("xT", c, n) for c in range(KC) for n in range(NTB)], [KC, 256])
        if stop == 6:
            return
        P.barrier()

        fT = self.carve(64 * 1024, 128, [NJ, TB], BF16)
        sq = merged[:, 4:6, :].rearrange("p a b -> p (a b)").rearrange("p (c t) -> p c t", c=KC)
        mbuf = merged[:, 0:4, :].rearrange("p a b -> p (a b)").bitcast(F32).rearrange("p (c t) -> p c t", c=KC)
        sgf = merged[:, 6, 0:TB]
        tmpf = merged[:, 7, 0:2 * TB].bitcast(F32)
        for n in range(NTB):
            tsl = slice(n * TB, (n + 1) * TB)
            self.rms_rstd(xT[:, :, tsl], [("xT", c, n) for c in range(KC)], sq, "p7a")
            for c in range(KC):
                self.stt(hT[:, c, tsl], xT[:, c, tsl], gfp[:, c:c + 1], self.rstd[:, :], ALU.mult, ALU.mult,
                         r=[("xT", c, n), "pf", "rstd"], w=[("hT", c, n)])
            for j in range(NJ):
                seg, sk = self.wload(l, "f%d" % j, 2048)
                seg = seg.rearrange("p (k m) -> p k m", k=KC)
                bg = self.bank()
                for kc in range(KC):
                    self.mm(self.psb(bg), seg[:, kc, 0:128], hT[:, kc, tsl], kc == 0, kc == KC - 1, r=[sk, ("hT", kc, n)], w=[("ps", bg)])
                bu = self.bank()
                for kc in range(KC):
                    self.mm(self.psb(bu), seg[:, kc, 128:256], hT[:, kc, tsl], kc == 0, kc == KC - 1, r=[sk, ("hT", kc, n)], w=[("ps", bu)])
                self.act(sgf, self.psb(bg), AF.Silu, r=[("ps", bg)], w=["sgf"])
                self.tt(fT[:, j, :], sgf, self.psb(bu), ALU.mult, r=["sgf", ("ps", bu)], w=[("fT", j)])
            for c in range(KC):
                seg, sk = self.wload(l, "fo%d" % c, NJ * 128)
                seg = seg.rearrange("p (k m) -> p k m", k=NJ)
                b = self.bank()
                for j in range(NJ):
                    self.mm(self.psb(b), seg[:, j, :], fT[:, j, :], j == 0, j == NJ - 1, r=[sk, ("fT", j)], w=[("ps", b)])
                self.act(mbuf[:, c, :], self.psb(b), AF.Copy, r=[("ps", b)], w=[("mbuf", c)])
            self.rms_rstd(mbuf, [("mbuf", c) for c in range(KC)], sq, "p7b")
            for c in range(KC):
                self.stt(tmpf, mbuf[:, c, :], gfo[:, c:c + 1], self.rstd[:, :], ALU.mult, ALU.mult,
                         r=[("mbuf", c), "pf", "rstd"], w=["tmpf"])
                self.tt(mbuf[:, c, :], tmpf, xT[:, c, tsl], ALU.add, r=["tmpf", ("xT", c, n)], w=[("mbuf", c)])
            self.dma("sp", xdst[:, :, tsl], mbuf[:, :, :], r=[("mbuf", c) for c in range(KC)], w=[], slot="xout")
        if "xout" not in self.final_slots:
            self.final_slots.append("xout")

    def phase_s5(self, l, hT, merged, pf, uz):
        P = self.P
        K = 1024
        Ec = self.carve(16 * K, 128, [1, S], F32)[:, 0, :]
        Es = self.carve(24 * K, 128, [1, S], F32)[:, 0, :]
        vre = self.carve(32 * K, 128, [1, S], F32)[:, 0, :]
        vim = self.carve(40 * K, 128, [1, S], F32)[:, 0, :]
        xre = self.carve(48 * K, 128, [1, S], BF16)[:, 0, :]
        xim = self.carve(52 * K, 128, [1, S], BF16)[:, 0, :]
        tmp1 = self.carve(56 * K, 128, [1, TB], F32)[:, 0, :]
        tmp2 = self.carve(58 * K, 128, [1, TB], F32)[:, 0, :]
        Bre = self.carve(60 * K, 128, [2, 4, 128], BF16)
        Bim = self.carve(62 * K, 128, [2, 4, 128], BF16)
        gt = self.carve(71 * K, 128, [1, TB], F32)[:, 0, :]
        sm = self.carve(64 * K, 128, [16, 16], F32)
        s1 = self.carve(66 * K, 128, [1, TB], BF16)[:, 0, :]
        s2 = self.carve(67 * K, 128, [1, TB], BF16)[:, 0, :]
        t3 = self.carve(68 * K, 128, [1, TB], F32)[:, 0, :]
        xw = self.carve(32 * K, 128, [8, 512], F32)
        smi = self.carve(70 * K, 128, [1, 16], I32)[:, 0, :]
        TWO_PI = 2.0 * math.pi

        def pfx(o):
            return pf[:, o:o + 512]

        def dve(fn, r, w):
            P.op("dve", fn, r=r, w=w)

        def zoh(lr, li, ld, wk, n, tag, itile):
            dt_, lrdt, th, kf, q, sn, cs, t0 = wk[:8]
            kk = ["z%s%d" % (tag, i) for i in range(8)]
            kdt, klrdt, kth, kkf, kq, ksn, kcs, kt0 = kk
            ki = "z%si" % tag
            self.act(dt_, ld, AF.Exp, r=["pf"], w=[kdt])
            self.tt(lrdt, lr, dt_, ALU.mult, r=["pf", kdt], w=[klrdt])
            self.tt(th, li, dt_, ALU.mult, r=["pf", kdt], w=[kth])
            self.ts(kf, th, 1.0 / TWO_PI, None, ALU.mult, ALU.bypass, r=[kth], w=[kkf])
            dve(lambda e: e.tensor_copy(out=itile, in_=kf), r=[kkf], w=[ki])
            dve(lambda e: e.tensor_copy(out=kf, in_=itile), r=[ki], w=[kkf])
            self.stt(q, kf, -TWO_PI, th, ALU.mult, ALU.add, r=[kkf, kth], w=[kq])
            self.act(sn, q, AF.Sin, r=[kq], w=[ksn], scale=0.25)
            self.act(cs, q, AF.Sin, r=[kq, "hpi"], w=[kcs], scale=0.25, bias=self.hpi[:, 0:1])
            for it in range(2):
                self.tt(t0, sn, sn, ALU.mult, r=[ksn], w=[kt0])
                self.stt(sn, sn, 2.0, cs, ALU.mult, ALU.mult, r=[ksn, kcs], w=[ksn])
                self.ts(cs, t0, -2.0, 1.0, ALU.mult, ALU.add, r=[kt0], w=[kcs])
            self.act(dt_, lrdt, AF.Exp, r=[klrdt], w=[kdt])
            return dict(mag=dt_, cos=cs, sin=sn, keys=[kdt, kcs, ksn], k=kk)

        wkx = [xw[:, i, :] for i in range(8)]
        smx = self.carve(56 * K, 128, [1, 512], I32)[:, 0, :]
        zx = zoh(pfx(PF_LRX), pfx(PF_LIX), pfx(PF_LDX), wkx, 512, "x", smx)
        K_ = zx["k"]
        mg_, cs_x, sn_x = wkx[0], wkx[6], wkx[5]
        are, aim, den, cre, cim = wkx[1], wkx[2], wkx[3], wkx[4], wkx[7]
        kare, kaim, kden, kcre, kcim = K_[1], K_[2], K_[3], K_[4], K_[7]
        kmg, kcs, ksn = K_[0], K_[6], K_[5]
        lr, li = pfx(PF_LRX), pfx(PF_LIX)
        self.tt(are, mg_, cs_x, ALU.mult, r=[kmg, kcs], w=[kare])
        self.tt(aim, mg_, sn_x, ALU.mult, r=[kmg, ksn], w=[kaim])
        self.ts(are, are, -1.0, None, ALU.add, ALU.bypass, r=[kare], w=[kare])
        t0, kt0 = wkx[0], K_[0]
        t1, kt1, t2, kt2 = wkx[5], K_[5], wkx[6], K_[6]
        self.tt(den, lr, lr, ALU.mult, r=["pf"], w=[kden])
        self.tt(cre, li, li, ALU.mult, r=["pf"], w=[kcre])
        self.tt(den, den, cre, ALU.add, r=[kden, kcre], w=[kden])
        dve(lambda e: e.reciprocal(out=den, in_=den), r=[kden], w=[kden])
        self.tt(cre, are, lr, ALU.mult, r=[kare, "pf"], w=[kcre])
        self.tt(t0, aim, li, ALU.mult, r=[kaim, "pf"], w=[kt0])
        self.tt(cre, cre, t0, ALU.add, r=[kcre, kt0], w=[kcre])
        self.tt(cre, cre, den, ALU.mult, r=[kcre, kden], w=[kcre])
        self.tt(cim, aim, lr, ALU.mult, r=[kaim, "pf"], w=[kcim])
        self.tt(t0, are, li, ALU.mult, r=[kare, "pf"], w=[kt0])
        self.tt(cim, cim, t0, ALU.subtract, r=[kcim, kt0], w=[kcim])
        self.tt(cim, cim, den, ALU.mult, r=[kcim, kden], w=[kcim])
        br, bi = pfx(PF_BRX), pfx(PF_BIX)
        self.tt(t1, cre, br, ALU.mult, r=[kcre, "pf"], w=[kt1])
        self.tt(t2, cim, bi, ALU.mult, r=[kcim, "pf"], w=[kt2])
        self.tt(t1, t1, t2, ALU.subtract, r=[kt1, kt2], w=[kt1])
        for v in range(2):
            self.ts(Bre[:, v, :, :].rearrange("p a b -> p (a b)"), t1, self.cf[:, CF_PAR + v:CF_PAR + v + 1], None, ALU.mult, ALU.bypass,
                    r=[kt1, "cf"], w=["Bre"])
        self.tt(t1, cre, bi, ALU.mult, r=[kcre, "pf"], w=[kt1])
        self.tt(t2, cim, br, ALU.mult, r=[kcim, "pf"], w=[kt2])
        self.tt(t1, t1, t2, ALU.add, r=[kt1, kt2], w=[kt1])
        for v in range(2):
            self.ts(Bim[:, v, :, :].rearrange("p a b -> p (a b)"), t1, self.cf[:, CF_PAR + v:CF_PAR + v + 1], None, ALU.mult, ALU.bypass,
                    r=[kt1, "cf"], w=["Bim"])

        wks = [sm[:, i, :] for i in range(8)]
        zs = zoh(pf[:, PF_LRS:PF_LRS + 16], pf[:, PF_LIS:PF_LIS + 16], pf[:, PF_LDS:PF_LDS + 16], wks, 16, "s", smi)
        mag, cth, sth = zs["mag"], zs["cos"], zs["sin"]
        nsc = sm[:, 8, :]

        segC, skC = self.wload(l, "sC", 4096)
        Cre = segC[:, 0:2048].rearrange("p (j m) -> p j m", j=16)
        Cim = segC[:, 2048:4096].rearrange("p (j m) -> p j m", j=16)
        segD, skD = self.wload(l, "sD", 512)
        Dg = segD.rearrange("p (c m) -> p c m", c=4)

        self.bankset = [0, 1, 2, 3]
        Ec2 = [self.carve((16 + 4 * i) * K, 128, [1, TB], F32)[:, 0, :] for i in range(2)]
        Es2 = [self.carve((18 + 4 * i) * K, 128, [1, TB], F32)[:, 0, :] for i in range(2)]
        vslot_re = [self.carve((32 + 4 * i) * K, 128, [1, TB], F32)[:, 0, :] for i in range(4)]
        vslot_im = [self.carve((34 + 4 * i) * K, 128, [1, TB], F32)[:, 0, :] for i in range(4)]
        pt4 = [self.carve(o * K, 128, [1, TB], F32)[:, 0, :] for o in (73, 24, 27, 29)]
        car = self.carve(26 * K, 128, [1, 16], F32)[:, 0, :]
        vi = 0
        pending = None
        ydefer = []
        for cc in range(4):
            ybanks = [4, 5, 6, 7]
            for jj in range(4):
                j = cc * 4 + jj
                rows = slice(64 * (jj // 2), 64 * (jj // 2) + 64)
                pv = jj % 2
                Ec, Es = Ec2[j % 2], Es2[j % 2]
                ke, ks = ("Ec", j % 2), ("Es", j % 2)
                dve(lambda e, j=j, Ec=Ec: e.tensor_copy(out=Ec[:, 0:1], in_=cth[:, j:j + 1]), r=zs["keys"], w=[ke])
                dve(lambda e, j=j, Es=Es: e.tensor_copy(out=Es[:, 0:1], in_=sth[:, j:j + 1]), r=zs["keys"], w=[ks])
                nn = 1
                lev = 0
                while nn < TB:
                    cs_ = Ec[:, nn - 1:nn]
                    ss_ = Es[:, nn - 1:nn]
                    ns_ = nsc[:, lev:lev + 1]
                    self.ts(ns_, ss_, -1.0, None, ALU.mult, ALU.bypass, r=[ks], w=["nsc"])
                    self.ts(Ec[:, nn:2 * nn], Ec[:, 0:nn], cs_, None, ALU.mult, ALU.bypass, r=[ke], w=[ke])
                    self.stt(Ec[:, nn:2 * nn], Es[:, 0:nn], ns_, Ec[:, nn:2 * nn], ALU.mult, ALU.add, r=[ks, "nsc", ke], w=[ke])
                    self.ts(Es[:, nn:2 * nn], Es[:, 0:nn], cs_, None, ALU.mult, ALU.bypass, r=[ks, ke], w=[ks])
                    self.stt(Es[:, nn:2 * nn], Ec[:, 0:nn], ss_, Es[:, nn:2 * nn], ALU.mult, ALU.add, r=[ke, ks], w=[ks])
                    nn *= 2
                    lev += 1
                cl, sl_, nsl = Ec[:, TB - 1:TB], Es[:, TB - 1:TB], nsc[:, 12:13]
                self.ts(nsl, sl_, -1.0, None, ALU.mult, ALU.bypass, r=[ks], w=["nsl"])
                for n in range(NTB):
                    tsl = slice(n * TB, (n + 1) * TB)
                    vre, vim = vslot_re[vi % 4], vslot_im[vi % 4]
                    kvr, kvi = ("vre", vi % 4), ("vim", vi % 4)
                    vi += 1
                    b1 = self.bank()
                    self.mm(self.psb(b1), Bre[rows, pv, cc, :], uz[rows, cc, tsl], True, True, r=["Bre", ("uz", cc, n)], w=[("ps", b1)])
                    b2 = self.bank()
                    self.mm(self.psb(b2), Bim[rows, pv, cc, :], uz[rows, cc, tsl], True, True, r=["Bim", ("uz", cc, n)], w=[("ps", b2)])
                    self.tt(vre, Ec, self.psb(b1), ALU.mult, r=[ke, ks, ("ps", b1)], w=[kvr])
                    self.tt(tmp1, Es, self.psb(b2), ALU.mult, r=[ks, ("ps", b2)], w=["tmp1"])
                    self.tt(vre, vre, tmp1, ALU.add, r=[kvr, "tmp1"], w=[kvr])
                    self.tt(vim, Ec, self.psb(b2), ALU.mult, r=[ke, ("ps", b2)], w=[kvi])
                    self.tt(tmp2, Es, self.psb(b1), ALU.mult, r=[ks, ("ps", b1)], w=["tmp2"])
                    self.tt(vim, vim, tmp2, ALU.subtract, r=[kvi, "tmp2"], w=[kvi])
                    if n == 0:
                        dve(lambda e, j=j, vre=vre: e.tensor_tensor_scan(out=vre, data0=mag[:, j:j + 1].to_broadcast([128, TB]), data1=vre,
                                                                        initial=0.0, op0=ALU.mult, op1=ALU.add), r=[kvr] + zs["keys"], w=[kvr])
                        dve(lambda e, j=j, vim=vim: e.tensor_tensor_scan(out=vim, data0=mag[:, j:j + 1].to_broadcast([128, TB]), data1=vim,
                                                                        initial=0.0, op0=ALU.mult, op1=ALU.add), r=[kvi] + zs["keys"], w=[kvi])
                    else:
                        dve(lambda e, j=j, vre=vre: e.tensor_tensor_scan(out=vre, data0=mag[:, j:j + 1].to_broadcast([128, TB]), data1=vre,
                                                                        initial=car[:, 0:1], op0=ALU.mult, op1=ALU.add), r=[kvr, "car"] + zs["keys"], w=[kvr])
                        dve(lambda e, j=j, vim=vim: e.tensor_tensor_scan(out=vim, data0=mag[:, j:j + 1].to_broadcast([128, TB]), data1=vim,
                                                                        initial=car[:, 1:2], op0=ALU.mult, op1=ALU.add), r=[kvi, "car"] + zs["keys"], w=[kvi])
                    if n < NTB - 1:
                        wr_l, wi_l = vre[:, TB - 1:TB], vim[:, TB - 1:TB]
                        self.ts(car[:, 2:3], wr_l, cl, None, ALU.mult, ALU.bypass, r=[kvr, ke], w=["car2"])
                        self.ts(car[:, 3:4], wr_l, sl_, None, ALU.mult, ALU.bypass, r=[kvr, ks], w=["car3"])
                        self.stt(car[:, 0:1], wi_l, nsl, car[:, 2:3], ALU.mult, ALU.add, r=[kvi, "nsl", "car2"], w=["car"])
                        self.stt(car[:, 1:2], wi_l, cl, car[:, 3:4], ALU.mult, ALU.add, r=[kvi, ke, "car3", "car"], w=["car"])
                    def ptt(out, in0, in1, op, r, w):
                        P.op("pool", lambda e: e.tensor_tensor(out=out, in0=in0, in1=in1, op=op), r=r, w=w)
                    yb = ybanks[n]
                    ptt(pt4[0], Ec, vre, ALU.mult, [ke, kvr], ["pt0"])
                    ptt(pt4[1], Es, vim, ALU.mult, [ks, kvi], ["pt1"])
                    if pending is not None:
                        pending()
                        pending = None
                    ptt(pt4[2], Es, vre, ALU.mult, [ks, kvr], ["pt2"])
                    ptt(pt4[3], Ec, vim, ALU.mult, [ke, kvi], ["pt3"])
                    ptt(xre[:, tsl], pt4[0], pt4[1], ALU.subtract, ["pt0", "pt1"], [("xre", n)])
                    ptt(pt4[2], pt4[2], pt4[3], ALU.add, ["pt2", "pt3"], ["pt2"])

                    def ymm(j=j, jj=jj, cc=cc, n=n, tsl=tsl, yb=yb):
                        self.mm(self.psb(yb), Cre[:, j, :], xre[:, tsl], jj == 0, False, r=[skC, ("xre", n)], w=[("ps", yb)])
                        self.mm(self.psb(yb), Cim[:, j, :], xim[:, tsl], False, False, r=[skC, ("xim", n)], w=[("ps", yb)])
                        if jj == 3:
                            self.mm(self.psb(yb), Dg[:, cc, :], uz[:, cc, tsl], False, True, r=[skD, ("uz", cc, n)], w=[("ps", yb)])

                    def fin(n=n, tsl=tsl):
                        xo = xim[:, tsl]
                        P.op("pool", lambda e: e.tensor_scalar(out=xo, in0=pt4[2], scalar1=-1.0, scalar2=0.0, op0=ALU.mult, op1=ALU.add),
                             r=["pt2"], w=[("xim", n)])
                    pending = fin
                    ydefer.append(ymm)
                    while len(ydefer) > 3:
                        ydefer.pop(0)()
                pending()
                pending = None
            while ydefer:
                ydefer.pop(0)()
            for n in range(NTB):
                tsl = slice(n * TB, (n + 1) * TB)
                yb = ybanks[n]
                self.act(gt, self.psb(yb), AF.Square, r=[("ps", yb)], w=["gt"])
                self.ts(gt, gt, 0.044715, 1.0, ALU.mult, ALU.add, r=["gt"], w=["gt"])
                self.tt(gt, gt, self.psb(yb), ALU.mult, r=["gt", ("ps", yb)], w=["gt"])
                self.act(t3, gt, AF.Sigmoid, r=["gt"], w=["t3"], scale=1.5957691216057308)
                self.tt(uz[:, cc, tsl], t3, self.psb(yb), ALU.mult, r=["t3", ("ps", yb)], w=[("uz", cc, n)])
        self.tap("zT", uz[:, :, :], [("uz", cc, n) for cc in range(4) for n in range(NTB)], [4, S])
        self.bankset = list(range(8))
        for c in range(KC):
            segl, skl = self.wload(l, "glu%d" % c, 1024)
            segl = segl.rearrange("p (a k m) -> p a k m", a=2, k=4)
            segg, skg = self.wload(l, "g2_%d" % c, 1024)
            segg = segg.rearrange("p (k m) -> p k m", k=KC)
            for n in range(NTB):
                tsl = slice(n * TB, (n + 1) * TB)
                ba = self.bank()
                for cc in range(4):
                    self.mm(self.psb(ba), segl[:, 0, cc, :], uz[:, cc, tsl], cc == 0, cc == 3, r=[skl, ("uz", cc, n)], w=[("ps", ba)])
                bb = self.bank()
                for cc in range(4):
                    self.mm(self.psb(bb), segl[:, 1, cc, :], uz[:, cc, tsl], cc == 0, cc == 3, r=[skl, ("uz", cc, n)], w=[("ps", bb)])
                bg = self.bank()
                for kc in range(KC):
                    self.mm(self.psb(bg), segg[:, kc, :], hT[:, kc, tsl], kc == 0, kc == KC - 1, r=[skg, ("hT", kc, n)], w=[("ps", bg)])
                self.act(s1, self.psb(bb), AF.Sigmoid, r=[("ps", bb)], w=["s1"])
                self.act(s2, self.psb(bg), AF.Sigmoid, r=[("ps", bg)], w=["s2"])
                self.tt(t3, s1, self.psb(ba), ALU.mult, r=["s1", ("ps", ba)], w=["t3"])
                self.tt(t3, t3, s2, ALU.mult, r=["t3", "s2"], w=["t3"])
                self.tt(merged[:, c, tsl], merged[:, c, tsl], t3, ALU.add, r=[("mg", c, n), "t3"], w=[("mg", c, n)])

    def phase_attn(self, l, hT, merged, kaugT, kiT, vaug, wi_s, ident, sel4, biasT, cmask, ones64, b31d):
        P = self.P
        K = 1024
        ps = self.ps
        qT = self.arena[0:65, 0: 16 * TB].rearrange("p (h q) -> p h q", h=16)
        qiT = self.arena[0:64, 8 * K: 8 * K + 8 * TB].rearrange("p (h q) -> p h q", h=8)
        OTn = self.arena[0:64, 12 * K: 12 * K + 16 * TB].rearrange("p (h q) -> p h q", h=16)
        score2 = [self.carve(40 * K, 128, [1, S], F32)[:, 0, :], self.pf_t[:, PF_LRX:PF_LRX + S]]
        rl = [self.carve(48 * K, 128, [1, TB], F32)[:, 0, :], self.carve(50 * K, 128, [1, TB], F32)[:, 0, :]]
        nm3 = [self.carve((52 + 4 * i) * K, 128, [1, S], BF16)[:, 0, :] for i in range(3)]
        self.idx_i = 0
        PT = [self.carve(64 * K, 128, [1, 1024], BF16)[:, 0, :], self.carve(66 * K, 128, [1, 1024], BF16)[:, 0, :]]
        ot = self.carve(68 * K, 64, [1, 1024], F32)[:, 0, :]
        aotmp = self.carve(60 * K, 128, [1, TB], F32)[:, 0, :]
        bis = self.carve(72 * K, 128, [1, 16], F32)[:, 0, :]
        sgt = self.carve(73 * K, 128, [1, TB], BF16)[:, 0, :]
        lnr = self.arena[64:65, 12 * K: 12 * K + 2048].bitcast(F32)
        rrow = self.arena[64:65, 14 * K: 14 * K + 1024]
        P.op("pool", lambda e: e.dma_start(out=self.arena[64:65, 0:16 * TB], in_=b31d, max_dma_last_dim=8192), w=["qT64"], slot="b31")

        def emit_ao(nn):
            tsl = slice(nn * TB, (nn + 1) * TB)
            self.bankset = [0, 1, 2, 3, 4, 5]
            for c in range(KC):
                sego, sko = self.wload(l, "ao%d" % c, 2048)
                sego = sego.rearrange("p (h m) -> p h m", h=16)
                segg, skg = self.wload(l, "g1_%d" % c, 1024)
                segg = segg.rearrange("p (k m) -> p k m", k=KC)
                by = self.bank()
                for h in range(16):
                    self.mm(self.psb(by), sego[0:64, h, :], OTn[:, h, :], h == 0, h == 15, r=[sko] + [("OTn", i) for i in range(4)], w=[("ps", by)])
                bg = self.bank()
                for kc in range(KC):
                    self.mm(self.psb(bg), segg[:, kc, :], hT[:, kc, tsl], kc == 0, kc == KC - 1, r=[skg, ("hT", kc, nn)], w=[("ps", bg)])
                self.act(sgt, self.psb(bg), AF.Sigmoid, r=[("ps", bg)], w=["sgt"])
                t3 = aotmp
                self.tt(t3, sgt, self.psb(by), ALU.mult, r=["sgt", ("ps", by)], w=["aotmp"])
                self.tt(merged[:, c, tsl], merged[:, c, tsl], t3, ALU.add, r=[("mg", c, nn), "aotmp"], w=[("mg", c, nn)])
                yield

        for n in range(NTB):
            tsl = slice(n * TB, (n + 1) * TB)
            self.bankset = list(range(8))
            for hp in range(8):
                seg, sk = self.wload(l, "q%d" % hp, 1024)
                seg = seg.rearrange("p (k m) -> p k m", k=KC)
                for half in range(2):
                    h = 2 * hp + half
                    b = self.bank()
                    for kc in range(KC):
                        self.mm(self.psb(b, 64), seg[:, kc, half * 64:(half + 1) * 64], hT[:, kc, tsl], kc == 0, kc == KC - 1,
                                r=[sk, ("hT", kc, n)], w=[("ps", b)])
                    self.act(qT[0:64, h, :], self.psb(b, 64), AF.Copy, r=[("ps", b)], w=[("qT", h)], scale=0.125)
            for hp in range(4):
                seg, sk = self.wload(l, "qi%d" % hp, 1024)
                seg = seg.rearrange("p (k m) -> p k m", k=KC)
                for half in range(2):
                    h = 2 * hp + half
                    b = self.bank()
                    for kc in range(KC):
                        self.mm(self.psb(b, 64), seg[:, kc, half * 64:(half + 1) * 64], hT[:, kc, tsl], kc == 0, kc == KC - 1,
                                r=[sk, ("hT", kc, n)], w=[("ps", b)])
                    self.act(qiT[0:64, h, :], self.psb(b, 64), AF.Copy, r=[("ps", b)], w=[("qiT", h)])
            if n == 0:
                self.tap("qT", qT[:, :, :], [("qT", h) for h in range(16)] + ["qT64"], [16, TB], parts=65)

            def genA(qq):
                qb = 4 * n + qq
                qsl = slice(qq * 128, (qq + 1) * 128)
                L = (qb + 1) * 128
                sc = score2[qb % 2]
                ngr = (L + 511) // 512
                for kg in range(ngr):
                    k0 = kg * 512
                    nk = min(512, L - k0)
                    sk_ = ("sc", qb % 2, kg)
                    for h in range(8):
                        b = 6 + (self.idx_i % 2)
                        r_ = rl[self.idx_i % 2]
                        rk = ("rl", self.idx_i % 2)
                        self.idx_i += 1
                        self.mm(self.psb(b, 128, nk), qiT[0:64, h, qsl], kiT[0:64, k0:k0 + nk], True, True,
                                r=[("qiT", h)] + [("kiT", i) for i in range(NTB)], w=[("ps", b)])
                        self.act(r_[:, 0:nk], self.psb(b, 128, nk), AF.Relu, r=[("ps", b)], w=[rk])
                        wcol = wi_s[:, qb, h:h + 1]
                        wk = [("wi", qb)]
                        if h == 0:
                            ndiag = nk - 128 if (k0 + nk == L) else nk
                            if ndiag > 0:
                                self.ts(sc[:, k0:k0 + ndiag], r_[:, 0:ndiag], wcol, None, ALU.mult, ALU.bypass, r=[rk] + wk, w=[sk_])
                            if k0 + nk == L:
                                self.stt(sc[:, L - 128:L], r_[:, nk - 128:nk], wcol, cmask, ALU.mult, ALU.add, r=[rk, "cf"] + wk, w=[sk_])
                        else:
                            self.stt(sc[:, k0:k0 + nk], r_[:, 0:nk], wcol, sc[:, k0:k0 + nk], ALU.mult, ALU.add,
                                     r=[rk, sk_] + wk, w=[sk_])
                        yield

            def genB(qq):
                qb = 4 * n + qq
                L = (qb + 1) * 128
                sc = score2[qb % 2]
                nmb = nm3[qb % 2]
                nmk = ("nm", qb % 2)
                ngr = (L + 511) // 512
                sck = [("sc", qb % 2, kg) for kg in range(ngr)]
                if qb >= 2:
                    o = 8 * (qb % 2)
                    cA, cB, cnt, tmpb, thr = bis[:, o:o + 1], bis[:, o + 1:o + 2], bis[:, o + 2:o + 3], bis[:, o + 3:o + 4], bis[:, o + 4:o + 5]
                    kp = "b%d" % (qb % 2)
                    P.op("dve", lambda e, cA=cA: e.memset(cA, 0.0), w=[kp + "c0"])
                    cur, nxt = cA, cB
                    curk, nxtk = kp + "c0", kp + "c1"
                    step = 4.0
                    for it in range(NBIS):
                        self.ts(nmb[:, 0:L], sc[:, 0:L], cur, None, ALU.is_ge, ALU.add, r=sck + [curk], w=[nmk, kp + "cnt"], accum_out=cnt)
                        self.ts(tmpb, cnt, 256.0, 2.0 * step, ALU.is_ge, ALU.mult, r=[kp + "cnt"], w=[kp + "tmpb"])
                        self.ts(nxt, tmpb, -step, cur, ALU.add, ALU.add, r=[kp + "tmpb", curk], w=[nxtk])
                        cur, nxt = nxt, cur
                        curk, nxtk = nxtk, curk
                        step *= 0.5
                        yield
                    self.ts(thr, cur, -2.0 * step - 1e-5, None, ALU.add, ALU.bypass, r=[curk], w=[kp + "thr"])
                    self.ts(nmb[:, 0:L], sc[:, 0:L], thr, NEG, ALU.is_lt, ALU.mult, r=sck + [kp + "thr"], w=[nmk])
                else:
                    self.ts(nmb[:, 0:L], sc[:, 0:L], -16.0, NEG, ALU.is_lt, ALU.mult, r=sck, w=[nmk])
                if qb == 5:
                    self.tap("score5", sc[:, 0:L], sck, [L])
                    self.tap("nm5", nmb[:, 0:L], [nmk], [L])
                yield

            def n_units_A(qq):
                qb = 4 * n + qq
                return 8 * (((qb + 1) * 128 + 511) // 512)

            def step_gen(g):
                if g is None:
                    return None
                try:
                    next(g)
                    return g
                except StopIteration:
                    return None

            def drain(g):
                while g is not None:
                    g = step_gen(g)

            def emit_attn(qq, gB, nB, gA, nA):
                qb = 4 * n + qq
                qsl = slice(qq * 128, (qq + 1) * 128)
                nmb = nm3[qb % 2]
                nmk = ("nm", qb % 2)
                niter = 2 * (qb + 1)
                perB = -(-nB // niter)
                perA = -(-nA // niter)
                for hh in range(2):
                    def emit_S(kb):
                        sb = kb % 2
                        near = (qb - kb) < 2
                        kk = 64 if near else 65
                        ksl = slice(kb * 128, (kb + 1) * 128)
                        for bk in range(2):
                            bnk = 2 * sb + bk
                            hs = slice(hh * 8 + bk * 4, hh * 8 + bk * 4 + 4)
                            outp = self.psb(bnk).rearrange("p (h q) -> p h q", h=4)
                            qk = [("qT", h) for h in range(hh * 8 + bk * 4, hh * 8 + bk * 4 + 4)] + ["qT64"]
                            self.mm(outp, kaugT[0:kk, ksl], qT[0:kk, hs, qsl], True, False,
                                    r=qk + [("kT", kb // 4), "kaug1"], w=[("ps", bnk)])
                            self.mm(outp, nmb[:, ksl], sel4, False, not near, r=[nmk, "cb"], w=[("ps", bnk)])
                            if near:
                                self.mm(outp, ident, biasT[:, qb - kb, hs, :], False, True, r=["cb"], w=[("ps", bnk)])
                        self.act(PT[sb][:, :], ps[:, 2 * sb * 512: 2 * sb * 512 + 1024], AF.Exp,
                                 r=[("ps", 2 * sb), ("ps", 2 * sb + 1)], w=[("PT", sb)])

                    def emit_PV(kb):
                        sb = kb % 2
                        for bk in range(2):
                            self.mm(self.psb(4 + bk, 65), vaug[:, kb, 0:65], PT[sb][:, bk * 512:(bk + 1) * 512], kb == 0, kb == qb,
                                    r=[("PT", sb), ("vaug", kb), "vaug1"], w=[("ps", 4 + bk)])

                    for kb in range(qb + 1):
                        emit_S(kb)
                        if kb > 0:
                            emit_PV(kb - 1)
                        for _ in range(perB):
                            gB = step_gen(gB)
                        for _ in range(perA):
                            gA = step_gen(gA)
                    emit_PV(qb)
                    if hh == 1:
                        drain(gB)
                        drain(gA)
                        gB = gA = None
                    self.act(lnr, ps[64:65, 2048:3072], AF.Ln, r=[("ps", 4), ("ps", 5)], w=["lnr"])
                    self.act(rrow, lnr, AF.Exp, r=["lnr"], w=["rrow"], scale=-1.0)
                    for bk in range(2):
                        self.mm(self.psb(6 + bk, 64), self.ones_bf[64:65, 0:64], rrow[:, bk * 512:(bk + 1) * 512], True, True,
                                r=["ones_bf", "rrow"], w=[("ps", 6 + bk)])
                    self.act(ot[:, :], ps[0:64, 2048:3072], AF.Copy, r=[("ps", 4), ("ps", 5)], w=["ot"])
                    for bk in range(2):
                        hs = slice(hh * 8 + bk * 4, hh * 8 + bk * 4 + 4)
                        self.tt(OTn[:, hs, qsl], ot[:, bk * 512:(bk + 1) * 512].rearrange("p (h q) -> p h q", h=4),
                                self.psb(6 + bk, 64).rearrange("p (h q) -> p h q", h=4), ALU.mult,
                                r=["ot", ("ps", 6 + bk)], w=[("OTn", hh * 2 + bk)])

            drain(genA(0))
            gB0, gA1 = genB(0), genA(1)
            gAO = emit_ao(n - 1) if n > 0 else None
            rnd = 0
            while gB0 is not None or gA1 is not None or gAO is not None:
                gB0 = step_gen(gB0)
                gA1 = step_gen(step_gen(gA1))
                if rnd % 2 == 1:
                    gAO = step_gen(gAO)
                rnd += 1
            for qq in range(4):
                gB = genB(qq + 1) if qq + 1 < 4 else None
                gA = genA(qq + 2) if qq + 2 < 4 else None
                emit_attn(qq, gB, NBIS + 1, gA, n_units_A(qq + 2) if qq + 2 < 4 else 0)
            if n == 0:
                self.tap("OTn", OTn[:, :, :], [("OTn", i) for i in range(4)], [16, TB], parts=64)
        for _ in emit_ao(NTB - 1):
            pass


def km(w):
    k, m = w.shape
    return np.ascontiguousarray(w.reshape(k // 128, 128, m).transpose(1, 0, 2)).reshape(128, (k // 128) * m)


def host_segments(inp, l):
    w = inp["w_in"][l]
    segs = {}
    segs["kk"] = km(np.concatenate([w[:, C_K:C_K + 64], w[:, C_KI:C_KI + 64]], axis=1))
    segs["vw"] = km(np.concatenate([w[:, C_V:C_V + 64], w[:, C_WI:C_WI + 8]], axis=1))
    for cc in range(4):
        segs["pu%d" % cc] = km(w[:, C_POOL + cc * 128: C_POOL + (cc + 1) * 128])
        segs["su%d" % cc] = km(w[:, C_S5 + cc * 128: C_S5 + (cc + 1) * 128])
    segs["mix"] = np.ascontiguousarray(inp["pool_mix_w"][l].transpose(1, 0, 2)).reshape(128, 512)
    for c in range(8):
        cs = slice(c * 128, (c + 1) * 128)
        segs["po%d" % c] = km(inp["pool_out_w"][l][:, cs])
        for b in range(3):
            segs["g%d_%d" % (b, c)] = km(w[:, C_G + b * 1024 + c * 128: C_G + b * 1024 + (c + 1) * 128])
        glu = inp["s5_glu_w"][l]
        segs["glu%d" % c] = np.concatenate([km(glu[:, cs]), km(glu[:, 1024 + c * 128: 1024 + (c + 1) * 128])], axis=1)
        segs["q%d" % c] = km(w[:, C_Q + c * 128: C_Q + (c + 1) * 128])
        ao = inp["attn_out_w"][l][:, cs].reshape(16, 64, 128).transpose(1, 0, 2).reshape(64, 2048)
        segs["ao%d" % c] = np.concatenate([ao, np.zeros((64, 2048), np.float32)], axis=0)
        segs["wo%d" % c] = km(inp["w_out"][l][:, cs])
        segs["fo%d" % c] = km(inp["ffn_w_out"][l][:, cs])
    for hp in range(4):
        segs["qi%d" % hp] = km(w[:, C_QI + hp * 128: C_QI + (hp + 1) * 128])
    fw = inp["ffn_w_in"][l]
    for j in range(NJ):
        segs["f%d" % j] = km(np.concatenate([fw[:, j * 128:(j + 1) * 128], fw[:, FH + j * 128: FH + (j + 1) * 128]], axis=1))
    cre = inp["s5_c_re"][l]
    cim = inp["s5_c_im"][l]
    Cre = np.zeros((128, 16, 128), np.float32)
    Cim = np.zeros((128, 16, 128), np.float32)
    for g in range(32):
        j, g2 = g // 2, g % 2
        jj = j % 4
        m0 = 32 * jj + 16 * g2
        Cre[g2 * 64:(g2 + 1) * 64, j, m0:m0 + 16] = cre[g].T
        Cim[g2 * 64:(g2 + 1) * 64, j, m0:m0 + 16] = cim[g].T
    segs["sC"] = np.concatenate([Cre.reshape(128, 2048), Cim.reshape(128, 2048)], axis=1)
    Dg = np.zeros((128, 4, 128), np.float32)
    d = inp["s5_d"][l]
    for cc in range(4):
        Dg[np.arange(128), cc, np.arange(128)] = d[cc * 128:(cc + 1) * 128]
    segs["sD"] = Dg.reshape(128, 512)
    return segs


def host_pf(inp, l):
    pf = np.zeros((128, NPF), np.float32)
    for off, name in ((PF_GMP, "norm_mix_pre"), (PF_GMO, "norm_mix_post"), (PF_GFP, "norm_ffn_pre"), (PF_GFO, "norm_ffn_post")):
        pf[:, off:off + 8] = inp[name][l].reshape(8, 128).T
    pf[:, PF_PSC:PF_PSC + 4] = inp["pool_scale"][l].reshape(4, 128).T
    lr, li, ld = inp["s5_lambda_re"][l], inp["s5_lambda_im"][l], inp["s5_log_dt"][l]
    for j in range(16):
        for g2 in range(2):
            g = 2 * j + g2
            pf[g2 * 64:(g2 + 1) * 64, PF_LRS + j] = lr[g]
            pf[g2 * 64:(g2 + 1) * 64, PF_LIS + j] = li[g]
            pf[g2 * 64:(g2 + 1) * 64, PF_LDS + j] = ld[g]
    br, bi = inp["s5_b_re"][l], inp["s5_b_im"][l]
    X = np.zeros((5, 128, 4, 128), np.float32)
    for cc in range(4):
        for p in range(128):
            g = cc * 8 + p // 16
            i = p % 16
            X[0, p, cc, :] = np.tile(lr[g], 2)
            X[1, p, cc, :] = np.tile(li[g], 2)
            X[2, p, cc, :] = ld[g]
            g2 = g % 2
            X[3, p, cc, g2 * 64:(g2 + 1) * 64] = br[g, :, i]
            X[4, p, cc, g2 * 64:(g2 + 1) * 64] = bi[g, :, i]
    for k, off in enumerate((PF_LRX, PF_LIX, PF_LDX, PF_BRX, PF_BIX)):
        pf[:, off:off + 512] = X[k].reshape(128, 512)
    return pf


def host_consts(inp):
    cf = np.zeros((128, NCF), np.float32)
    q = np.arange(128)[:, None]
    s = np.arange(128)[None, :]
    cf[:, CF_CMASK:CF_CMASK + 128] = np.where(s > q, np.float32(-1e4), np.float32(0.0))
    cf[:, CF_INVC:CF_INVC + 16] = (1.0 / np.arange(1, 17, dtype=np.float32))[None, :]
    cf[:, CF_ONES:CF_ONES + 64] = 1.0
    par = ((np.arange(128) // 32) % 2).astype(np.float32)
    cf[:, CF_PAR] = 1.0 - par
    cf[:, CF_PAR + 1] = par
    cb = np.zeros((128, NCB), np.float32)
    cb[:, CB_ID:CB_ID + 128] = np.eye(128, dtype=np.float32)
    cb[:, CB_SEL:CB_SEL + 512] = np.tile(np.eye(128, dtype=np.float32), (1, 4))
    rb = inp["rel_bias"]
    sl = np.arange(128)[:, None]
    ql = np.arange(128)[None, :]
    bt = np.zeros((128, 2, 16, 128), np.float32)
    for kind in range(2):
        dist = ql - sl + 128 * kind
        idx = rel_bucket_np(np.maximum(dist, 0))
        bt[:, kind, :, :] = rb[idx].transpose(0, 2, 1)
    cb[:, CB_BIAS:CB_BIAS + 4096] = bt.reshape(128, 4096)
    b31 = np.ascontiguousarray(np.repeat(rb[31][:, None], TB, axis=1)).reshape(1, 16 * TB).astype(np.float32)
    return cf, cb, b31


_CACHE = {}


def get_program(nlayers=NL, stop=None, taps=()):
    key = (nlayers, stop, tuple(taps))
    if key not in _CACHE:
        b0 = Builder(nlayers, stop, taps)
        b0.wtotal = 1 << 20
        b0.build()
        b = Builder(nlayers, stop, taps)
        b.wtotal = max(b0.seg_total, 16)
        nc = b.build()
        _CACHE[key] = (nc, b)
    return _CACHE[key]


def run(inputs, nlayers=NL, stop=None, taps=()):
    nc, b = get_program(nlayers, stop, taps)
    inp = {k: np.asarray(v, dtype=np.float32) for k, v in inputs.items()}
    ws = np.zeros((NL, 128, b.wtotal), np.float32)
    pfs = np.zeros((NL, 128, NPF), np.float32)
    for l in range(NL):
        segs = host_segments(inp, l)
        for name, (off, n) in b.seg_off.items():
            a = segs[name]
            assert a.shape == (128, n), (name, a.shape, n)
            ws[l, :, off:off + n] = a
        pfs[l] = host_pf(inp, l)
    cf, cb, b31 = host_consts(inp)
    x = inp["x"]
    in_maps = []
    for c in range(8):
        xt = np.ascontiguousarray(x[c].T.reshape(KC, 128, S).transpose(1, 0, 2))
        in_maps.append({"x": xt, "wstream": ws, "pf": pfs, "cf": cf, "cb": cb, "b31": b31})
    res = run_bass_kernel_spmd(nc, in_maps, core_ids=list(range(8)))
    return res, b


def kernel(**inputs):
    res, b = run(inputs)
    outs = []
    for c in range(8):
        o = res.results[c]["out"]
        outs.append(np.ascontiguousarray(o.transpose(1, 0, 2).reshape(D, S).T))
    return np.stack(outs, axis=0).astype(np.float32)
```

```python
import contextlib
import math
import numpy as np
import concourse.bass as bass
import concourse.mybir as mybir
from concourse.bass_utils import run_bass_kernel_spmd

F32 = mybir.dt.float32
BF16 = mybir.dt.bfloat16
I32 = mybir.dt.int32
ALU = mybir.AluOpType
AF = mybir.ActivationFunctionType

ENGS = ("pe", "act", "dve", "pool", "sp")


class Op:
    __slots__ = ("eng", "fn", "waits", "signal", "slot", "seq", "dcount")

    def __init__(self, eng, fn):
        self.eng = eng
        self.fn = fn
        self.waits = {}
        self.signal = False
        self.slot = None
        self.seq = 0
        self.dcount = 0


class Prog:
    def __init__(self):
        self.ops = {e: [] for e in ENGS}
        self.last_w = {}
        self.readers = {}
        self.slot_count = {}
        self.pending = {e: {} for e in ENGS}

    def _add_wait(self, op, tok, raw):
        kind, who, val = tok
        if kind == "E":
            if who == op.eng and not raw:
                return
            if who == "pe" and op.eng == "pe":
                return
            self.ops[who][val].signal = True
        k = (kind, who)
        if op.waits.get(k, -1) < val:
            op.waits[k] = val

    def op(self, eng, fn, r=(), w=(), slot=None):
        o = Op(eng, fn)
        o.seq = len(self.ops[eng])
        if slot is not None:
            o.slot = slot
            self.slot_count[slot] = self.slot_count.get(slot, 0) + 1
            o.dcount = self.slot_count[slot]
            tok = ("D", slot, o.dcount)
        else:
            tok = ("E", eng, o.seq)
        for k, v in self.pending[eng].items():
            if o.waits.get(k, -1) < v:
                o.waits[k] = v
        self.pending[eng] = {}
        for k in r:
            lw = self.last_w.get(k)
            if lw is not None:
                self._add_wait(o, lw, True)
        for k in w:
            lw = self.last_w.get(k)
            if lw is not None:
                self._add_wait(o, lw, False)
            for t in self.readers.get(k, ()):
                self._add_wait(o, t, False)
        self.ops[eng].append(o)
        for k in r:
            self.readers.setdefault(k, []).append(tok)
        for k in w:
            self.last_w[k] = tok
            self.readers[k] = []
        return o

    def barrier(self):
        toks = {}
        for e in ENGS:
            for o in reversed(self.ops[e]):
                if o.slot is None:
                    o.signal = True
                    toks[("E", e)] = o.seq
                    break
        for s, c in self.slot_count.items():
            toks[("D", s)] = c
        for e in ENGS:
            for k, v in toks.items():
                if k == ("E", e):
                    continue
                if self.pending[e].get(k, -1) < v:
                    self.pending[e][k] = v
        self.last_w = {}
        self.readers = {}

    def emit(self, nc, final_slots=()):
        with contextlib.ExitStack() as st:
            esem = {e: st.enter_context(nc.semaphore("s_" + e)) for e in ENGS}
            dsem = {s: st.enter_context(nc.semaphore("d_%d" % i)) for i, s in enumerate(self.slot_count)}
            sigcount = {}
            for e in ENGS:
                c = 0
                arr = []
                for o in self.ops[e]:
                    if o.slot is None and o.signal:
                        c += 1
                    arr.append(c)
                sigcount[e] = arr
            block = st.enter_context(nc.Block())
            prog = self

            def make(e):
                def body(eng):
                    waited = {}
                    for o in prog.ops[e]:
                        for (kind, who), val in o.waits.items():
                            if kind == "E":
                                sem = esem[who]
                                v = sigcount[who][val]
                            else:
                                sem = dsem[who]
                                v = 16 * val
                            if waited.get((kind, who), -1) >= v:
                                continue
                            waited[(kind, who)] = v
                            eng.wait_ge(sem, v)
                        ins = o.fn(eng)
                        if o.slot is not None:
                            ins.then_inc(dsem[o.slot], 16)
                        elif o.signal:
                            ins.then_inc(esem[e], 1)
                    if e == "sp":
                        for s in final_slots:
                            eng.wait_ge(dsem[s], 16 * prog.slot_count[s])
                return body

            block.tensor(make("pe"))
            block.scalar(make("act"))
            block.vector(make("dve"))
            block.gpsimd(make("pool"))
            block.sync(make("sp"))


S = 2048
D = 1024
TB = 512
NTB = 4
KC = 8
NL = 2
FH = 2816
NJ = 22
EPS = 1e-6
IDX_SCALE = (8 ** -0.5) * (64 ** -0.5)
NEG = -30000.0
NBIS = 15

C_POOL, C_Q, C_K, C_V, C_QI, C_KI, C_WI, C_S5, C_G = 0, 512, 1536, 1600, 1664, 2176, 2240, 2248, 2760

PF_GMP, PF_GMO, PF_GFP, PF_GFO, PF_PSC = 0, 8, 16, 24, 32
PF_LRS, PF_LIS, PF_LDS = 36, 52, 68
PF_LRX, PF_LIX, PF_LDX, PF_BRX, PF_BIX = 84, 596, 1108, 1620, 2132
NPF = 2644
CF_CMASK, CF_INVC, CF_ONES, CF_PAR = 0, 128, 144, 208
NCF = 212
CB_ID, CB_SEL, CB_BIAS = 0, 128, 640
NCB = 640 + 4096

ARENA = 86 * 1024
RSLOT = 4096
NRING = 4
KV0 = 75 * 1024


def rel_bucket_np(dist):
    dist = np.asarray(dist, np.int32)
    d_f = np.maximum(dist, 1).astype(np.float32)
    large = 16 + (np.log(d_f / np.float32(16)) / np.float32(math.log(128 / 16)) * np.float32(16)).astype(np.int32)
    large = np.minimum(large, 31)
    return np.where(dist < 16, dist, large)


class Builder:
    def __init__(self, nlayers=NL, stop=None, taps=()):
        self.nlayers = nlayers
        self.stop = stop
        self.taps = taps
        self.seg_off = {}
        self.seg_total = 0
        self.ring_i = 0
        self.bank_i = 0
        self.bankset = list(range(8))
        self.tapouts = {}

    def carve(self, off, parts, shape, dt):
        esz = 2 if dt == BF16 else 4
        n = int(np.prod(shape))
        assert off % 4 == 0 and off + n * esz <= ARENA, (off, n, esz)
        ap = self.arena[0:parts, off // 2: off // 2 + n * esz // 2]
        if dt != BF16:
            ap = ap.bitcast(dt)
        if len(shape) == 2:
            return ap.rearrange("p (a b) -> p a b", a=shape[0])
        if len(shape) == 3:
            return ap.rearrange("p (a b c) -> p a b c", a=shape[0], b=shape[1])
        return ap

    def bank(self):
        b = self.bankset[self.bank_i % len(self.bankset)]
        self.bank_i += 1
        return b

    def psb(self, b, parts=128, n=512):
        return self.ps[0:parts, b * 512: b * 512 + n]

    def wload(self, l, name, n):
        assert n <= RSLOT
        if name not in self.seg_off:
            self.seg_off[name] = (self.seg_total, n)
            self.seg_total += n
        off, n0 = self.seg_off[name]
        assert n0 == n
        slot = self.ring_i % NRING
        self.ring_i += 1
        dst = self.ring[:, slot * RSLOT: slot * RSLOT + n]
        src = self.wstream[l, :, off:off + n]
        key = ("ring", slot)
        self.P.op("pool", lambda e: e.dma_start(out=dst, in_=src, max_dma_last_dim=8192), w=[key], slot="ring%d" % slot)
        return dst, key

    def mm(self, out, lhsT, rhs, start, stop, r, w):
        self.P.op("pe", lambda e: e.matmul(out, lhsT, rhs, start=start, stop=stop), r=r, w=w)

    def act(self, out, in_, func, r, w, scale=1.0, bias=0.0):
        self.P.op("act", lambda e: e.activation(out=out, in_=in_, func=func, bias=bias, scale=scale), r=r, w=w)

    def tt(self, out, in0, in1, op, r, w):
        self.P.op("dve", lambda e: e.tensor_tensor(out=out, in0=in0, in1=in1, op=op), r=r, w=w)

    def ts(self, out, in0, s1, s2, op0, op1, r, w, accum_out=None):
        if accum_out is None:
            self.P.op("dve", lambda e: e.tensor_scalar(out=out, in0=in0, scalar1=s1, scalar2=s2, op0=op0, op1=op1), r=r, w=w)
        else:
            self.P.op("dve", lambda e: e.tensor_scalar(out=out, in0=in0, scalar1=s1, scalar2=s2, op0=op0, op1=op1, accum_out=accum_out), r=r, w=w)

    def stt(self, out, in0, scalar, in1, op0, op1, r, w):
        self.P.op("dve", lambda e: e.scalar_tensor_tensor(out=out, in0=in0, scalar=scalar, in1=in1, op0=op0, op1=op1), r=r, w=w)

    def dma(self, eng, out, in_, r, w, slot):
        self.P.op(eng, lambda e: e.dma_start(out=out, in_=in_), r=r, w=w, slot=slot)

    def tap(self, name, ap, keys, shape, parts=128):
        if name not in self.taps:
            return
        t = self.nc.dram_tensor("tap_" + name, [parts] + list(shape), ap.dtype, kind="ExternalOutput").ap()
        self.tapouts[name] = t
        self.P.op("sp", lambda e: e.dma_start(out=t, in_=ap), r=keys, slot="tap_" + name)
        self.final_slots.append("tap_" + name)

    def rms_rstd(self, src_f32, src_keys, sq, tag):
        P = self.P
        for c in range(KC):
            self.act(sq[:, c, :], src_f32[:, c, :], AF.Square, r=[src_keys[c]], w=[("sq", c)])
        b = self.bank()
        for c in range(KC):
            self.mm(self.psb(b), self.ones_bf[:, :], sq[:, c, :], c == 0, c == KC - 1, r=[("sq", c), "ones_bf"], w=[("ps", b)])
        self.act(self.rstd[:, :], self.psb(b), AF.Sqrt, r=[("ps", b), "epsc"], w=["rstd"], scale=1.0 / D, bias=self.epsc[:, 0:1])
        P.op("dve", lambda e: e.reciprocal(out=self.rstd[:, :], in_=self.rstd[:, :]), r=["rstd"], w=["rstd"])

    def build(self):
        nc = bass.Bass("TRN2", target_bir_lowering=False)
        self.nc = nc
        self.final_slots = []
        P = self.P = Prog()
        xin = nc.dram_tensor("x", [128, KC, S], F32, kind="ExternalInput").ap()
        out = nc.dram_tensor("out", [128, KC, S], F32, kind="ExternalOutput").ap()
        xres = nc.dram_tensor("xres", [128, KC, S], F32, kind="Internal").ap()
        self.wstream = nc.dram_tensor("wstream", [NL, 128, self.wtotal], F32, kind="ExternalInput").ap()
        pfd = nc.dram_tensor("pf", [NL, 128, NPF], F32, kind="ExternalInput").ap()
        cfd = nc.dram_tensor("cf", [128, NCF], F32, kind="ExternalInput").ap()
        cbd = nc.dram_tensor("cb", [128, NCB], F32, kind="ExternalInput").ap()
        b31d = nc.dram_tensor("b31", [1, 16 * TB], F32, kind="ExternalInput").ap()
        with contextlib.ExitStack() as st:
            E = st.enter_context
            self.arena = E(nc.sbuf_tensor("arena", [128, ARENA // 2], BF16))
            hT = E(nc.sbuf_tensor("hT", [128, KC, S], BF16))
            merged = E(nc.sbuf_tensor("merged", [128, KC, S], BF16))
            self.ring = E(nc.sbuf_tensor("ring", [128, NRING * RSLOT], BF16))
            pf = E(nc.sbuf_tensor("pfs", [128, NPF], F32))
            self.pf_t = pf
            cf = E(nc.sbuf_tensor("cfs", [128, NCF], F32))
            cb = E(nc.sbuf_tensor("cbs", [128, NCB], BF16))
            self.ones_bf = E(nc.sbuf_tensor("ones_bf", [128, 128], BF16))
            self.rstd = E(nc.sbuf_tensor("rstd", [128, TB], F32))
            self.epsc = E(nc.sbuf_tensor("epsc", [128, 8], F32))
            self.hpi = E(nc.sbuf_tensor("hpi", [128, 8], F32))
            self.ps = E(nc.psum_tensor("ps", [128, 4096], F32))
            ps = self.ps
            ident = cb[:, CB_ID:CB_ID + 128]
            sel4 = cb[:, CB_SEL:CB_SEL + 512].rearrange("p (h q) -> p h q", h=4)
            biasT = cb[:, CB_BIAS:CB_BIAS + 4096].rearrange("p (k h q) -> p k h q", k=2, h=16)
            cmask = cf[:, CF_CMASK:CF_CMASK + 128]
            invc = cf[:, CF_INVC:CF_INVC + 16]
            ones64 = cf[:, CF_ONES:CF_ONES + 64]
            self.cf = cf

            P.op("dve", lambda e: e.memset(self.ones_bf[:, :], 1.0), w=["ones_bf"])
            P.op("dve", lambda e: e.memset(self.epsc[:, :], EPS), w=["epsc"])
            P.op("dve", lambda e: e.memset(self.hpi[:, :], math.pi / 2), w=["hpi"])
            self.dma("sp", cf[:, :], cfd, r=[], w=["cf"], slot="cf")
            P.op("pool", lambda e: e.dma_start(out=cb[:, :], in_=cbd, max_dma_last_dim=8192), w=["cb"], slot="cb")

            for l in range(self.nlayers):
                xsrc = xin if l == 0 else xres
                xdst = out if l == self.nlayers - 1 else xres
                self.layer(l, xsrc, xdst, hT, merged, pf, pfd, ident, sel4, biasT, cmask, invc, ones64, b31d)
                if self.stop is not None:
                    break
            P.barrier()
            P.emit(nc, final_slots=self.final_slots)
        return nc

    def layer(self, l, xsrc, xdst, hT, merged, pf, pfd, ident, sel4, biasT, cmask, invc, ones64, b31d):
        P = self.P
        nc = self.nc
        ps = self.ps
        stop = self.stop
        P.barrier()
        self.bankset = list(range(8))
        self.dma("sp", pf[:, :], pfd[l], r=[], w=["pf"], slot="pf")
        gmp = pf[:, PF_GMP:PF_GMP + 8]
        gmo = pf[:, PF_GMO:PF_GMO + 8]
        gfp = pf[:, PF_GFP:PF_GFP + 8]
        gfo = pf[:, PF_GFO:PF_GFO + 8]
        psc = pf[:, PF_PSC:PF_PSC + 4]

        xblk = self.carve(0, 128, [KC, TB], F32)
        sq = self.carve(16 * 1024, 128, [KC, TB], BF16)
        for n in range(NTB):
            tsl = slice(n * TB, (n + 1) * TB)
            self.dma("sp", xblk[:, :, :], xsrc[:, :, tsl], r=[], w=[("xblk", c) for c in range(KC)], slot="xblk")
            self.rms_rstd(xblk, [("xblk", c) for c in range(KC)], sq, "p1")
            for c in range(KC):
                self.stt(hT[:, c, tsl], xblk[:, c, :], gmp[:, c:c + 1], self.rstd[:, :], ALU.mult, ALU.mult,
                         r=[("xblk", c), "pf", "rstd"], w=[("hT", c, n)])
        self.tap("hT", hT[:, :, :], [("hT", c, n) for c in range(KC) for n in range(NTB)], [KC, S])
        if stop == 1:
            return
        P.barrier()

        kaugT = self.arena[0:65, KV0 // 2: KV0 // 2 + S]
        kiT = self.arena[0:64, KV0 // 2 + S: KV0 // 2 + 2 * S]
        vaug = self.arena[:, KV0 // 2 + 2 * S: KV0 // 2 + 2 * S + 16 * 66].rearrange("p (t d) -> p t d", t=16)
        wi_off = KV0 + 2 * (2 * S + 16 * 66)
        wi_s = self.arena[:, wi_off // 2: wi_off // 2 + 256].bitcast(F32).rearrange("p (t h) -> p t h", t=16)
        assert wi_off + 512 <= ARENA
        uz = self.carve(0, 128, [4, S], BF16)
        upp = [self.carve(16 * 1024, 128, [1, S], F32)[:, 0, :], self.carve(24 * 1024, 128, [1, S], F32)[:, 0, :]]
        dT = self.carve(32 * 1024, 128, [4, S], BF16)
        yp = self.carve(48 * 1024, 128, [4, S], BF16)
        sgt = self.carve(64 * 1024, 128, [1, TB], BF16)[:, 0, :]

        def hkeys(n):
            return [("hT", c, n) for c in range(KC)]

        seg, sk = self.wload(l, "kk", 1024)
        seg = seg.rearrange("p (k m) -> p k m", k=KC)
        P.op("dve", lambda e: e.memset(kaugT[64:65, :], 1.0), w=["kaug1"])
        P.op("dve", lambda e: e.memset(vaug[:, :, 64:65], 1.0), w=["vaug1"])
        for n in range(NTB):
            tsl = slice(n * TB, (n + 1) * TB)
            for half, dst, key in ((0, kaugT, "kT"), (1, kiT, "kiT")):
                b = self.bank()
                for kc in range(KC):
                    self.mm(self.psb(b, 64), seg[:, kc, half * 64:(half + 1) * 64], hT[:, kc, tsl], kc == 0, kc == KC - 1,
                            r=[sk, ("hT", kc, n)], w=[("ps", b)])
                self.act(dst[0:64, tsl], self.psb(b, 64), AF.Copy, r=[("ps", b)], w=[(key, n)])
        seg, sk = self.wload(l, "vw", KC * 72)
        seg = seg.rearrange("p (k m) -> p k m", k=KC)
        for tt_ in range(16):
            n = tt_ // 4
            b = self.bank()
            for kc in range(KC):
                self.mm(ps[:, b * 512: b * 512 + 72], hT[:, kc, tt_ * 128:(tt_ + 1) * 128], seg[:, kc, :], kc == 0, kc == KC - 1,
                        r=[sk, ("hT", kc, n)], w=[("ps", b)])
            self.act(vaug[:, tt_, 0:64], ps[:, b * 512: b * 512 + 64], AF.Copy, r=[("ps", b)], w=[("vaug", tt_)])
            self.act(wi_s[:, tt_, :], ps[:, b * 512 + 64: b * 512 + 72], AF.Copy, r=[("ps", b)], w=[("wi", tt_)], scale=IDX_SCALE)
        for cc in range(4):
            seg, sk = self.wload(l, "su%d" % cc, 1024)
            seg = seg.rearrange("p (k m) -> p k m", k=KC)
            for n in range(NTB):
                tsl = slice(n * TB, (n + 1) * TB)
                b = self.bank()
                for kc in range(KC):
                    self.mm(self.psb(b), seg[:, kc, :], hT[:, kc, tsl], kc == 0, kc == KC - 1, r=[sk, ("hT", kc, n)], w=[("ps", b)])
                self.act(uz[:, cc, tsl], self.psb(b), AF.Copy, r=[("ps", b)], w=[("uz", cc, n)])
        for cc in range(4):
            seg, sk = self.wload(l, "pu%d" % cc, 1024)
            seg = seg.rearrange("p (k m) -> p k m", k=KC)
            u0 = upp[0]
            for n in range(NTB):
                tsl = slice(n * TB, (n + 1) * TB)
                b = self.bank()
                for kc in range(KC):
                    self.mm(self.psb(b), seg[:, kc, :], hT[:, kc, tsl], kc == 0, kc == KC - 1, r=[sk, ("hT", kc, n)], w=[("ps", b)])
                self.act(u0[:, tsl], self.psb(b), AF.Copy, r=[("ps", b)], w=["up0"])
            wlen = 2 ** (cc + 1)
            sA = upp[1]
            sB = self.carve(66 * 1024, 128, [1, S], F32)[:, 0, :]
            bufs = [(sA, "upA"), (sB, "upB")]
            k = 1
            bi = 0
            src, srck = u0, "up0"
            while k < wlen:
                dstb, dstk = bufs[bi % 2]
                self.tt(dstb[:, k:], src[:, k:], src[:, :S - k], ALU.add, r=[srck], w=[dstk])
                P.op("dve", lambda e, d=dstb, s_=src, k=k: e.tensor_copy(out=d[:, 0:k], in_=s_[:, 0:k]), r=[srck], w=[dstk])
                src, srck = dstb, dstk
                bi += 1
                k *= 2
            self.stt(dT[:, cc, :], src[:, :], 1.0 / wlen, u0[:, :], ALU.mult, ALU.subtract, r=[srck, "up0"], w=[("dT", cc)])
            tmpc = self.carve(64 * 1024 + 1024, 128, [1, 16], F32)[:, 0, :]
            self.tt(tmpc[:, 0:wlen - 1], src[:, 0:wlen - 1], invc[:, 0:wlen - 1], ALU.mult, r=[srck, "cf"], w=["tmpc"])
            self.tt(dT[:, cc, 0:wlen - 1], tmpc[:, 0:wlen - 1], u0[:, 0:wlen - 1], ALU.subtract, r=["tmpc", "up0", ("dT", cc)], w=[("dT", cc)])
        self.tap("dT", dT[:, :, :], [("dT", cc) for cc in range(4)], [4, S])
        self.tap("kT", kaugT[:, :], [("kT", n) for n in range(NTB)] + ["kaug1"], [S], parts=65)
        self.tap("vaug", vaug[:, :, :], [("vaug", t) for t in range(16)] + ["vaug1"], [16, 66])
        self.tap("wi", wi_s[:, :, :], [("wi", t) for t in range(16)], [16, 8])
        if stop == 2:
            return

        seg, sk = self.wload(l, "mix", 512)
        seg = seg.rearrange("p (g m) -> p g m", g=4)
        for cc in range(4):
            for n in range(NTB):
                tsl = slice(n * TB, (n + 1) * TB)
                b = self.bank()
                self.mm(self.psb(b), seg[:, cc, :], dT[:, cc, tsl], True, True, r=[sk, ("dT", cc)], w=[("ps", b)])
                self.act(yp[:, cc, tsl], self.psb(b), AF.Copy, r=[("ps", b), "pf"], w=[("yp", cc, n)], scale=psc[:, cc:cc + 1])
        for c in range(KC):
            segp, skp = self.wload(l, "po%d" % c, 512)
            segp = segp.rearrange("p (k m) -> p k m", k=4)
            segg, skg = self.wload(l, "g0_%d" % c, 1024)
            segg = segg.rearrange("p (k m) -> p k m", k=KC)
            for n in range(NTB):
                tsl = slice(n * TB, (n + 1) * TB)
                by = self.bank()
                for cc in range(4):
                    self.mm(self.psb(by), segp[:, cc, :], yp[:, cc, tsl], cc == 0, cc == 3, r=[skp, ("yp", cc, n)], w=[("ps", by)])
                bg = self.bank()
                for kc in range(KC):
                    self.mm(self.psb(bg), segg[:, kc, :], hT[:, kc, tsl], kc == 0, kc == KC - 1, r=[skg, ("hT", kc, n)], w=[("ps", bg)])
                self.act(sgt[:, :], self.psb(bg), AF.Sigmoid, r=[("ps", bg)], w=["sgt"])
                self.tt(merged[:, c, tsl], sgt[:, :], self.psb(by), ALU.mult, r=["sgt", ("ps", by)], w=[("mg", c, n)])
        self.tap("mg3", merged[:, :, S - 256:S], [("mg", c, n) for c in range(KC) for n in range(NTB)], [KC, 256])
        if stop == 3:
            return
        P.barrier()

        self.phase_s5(l, hT, merged, pf, uz)
        self.tap("mg4", merged[:, :, S - 256:S], [("mg", c, n) for c in range(KC) for n in range(NTB)], [KC, 256])
        if stop == 4:
            return
        P.barrier()

        self.phase_attn(l, hT, merged, kaugT, kiT, vaug, wi_s, ident, sel4, biasT, cmask, ones64, b31d)
        self.tap("mg5", merged[:, :, S - 256:S], [("mg", c, n) for c in range(KC) for n in range(NTB)], [KC, 256])
        if stop == 5:
            return
        P.barrier()

        self.bankset = list(range(8))
        xT = self.carve(0, 128, [KC, S], F32)
        sq = hT[:, 4:6, :].rearrange("p a b -> p (a b)").rearrange("p (c t) -> p c t", c=KC)
        xblk = hT[:, 0:4, :].rearrange("p a b -> p (a b)").bitcast(F32).rearrange("p (c t) -> p c t", c=KC)
        tmpf = self.carve(64 * 1024, 128, [1, TB], F32)[:, 0, :]
        for c in range(KC):
            seg, sk = self.wload(l, "wo%d" % c, 1024)
            seg = seg.rearrange("p (k m) -> p k m", k=KC)
            for n in range(NTB):
                tsl = slice(n * TB, (n + 1) * TB)
                b = self.bank()
                for kc in range(KC):
                    self.mm(self.psb(b), seg[:, kc, :], merged[:, kc, tsl], kc == 0, kc == KC - 1, r=[sk, ("mg", kc, n)], w=[("ps", b)])
                self.act(xT[:, c, tsl], self.psb(b), AF.Copy, r=[("ps", b)], w=[("xT", c, n)])
        for n in range(NTB):
            tsl = slice(n * TB, (n + 1) * TB)
            self.dma("sp", xblk[:, :, :], xsrc[:, :, tsl], r=[], w=[("xblk", c) for c in range(KC)], slot="xblk")
            self.rms_rstd(xT[:, :, tsl], [("xT", c, n) for c in range(KC)], sq, "p6")
            for c in range(KC):
                self.stt(tmpf[:, :], xT[:, c, tsl], gmo[:, c:c + 1], self.rstd[:, :], ALU.mult, ALU.mult,
                         r=[("xT", c, n), "pf", "rstd"], w=["tmpf"])
                self.tt(xT[:, c, tsl], tmpf[:, :], xblk[:, c, :], ALU.add, r=["tmpf", ("xblk", c)], w=[("xT", c, n)])
        self.tap("x6", xT[:, :, S - 256:S], [("xT", c, n) for c in range(KC) for n in range(NTB)], [KC, 256])
        if stop == 6:
            return
        P.barrier()

        fT = self.carve(64 * 1024, 128, [NJ, TB], BF16)
        sq = merged[:, 4:6, :].rearrange("p a b -> p (a b)").rearrange("p (c t) -> p c t", c=KC)
        mbuf = merged[:, 0:4, :].rearrange("p a b -> p (a b)").bitcast(F32).rearrange("p (c t) -> p c t", c=KC)
        sgf = merged[:, 6, 0:TB]
        tmpf = merged[:, 7, 0:2 * TB].bitcast(F32)
        def prenorm(n):
            tsl = slice(n * TB, (n + 1) * TB)
            self.rms_rstd(xT[:, :, tsl], [("xT", c, n) for c in range(KC)], sq, "p7a")
            for c in range(KC):
                self.stt(hT[:, c, tsl], xT[:, c, tsl], gfp[:, c:c + 1], self.rstd[:, :], ALU.mult, ALU.mult,
                         r=[("xT", c, n), "pf", "rstd"], w=[("hT", c, n)])

        prenorm(0)
        for n in range(NTB):
            tsl = slice(n * TB, (n + 1) * TB)
            for j in range(NJ):
                seg, sk = self.wload(l, "f%d" % j, 2048)
                seg = seg.rearrange("p (k m) -> p k m", k=KC)
                bg = self.bank()
                for kc in range(KC):
                    self.mm(self.psb(bg), seg[:, kc, 0:128], hT[:, kc, tsl], kc == 0, kc == KC - 1, r=[sk, ("hT", kc, n)], w=[("ps", bg)])
                bu = self.bank()
                for kc in range(KC):
                    self.mm(self.psb(bu), seg[:, kc, 128:256], hT[:, kc, tsl], kc == 0, kc == KC - 1, r=[sk, ("hT", kc, n)], w=[("ps", bu)])
                self.act(sgf, self.psb(bg), AF.Silu, r=[("ps", bg)], w=["sgf"])
                self.tt(fT[:, j, :], sgf, self.psb(bu), ALU.mult, r=["sgf", ("ps", bu)], w=[("fT", j)])
            if n + 1 < NTB:
                prenorm(n + 1)
            for c in range(KC):
                seg, sk = self.wload(l, "fo%d" % c, NJ * 128)
                seg = seg.rearrange("p (k m) -> p k m", k=NJ)
                b = self.bank()
                for j in range(NJ):
                    self.mm(self.psb(b), seg[:, j, :], fT[:, j, :], j == 0, j == NJ - 1, r=[sk, ("fT", j)], w=[("ps", b)])
                self.act(mbuf[:, c, :], self.psb(b), AF.Copy, r=[("ps", b)], w=[("mbuf", c)])
            self.rms_rstd(mbuf, [("mbuf", c) for c in range(KC)], sq, "p7b")
            for c in range(KC):
                self.stt(tmpf, mbuf[:, c, :], gfo[:, c:c + 1], self.rstd[:, :], ALU.mult, ALU.mult,
                         r=[("mbuf", c), "pf", "rstd"], w=["tmpf"])
                self.tt(mbuf[:, c, :], tmpf, xT[:, c, tsl], ALU.add, r=["tmpf", ("xT", c, n)], w=[("mbuf", c)])
            self.dma("sp", xdst[:, :, tsl], mbuf[:, :, :], r=[("mbuf", c) for c in range(KC)], w=[], slot="xout")
        if "xout" not in self.final_slots:
            self.final_slots.append("xout")

    def phase_s5(self, l, hT, merged, pf, uz):
        P = self.P
        K = 1024
        Ec = self.carve(16 * K, 128, [1, S], F32)[:, 0, :]
        Es = self.carve(24 * K, 128, [1, S], F32)[:, 0, :]
        vre = self.carve(32 * K, 128, [1, S], F32)[:, 0, :]
        vim = self.carve(40 * K, 128, [1, S], F32)[:, 0, :]
        xre = self.carve(48 * K, 128, [1, S], BF16)[:, 0, :]
        xim = self.carve(52 * K, 128, [1, S], BF16)[:, 0, :]
        tmp1 = self.carve(56 * K, 128, [1, TB], F32)[:, 0, :]
        tmp2 = self.carve(58 * K, 128, [1, TB], F32)[:, 0, :]
        Bre = self.carve(60 * K, 128, [2, 4, 128], BF16)
        Bim = self.carve(62 * K, 128, [2, 4, 128], BF16)
        gt = self.carve(71 * K, 128, [1, TB], F32)[:, 0, :]
        sm = self.carve(64 * K, 128, [16, 16], F32)
        s1 = self.carve(66 * K, 128, [1, TB], BF16)[:, 0, :]
        s2 = self.carve(67 * K, 128, [1, TB], BF16)[:, 0, :]
        t3 = self.carve(68 * K, 128, [1, TB], F32)[:, 0, :]
        xw = self.carve(32 * K, 128, [8, 512], F32)
        smi = self.carve(70 * K, 128, [1, 16], I32)[:, 0, :]
        TWO_PI = 2.0 * math.pi

        def pfx(o):
            return pf[:, o:o + 512]

        def dve(fn, r, w):
            P.op("dve", fn, r=r, w=w)

        def zoh(lr, li, ld, wk, n, tag, itile):
            dt_, lrdt, th, kf, q, sn, cs, t0 = wk[:8]
            kk = ["z%s%d" % (tag, i) for i in range(8)]
            kdt, klrdt, kth, kkf, kq, ksn, kcs, kt0 = kk
            ki = "z%si" % tag
            self.act(dt_, ld, AF.Exp, r=["pf"], w=[kdt])
            self.tt(lrdt, lr, dt_, ALU.mult, r=["pf", kdt], w=[klrdt])
            self.tt(th, li, dt_, ALU.mult, r=["pf", kdt], w=[kth])
            self.ts(kf, th, 1.0 / TWO_PI, None, ALU.mult, ALU.bypass, r=[kth], w=[kkf])
            dve(lambda e: e.tensor_copy(out=itile, in_=kf), r=[kkf], w=[ki])
            dve(lambda e: e.tensor_copy(out=kf, in_=itile), r=[ki], w=[kkf])
            self.stt(q, kf, -TWO_PI, th, ALU.mult, ALU.add, r=[kkf, kth], w=[kq])
            self.act(sn, q, AF.Sin, r=[kq], w=[ksn], scale=0.25)
            self.act(cs, q, AF.Sin, r=[kq, "hpi"], w=[kcs], scale=0.25, bias=self.hpi[:, 0:1])
            for it in range(2):
                self.tt(t0, sn, sn, ALU.mult, r=[ksn], w=[kt0])
                self.stt(sn, sn, 2.0, cs, ALU.mult, ALU.mult, r=[ksn, kcs], w=[ksn])
                self.ts(cs, t0, -2.0, 1.0, ALU.mult, ALU.add, r=[kt0], w=[kcs])
            self.act(dt_, lrdt, AF.Exp, r=[klrdt], w=[kdt])
            return dict(mag=dt_, cos=cs, sin=sn, keys=[kdt, kcs, ksn], k=kk)

        wkx = [xw[:, i, :] for i in range(8)]
        smx = self.carve(56 * K, 128, [1, 512], I32)[:, 0, :]
        zx = zoh(pfx(PF_LRX), pfx(PF_LIX), pfx(PF_LDX), wkx, 512, "x", smx)
        K_ = zx["k"]
        mg_, cs_x, sn_x = wkx[0], wkx[6], wkx[5]
        are, aim, den, cre, cim = wkx[1], wkx[2], wkx[3], wkx[4], wkx[7]
        kare, kaim, kden, kcre, kcim = K_[1], K_[2], K_[3], K_[4], K_[7]
        kmg, kcs, ksn = K_[0], K_[6], K_[5]
        lr, li = pfx(PF_LRX), pfx(PF_LIX)
        self.tt(are, mg_, cs_x, ALU.mult, r=[kmg, kcs], w=[kare])
        self.tt(aim, mg_, sn_x, ALU.mult, r=[kmg, ksn], w=[kaim])
        self.ts(are, are, -1.0, None, ALU.add, ALU.bypass, r=[kare], w=[kare])
        t0, kt0 = wkx[0], K_[0]
        t1, kt1, t2, kt2 = wkx[5], K_[5], wkx[6], K_[6]
        self.tt(den, lr, lr, ALU.mult, r=["pf"], w=[kden])
        self.tt(cre, li, li, ALU.mult, r=["pf"], w=[kcre])
        self.tt(den, den, cre, ALU.add, r=[kden, kcre], w=[kden])
        dve(lambda e: e.reciprocal(out=den, in_=den), r=[kden], w=[kden])
        self.tt(cre, are, lr, ALU.mult, r=[kare, "pf"], w=[kcre])
        self.tt(t0, aim, li, ALU.mult, r=[kaim, "pf"], w=[kt0])
        self.tt(cre, cre, t0, ALU.add, r=[kcre, kt0], w=[kcre])
        self.tt(cre, cre, den, ALU.mult, r=[kcre, kden], w=[kcre])
        self.tt(cim, aim, lr, ALU.mult, r=[kaim, "pf"], w=[kcim])
        self.tt(t0, are, li, ALU.mult, r=[kare, "pf"], w=[kt0])
        self.tt(cim, cim, t0, ALU.subtract, r=[kcim, kt0], w=[kcim])
        self.tt(cim, cim, den, ALU.mult, r=[kcim, kden], w=[kcim])
        br, bi = pfx(PF_BRX), pfx(PF_BIX)
        self.tt(t1, cre, br, ALU.mult, r=[kcre, "pf"], w=[kt1])
        self.tt(t2, cim, bi, ALU.mult, r=[kcim, "pf"], w=[kt2])
        self.tt(t1, t1, t2, ALU.subtract, r=[kt1, kt2], w=[kt1])
        for v in range(2):
            self.ts(Bre[:, v, :, :].rearrange("p a b -> p (a b)"), t1, self.cf[:, CF_PAR + v:CF_PAR + v + 1], None, ALU.mult, ALU.bypass,
                    r=[kt1, "cf"], w=["Bre"])
        self.tt(t1, cre, bi, ALU.mult, r=[kcre, "pf"], w=[kt1])
        self.tt(t2, cim, br, ALU.mult, r=[kcim, "pf"], w=[kt2])
        self.tt(t1, t1, t2, ALU.add, r=[kt1, kt2], w=[kt1])
        for v in range(2):
            self.ts(Bim[:, v, :, :].rearrange("p a b -> p (a b)"), t1, self.cf[:, CF_PAR + v:CF_PAR + v + 1], None, ALU.mult, ALU.bypass,
                    r=[kt1, "cf"], w=["Bim"])

        wks = [sm[:, i, :] for i in range(8)]
        zs = zoh(pf[:, PF_LRS:PF_LRS + 16], pf[:, PF_LIS:PF_LIS + 16], pf[:, PF_LDS:PF_LDS + 16], wks, 16, "s", smi)
        mag, cth, sth = zs["mag"], zs["cos"], zs["sin"]
        nsc = sm[:, 8, :]

        segC, skC = self.wload(l, "sC", 4096)
        Cre = segC[:, 0:2048].rearrange("p (j m) -> p j m", j=16)
        Cim = segC[:, 2048:4096].rearrange("p (j m) -> p j m", j=16)
        segD, skD = self.wload(l, "sD", 512)
        Dg = segD.rearrange("p (c m) -> p c m", c=4)

        self.bankset = [0, 1, 2, 3]
        Ec2 = [self.carve((16 + 4 * i) * K, 128, [1, TB], F32)[:, 0, :] for i in range(2)]
        Es2 = [self.carve((18 + 4 * i) * K, 128, [1, TB], F32)[:, 0, :] for i in range(2)]
        vslot_re = [self.carve((32 + 4 * i) * K, 128, [1, TB], F32)[:, 0, :] for i in range(4)]
        vslot_im = [self.carve((34 + 4 * i) * K, 128, [1, TB], F32)[:, 0, :] for i in range(4)]
        pt4 = [self.carve(o * K, 128, [1, TB], F32)[:, 0, :] for o in (73, 24, 27, 29)]
        car = self.carve(26 * K, 128, [1, 16], F32)[:, 0, :]
        vi = 0
        pending = None
        ydefer = []
        for cc in range(4):
            ybanks = [4, 5, 6, 7]
            for jj in range(4):
                j = cc * 4 + jj
                rows = slice(64 * (jj // 2), 64 * (jj // 2) + 64)
                pv = jj % 2
                Ec, Es = Ec2[j % 2], Es2[j % 2]
                ke, ks = ("Ec", j % 2), ("Es", j % 2)
                dve(lambda e, j=j, Ec=Ec: e.tensor_copy(out=Ec[:, 0:1], in_=cth[:, j:j + 1]), r=zs["keys"], w=[ke])
                dve(lambda e, j=j, Es=Es: e.tensor_copy(out=Es[:, 0:1], in_=sth[:, j:j + 1]), r=zs["keys"], w=[ks])
                nn = 1
                lev = 0
                while nn < TB:
                    cs_ = Ec[:, nn - 1:nn]
                    ss_ = Es[:, nn - 1:nn]
                    ns_ = nsc[:, lev:lev + 1]
                    self.ts(ns_, ss_, -1.0, None, ALU.mult, ALU.bypass, r=[ks], w=["nsc"])
                    self.ts(Ec[:, nn:2 * nn], Ec[:, 0:nn], cs_, None, ALU.mult, ALU.bypass, r=[ke], w=[ke])
                    self.stt(Ec[:, nn:2 * nn], Es[:, 0:nn], ns_, Ec[:, nn:2 * nn], ALU.mult, ALU.add, r=[ks, "nsc", ke], w=[ke])
                    self.ts(Es[:, nn:2 * nn], Es[:, 0:nn], cs_, None, ALU.mult, ALU.bypass, r=[ks, ke], w=[ks])
                    self.stt(Es[:, nn:2 * nn], Ec[:, 0:nn], ss_, Es[:, nn:2 * nn], ALU.mult, ALU.add, r=[ke, ks], w=[ks])
                    nn *= 2
                    lev += 1
                cl, sl_, nsl = Ec[:, TB - 1:TB], Es[:, TB - 1:TB], nsc[:, 12:13]
                self.ts(nsl, sl_, -1.0, None, ALU.mult, ALU.bypass, r=[ks], w=["nsl"])
                for n in range(NTB):
                    tsl = slice(n * TB, (n + 1) * TB)
                    vre, vim = vslot_re[vi % 4], vslot_im[vi % 4]
                    kvr, kvi = ("vre", vi % 4), ("vim", vi % 4)
                    vi += 1
                    b1 = self.bank()
                    self.mm(self.psb(b1), Bre[rows, pv, cc, :], uz[rows, cc, tsl], True, True, r=["Bre", ("uz", cc, n)], w=[("ps", b1)])
                    b2 = self.bank()
                    self.mm(self.psb(b2), Bim[rows, pv, cc, :], uz[rows, cc, tsl], True, True, r=["Bim", ("uz", cc, n)], w=[("ps", b2)])
                    self.tt(vre, Ec, self.psb(b1), ALU.mult, r=[ke, ks, ("ps", b1)], w=[kvr])
                    self.tt(tmp1, Es, self.psb(b2), ALU.mult, r=[ks, ("ps", b2)], w=["tmp1"])
                    self.tt(vre, vre, tmp1, ALU.add, r=[kvr, "tmp1"], w=[kvr])
                    self.tt(vim, Ec, self.psb(b2), ALU.mult, r=[ke, ("ps", b2)], w=[kvi])
                    self.tt(tmp2, Es, self.psb(b1), ALU.mult, r=[ks, ("ps", b1)], w=["tmp2"])
                    self.tt(vim, vim, tmp2, ALU.subtract, r=[kvi, "tmp2"], w=[kvi])
                    if n == 0:
                        dve(lambda e, j=j, vre=vre: e.tensor_tensor_scan(out=vre, data0=mag[:, j:j + 1].to_broadcast([128, TB]), data1=vre,
                                                                        initial=0.0, op0=ALU.mult, op1=ALU.add), r=[kvr] + zs["keys"], w=[kvr])
                        dve(lambda e, j=j, vim=vim: e.tensor_tensor_scan(out=vim, data0=mag[:, j:j + 1].to_broadcast([128, TB]), data1=vim,
                                                                        initial=0.0, op0=ALU.mult, op1=ALU.add), r=[kvi] + zs["keys"], w=[kvi])
                    else:
                        dve(lambda e, j=j, vre=vre: e.tensor_tensor_scan(out=vre, data0=mag[:, j:j + 1].to_broadcast([128, TB]), data1=vre,
                                                                        initial=car[:, 0:1], op0=ALU.mult, op1=ALU.add), r=[kvr, "car"] + zs["keys"], w=[kvr])
                        dve(lambda e, j=j, vim=vim: e.tensor_tensor_scan(out=vim, data0=mag[:, j:j + 1].to_broadcast([128, TB]), data1=vim,
                                                                        initial=car[:, 1:2], op0=ALU.mult, op1=ALU.add), r=[kvi, "car"] + zs["keys"], w=[kvi])
                    if n < NTB - 1:
                        wr_l, wi_l = vre[:, TB - 1:TB], vim[:, TB - 1:TB]
                        self.ts(car[:, 2:3], wr_l, cl, None, ALU.mult, ALU.bypass, r=[kvr, ke], w=["car2"])
                        self.ts(car[:, 3:4], wr_l, sl_, None, ALU.mult, ALU.bypass, r=[kvr, ks], w=["car3"])
                        self.stt(car[:, 0:1], wi_l, nsl, car[:, 2:3], ALU.mult, ALU.add, r=[kvi, "nsl", "car2"], w=["car"])
                        self.stt(car[:, 1:2], wi_l, cl, car[:, 3:4], ALU.mult, ALU.add, r=[kvi, ke, "car3", "car"], w=["car"])
                    def ptt(out, in0, in1, op, r, w):
                        P.op("pool", lambda e: e.tensor_tensor(out=out, in0=in0, in1=in1, op=op), r=r, w=w)
                    yb = ybanks[n]
                    ptt(pt4[0], Ec, vre, ALU.mult, [ke, kvr], ["pt0"])
                    ptt(pt4[1], Es, vim, ALU.mult, [ks, kvi], ["pt1"])
                    if pending is not None:
                        pending()
                        pending = None
                    ptt(pt4[2], Es, vre, ALU.mult, [ks, kvr], ["pt2"])
                    ptt(pt4[3], Ec, vim, ALU.mult, [ke, kvi], ["pt3"])
                    ptt(xre[:, tsl], pt4[0], pt4[1], ALU.subtract, ["pt0", "pt1"], [("xre", n)])
                    ptt(pt4[2], pt4[2], pt4[3], ALU.add, ["pt2", "pt3"], ["pt2"])

                    def ymm(j=j, jj=jj, cc=cc, n=n, tsl=tsl, yb=yb):
                        self.mm(self.psb(yb), Cre[:, j, :], xre[:, tsl], jj == 0, False, r=[skC, ("xre", n)], w=[("ps", yb)])
                        self.mm(self.psb(yb), Cim[:, j, :], xim[:, tsl], False, False, r=[skC, ("xim", n)], w=[("ps", yb)])
                        if jj == 3:
                            self.mm(self.psb(yb), Dg[:, cc, :], uz[:, cc, tsl], False, True, r=[skD, ("uz", cc, n)], w=[("ps", yb)])

                    def fin(n=n, tsl=tsl):
                        xo = xim[:, tsl]
                        P.op("pool", lambda e: e.tensor_scalar(out=xo, in0=pt4[2], scalar1=-1.0, scalar2=0.0, op0=ALU.mult, op1=ALU.add),
                             r=["pt2"], w=[("xim", n)])
                    pending = fin
                    ydefer.append(ymm)
                    while len(ydefer) > 3:
                        ydefer.pop(0)()
                pending()
                pending = None
            while ydefer:
                ydefer.pop(0)()
            for n in range(NTB):
                tsl = slice(n * TB, (n + 1) * TB)
                yb = ybanks[n]
                self.act(gt, self.psb(yb), AF.Square, r=[("ps", yb)], w=["gt"])
                self.ts(gt, gt, 0.044715, 1.0, ALU.mult, ALU.add, r=["gt"], w=["gt"])
                self.tt(gt, gt, self.psb(yb), ALU.mult, r=["gt", ("ps", yb)], w=["gt"])
                self.act(t3, gt, AF.Sigmoid, r=["gt"], w=["t3"], scale=1.5957691216057308)
                self.tt(uz[:, cc, tsl], t3, self.psb(yb), ALU.mult, r=["t3", ("ps", yb)], w=[("uz", cc, n)])
        self.tap("zT", uz[:, :, :], [("uz", cc, n) for cc in range(4) for n in range(NTB)], [4, S])
        self.bankset = list(range(8))
        for c in range(KC):
            segl, skl = self.wload(l, "glu%d" % c, 1024)
            segl = segl.rearrange("p (a k m) -> p a k m", a=2, k=4)
            segg, skg = self.wload(l, "g2_%d" % c, 1024)
            segg = segg.rearrange("p (k m) -> p k m", k=KC)
            for n in range(NTB):
                tsl = slice(n * TB, (n + 1) * TB)
                ba = self.bank()
                for cc in range(4):
                    self.mm(self.psb(ba), segl[:, 0, cc, :], uz[:, cc, tsl], cc == 0, cc == 3, r=[skl, ("uz", cc, n)], w=[("ps", ba)])
                bb = self.bank()
                for cc in range(4):
                    self.mm(self.psb(bb), segl[:, 1, cc, :], uz[:, cc, tsl], cc == 0, cc == 3, r=[skl, ("uz", cc, n)], w=[("ps", bb)])
                bg = self.bank()
                for kc in range(KC):
                    self.mm(self.psb(bg), segg[:, kc, :], hT[:, kc, tsl], kc == 0, kc == KC - 1, r=[skg, ("hT", kc, n)], w=[("ps", bg)])
                self.act(s1, self.psb(bb), AF.Sigmoid, r=[("ps", bb)], w=["s1"])
                self.act(s2, self.psb(bg), AF.Sigmoid, r=[("ps", bg)], w=["s2"])
                self.tt(t3, s1, self.psb(ba), ALU.mult, r=["s1", ("ps", ba)], w=["t3"])
                self.tt(t3, t3, s2, ALU.mult, r=["t3", "s2"], w=["t3"])
                self.tt(merged[:, c, tsl], merged[:, c, tsl], t3, ALU.add, r=[("mg", c, n), "t3"], w=[("mg", c, n)])

    def phase_attn(self, l, hT, merged, kaugT, kiT, vaug, wi_s, ident, sel4, biasT, cmask, ones64, b31d):
        P = self.P
        K = 1024
        ps = self.ps
        qT = self.arena[0:65, 0: 16 * TB].rearrange("p (h q) -> p h q", h=16)
        qiT = self.arena[0:64, 8 * K: 8 * K + 8 * TB].rearrange("p (h q) -> p h q", h=8)
        OTn = self.arena[0:64, 12 * K: 12 * K + 16 * TB].rearrange("p (h q) -> p h q", h=16)
        score2 = [self.carve(40 * K, 128, [1, S], F32)[:, 0, :], self.pf_t[:, PF_LRX:PF_LRX + S]]
        rl = [self.carve(48 * K, 128, [1, TB], F32)[:, 0, :], self.carve(50 * K, 128, [1, TB], F32)[:, 0, :]]
        nm3 = [self.carve((52 + 4 * i) * K, 128, [1, S], BF16)[:, 0, :] for i in range(3)]
        self.idx_i = 0
        PT = [self.carve(64 * K, 128, [1, 1024], BF16)[:, 0, :], self.carve(66 * K, 128, [1, 1024], BF16)[:, 0, :]]
        ot = self.carve(68 * K, 64, [1, 1024], F32)[:, 0, :]
        aotmp = self.carve(60 * K, 128, [1, TB], F32)[:, 0, :]
        bis = self.carve(72 * K, 128, [1, 16], F32)[:, 0, :]
        sgt = self.carve(73 * K, 128, [1, TB], BF16)[:, 0, :]
        lnr = self.arena[64:65, 12 * K: 12 * K + 2048].bitcast(F32)
        rrow = self.arena[64:65, 14 * K: 14 * K + 1024]
        P.op("pool", lambda e: e.dma_start(out=self.arena[64:65, 0:16 * TB], in_=b31d, max_dma_last_dim=8192), w=["qT64"], slot="b31")

        def emit_ao(nn):
            tsl = slice(nn * TB, (nn + 1) * TB)
            self.bankset = [0, 1, 2, 3, 4, 5]
            for c in range(KC):
                sego, sko = self.wload(l, "ao%d" % c, 2048)
                sego = sego.rearrange("p (h m) -> p h m", h=16)
                segg, skg = self.wload(l, "g1_%d" % c, 1024)
                segg = segg.rearrange("p (k m) -> p k m", k=KC)
                by = self.bank()
                for h in range(16):
                    self.mm(self.psb(by), sego[0:64, h, :], OTn[:, h, :], h == 0, h == 15, r=[sko] + [("OTn", i) for i in range(4)], w=[("ps", by)])
                bg = self.bank()
                for kc in range(KC):
                    self.mm(self.psb(bg), segg[:, kc, :], hT[:, kc, tsl], kc == 0, kc == KC - 1, r=[skg, ("hT", kc, nn)], w=[("ps", bg)])
                self.act(sgt, self.psb(bg), AF.Sigmoid, r=[("ps", bg)], w=["sgt"])
                t3 = aotmp
                self.tt(t3, sgt, self.psb(by), ALU.mult, r=["sgt", ("ps", by)], w=["aotmp"])
                self.tt(merged[:, c, tsl], merged[:, c, tsl], t3, ALU.add, r=[("mg", c, nn), "aotmp"], w=[("mg", c, nn)])
                yield

        for n in range(NTB):
            tsl = slice(n * TB, (n + 1) * TB)
            self.bankset = list(range(8))
            for hp in range(8):
                seg, sk = self.wload(l, "q%d" % hp, 1024)
                seg = seg.rearrange("p (k m) -> p k m", k=KC)
                for half in range(2):
                    h = 2 * hp + half
                    b = self.bank()
                    for kc in range(KC):
                        self.mm(self.psb(b, 64), seg[:, kc, half * 64:(half + 1) * 64], hT[:, kc, tsl], kc == 0, kc == KC - 1,
                                r=[sk, ("hT", kc, n)], w=[("ps", b)])
                    self.act(qT[0:64, h, :], self.psb(b, 64), AF.Copy, r=[("ps", b)], w=[("qT", h)], scale=0.125)
            for hp in range(4):
                seg, sk = self.wload(l, "qi%d" % hp, 1024)
                seg = seg.rearrange("p (k m) -> p k m", k=KC)
                for half in range(2):
                    h = 2 * hp + half
                    b = self.bank()
                    for kc in range(KC):
                        self.mm(self.psb(b, 64), seg[:, kc, half * 64:(half + 1) * 64], hT[:, kc, tsl], kc == 0, kc == KC - 1,
                                r=[sk, ("hT", kc, n)], w=[("ps", b)])
                    self.act(qiT[0:64, h, :], self.psb(b, 64), AF.Copy, r=[("ps", b)], w=[("qiT", h)])
            if n == 0:
                self.tap("qT", qT[:, :, :], [("qT", h) for h in range(16)] + ["qT64"], [16, TB], parts=65)

            def genA(qq):
                qb = 4 * n + qq
                qsl = slice(qq * 128, (qq + 1) * 128)
                L = (qb + 1) * 128
                sc = score2[qb % 2]
                ngr = (L + 511) // 512
                for kg in range(ngr):
                    k0 = kg * 512
                    nk = min(512, L - k0)
                    sk_ = ("sc", qb % 2, kg)
                    for h in range(8):
                        b = 6 + (self.idx_i % 2)
                        r_ = rl[self.idx_i % 2]
                        rk = ("rl", self.idx_i % 2)
                        self.idx_i += 1
                        self.mm(self.psb(b, 128, nk), qiT[0:64, h, qsl], kiT[0:64, k0:k0 + nk], True, True,
                                r=[("qiT", h)] + [("kiT", i) for i in range(NTB)], w=[("ps", b)])
                        self.act(r_[:, 0:nk], self.psb(b, 128, nk), AF.Relu, r=[("ps", b)], w=[rk])
                        wcol = wi_s[:, qb, h:h + 1]
                        wk = [("wi", qb)]
                        if h == 0:
                            ndiag = nk - 128 if (k0 + nk == L) else nk
                            if ndiag > 0:
                                self.ts(sc[:, k0:k0 + ndiag], r_[:, 0:ndiag], wcol, None, ALU.mult, ALU.bypass, r=[rk] + wk, w=[sk_])
                            if k0 + nk == L:
                                self.stt(sc[:, L - 128:L], r_[:, nk - 128:nk], wcol, cmask, ALU.mult, ALU.add, r=[rk, "cf"] + wk, w=[sk_])
                        else:
                            self.stt(sc[:, k0:k0 + nk], r_[:, 0:nk], wcol, sc[:, k0:k0 + nk], ALU.mult, ALU.add,
                                     r=[rk, sk_] + wk, w=[sk_])
                        yield

            def genB(qq):
                qb = 4 * n + qq
                L = (qb + 1) * 128
                sc = score2[qb % 2]
                nmb = nm3[qb % 2]
                nmk = ("nm", qb % 2)
                ngr = (L + 511) // 512
                sck = [("sc", qb % 2, kg) for kg in range(ngr)]
                if qb >= 2:
                    o = 8 * (qb % 2)
                    cA, cB, cnt, tmpb, thr = bis[:, o:o + 1], bis[:, o + 1:o + 2], bis[:, o + 2:o + 3], bis[:, o + 3:o + 4], bis[:, o + 4:o + 5]
                    kp = "b%d" % (qb % 2)
                    P.op("dve", lambda e, cA=cA: e.memset(cA, 0.0), w=[kp + "c0"])
                    cur, nxt = cA, cB
                    curk, nxtk = kp + "c0", kp + "c1"
                    step = 4.0
                    for it in range(NBIS):
                        self.ts(nmb[:, 0:L], sc[:, 0:L], cur, None, ALU.is_ge, ALU.add, r=sck + [curk], w=[nmk, kp + "cnt"], accum_out=cnt)
                        self.ts(tmpb, cnt, 256.0, 2.0 * step, ALU.is_ge, ALU.mult, r=[kp + "cnt"], w=[kp + "tmpb"])
                        self.ts(nxt, tmpb, -step, cur, ALU.add, ALU.add, r=[kp + "tmpb", curk], w=[nxtk])
                        cur, nxt = nxt, cur
                        curk, nxtk = nxtk, curk
                        step *= 0.5
                        yield
                    self.ts(thr, cur, -2.0 * step - 1e-5, None, ALU.add, ALU.bypass, r=[curk], w=[kp + "thr"])
                    self.ts(nmb[:, 0:L], sc[:, 0:L], thr, NEG, ALU.is_lt, ALU.mult, r=sck + [kp + "thr"], w=[nmk])
                else:
                    self.ts(nmb[:, 0:L], sc[:, 0:L], -16.0, NEG, ALU.is_lt, ALU.mult, r=sck, w=[nmk])
                if qb == 5:
                    self.tap("score5", sc[:, 0:L], sck, [L])
                    self.tap("nm5", nmb[:, 0:L], [nmk], [L])
                yield

            def n_units_A(qq):
                qb = 4 * n + qq
                return 8 * (((qb + 1) * 128 + 511) // 512)

            def step_gen(g):
                if g is None:
                    return None
                try:
                    next(g)
                    return g
                except StopIteration:
                    return None

            def drain(g):
                while g is not None:
                    g = step_gen(g)

            def emit_attn(qq, gB, nB, gA, nA):
                qb = 4 * n + qq
                qsl = slice(qq * 128, (qq + 1) * 128)
                nmb = nm3[qb % 2]
                nmk = ("nm", qb % 2)
                niter = 2 * (qb + 1)
                perB = -(-nB // niter)
                perA = -(-nA // niter)
                for hh in range(2):
                    def emit_S(kb):
                        sb = kb % 2
                        near = (qb - kb) < 2
                        kk = 64 if near else 65
                        ksl = slice(kb * 128, (kb + 1) * 128)
                        for bk in range(2):
                            bnk = 2 * sb + bk
                            hs = slice(hh * 8 + bk * 4, hh * 8 + bk * 4 + 4)
                            outp = self.psb(bnk).rearrange("p (h q) -> p h q", h=4)
                            qk = [("qT", h) for h in range(hh * 8 + bk * 4, hh * 8 + bk * 4 + 4)] + ["qT64"]
                            self.mm(outp, kaugT[0:kk, ksl], qT[0:kk, hs, qsl], True, False,
                                    r=qk + [("kT", kb // 4), "kaug1"], w=[("ps", bnk)])
                            self.mm(outp, nmb[:, ksl], sel4, False, not near, r=[nmk, "cb"], w=[("ps", bnk)])
                            if near:
                                self.mm(outp, ident, biasT[:, qb - kb, hs, :], False, True, r=["cb"], w=[("ps", bnk)])
                        self.act(PT[sb][:, :], ps[:, 2 * sb * 512: 2 * sb * 512 + 1024], AF.Exp,
                                 r=[("ps", 2 * sb), ("ps", 2 * sb + 1)], w=[("PT", sb)])

                    def emit_PV(kb):
                        sb = kb % 2
                        for bk in range(2):
                            self.mm(self.psb(4 + bk, 65), vaug[:, kb, 0:65], PT[sb][:, bk * 512:(bk + 1) * 512], kb == 0, kb == qb,
                                    r=[("PT", sb), ("vaug", kb), "vaug1"], w=[("ps", 4 + bk)])

                    for kb in range(qb + 1):
                        emit_S(kb)
                        if kb > 0:
                            emit_PV(kb - 1)
                        for _ in range(perB):
                            gB = step_gen(gB)
                        for _ in range(perA):
                            gA = step_gen(gA)
                    emit_PV(qb)
                    if hh == 1:
                        drain(gB)
                        drain(gA)
                        gB = gA = None
                    self.act(lnr, ps[64:65, 2048:3072], AF.Ln, r=[("ps", 4), ("ps", 5)], w=["lnr"])
                    self.act(rrow, lnr, AF.Exp, r=["lnr"], w=["rrow"], scale=-1.0)
                    for bk in range(2):
                        self.mm(self.psb(6 + bk, 64), self.ones_bf[64:65, 0:64], rrow[:, bk * 512:(bk + 1) * 512], True, True,
                                r=["ones_bf", "rrow"], w=[("ps", 6 + bk)])
                    self.act(ot[:, :], ps[0:64, 2048:3072], AF.Copy, r=[("ps", 4), ("ps", 5)], w=["ot"])
                    for bk in range(2):
                        hs = slice(hh * 8 + bk * 4, hh * 8 + bk * 4 + 4)
                        self.tt(OTn[:, hs, qsl], ot[:, bk * 512:(bk + 1) * 512].rearrange("p (h q) -> p h q", h=4),
                                self.psb(6 + bk, 64).rearrange("p (h q) -> p h q", h=4), ALU.mult,
                                r=["ot", ("ps", 6 + bk)], w=[("OTn", hh * 2 + bk)])

            drain(genA(0))
            gB0, gA1 = genB(0), genA(1)
            gAO = emit_ao(n - 1) if n > 0 else None
            rnd = 0
            while gB0 is not None or gA1 is not None or gAO is not None:
                gB0 = step_gen(gB0)
                gA1 = step_gen(step_gen(gA1))
                if rnd % 2 == 1:
                    gAO = step_gen(gAO)
                rnd += 1
            for qq in range(4):
                gB = genB(qq + 1) if qq + 1 < 4 else None
                gA = genA(qq + 2) if qq + 2 < 4 else None
                emit_attn(qq, gB, NBIS + 1, gA, n_units_A(qq + 2) if qq + 2 < 4 else 0)
            if n == 0:
                self.tap("OTn", OTn[:, :, :], [("OTn", i) for i in range(4)], [16, TB], parts=64)
        for _ in emit_ao(NTB - 1):
            pass


def km(w):
    k, m = w.shape
    return np.ascontiguousarray(w.reshape(k // 128, 128, m).transpose(1, 0, 2)).reshape(128, (k // 128) * m)


def host_segments(inp, l):
    w = inp["w_in"][l]
    segs = {}
    segs["kk"] = km(np.concatenate([w[:, C_K:C_K + 64], w[:, C_KI:C_KI + 64]], axis=1))
    segs["vw"] = km(np.concatenate([w[:, C_V:C_V + 64], w[:, C_WI:C_WI + 8]], axis=1))
    for cc in range(4):
        segs["pu%d" % cc] = km(w[:, C_POOL + cc * 128: C_POOL + (cc + 1) * 128])
        segs["su%d" % cc] = km(w[:, C_S5 + cc * 128: C_S5 + (cc + 1) * 128])
    segs["mix"] = np.ascontiguousarray(inp["pool_mix_w"][l].transpose(1, 0, 2)).reshape(128, 512)
    for c in range(8):
        cs = slice(c * 128, (c + 1) * 128)
        segs["po%d" % c] = km(inp["pool_out_w"][l][:, cs])
        for b in range(3):
            segs["g%d_%d" % (b, c)] = km(w[:, C_G + b * 1024 + c * 128: C_G + b * 1024 + (c + 1) * 128])
        glu = inp["s5_glu_w"][l]
        segs["glu%d" % c] = np.concatenate([km(glu[:, cs]), km(glu[:, 1024 + c * 128: 1024 + (c + 1) * 128])], axis=1)
        segs["q%d" % c] = km(w[:, C_Q + c * 128: C_Q + (c + 1) * 128])
        ao = inp["attn_out_w"][l][:, cs].reshape(16, 64, 128).transpose(1, 0, 2).reshape(64, 2048)
        segs["ao%d" % c] = np.concatenate([ao, np.zeros((64, 2048), np.float32)], axis=0)
        segs["wo%d" % c] = km(inp["w_out"][l][:, cs])
        segs["fo%d" % c] = km(inp["ffn_w_out"][l][:, cs])
    for hp in range(4):
        segs["qi%d" % hp] = km(w[:, C_QI + hp * 128: C_QI + (hp + 1) * 128])
    fw = inp["ffn_w_in"][l]
    for j in range(NJ):
        segs["f%d" % j] = km(np.concatenate([fw[:, j * 128:(j + 1) * 128], fw[:, FH + j * 128: FH + (j + 1) * 128]], axis=1))
    cre = inp["s5_c_re"][l]
    cim = inp["s5_c_im"][l]
    Cre = np.zeros((128, 16, 128), np.float32)
    Cim = np.zeros((128, 16, 128), np.float32)
    for g in range(32):
        j, g2 = g // 2, g % 2
        jj = j % 4
        m0 = 32 * jj + 16 * g2
        Cre[g2 * 64:(g2 + 1) * 64, j, m0:m0 + 16] = cre[g].T
        Cim[g2 * 64:(g2 + 1) * 64, j, m0:m0 + 16] = cim[g].T
    segs["sC"] = np.concatenate([Cre.reshape(128, 2048), Cim.reshape(128, 2048)], axis=1)
    Dg = np.zeros((128, 4, 128), np.float32)
    d = inp["s5_d"][l]
    for cc in range(4):
        Dg[np.arange(128), cc, np.arange(128)] = d[cc * 128:(cc + 1) * 128]
    segs["sD"] = Dg.reshape(128, 512)
    return segs


def host_pf(inp, l):
    pf = np.zeros((128, NPF), np.float32)
    for off, name in ((PF_GMP, "norm_mix_pre"), (PF_GMO, "norm_mix_post"), (PF_GFP, "norm_ffn_pre"), (PF_GFO, "norm_ffn_post")):
        pf[:, off:off + 8] = inp[name][l].reshape(8, 128).T
    pf[:, PF_PSC:PF_PSC + 4] = inp["pool_scale"][l].reshape(4, 128).T
    lr, li, ld = inp["s5_lambda_re"][l], inp["s5_lambda_im"][l], inp["s5_log_dt"][l]
    for j in range(16):
        for g2 in range(2):
            g = 2 * j + g2
            pf[g2 * 64:(g2 + 1) * 64, PF_LRS + j] = lr[g]
            pf[g2 * 64:(g2 + 1) * 64, PF_LIS + j] = li[g]
            pf[g2 * 64:(g2 + 1) * 64, PF_LDS + j] = ld[g]
    br, bi = inp["s5_b_re"][l], inp["s5_b_im"][l]
    X = np.zeros((5, 128, 4, 128), np.float32)
    for cc in range(4):
        for p in range(128):
            g = cc * 8 + p // 16
            i = p % 16
            X[0, p, cc, :] = np.tile(lr[g], 2)
            X[1, p, cc, :] = np.tile(li[g], 2)
            X[2, p, cc, :] = ld[g]
            g2 = g % 2
            X[3, p, cc, g2 * 64:(g2 + 1) * 64] = br[g, :, i]
            X[4, p, cc, g2 * 64:(g2 + 1) * 64] = bi[g, :, i]
    for k, off in enumerate((PF_LRX, PF_LIX, PF_LDX, PF_BRX, PF_BIX)):
        pf[:, off:off + 512] = X[k].reshape(128, 512)
    return pf


def host_consts(inp):
    cf = np.zeros((128, NCF), np.float32)
    q = np.arange(128)[:, None]
    s = np.arange(128)[None, :]
    cf[:, CF_CMASK:CF_CMASK + 128] = np.where(s > q, np.float32(-1e4), np.float32(0.0))
    cf[:, CF_INVC:CF_INVC + 16] = (1.0 / np.arange(1, 17, dtype=np.float32))[None, :]
    cf[:, CF_ONES:CF_ONES + 64] = 1.0
    par = ((np.arange(128) // 32) % 2).astype(np.float32)
    cf[:, CF_PAR] = 1.0 - par
    cf[:, CF_PAR + 1] = par
    cb = np.zeros((128, NCB), np.float32)
    cb[:, CB_ID:CB_ID + 128] = np.eye(128, dtype=np.float32)
    cb[:, CB_SEL:CB_SEL + 512] = np.tile(np.eye(128, dtype=np.float32), (1, 4))
    rb = inp["rel_bias"]
    sl = np.arange(128)[:, None]
    ql = np.arange(128)[None, :]
    bt = np.zeros((128, 2, 16, 128), np.float32)
    for kind in range(2):
        dist = ql - sl + 128 * kind
        idx = rel_bucket_np(np.maximum(dist, 0))
        bt[:, kind, :, :] = rb[idx].transpose(0, 2, 1)
    cb[:, CB_BIAS:CB_BIAS + 4096] = bt.reshape(128, 4096)
    b31 = np.ascontiguousarray(np.repeat(rb[31][:, None], TB, axis=1)).reshape(1, 16 * TB).astype(np.float32)
    return cf, cb, b31


_CACHE = {}


def get_program(nlayers=NL, stop=None, taps=()):
    key = (nlayers, stop, tuple(taps))
    if key not in _CACHE:
        b0 = Builder(nlayers, stop, taps)
        b0.wtotal = 1 << 20
        b0.build()
        b = Builder(nlayers, stop, taps)
        b.wtotal = max(b0.seg_total, 16)
        nc = b.build()
        _CACHE[key] = (nc, b)
    return _CACHE[key]


def run(inputs, nlayers=NL, stop=None, taps=()):
    nc, b = get_program(nlayers, stop, taps)
    inp = {k: np.asarray(v, dtype=np.float32) for k, v in inputs.items()}
    ws = np.zeros((NL, 128, b.wtotal), np.float32)
    pfs = np.zeros((NL, 128, NPF), np.float32)
    for l in range(NL):
        segs = host_segments(inp, l)
        for name, (off, n) in b.seg_off.items():
            a = segs[name]
            assert a.shape == (128, n), (name, a.shape, n)
            ws[l, :, off:off + n] = a
        pfs[l] = host_pf(inp, l)
    cf, cb, b31 = host_consts(inp)
    x = inp["x"]
    in_maps = []
    for c in range(8):
        xt = np.ascontiguousarray(x[c].T.reshape(KC, 128, S).transpose(1, 0, 2))
        in_maps.append({"x": xt, "wstream": ws, "pf": pfs, "cf": cf, "cb": cb, "b31": b31})
    res = run_bass_kernel_spmd(nc, in_maps, core_ids=list(range(8)))
    return res, b


def kernel(**inputs):
    res, b = run(inputs)
    outs = []
    for c in range(8):
        o = res.results[c]["out"]
        outs.append(np.ascontiguousarray(o.transpose(1, 0, 2).reshape(D, S).T))
    return np.stack(outs, axis=0).astype(np.float32)
```

```python
import contextlib
import math
import numpy as np
import concourse.bass as bass
import concourse.mybir as mybir
from concourse.bass_utils import run_bass_kernel_spmd

F32 = mybir.dt.float32
BF16 = mybir.dt.bfloat16
I32 = mybir.dt.int32
ALU = mybir.AluOpType
AF = mybir.ActivationFunctionType

ENGS = ("pe", "act", "dve", "pool", "sp")


class Op:
    __slots__ = ("eng", "fn", "waits", "signal", "slot", "seq", "dcount")

    def __init__(self, eng, fn):
        self.eng = eng
        self.fn = fn
        self.waits = {}
        self.signal = False
        self.slot = None
        self.seq = 0
        self.dcount = 0


class Prog:
    def __init__(self):
        self.ops = {e: [] for e in ENGS}
        self.last_w = {}
        self.readers = {}
        self.slot_count = {}
        self.pending = {e: {} for e in ENGS}

    def _add_wait(self, op, tok, raw):
        kind, who, val = tok
        if kind == "E":
            if who == op.eng and not raw:
                return
            if who == "pe" and op.eng == "pe":
                return
            self.ops[who][val].signal = True
        k = (kind, who)
        if op.waits.get(k, -1) < val:
            op.waits[k] = val

    def op(self, eng, fn, r=(), w=(), slot=None):
        o = Op(eng, fn)
        o.seq = len(self.ops[eng])
        if slot is not None:
            o.slot = slot
            self.slot_count[slot] = self.slot_count.get(slot, 0) + 1
            o.dcount = self.slot_count[slot]
            tok = ("D", slot, o.dcount)
        else:
            tok = ("E", eng, o.seq)
        for k, v in self.pending[eng].items():
            if o.waits.get(k, -1) < v:
                o.waits[k] = v
        self.pending[eng] = {}
        for k in r:
            lw = self.last_w.get(k)
            if lw is not None:
                self._add_wait(o, lw, True)
        for k in w:
            lw = self.last_w.get(k)
            if lw is not None:
                self._add_wait(o, lw, False)
            for t in self.readers.get(k, ()):
                self._add_wait(o, t, False)
        self.ops[eng].append(o)
        for k in r:
            self.readers.setdefault(k, []).append(tok)
        for k in w:
            self.last_w[k] = tok
            self.readers[k] = []
        return o

    def barrier(self):
        toks = {}
        for e in ENGS:
            for o in reversed(self.ops[e]):
                if o.slot is None:
                    o.signal = True
                    toks[("E", e)] = o.seq
                    break
        for s, c in self.slot_count.items():
            toks[("D", s)] = c
        for e in ENGS:
            for k, v in toks.items():
                if k == ("E", e):
                    continue
                if self.pending[e].get(k, -1) < v:
                    self.pending[e][k] = v
        self.last_w = {}
        self.readers = {}

    def emit(self, nc, final_slots=()):
        with contextlib.ExitStack() as st:
            esem = {e: st.enter_context(nc.semaphore("s_" + e)) for e in ENGS}
            dsem = {s: st.enter_context(nc.semaphore("d_%d" % i)) for i, s in enumerate(self.slot_count)}
            sigcount = {}
            for e in ENGS:
                c = 0
                arr = []
                for o in self.ops[e]:
                    if o.slot is None and o.signal:
                        c += 1
                    arr.append(c)
                sigcount[e] = arr
            block = st.enter_context(nc.Block())
            prog = self

            def make(e):
                def body(eng):
                    waited = {}
                    for o in prog.ops[e]:
                        for (kind, who), val in o.waits.items():
                            if kind == "E":
                                sem = esem[who]
                                v = sigcount[who][val]
                            else:
                                sem = dsem[who]
                                v = 16 * val
                            if waited.get((kind, who), -1) >= v:
                                continue
                            waited[(kind, who)] = v
                            eng.wait_ge(sem, v)
                        ins = o.fn(eng)
                        if o.slot is not None:
                            ins.then_inc(dsem[o.slot], 16)
                        elif o.signal:
                            ins.then_inc(esem[e], 1)
                    if e == "sp":
                        for s in final_slots:
                            eng.wait_ge(dsem[s], 16 * prog.slot_count[s])
                return body

            block.tensor(make("pe"))
            block.scalar(make("act"))
            block.vector(make("dve"))
            block.gpsimd(make("pool"))
            block.sync(make("sp"))


S = 2048
D = 1024
TB = 512
NTB = 4
KC = 8
NL = 2
FH = 2816
NJ = 22
EPS = 1e-6
IDX_SCALE = (8 ** -0.5) * (64 ** -0.5)
NEG = -30000.0
NBIS = 15

C_POOL, C_Q, C_K, C_V, C_QI, C_KI, C_WI, C_S5, C_G = 0, 512, 1536, 1600, 1664, 2176, 2240, 2248, 2760

PF_GMP, PF_GMO, PF_GFP, PF_GFO, PF_PSC = 0, 8, 16, 24, 32
PF_LRS, PF_LIS, PF_LDS = 36, 52, 68
PF_LRX, PF_LIX, PF_LDX, PF_BRX, PF_BIX = 84, 596, 1108, 1620, 2132
NPF = 2644
CF_CMASK, CF_INVC, CF_ONES, CF_PAR = 0, 128, 144, 208
NCF = 212
CB_ID, CB_SEL, CB_BIAS = 0, 128, 640
NCB = 640 + 4096

ARENA = 86 * 1024
RSLOT = 4096
NRING = 4
KV0 = 75 * 1024


def rel_bucket_np(dist):
    dist = np.asarray(dist, np.int32)
    d_f = np.maximum(dist, 1).astype(np.float32)
    large = 16 + (np.log(d_f / np.float32(16)) / np.float32(math.log(128 / 16)) * np.float32(16)).astype(np.int32)
    large = np.minimum(large, 31)
    return np.where(dist < 16, dist, large)


class Builder:
    def __init__(self, nlayers=NL, stop=None, taps=()):
        self.nlayers = nlayers
        self.stop = stop
        self.taps = taps
        self.seg_off = {}
        self.seg_total = 0
        self.ring_i = 0
        self.bank_i = 0
        self.bankset = list(range(8))
        self.tapouts = {}

    def carve(self, off, parts, shape, dt):
        esz = 2 if dt == BF16 else 4
        n = int(np.prod(shape))
        assert off % 4 == 0 and off + n * esz <= ARENA, (off, n, esz)
        ap = self.arena[0:parts, off // 2: off // 2 + n * esz // 2]
        if dt != BF16:
            ap = ap.bitcast(dt)
        if len(shape) == 2:
            return ap.rearrange("p (a b) -> p a b", a=shape[0])
        if len(shape) == 3:
            return ap.rearrange("p (a b c) -> p a b c", a=shape[0], b=shape[1])
        return ap

    def bank(self):
        b = self.bankset[self.bank_i % len(self.bankset)]
        self.bank_i += 1
        return b

    def psb(self, b, parts=128, n=512):
        return self.ps[0:parts, b * 512: b * 512 + n]

    def wload(self, l, name, n):
        assert n <= RSLOT
        if name not in self.seg_off:
            self.seg_off[name] = (self.seg_total, n)
            self.seg_total += n
        off, n0 = self.seg_off[name]
        assert n0 == n
        slot = self.ring_i % NRING
        self.ring_i += 1
        dst = self.ring[:, slot * RSLOT: slot * RSLOT + n]
        src = self.wstream[l, :, off:off + n]
        key = ("ring", slot)
        self.P.op("pool", lambda e: e.dma_start(out=dst, in_=src, max_dma_last_dim=8192), w=[key], slot="ring%d" % slot)
        return dst, key

    def mm(self, out, lhsT, rhs, start, stop, r, w):
        self.P.op("pe", lambda e: e.matmul(out, lhsT, rhs, start=start, stop=stop), r=r, w=w)

    def act(self, out, in_, func, r, w, scale=1.0, bias=0.0):
        self.P.op("act", lambda e: e.activation(out=out, in_=in_, func=func, bias=bias, scale=scale), r=r, w=w)

    def tt(self, out, in0, in1, op, r, w):
        self.P.op("dve", lambda e: e.tensor_tensor(out=out, in0=in0, in1=in1, op=op), r=r, w=w)

    def ts(self, out, in0, s1, s2, op0, op1, r, w, accum_out=None):
        if accum_out is None:
            self.P.op("dve", lambda e: e.tensor_scalar(out=out, in0=in0, scalar1=s1, scalar2=s2, op0=op0, op1=op1), r=r, w=w)
        else:
            self.P.op("dve", lambda e: e.tensor_scalar(out=out, in0=in0, scalar1=s1, scalar2=s2, op0=op0, op1=op1, accum_out=accum_out), r=r, w=w)

    def stt(self, out, in0, scalar, in1, op0, op1, r, w):
        self.P.op("dve", lambda e: e.scalar_tensor_tensor(out=out, in0=in0, scalar=scalar, in1=in1, op0=op0, op1=op1), r=r, w=w)

    def dma(self, eng, out, in_, r, w, slot):
        self.P.op(eng, lambda e: e.dma_start(out=out, in_=in_), r=r, w=w, slot=slot)

    def tap(self, name, ap, keys, shape, parts=128):
        if name not in self.taps:
            return
        t = self.nc.dram_tensor("tap_" + name, [parts] + list(shape), ap.dtype, kind="ExternalOutput").ap()
        self.tapouts[name] = t
        self.P.op("sp", lambda e: e.dma_start(out=t, in_=ap), r=keys, slot="tap_" + name)
        self.final_slots.append("tap_" + name)

    def rms_rstd(self, src_f32, src_keys, sq, tag):
        P = self.P
        for c in range(KC):
            self.act(sq[:, c, :], src_f32[:, c, :], AF.Square, r=[src_keys[c]], w=[("sq", c)])
        b = self.bank()
        for c in range(KC):
            self.mm(self.psb(b), self.ones_bf[:, :], sq[:, c, :], c == 0, c == KC - 1, r=[("sq", c), "ones_bf"], w=[("ps", b)])
        self.act(self.rstd[:, :], self.psb(b), AF.Sqrt, r=[("ps", b), "epsc"], w=["rstd"], scale=1.0 / D, bias=self.epsc[:, 0:1])
        P.op("dve", lambda e: e.reciprocal(out=self.rstd[:, :], in_=self.rstd[:, :]), r=["rstd"], w=["rstd"])

    def build(self):
        nc = bass.Bass("TRN2", target_bir_lowering=False)
        self.nc = nc
        self.final_slots = []
        P = self.P = Prog()
        xin = nc.dram_tensor("x", [128, KC, S], F32, kind="ExternalInput").ap()
        out = nc.dram_tensor("out", [128, KC, S], F32, kind="ExternalOutput").ap()
        xres = nc.dram_tensor("xres", [128, KC, S], F32, kind="Internal").ap()
        self.wstream = nc.dram_tensor("wstream", [NL, 128, self.wtotal], F32, kind="ExternalInput").ap()
        pfd = nc.dram_tensor("pf", [NL, 128, NPF], F32, kind="ExternalInput").ap()
        cfd = nc.dram_tensor("cf", [128, NCF], F32, kind="ExternalInput").ap()
        cbd = nc.dram_tensor("cb", [128, NCB], F32, kind="ExternalInput").ap()
        b31d = nc.dram_tensor("b31", [1, 16 * TB], F32, kind="ExternalInput").ap()
        with contextlib.ExitStack() as st:
            E = st.enter_context
            self.arena = E(nc.sbuf_tensor("arena", [128, ARENA // 2], BF16))
            hT = E(nc.sbuf_tensor("hT", [128, KC, S], BF16))
            merged = E(nc.sbuf_tensor("merged", [128, KC, S], BF16))
            self.ring = E(nc.sbuf_tensor("ring", [128, NRING * RSLOT], BF16))
            pf = E(nc.sbuf_tensor("pfs", [128, NPF], F32))
            self.pf_t = pf
            cf = E(nc.sbuf_tensor("cfs", [128, NCF], F32))
            cb = E(nc.sbuf_tensor("cbs", [128, NCB], BF16))
            self.ones_bf = E(nc.sbuf_tensor("ones_bf", [128, 128], BF16))
            self.rstd = E(nc.sbuf_tensor("rstd", [128, TB], F32))
            self.epsc = E(nc.sbuf_tensor("epsc", [128, 8], F32))
            self.hpi = E(nc.sbuf_tensor("hpi", [128, 8], F32))
            self.ps = E(nc.psum_tensor("ps", [128, 4096], F32))
            ps = self.ps
            ident = cb[:, CB_ID:CB_ID + 128]
            sel4 = cb[:, CB_SEL:CB_SEL + 512].rearrange("p (h q) -> p h q", h=4)
            biasT = cb[:, CB_BIAS:CB_BIAS + 4096].rearrange("p (k h q) -> p k h q", k=2, h=16)
            cmask = cf[:, CF_CMASK:CF_CMASK + 128]
            invc = cf[:, CF_INVC:CF_INVC + 16]
            ones64 = cf[:, CF_ONES:CF_ONES + 64]
            self.cf = cf

            P.op("dve", lambda e: e.memset(self.ones_bf[:, :], 1.0), w=["ones_bf"])
            P.op("dve", lambda e: e.memset(self.epsc[:, :], EPS), w=["epsc"])
            P.op("dve", lambda e: e.memset(self.hpi[:, :], math.pi / 2), w=["hpi"])
            self.dma("sp", cf[:, :], cfd, r=[], w=["cf"], slot="cf")
            P.op("pool", lambda e: e.dma_start(out=cb[:, :], in_=cbd, max_dma_last_dim=8192), w=["cb"], slot="cb")

            for l in range(self.nlayers):
                xsrc = xin if l == 0 else xres
                xdst = out if l == self.nlayers - 1 else xres
                self.layer(l, xsrc, xdst, hT, merged, pf, pfd, ident, sel4, biasT, cmask, invc, ones64, b31d)
                if self.stop is not None:
                    break
            P.barrier()
            P.emit(nc, final_slots=self.final_slots)
        return nc

    def layer(self, l, xsrc, xdst, hT, merged, pf, pfd, ident, sel4, biasT, cmask, invc, ones64, b31d):
        P = self.P
        nc = self.nc
        ps = self.ps
        stop = self.stop
        P.barrier()
        self.bankset = list(range(8))
        self.dma("sp", pf[:, :], pfd[l], r=[], w=["pf"], slot="pf")
        gmp = pf[:, PF_GMP:PF_GMP + 8]
        gmo = pf[:, PF_GMO:PF_GMO + 8]
        gfp = pf[:, PF_GFP:PF_GFP + 8]
        gfo = pf[:, PF_GFO:PF_GFO + 8]
        psc = pf[:, PF_PSC:PF_PSC + 4]

        xblk = self.carve(0, 128, [KC, TB], F32)
        sq = self.carve(16 * 1024, 128, [KC, TB], BF16)
        for n in range(NTB):
            tsl = slice(n * TB, (n + 1) * TB)
            self.dma("sp", xblk[:, :, :], xsrc[:, :, tsl], r=[], w=[("xblk", c) for c in range(KC)], slot="xblk")
            self.rms_rstd(xblk, [("xblk", c) for c in range(KC)], sq, "p1")
            for c in range(KC):
                self.stt(hT[:, c, tsl], xblk[:, c, :], gmp[:, c:c + 1], self.rstd[:, :], ALU.mult, ALU.mult,
                         r=[("xblk", c), "pf", "rstd"], w=[("hT", c, n)])
        self.tap("hT", hT[:, :, :], [("hT", c, n) for c in range(KC) for n in range(NTB)], [KC, S])
        if stop == 1:
            return
        p1done = [("hT", c, NTB - 1) for c in range(KC)]

        kaugT = self.arena[0:65, KV0 // 2: KV0 // 2 + S]
        kiT = self.arena[0:64, KV0 // 2 + S: KV0 // 2 + 2 * S]
        vaug = self.arena[:, KV0 // 2 + 2 * S: KV0 // 2 + 2 * S + 16 * 66].rearrange("p (t d) -> p t d", t=16)
        wi_off = KV0 + 2 * (2 * S + 16 * 66)
        wi_s = self.arena[:, wi_off // 2: wi_off // 2 + 256].bitcast(F32).rearrange("p (t h) -> p t h", t=16)
        assert wi_off + 512 <= ARENA
        uz = self.carve(0, 128, [4, S], BF16)
        upp = [self.carve(16 * 1024, 128, [1, S], F32)[:, 0, :], self.carve(24 * 1024, 128, [1, S], F32)[:, 0, :]]
        dT = self.carve(32 * 1024, 128, [4, S], BF16)
        yp = self.carve(48 * 1024, 128, [4, S], BF16)
        sgt = self.carve(64 * 1024, 128, [1, TB], BF16)[:, 0, :]

        def hkeys(n):
            return [("hT", c, n) for c in range(KC)]

        seg, sk = self.wload(l, "kk", 1024)
        seg = seg.rearrange("p (k m) -> p k m", k=KC)
        P.op("dve", lambda e: e.memset(kaugT[64:65, :], 1.0), w=["kaug1"])
        P.op("dve", lambda e: e.memset(vaug[:, :, 64:65], 1.0), w=["vaug1"])
        for n in range(NTB):
            tsl = slice(n * TB, (n + 1) * TB)
            for half, dst, key in ((0, kaugT, "kT"), (1, kiT, "kiT")):
                b = self.bank()
                for kc in range(KC):
                    self.mm(self.psb(b, 64), seg[:, kc, half * 64:(half + 1) * 64], hT[:, kc, tsl], kc == 0, kc == KC - 1,
                            r=[sk, ("hT", kc, n)], w=[("ps", b)])
                self.act(dst[0:64, tsl], self.psb(b, 64), AF.Copy, r=[("ps", b)], w=[(key, n)])
        seg, sk = self.wload(l, "vw", KC * 72)
        seg = seg.rearrange("p (k m) -> p k m", k=KC)
        for tt_ in range(16):
            n = tt_ // 4
            b = self.bank()
            for kc in range(KC):
                self.mm(ps[:, b * 512: b * 512 + 72], hT[:, kc, tt_ * 128:(tt_ + 1) * 128], seg[:, kc, :], kc == 0, kc == KC - 1,
                        r=[sk, ("hT", kc, n)], w=[("ps", b)])
            self.act(vaug[:, tt_, 0:64], ps[:, b * 512: b * 512 + 64], AF.Copy, r=[("ps", b)], w=[("vaug", tt_)])
            self.act(wi_s[:, tt_, :], ps[:, b * 512 + 64: b * 512 + 72], AF.Copy, r=[("ps", b)], w=[("wi", tt_)], scale=IDX_SCALE)
        for cc in range(4):
            seg, sk = self.wload(l, "su%d" % cc, 1024)
            seg = seg.rearrange("p (k m) -> p k m", k=KC)
            for n in range(NTB):
                tsl = slice(n * TB, (n + 1) * TB)
                b = self.bank()
                for kc in range(KC):
                    self.mm(self.psb(b), seg[:, kc, :], hT[:, kc, tsl], kc == 0, kc == KC - 1, r=[sk, ("hT", kc, n)], w=[("ps", b)])
                self.act(uz[:, cc, tsl], self.psb(b), AF.Copy, r=[("ps", b)] + p1done, w=[("uz", cc, n)])
        for cc in range(4):
            seg, sk = self.wload(l, "pu%d" % cc, 1024)
            seg = seg.rearrange("p (k m) -> p k m", k=KC)
            u0 = upp[0]
            for n in range(NTB):
                tsl = slice(n * TB, (n + 1) * TB)
                b = self.bank()
                for kc in range(KC):
                    self.mm(self.psb(b), seg[:, kc, :], hT[:, kc, tsl], kc == 0, kc == KC - 1, r=[sk, ("hT", kc, n)], w=[("ps", b)])
                self.act(u0[:, tsl], self.psb(b), AF.Copy, r=[("ps", b)] + p1done, w=["up0"])
            wlen = 2 ** (cc + 1)
            sA = upp[1]
            sB = self.carve(66 * 1024, 128, [1, S], F32)[:, 0, :]
            bufs = [(sA, "upA"), (sB, "upB")]
            k = 1
            bi = 0
            src, srck = u0, "up0"
            while k < wlen:
                dstb, dstk = bufs[bi % 2]
                self.tt(dstb[:, k:], src[:, k:], src[:, :S - k], ALU.add, r=[srck], w=[dstk])
                P.op("dve", lambda e, d=dstb, s_=src, k=k: e.tensor_copy(out=d[:, 0:k], in_=s_[:, 0:k]), r=[srck], w=[dstk])
                src, srck = dstb, dstk
                bi += 1
                k *= 2
            self.stt(dT[:, cc, :], src[:, :], 1.0 / wlen, u0[:, :], ALU.mult, ALU.subtract, r=[srck, "up0"], w=[("dT", cc)])
            tmpc = self.carve(64 * 1024 + 1024, 128, [1, 16], F32)[:, 0, :]
            self.tt(tmpc[:, 0:wlen - 1], src[:, 0:wlen - 1], invc[:, 0:wlen - 1], ALU.mult, r=[srck, "cf"], w=["tmpc"])
            self.tt(dT[:, cc, 0:wlen - 1], tmpc[:, 0:wlen - 1], u0[:, 0:wlen - 1], ALU.subtract, r=["tmpc", "up0", ("dT", cc)], w=[("dT", cc)])
        self.tap("dT", dT[:, :, :], [("dT", cc) for cc in range(4)], [4, S])
        self.tap("kT", kaugT[:, :], [("kT", n) for n in range(NTB)] + ["kaug1"], [S], parts=65)
        self.tap("vaug", vaug[:, :, :], [("vaug", t) for t in range(16)] + ["vaug1"], [16, 66])
        self.tap("wi", wi_s[:, :, :], [("wi", t) for t in range(16)], [16, 8])
        if stop == 2:
            return

        seg, sk = self.wload(l, "mix", 512)
        seg = seg.rearrange("p (g m) -> p g m", g=4)
        for cc in range(4):
            for n in range(NTB):
                tsl = slice(n * TB, (n + 1) * TB)
                b = self.bank()
                self.mm(self.psb(b), seg[:, cc, :], dT[:, cc, tsl], True, True, r=[sk, ("dT", cc)], w=[("ps", b)])
                self.act(yp[:, cc, tsl], self.psb(b), AF.Copy, r=[("ps", b), "pf"], w=[("yp", cc, n)], scale=psc[:, cc:cc + 1])
        for c in range(KC):
            segp, skp = self.wload(l, "po%d" % c, 512)
            segp = segp.rearrange("p (k m) -> p k m", k=4)
            segg, skg = self.wload(l, "g0_%d" % c, 1024)
            segg = segg.rearrange("p (k m) -> p k m", k=KC)
            for n in range(NTB):
                tsl = slice(n * TB, (n + 1) * TB)
                by = self.bank()
                for cc in range(4):
                    self.mm(self.psb(by), segp[:, cc, :], yp[:, cc, tsl], cc == 0, cc == 3, r=[skp, ("yp", cc, n)], w=[("ps", by)])
                bg = self.bank()
                for kc in range(KC):
                    self.mm(self.psb(bg), segg[:, kc, :], hT[:, kc, tsl], kc == 0, kc == KC - 1, r=[skg, ("hT", kc, n)], w=[("ps", bg)])
                self.act(sgt[:, :], self.psb(bg), AF.Sigmoid, r=[("ps", bg)], w=["sgt"])
                self.tt(merged[:, c, tsl], sgt[:, :], self.psb(by), ALU.mult, r=["sgt", ("ps", by)], w=[("mg", c, n)])
        self.tap("mg3", merged[:, :, S - 256:S], [("mg", c, n) for c in range(KC) for n in range(NTB)], [KC, 256])
        if stop == 3:
            return
        P.barrier()

        self.phase_s5(l, hT, merged, pf, uz)
        self.tap("mg4", merged[:, :, S - 256:S], [("mg", c, n) for c in range(KC) for n in range(NTB)], [KC, 256])
        if stop == 4:
            return
        P.barrier()

        self.phase_attn(l, hT, merged, kaugT, kiT, vaug, wi_s, ident, sel4, biasT, cmask, ones64, b31d)
        self.tap("mg5", merged[:, :, S - 256:S], [("mg", c, n) for c in range(KC) for n in range(NTB)], [KC, 256])
        if stop == 5:
            return
        P.barrier()

        self.bankset = list(range(8))
        xT = self.carve(0, 128, [KC, S], F32)
        sq = hT[:, 4:6, :].rearrange("p a b -> p (a b)").rearrange("p (c t) -> p c t", c=KC)
        xblk = hT[:, 0:4, :].rearrange("p a b -> p (a b)").bitcast(F32).rearrange("p (c t) -> p c t", c=KC)
        tmpf = self.carve(64 * 1024, 128, [1, TB], F32)[:, 0, :]
        for c in range(KC):
            seg, sk = self.wload(l, "wo%d" % c, 1024)
            seg = seg.rearrange("p (k m) -> p k m", k=KC)
            for n in range(NTB):
                tsl = slice(n * TB, (n + 1) * TB)
                b = self.bank()
                for kc in range(KC):
                    self.mm(self.psb(b), seg[:, kc, :], merged[:, kc, tsl], kc == 0, kc == KC - 1, r=[sk, ("mg", kc, n)], w=[("ps", b)])
                self.act(xT[:, c, tsl], self.psb(b), AF.Copy, r=[("ps", b)], w=[("xT", c, n)])
        for n in range(NTB):
            tsl = slice(n * TB, (n + 1) * TB)
            self.dma("sp", xblk[:, :, :], xsrc[:, :, tsl], r=[], w=[("xblk", c) for c in range(KC)], slot="xblk")
            self.rms_rstd(xT[:, :, tsl], [("xT", c, n) for c in range(KC)], sq, "p6")
            for c in range(KC):
                self.stt(tmpf[:, :], xT[:, c, tsl], gmo[:, c:c + 1], self.rstd[:, :], ALU.mult, ALU.mult,
                         r=[("xT", c, n), "pf", "rstd"], w=["tmpf"])
                self.tt(xT[:, c, tsl], tmpf[:, :], xblk[:, c, :], ALU.add, r=["tmpf", ("xblk", c)], w=[("xT", c, n)])
        self.tap("x6", xT[:, :, S - 256:S], [("xT", c, n) for c in range(KC) for n in range(NTB)], [KC, 256])
        if stop == 6:
            return
        P.barrier()

        fT = self.carve(64 * 1024, 128, [NJ, TB], BF16)
        sq = merged[:, 4:6, :].rearrange("p a b -> p (a b)").rearrange("p (c t) -> p c t", c=KC)
        mbuf = merged[:, 0:4, :].rearrange("p a b -> p (a b)").bitcast(F32).rearrange("p (c t) -> p c t", c=KC)
        sgf = merged[:, 6, 0:TB]
        tmpf = merged[:, 7, 0:2 * TB].bitcast(F32)
        def prenorm(n):
            tsl = slice(n * TB, (n + 1) * TB)
            self.rms_rstd(xT[:, :, tsl], [("xT", c, n) for c in range(KC)], sq, "p7a")
            for c in range(KC):
                self.stt(hT[:, c, tsl], xT[:, c, tsl], gfp[:, c:c + 1], self.rstd[:, :], ALU.mult, ALU.mult,
                         r=[("xT", c, n), "pf", "rstd"], w=[("hT", c, n)])

        prenorm(0)
        for n in range(NTB):
            tsl = slice(n * TB, (n + 1) * TB)
            for j in range(NJ):
                seg, sk = self.wload(l, "f%d" % j, 2048)
                seg = seg.rearrange("p (k m) -> p k m", k=KC)
                bg = self.bank()
                for kc in range(KC):
                    self.mm(self.psb(bg), seg[:, kc, 0:128], hT[:, kc, tsl], kc == 0, kc == KC - 1, r=[sk, ("hT", kc, n)], w=[("ps", bg)])
                bu = self.bank()
                for kc in range(KC):
                    self.mm(self.psb(bu), seg[:, kc, 128:256], hT[:, kc, tsl], kc == 0, kc == KC - 1, r=[sk, ("hT", kc, n)], w=[("ps", bu)])
                self.act(sgf, self.psb(bg), AF.Silu, r=[("ps", bg)], w=["sgf"])
                self.tt(fT[:, j, :], sgf, self.psb(bu), ALU.mult, r=["sgf", ("ps", bu)], w=[("fT", j)])
            if n + 1 < NTB:
                prenorm(n + 1)
            for c in range(KC):
                seg, sk = self.wload(l, "fo%d" % c, NJ * 128)
                seg = seg.rearrange("p (k m) -> p k m", k=NJ)
                b = self.bank()
                for j in range(NJ):
                    self.mm(self.psb(b), seg[:, j, :], fT[:, j, :], j == 0, j == NJ - 1, r=[sk, ("fT", j)], w=[("ps", b)])
                self.act(mbuf[:, c, :], self.psb(b), AF.Copy, r=[("ps", b)], w=[("mbuf", c)])
            self.rms_rstd(mbuf, [("mbuf", c) for c in range(KC)], sq, "p7b")
            for c in range(KC):
                self.stt(tmpf, mbuf[:, c, :], gfo[:, c:c + 1], self.rstd[:, :], ALU.mult, ALU.mult,
                         r=[("mbuf", c), "pf", "rstd"], w=["tmpf"])
                self.tt(mbuf[:, c, :], tmpf, xT[:, c, tsl], ALU.add, r=["tmpf", ("xT", c, n)], w=[("mbuf", c)])
            self.dma("sp", xdst[:, :, tsl], mbuf[:, :, :], r=[("mbuf", c) for c in range(KC)], w=[], slot="xout")
        if "xout" not in self.final_slots:
            self.final_slots.append("xout")

    def phase_s5(self, l, hT, merged, pf, uz):
        P = self.P
        K = 1024
        Ec = self.carve(16 * K, 128, [1, S], F32)[:, 0, :]
        Es = self.carve(24 * K, 128, [1, S], F32)[:, 0, :]
        vre = self.carve(32 * K, 128, [1, S], F32)[:, 0, :]
        vim = self.carve(40 * K, 128, [1, S], F32)[:, 0, :]
        xre = self.carve(48 * K, 128, [1, S], BF16)[:, 0, :]
        xim = self.carve(52 * K, 128, [1, S], BF16)[:, 0, :]
        tmp1 = self.carve(56 * K, 128, [1, TB], F32)[:, 0, :]
        tmp2 = self.carve(58 * K, 128, [1, TB], F32)[:, 0, :]
        Bre = self.carve(60 * K, 128, [2, 4, 128], BF16)
        Bim = self.carve(62 * K, 128, [2, 4, 128], BF16)
        gt = self.carve(71 * K, 128, [1, TB], F32)[:, 0, :]
        sm = self.carve(64 * K, 128, [16, 16], F32)
        s1 = self.carve(66 * K, 128, [1, TB], BF16)[:, 0, :]
        s2 = self.carve(67 * K, 128, [1, TB], BF16)[:, 0, :]
        t3 = self.carve(68 * K, 128, [1, TB], F32)[:, 0, :]
        xw = self.carve(32 * K, 128, [8, 512], F32)
        smi = self.carve(70 * K, 128, [1, 16], I32)[:, 0, :]
        TWO_PI = 2.0 * math.pi

        def pfx(o):
            return pf[:, o:o + 512]

        def dve(fn, r, w):
            P.op("dve", fn, r=r, w=w)

        def zoh(lr, li, ld, wk, n, tag, itile):
            dt_, lrdt, th, kf, q, sn, cs, t0 = wk[:8]
            kk = ["z%s%d" % (tag, i) for i in range(8)]
            kdt, klrdt, kth, kkf, kq, ksn, kcs, kt0 = kk
            ki = "z%si" % tag
            self.act(dt_, ld, AF.Exp, r=["pf"], w=[kdt])
            self.tt(lrdt, lr, dt_, ALU.mult, r=["pf", kdt], w=[klrdt])
            self.tt(th, li, dt_, ALU.mult, r=["pf", kdt], w=[kth])
            self.ts(kf, th, 1.0 / TWO_PI, None, ALU.mult, ALU.bypass, r=[kth], w=[kkf])
            dve(lambda e: e.tensor_copy(out=itile, in_=kf), r=[kkf], w=[ki])
            dve(lambda e: e.tensor_copy(out=kf, in_=itile), r=[ki], w=[kkf])
            self.stt(q, kf, -TWO_PI, th, ALU.mult, ALU.add, r=[kkf, kth], w=[kq])
            self.act(sn, q, AF.Sin, r=[kq], w=[ksn], scale=0.25)
            self.act(cs, q, AF.Sin, r=[kq, "hpi"], w=[kcs], scale=0.25, bias=self.hpi[:, 0:1])
            for it in range(2):
                self.tt(t0, sn, sn, ALU.mult, r=[ksn], w=[kt0])
                self.stt(sn, sn, 2.0, cs, ALU.mult, ALU.mult, r=[ksn, kcs], w=[ksn])
                self.ts(cs, t0, -2.0, 1.0, ALU.mult, ALU.add, r=[kt0], w=[kcs])
            self.act(dt_, lrdt, AF.Exp, r=[klrdt], w=[kdt])
            return dict(mag=dt_, cos=cs, sin=sn, keys=[kdt, kcs, ksn], k=kk)

        wkx = [xw[:, i, :] for i in range(8)]
        smx = self.carve(56 * K, 128, [1, 512], I32)[:, 0, :]
        zx = zoh(pfx(PF_LRX), pfx(PF_LIX), pfx(PF_LDX), wkx, 512, "x", smx)
        K_ = zx["k"]
        mg_, cs_x, sn_x = wkx[0], wkx[6], wkx[5]
        are, aim, den, cre, cim = wkx[1], wkx[2], wkx[3], wkx[4], wkx[7]
        kare, kaim, kden, kcre, kcim = K_[1], K_[2], K_[3], K_[4], K_[7]
        kmg, kcs, ksn = K_[0], K_[6], K_[5]
        lr, li = pfx(PF_LRX), pfx(PF_LIX)
        self.tt(are, mg_, cs_x, ALU.mult, r=[kmg, kcs], w=[kare])
        self.tt(aim, mg_, sn_x, ALU.mult, r=[kmg, ksn], w=[kaim])
        self.ts(are, are, -1.0, None, ALU.add, ALU.bypass, r=[kare], w=[kare])
        t0, kt0 = wkx[0], K_[0]
        t1, kt1, t2, kt2 = wkx[5], K_[5], wkx[6], K_[6]
        self.tt(den, lr, lr, ALU.mult, r=["pf"], w=[kden])
        self.tt(cre, li, li, ALU.mult, r=["pf"], w=[kcre])
        self.tt(den, den, cre, ALU.add, r=[kden, kcre], w=[kden])
        dve(lambda e: e.reciprocal(out=den, in_=den), r=[kden], w=[kden])
        self.tt(cre, are, lr, ALU.mult, r=[kare, "pf"], w=[kcre])
        self.tt(t0, aim, li, ALU.mult, r=[kaim, "pf"], w=[kt0])
        self.tt(cre, cre, t0, ALU.add, r=[kcre, kt0], w=[kcre])
        self.tt(cre, cre, den, ALU.mult, r=[kcre, kden], w=[kcre])
        self.tt(cim, aim, lr, ALU.mult, r=[kaim, "pf"], w=[kcim])
        self.tt(t0, are, li, ALU.mult, r=[kare, "pf"], w=[kt0])
        self.tt(cim, cim, t0, ALU.subtract, r=[kcim, kt0], w=[kcim])
        self.tt(cim, cim, den, ALU.mult, r=[kcim, kden], w=[kcim])
        br, bi = pfx(PF_BRX), pfx(PF_BIX)
        self.tt(t1, cre, br, ALU.mult, r=[kcre, "pf"], w=[kt1])
        self.tt(t2, cim, bi, ALU.mult, r=[kcim, "pf"], w=[kt2])
        self.tt(t1, t1, t2, ALU.subtract, r=[kt1, kt2], w=[kt1])
        for v in range(2):
            self.ts(Bre[:, v, :, :].rearrange("p a b -> p (a b)"), t1, self.cf[:, CF_PAR + v:CF_PAR + v + 1], None, ALU.mult, ALU.bypass,
                    r=[kt1, "cf"], w=["Bre"])
        self.tt(t1, cre, bi, ALU.mult, r=[kcre, "pf"], w=[kt1])
        self.tt(t2, cim, br, ALU.mult, r=[kcim, "pf"], w=[kt2])
        self.tt(t1, t1, t2, ALU.add, r=[kt1, kt2], w=[kt1])
        for v in range(2):
            self.ts(Bim[:, v, :, :].rearrange("p a b -> p (a b)"), t1, self.cf[:, CF_PAR + v:CF_PAR + v + 1], None, ALU.mult, ALU.bypass,
                    r=[kt1, "cf"], w=["Bim"])

        wks = [sm[:, i, :] for i in range(8)]
        zs = zoh(pf[:, PF_LRS:PF_LRS + 16], pf[:, PF_LIS:PF_LIS + 16], pf[:, PF_LDS:PF_LDS + 16], wks, 16, "s", smi)
        mag, cth, sth = zs["mag"], zs["cos"], zs["sin"]
        nsc = sm[:, 8, :]

        segC, skC = self.wload(l, "sC", 4096)
        Cre = segC[:, 0:2048].rearrange("p (j m) -> p j m", j=16)
        Cim = segC[:, 2048:4096].rearrange("p (j m) -> p j m", j=16)
        segD, skD = self.wload(l, "sD", 512)
        Dg = segD.rearrange("p (c m) -> p c m", c=4)

        self.bankset = [0, 1, 2, 3]
        Ec2 = [self.carve((16 + 4 * i) * K, 128, [1, TB], F32)[:, 0, :] for i in range(2)]
        Es2 = [self.carve((18 + 4 * i) * K, 128, [1, TB], F32)[:, 0, :] for i in range(2)]
        vslot_re = [self.carve((32 + 4 * i) * K, 128, [1, TB], F32)[:, 0, :] for i in range(4)]
        vslot_im = [self.carve((34 + 4 * i) * K, 128, [1, TB], F32)[:, 0, :] for i in range(4)]
        pt4 = [self.carve(o * K, 128, [1, TB], F32)[:, 0, :] for o in (73, 24, 27, 29)]
        car = self.carve(26 * K, 128, [1, 16], F32)[:, 0, :]
        vi = 0
        pending = None
        ydefer = []
        for cc in range(4):
            ybanks = [4, 5, 6, 7]
            for jj in range(4):
                j = cc * 4 + jj
                rows = slice(64 * (jj // 2), 64 * (jj // 2) + 64)
                pv = jj % 2
                Ec, Es = Ec2[j % 2], Es2[j % 2]
                ke, ks = ("Ec", j % 2), ("Es", j % 2)
                dve(lambda e, j=j, Ec=Ec: e.tensor_copy(out=Ec[:, 0:1], in_=cth[:, j:j + 1]), r=zs["keys"], w=[ke])
                dve(lambda e, j=j, Es=Es: e.tensor_copy(out=Es[:, 0:1], in_=sth[:, j:j + 1]), r=zs["keys"], w=[ks])
                nn = 1
                lev = 0
                while nn < TB:
                    cs_ = Ec[:, nn - 1:nn]
                    ss_ = Es[:, nn - 1:nn]
                    ns_ = nsc[:, lev:lev + 1]
                    self.ts(ns_, ss_, -1.0, None, ALU.mult, ALU.bypass, r=[ks], w=["nsc"])
                    self.ts(Ec[:, nn:2 * nn], Ec[:, 0:nn], cs_, None, ALU.mult, ALU.bypass, r=[ke], w=[ke])
                    self.stt(Ec[:, nn:2 * nn], Es[:, 0:nn], ns_, Ec[:, nn:2 * nn], ALU.mult, ALU.add, r=[ks, "nsc", ke], w=[ke])
                    self.ts(Es[:, nn:2 * nn], Es[:, 0:nn], cs_, None, ALU.mult, ALU.bypass, r=[ks, ke], w=[ks])
                    self.stt(Es[:, nn:2 * nn], Ec[:, 0:nn], ss_, Es[:, nn:2 * nn], ALU.mult, ALU.add, r=[ke, ks], w=[ks])
                    nn *= 2
                    lev += 1
                cl, sl_, nsl = Ec[:, TB - 1:TB], Es[:, TB - 1:TB], nsc[:, 12:13]
                self.ts(nsl, sl_, -1.0, None, ALU.mult, ALU.bypass, r=[ks], w=["nsl"])
                for n in range(NTB):
                    tsl = slice(n * TB, (n + 1) * TB)
                    vre, vim = vslot_re[vi % 4], vslot_im[vi % 4]
                    kvr, kvi = ("vre", vi % 4), ("vim", vi % 4)
                    vi += 1
                    b1 = self.bank()
                    self.mm(self.psb(b1), Bre[rows, pv, cc, :], uz[rows, cc, tsl], True, True, r=["Bre", ("uz", cc, n)], w=[("ps", b1)])
                    b2 = self.bank()
                    self.mm(self.psb(b2), Bim[rows, pv, cc, :], uz[rows, cc, tsl], True, True, r=["Bim", ("uz", cc, n)], w=[("ps", b2)])
                    self.tt(vre, Ec, self.psb(b1), ALU.mult, r=[ke, ks, ("ps", b1)], w=[kvr])
                    self.tt(tmp1, Es, self.psb(b2), ALU.mult, r=[ks, ("ps", b2)], w=["tmp1"])
                    self.tt(vre, vre, tmp1, ALU.add, r=[kvr, "tmp1"], w=[kvr])
                    self.tt(vim, Ec, self.psb(b2), ALU.mult, r=[ke, ("ps", b2)], w=[kvi])
                    self.tt(tmp2, Es, self.psb(b1), ALU.mult, r=[ks, ("ps", b1)], w=["tmp2"])
                    self.tt(vim, vim, tmp2, ALU.subtract, r=[kvi, "tmp2"], w=[kvi])
                    if n == 0:
                        dve(lambda e, j=j, vre=vre: e.tensor_tensor_scan(out=vre, data0=mag[:, j:j + 1].to_broadcast([128, TB]), data1=vre,
                                                                        initial=0.0, op0=ALU.mult, op1=ALU.add), r=[kvr] + zs["keys"], w=[kvr])
                        dve(lambda e, j=j, vim=vim: e.tensor_tensor_scan(out=vim, data0=mag[:, j:j + 1].to_broadcast([128, TB]), data1=vim,
                                                                        initial=0.0, op0=ALU.mult, op1=ALU.add), r=[kvi] + zs["keys"], w=[kvi])
                    else:
                        dve(lambda e, j=j, vre=vre: e.tensor_tensor_scan(out=vre, data0=mag[:, j:j + 1].to_broadcast([128, TB]), data1=vre,
                                                                        initial=car[:, 0:1], op0=ALU.mult, op1=ALU.add), r=[kvr, "car"] + zs["keys"], w=[kvr])
                        dve(lambda e, j=j, vim=vim: e.tensor_tensor_scan(out=vim, data0=mag[:, j:j + 1].to_broadcast([128, TB]), data1=vim,
                                                                        initial=car[:, 1:2], op0=ALU.mult, op1=ALU.add), r=[kvi, "car"] + zs["keys"], w=[kvi])
                    if n < NTB - 1:
                        wr_l, wi_l = vre[:, TB - 1:TB], vim[:, TB - 1:TB]
                        self.ts(car[:, 2:3], wr_l, cl, None, ALU.mult, ALU.bypass, r=[kvr, ke], w=["car2"])
                        self.ts(car[:, 3:4], wr_l, sl_, None, ALU.mult, ALU.bypass, r=[kvr, ks], w=["car3"])
                        self.stt(car[:, 0:1], wi_l, nsl, car[:, 2:3], ALU.mult, ALU.add, r=[kvi, "nsl", "car2"], w=["car"])
                        self.stt(car[:, 1:2], wi_l, cl, car[:, 3:4], ALU.mult, ALU.add, r=[kvi, ke, "car3", "car"], w=["car"])
                    def ptt(out, in0, in1, op, r, w):
                        P.op("pool", lambda e: e.tensor_tensor(out=out, in0=in0, in1=in1, op=op), r=r, w=w)
                    yb = ybanks[n]
                    ptt(pt4[0], Ec, vre, ALU.mult, [ke, kvr], ["pt0"])
                    ptt(pt4[1], Es, vim, ALU.mult, [ks, kvi], ["pt1"])
                    if pending is not None:
                        pending()
                        pending = None
                    ptt(pt4[2], Es, vre, ALU.mult, [ks, kvr], ["pt2"])
                    ptt(pt4[3], Ec, vim, ALU.mult, [ke, kvi], ["pt3"])
                    ptt(xre[:, tsl], pt4[0], pt4[1], ALU.subtract, ["pt0", "pt1"], [("xre", n)])
                    ptt(pt4[2], pt4[2], pt4[3], ALU.add, ["pt2", "pt3"], ["pt2"])

                    def ymm(j=j, jj=jj, cc=cc, n=n, tsl=tsl, yb=yb):
                        self.mm(self.psb(yb), Cre[:, j, :], xre[:, tsl], jj == 0, False, r=[skC, ("xre", n)], w=[("ps", yb)])
                        self.mm(self.psb(yb), Cim[:, j, :], xim[:, tsl], False, False, r=[skC, ("xim", n)], w=[("ps", yb)])
                        if jj == 3:
                            self.mm(self.psb(yb), Dg[:, cc, :], uz[:, cc, tsl], False, True, r=[skD, ("uz", cc, n)], w=[("ps", yb)])

                    def fin(n=n, tsl=tsl):
                        xo = xim[:, tsl]
                        P.op("pool", lambda e: e.tensor_scalar(out=xo, in0=pt4[2], scalar1=-1.0, scalar2=0.0, op0=ALU.mult, op1=ALU.add),
                             r=["pt2"], w=[("xim", n)])
                    pending = fin
                    ydefer.append(ymm)
                    while len(ydefer) > 3:
                        ydefer.pop(0)()
                pending()
                pending = None
            while ydefer:
                ydefer.pop(0)()
            for n in range(NTB):
                tsl = slice(n * TB, (n + 1) * TB)
                yb = ybanks[n]
                self.act(gt, self.psb(yb), AF.Square, r=[("ps", yb)], w=["gt"])
                self.ts(gt, gt, 0.044715, 1.0, ALU.mult, ALU.add, r=["gt"], w=["gt"])
                self.tt(gt, gt, self.psb(yb), ALU.mult, r=["gt", ("ps", yb)], w=["gt"])
                self.act(t3, gt, AF.Sigmoid, r=["gt"], w=["t3"], scale=1.5957691216057308)
                self.tt(uz[:, cc, tsl], t3, self.psb(yb), ALU.mult, r=["t3", ("ps", yb)], w=[("uz", cc, n)])
        self.tap("zT", uz[:, :, :], [("uz", cc, n) for cc in range(4) for n in range(NTB)], [4, S])
        self.bankset = list(range(8))
        for c in range(KC):
            segl, skl = self.wload(l, "glu%d" % c, 1024)
            segl = segl.rearrange("p (a k m) -> p a k m", a=2, k=4)
            segg, skg = self.wload(l, "g2_%d" % c, 1024)
            segg = segg.rearrange("p (k m) -> p k m", k=KC)
            for n in range(NTB):
                tsl = slice(n * TB, (n + 1) * TB)
                ba = self.bank()
                for cc in range(4):
                    self.mm(self.psb(ba), segl[:, 0, cc, :], uz[:, cc, tsl], cc == 0, cc == 3, r=[skl, ("uz", cc, n)], w=[("ps", ba)])
                bb = self.bank()
                for cc in range(4):
                    self.mm(self.psb(bb), segl[:, 1, cc, :], uz[:, cc, tsl], cc == 0, cc == 3, r=[skl, ("uz", cc, n)], w=[("ps", bb)])
                bg = self.bank()
                for kc in range(KC):
                    self.mm(self.psb(bg), segg[:, kc, :], hT[:, kc, tsl], kc == 0, kc == KC - 1, r=[skg, ("hT", kc, n)], w=[("ps", bg)])
                self.act(s1, self.psb(bb), AF.Sigmoid, r=[("ps", bb)], w=["s1"])
                self.act(s2, self.psb(bg), AF.Sigmoid, r=[("ps", bg)], w=["s2"])
                self.tt(t3, s1, self.psb(ba), ALU.mult, r=["s1", ("ps", ba)], w=["t3"])
                self.tt(t3, t3, s2, ALU.mult, r=["t3", "s2"], w=["t3"])
                self.tt(merged[:, c, tsl], merged[:, c, tsl], t3, ALU.add, r=[("mg", c, n), "t3"], w=[("mg", c, n)])

    def phase_attn(self, l, hT, merged, kaugT, kiT, vaug, wi_s, ident, sel4, biasT, cmask, ones64, b31d):
        P = self.P
        K = 1024
        ps = self.ps
        qT = self.arena[0:65, 0: 16 * TB].rearrange("p (h q) -> p h q", h=16)
        qiT = self.arena[0:64, 8 * K: 8 * K + 8 * TB].rearrange("p (h q) -> p h q", h=8)
        OTn = self.arena[0:64, 12 * K: 12 * K + 16 * TB].rearrange("p (h q) -> p h q", h=16)
        score2 = [self.carve(40 * K, 128, [1, S], F32)[:, 0, :], self.pf_t[:, PF_LRX:PF_LRX + S]]
        rl = [self.carve(48 * K, 128, [1, TB], F32)[:, 0, :], self.carve(50 * K, 128, [1, TB], F32)[:, 0, :]]
        nm3 = [self.carve((52 + 4 * i) * K, 128, [1, S], BF16)[:, 0, :] for i in range(3)]
        self.idx_i = 0
        PT = [self.carve(64 * K, 128, [1, 1024], BF16)[:, 0, :], self.carve(66 * K, 128, [1, 1024], BF16)[:, 0, :]]
        ot = self.carve(68 * K, 64, [1, 1024], F32)[:, 0, :]
        aotmp = self.carve(60 * K, 128, [1, TB], F32)[:, 0, :]
        bis = self.carve(72 * K, 128, [1, 16], F32)[:, 0, :]
        sgt = self.carve(73 * K, 128, [1, TB], BF16)[:, 0, :]
        lnr = self.arena[64:65, 12 * K: 12 * K + 2048].bitcast(F32)
        rrow = self.arena[64:65, 14 * K: 14 * K + 1024]
        P.op("pool", lambda e: e.dma_start(out=self.arena[64:65, 0:16 * TB], in_=b31d, max_dma_last_dim=8192), w=["qT64"], slot="b31")

        def emit_ao(nn):
            tsl = slice(nn * TB, (nn + 1) * TB)
            self.bankset = [0, 1, 2, 3, 4, 5]
            for c in range(KC):
                sego, sko = self.wload(l, "ao%d" % c, 2048)
                sego = sego.rearrange("p (h m) -> p h m", h=16)
                segg, skg = self.wload(l, "g1_%d" % c, 1024)
                segg = segg.rearrange("p (k m) -> p k m", k=KC)
                by = self.bank()
                for h in range(16):
                    self.mm(self.psb(by), sego[0:64, h, :], OTn[:, h, :], h == 0, h == 15, r=[sko] + [("OTn", i) for i in range(4)], w=[("ps", by)])
                bg = self.bank()
                for kc in range(KC):
                    self.mm(self.psb(bg), segg[:, kc, :], hT[:, kc, tsl], kc == 0, kc == KC - 1, r=[skg, ("hT", kc, nn)], w=[("ps", bg)])
                self.act(sgt, self.psb(bg), AF.Sigmoid, r=[("ps", bg)], w=["sgt"])
                t3 = aotmp
                self.tt(t3, sgt, self.psb(by), ALU.mult, r=["sgt", ("ps", by)], w=["aotmp"])
                self.tt(merged[:, c, tsl], merged[:, c, tsl], t3, ALU.add, r=[("mg", c, nn), "aotmp"], w=[("mg", c, nn)])
                yield

        for n in range(NTB):
            tsl = slice(n * TB, (n + 1) * TB)
            self.bankset = list(range(8))
            for hp in range(8):
                seg, sk = self.wload(l, "q%d" % hp, 1024)
                seg = seg.rearrange("p (k m) -> p k m", k=KC)
                for half in range(2):
                    h = 2 * hp + half
                    b = self.bank()
                    for kc in range(KC):
                        self.mm(self.psb(b, 64), seg[:, kc, half * 64:(half + 1) * 64], hT[:, kc, tsl], kc == 0, kc == KC - 1,
                                r=[sk, ("hT", kc, n)], w=[("ps", b)])
                    self.act(qT[0:64, h, :], self.psb(b, 64), AF.Copy, r=[("ps", b)], w=[("qT", h)], scale=0.125)
            for hp in range(4):
                seg, sk = self.wload(l, "qi%d" % hp, 1024)
                seg = seg.rearrange("p (k m) -> p k m", k=KC)
                for half in range(2):
                    h = 2 * hp + half
                    b = self.bank()
                    for kc in range(KC):
                        self.mm(self.psb(b, 64), seg[:, kc, half * 64:(half + 1) * 64], hT[:, kc, tsl], kc == 0, kc == KC - 1,
                                r=[sk, ("hT", kc, n)], w=[("ps", b)])
                    self.act(qiT[0:64, h, :], self.psb(b, 64), AF.Copy, r=[("ps", b)], w=[("qiT", h)])
            if n == 0:
                self.tap("qT", qT[:, :, :], [("qT", h) for h in range(16)] + ["qT64"], [16, TB], parts=65)

            def genA(qq):
                qb = 4 * n + qq
                qsl = slice(qq * 128, (qq + 1) * 128)
                L = (qb + 1) * 128
                sc = score2[qb % 2]
                ngr = (L + 511) // 512
                for kg in range(ngr):
                    k0 = kg * 512
                    nk = min(512, L - k0)
                    sk_ = ("sc", qb % 2, kg)
                    for h in range(8):
                        b = 6 + (self.idx_i % 2)
                        r_ = rl[self.idx_i % 2]
                        rk = ("rl", self.idx_i % 2)
                        self.idx_i += 1
                        self.mm(self.psb(b, 128, nk), qiT[0:64, h, qsl], kiT[0:64, k0:k0 + nk], True, True,
                                r=[("qiT", h)] + [("kiT", i) for i in range(NTB)], w=[("ps", b)])
                        self.act(r_[:, 0:nk], self.psb(b, 128, nk), AF.Relu, r=[("ps", b)], w=[rk])
                        wcol = wi_s[:, qb, h:h + 1]
                        wk = [("wi", qb)]
                        if h == 0:
                            ndiag = nk - 128 if (k0 + nk == L) else nk
                            if ndiag > 0:
                                self.ts(sc[:, k0:k0 + ndiag], r_[:, 0:ndiag], wcol, None, ALU.mult, ALU.bypass, r=[rk] + wk, w=[sk_])
                            if k0 + nk == L:
                                self.stt(sc[:, L - 128:L], r_[:, nk - 128:nk], wcol, cmask, ALU.mult, ALU.add, r=[rk, "cf"] + wk, w=[sk_])
                        else:
                            self.stt(sc[:, k0:k0 + nk], r_[:, 0:nk], wcol, sc[:, k0:k0 + nk], ALU.mult, ALU.add,
                                     r=[rk, sk_] + wk, w=[sk_])
                        yield

            def genB(qq):
                qb = 4 * n + qq
                L = (qb + 1) * 128
                sc = score2[qb % 2]
                nmb = nm3[qb % 2]
                nmk = ("nm", qb % 2)
                ngr = (L + 511) // 512
                sck = [("sc", qb % 2, kg) for kg in range(ngr)]
                if qb >= 2:
                    o = 8 * (qb % 2)
                    cA, cB, cnt, tmpb, thr = bis[:, o:o + 1], bis[:, o + 1:o + 2], bis[:, o + 2:o + 3], bis[:, o + 3:o + 4], bis[:, o + 4:o + 5]
                    kp = "b%d" % (qb % 2)
                    P.op("dve", lambda e, cA=cA: e.memset(cA, 0.0), w=[kp + "c0"])
                    cur, nxt = cA, cB
                    curk, nxtk = kp + "c0", kp + "c1"
                    step = 4.0
                    for it in range(NBIS):
                        self.ts(nmb[:, 0:L], sc[:, 0:L], cur, None, ALU.is_ge, ALU.add, r=sck + [curk], w=[nmk, kp + "cnt"], accum_out=cnt)
                        self.ts(tmpb, cnt, 256.0, 2.0 * step, ALU.is_ge, ALU.mult, r=[kp + "cnt"], w=[kp + "tmpb"])
                        self.ts(nxt, tmpb, -step, cur, ALU.add, ALU.add, r=[kp + "tmpb", curk], w=[nxtk])
                        cur, nxt = nxt, cur
                        curk, nxtk = nxtk, curk
                        step *= 0.5
                        yield
                    self.ts(thr, cur, -2.0 * step - 1e-5, None, ALU.add, ALU.bypass, r=[curk], w=[kp + "thr"])
                    self.ts(nmb[:, 0:L], sc[:, 0:L], thr, NEG, ALU.is_lt, ALU.mult, r=sck + [kp + "thr"], w=[nmk])
                else:
                    self.ts(nmb[:, 0:L], sc[:, 0:L], -16.0, NEG, ALU.is_lt, ALU.mult, r=sck, w=[nmk])
                if qb == 5:
                    self.tap("score5", sc[:, 0:L], sck, [L])
                    self.tap("nm5", nmb[:, 0:L], [nmk], [L])
                yield

            def n_units_A(qq):
                qb = 4 * n + qq
                return 8 * (((qb + 1) * 128 + 511) // 512)

            def step_gen(g):
                if g is None:
                    return None
                try:
                    next(g)
                    return g
                except StopIteration:
                    return None

            def drain(g):
                while g is not None:
                    g = step_gen(g)

            def emit_attn(qq, gB, nB, gA, nA):
                qb = 4 * n + qq
                qsl = slice(qq * 128, (qq + 1) * 128)
                nmb = nm3[qb % 2]
                nmk = ("nm", qb % 2)
                niter = 2 * (qb + 1)
                perB = -(-nB // niter)
                perA = -(-nA // niter)
                for hh in range(2):
                    def emit_S(kb):
                        sb = kb % 2
                        near = (qb - kb) < 2
                        kk = 64 if near else 65
                        ksl = slice(kb * 128, (kb + 1) * 128)
                        for bk in range(2):
                            bnk = 2 * sb + bk
                            hs = slice(hh * 8 + bk * 4, hh * 8 + bk * 4 + 4)
                            outp = self.psb(bnk).rearrange("p (h q) -> p h q", h=4)
                            qk = [("qT", h) for h in range(hh * 8 + bk * 4, hh * 8 + bk * 4 + 4)] + ["qT64"]
                            self.mm(outp, kaugT[0:kk, ksl], qT[0:kk, hs, qsl], True, False,
                                    r=qk + [("kT", kb // 4), "kaug1"], w=[("ps", bnk)])
                            self.mm(outp, nmb[:, ksl], sel4, False, not near, r=[nmk, "cb"], w=[("ps", bnk)])
                            if near:
                                self.mm(outp, ident, biasT[:, qb - kb, hs, :], False, True, r=["cb"], w=[("ps", bnk)])
                        self.act(PT[sb][:, :], ps[:, 2 * sb * 512: 2 * sb * 512 + 1024], AF.Exp,
                                 r=[("ps", 2 * sb), ("ps", 2 * sb + 1)], w=[("PT", sb)])

                    def emit_PV(kb):
                        sb = kb % 2
                        for bk in range(2):
                            self.mm(self.psb(4 + bk, 65), vaug[:, kb, 0:65], PT[sb][:, bk * 512:(bk + 1) * 512], kb == 0, kb == qb,
                                    r=[("PT", sb), ("vaug", kb), "vaug1"], w=[("ps", 4 + bk)])

                    for kb in range(qb + 1):
                        emit_S(kb)
                        if kb > 0:
                            emit_PV(kb - 1)
                        for _ in range(perB):
                            gB = step_gen(gB)
                        for _ in range(perA):
                            gA = step_gen(gA)
                    emit_PV(qb)
                    if hh == 1:
                        drain(gB)
                        drain(gA)
                        gB = gA = None
                    self.act(lnr, ps[64:65, 2048:3072], AF.Ln, r=[("ps", 4), ("ps", 5)], w=["lnr"])
                    self.act(rrow, lnr, AF.Exp, r=["lnr"], w=["rrow"], scale=-1.0)
                    for bk in range(2):
                        self.mm(self.psb(6 + bk, 64), self.ones_bf[64:65, 0:64], rrow[:, bk * 512:(bk + 1) * 512], True, True,
                                r=["ones_bf", "rrow"], w=[("ps", 6 + bk)])
                    self.act(ot[:, :], ps[0:64, 2048:3072], AF.Copy, r=[("ps", 4), ("ps", 5)], w=["ot"])
                    for bk in range(2):
                        hs = slice(hh * 8 + bk * 4, hh * 8 + bk * 4 + 4)
                        self.tt(OTn[:, hs, qsl], ot[:, bk * 512:(bk + 1) * 512].rearrange("p (h q) -> p h q", h=4),
                                self.psb(6 + bk, 64).rearrange("p (h q) -> p h q", h=4), ALU.mult,
                                r=["ot", ("ps", 6 + bk)], w=[("OTn", hh * 2 + bk)])

            drain(genA(0))
            gB0, gA1 = genB(0), genA(1)
            gAO = emit_ao(n - 1) if n > 0 else None
            rnd = 0
            while gB0 is not None or gA1 is not None or gAO is not None:
                gB0 = step_gen(gB0)
                gA1 = step_gen(step_gen(gA1))
                if rnd % 2 == 1:
                    gAO = step_gen(gAO)
                rnd += 1
            for qq in range(4):
                gB = genB(qq + 1) if qq + 1 < 4 else None
                gA = genA(qq + 2) if qq + 2 < 4 else None
                emit_attn(qq, gB, NBIS + 1, gA, n_units_A(qq + 2) if qq + 2 < 4 else 0)
            if n == 0:
                self.tap("OTn", OTn[:, :, :], [("OTn", i) for i in range(4)], [16, TB], parts=64)
        for _ in emit_ao(NTB - 1):
            pass


def km(w):
    k, m = w.shape
    return np.ascontiguousarray(w.reshape(k // 128, 128, m).transpose(1, 0, 2)).reshape(128, (k // 128) * m)


def host_segments(inp, l):
    w = inp["w_in"][l]
    segs = {}
    segs["kk"] = km(np.concatenate([w[:, C_K:C_K + 64], w[:, C_KI:C_KI + 64]], axis=1))
    segs["vw"] = km(np.concatenate([w[:, C_V:C_V + 64], w[:, C_WI:C_WI + 8]], axis=1))
    for cc in range(4):
        segs["pu%d" % cc] = km(w[:, C_POOL + cc * 128: C_POOL + (cc + 1) * 128])
        segs["su%d" % cc] = km(w[:, C_S5 + cc * 128: C_S5 + (cc + 1) * 128])
    segs["mix"] = np.ascontiguousarray(inp["pool_mix_w"][l].transpose(1, 0, 2)).reshape(128, 512)
    for c in range(8):
        cs = slice(c * 128, (c + 1) * 128)
        segs["po%d" % c] = km(inp["pool_out_w"][l][:, cs])
        for b in range(3):
            segs["g%d_%d" % (b, c)] = km(w[:, C_G + b * 1024 + c * 128: C_G + b * 1024 + (c + 1) * 128])
        glu = inp["s5_glu_w"][l]
        segs["glu%d" % c] = np.concatenate([km(glu[:, cs]), km(glu[:, 1024 + c * 128: 1024 + (c + 1) * 128])], axis=1)
        segs["q%d" % c] = km(w[:, C_Q + c * 128: C_Q + (c + 1) * 128])
        ao = inp["attn_out_w"][l][:, cs].reshape(16, 64, 128).transpose(1, 0, 2).reshape(64, 2048)
        segs["ao%d" % c] = np.concatenate([ao, np.zeros((64, 2048), np.float32)], axis=0)
        segs["wo%d" % c] = km(inp["w_out"][l][:, cs])
        segs["fo%d" % c] = km(inp["ffn_w_out"][l][:, cs])
    for hp in range(4):
        segs["qi%d" % hp] = km(w[:, C_QI + hp * 128: C_QI + (hp + 1) * 128])
    fw = inp["ffn_w_in"][l]
    for j in range(NJ):
        segs["f%d" % j] = km(np.concatenate([fw[:, j * 128:(j + 1) * 128], fw[:, FH + j * 128: FH + (j + 1) * 128]], axis=1))
    cre = inp["s5_c_re"][l]
    cim = inp["s5_c_im"][l]
    Cre = np.zeros((128, 16, 128), np.float32)
    Cim = np.zeros((128, 16, 128), np.float32)
    for g in range(32):
        j, g2 = g // 2, g % 2
        jj = j % 4
        m0 = 32 * jj + 16 * g2
        Cre[g2 * 64:(g2 + 1) * 64, j, m0:m0 + 16] = cre[g].T
        Cim[g2 * 64:(g2 + 1) * 64, j, m0:m0 + 16] = cim[g].T
    segs["sC"] = np.concatenate([Cre.reshape(128, 2048), Cim.reshape(128, 2048)], axis=1)
    Dg = np.zeros((128, 4, 128), np.float32)
    d = inp["s5_d"][l]
    for cc in range(4):
        Dg[np.arange(128), cc, np.arange(128)] = d[cc * 128:(cc + 1) * 128]
    segs["sD"] = Dg.reshape(128, 512)
    return segs


def host_pf(inp, l):
    pf = np.zeros((128, NPF), np.float32)
    for off, name in ((PF_GMP, "norm_mix_pre"), (PF_GMO, "norm_mix_post"), (PF_GFP, "norm_ffn_pre"), (PF_GFO, "norm_ffn_post")):
        pf[:, off:off + 8] = inp[name][l].reshape(8, 128).T
    pf[:, PF_PSC:PF_PSC + 4] = inp["pool_scale"][l].reshape(4, 128).T
    lr, li, ld = inp["s5_lambda_re"][l], inp["s5_lambda_im"][l], inp["s5_log_dt"][l]
    for j in range(16):
        for g2 in range(2):
            g = 2 * j + g2
            pf[g2 * 64:(g2 + 1) * 64, PF_LRS + j] = lr[g]
            pf[g2 * 64:(g2 + 1) * 64, PF_LIS + j] = li[g]
            pf[g2 * 64:(g2 + 1) * 64, PF_LDS + j] = ld[g]
    br, bi = inp["s5_b_re"][l], inp["s5_b_im"][l]
    X = np.zeros((5, 128, 4, 128), np.float32)
    for cc in range(4):
        for p in range(128):
            g = cc * 8 + p // 16
            i = p % 16
            X[0, p, cc, :] = np.tile(lr[g], 2)
            X[1, p, cc, :] = np.tile(li[g], 2)
            X[2, p, cc, :] = ld[g]
            g2 = g % 2
            X[3, p, cc, g2 * 64:(g2 + 1) * 64] = br[g, :, i]
            X[4, p, cc, g2 * 64:(g2 + 1) * 64] = bi[g, :, i]
    for k, off in enumerate((PF_LRX, PF_LIX, PF_LDX, PF_BRX, PF_BIX)):
        pf[:, off:off + 512] = X[k].reshape(128, 512)
    return pf


def host_consts(inp):
    cf = np.zeros((128, NCF), np.float32)
    q = np.arange(128)[:, None]
    s = np.arange(128)[None, :]
    cf[:, CF_CMASK:CF_CMASK + 128] = np.where(s > q, np.float32(-1e4), np.float32(0.0))
    cf[:, CF_INVC:CF_INVC + 16] = (1.0 / np.arange(1, 17, dtype=np.float32))[None, :]
    cf[:, CF_ONES:CF_ONES + 64] = 1.0
    par = ((np.arange(128) // 32) % 2).astype(np.float32)
    cf[:, CF_PAR] = 1.0 - par
    cf[:, CF_PAR + 1] = par
    cb = np.zeros((128, NCB), np.float32)
    cb[:, CB_ID:CB_ID + 128] = np.eye(128, dtype=np.float32)
    cb[:, CB_SEL:CB_SEL + 512] = np.tile(np.eye(128, dtype=np.float32), (1, 4))
    rb = inp["rel_bias"]
    sl = np.arange(128)[:, None]
    ql = np.arange(128)[None, :]
    bt = np.zeros((128, 2, 16, 128), np.float32)
    for kind in range(2):
        dist = ql - sl + 128 * kind
        idx = rel_bucket_np(np.maximum(dist, 0))
        bt[:, kind, :, :] = rb[idx].transpose(0, 2, 1)
    cb[:, CB_BIAS:CB_BIAS + 4096] = bt.reshape(128, 4096)
    b31 = np.ascontiguousarray(np.repeat(rb[31][:, None], TB, axis=1)).reshape(1, 16 * TB).astype(np.float32)
    return cf, cb, b31


_CACHE = {}


def get_program(nlayers=NL, stop=None, taps=()):
    key = (nlayers, stop, tuple(taps))
    if key not in _CACHE:
        b0 = Builder(nlayers, stop, taps)
        b0.wtotal = 1 << 20
        b0.build()
        b = Builder(nlayers, stop, taps)
        b.wtotal = max(b0.seg_total, 16)
        nc = b.build()
        _CACHE[key] = (nc, b)
    return _CACHE[key]


def run(inputs, nlayers=NL, stop=None, taps=()):
    nc, b = get_program(nlayers, stop, taps)
    inp = {k: np.asarray(v, dtype=np.float32) for k, v in inputs.items()}
    ws = np.zeros((NL, 128, b.wtotal), np.float32)
    pfs = np.zeros((NL, 128, NPF), np.float32)
    for l in range(NL):
        segs = host_segments(inp, l)
        for name, (off, n) in b.seg_off.items():
            a = segs[name]
            assert a.shape == (128, n), (name, a.shape, n)
            ws[l, :, off:off + n] = a
        pfs[l] = host_pf(inp, l)
    cf, cb, b31 = host_consts(inp)
    x = inp["x"]
    in_maps = []
    for c in range(8):
        xt = np.ascontiguousarray(x[c].T.reshape(KC, 128, S).transpose(1, 0, 2))
        in_maps.append({"x": xt, "wstream": ws, "pf": pfs, "cf": cf, "cb": cb, "b31": b31})
    res = run_bass_kernel_spmd(nc, in_maps, core_ids=list(range(8)))
    return res, b


def kernel(**inputs):
    res, b = run(inputs)
    outs = []
    for c in range(8):
        o = res.results[c]["out"]
        outs.append(np.ascontiguousarray(o.transpose(1, 0, 2).reshape(D, S).T))
    return np.stack(outs, axis=0).astype(np.float32)
```

```python
import contextlib
import math
import numpy as np
import concourse.bass as bass
import concourse.mybir as mybir
from concourse.bass_utils import run_bass_kernel_spmd

F32 = mybir.dt.float32
BF16 = mybir.dt.bfloat16
I32 = mybir.dt.int32
ALU = mybir.AluOpType
AF = mybir.ActivationFunctionType

ENGS = ("pe", "act", "dve", "pool", "sp")


class Op:
    __slots__ = ("eng", "fn", "waits", "signal", "slot", "seq", "dcount")

    def __init__(self, eng, fn):
        self.eng = eng
        self.fn = fn
        self.waits = {}
        self.signal = False
        self.slot = None
        self.seq = 0
        self.dcount = 0


class Prog:
    def __init__(self):
        self.ops = {e: [] for e in ENGS}
        self.last_w = {}
        self.readers = {}
        self.slot_count = {}
        self.pending = {e: {} for e in ENGS}

    def _add_wait(self, op, tok, raw):
        kind, who, val = tok
        if kind == "E":
            if who == op.eng and not raw:
                return
            if who == "pe" and op.eng == "pe":
                return
            self.ops[who][val].signal = True
        k = (kind, who)
        if op.waits.get(k, -1) < val:
            op.waits[k] = val

    def op(self, eng, fn, r=(), w=(), slot=None):
        o = Op(eng, fn)
        o.seq = len(self.ops[eng])
        if slot is not None:
            o.slot = slot
            self.slot_count[slot] = self.slot_count.get(slot, 0) + 1
            o.dcount = self.slot_count[slot]
            tok = ("D", slot, o.dcount)
        else:
            tok = ("E", eng, o.seq)
        for k, v in self.pending[eng].items():
            if o.waits.get(k, -1) < v:
                o.waits[k] = v
        self.pending[eng] = {}
        for k in r:
            lw = self.last_w.get(k)
            if lw is not None:
                self._add_wait(o, lw, True)
        for k in w:
            lw = self.last_w.get(k)
            if lw is not None:
                self._add_wait(o, lw, False)
            for t in self.readers.get(k, ()):
                self._add_wait(o, t, False)
        self.ops[eng].append(o)
        for k in r:
            self.readers.setdefault(k, []).append(tok)
        for k in w:
            self.last_w[k] = tok
            self.readers[k] = []
        return o

    def barrier(self):
        toks = {}
        for e in ENGS:
            for o in reversed(self.ops[e]):
                if o.slot is None:
                    o.signal = True
                    toks[("E", e)] = o.seq
                    break
        for s, c in self.slot_count.items():
            toks[("D", s)] = c
        for e in ENGS:
            for k, v in toks.items():
                if k == ("E", e):
                    continue
                if self.pending[e].get(k, -1) < v:
                    self.pending[e][k] = v
        self.last_w = {}
        self.readers = {}

    def emit(self, nc, final_slots=()):
        with contextlib.ExitStack() as st:
            esem = {e: st.enter_context(nc.semaphore("s_" + e)) for e in ENGS}
            dsem = {s: st.enter_context(nc.semaphore("d_%d" % i)) for i, s in enumerate(self.slot_count)}
            sigcount = {}
            for e in ENGS:
                c = 0
                arr = []
                for o in self.ops[e]:
                    if o.slot is None and o.signal:
                        c += 1
                    arr.append(c)
                sigcount[e] = arr
            block = st.enter_context(nc.Block())
            prog = self

            def make(e):
                def body(eng):
                    waited = {}
                    for o in prog.ops[e]:
                        for (kind, who), val in o.waits.items():
                            if kind == "E":
                                sem = esem[who]
                                v = sigcount[who][val]
                            else:
                                sem = dsem[who]
                                v = 16 * val
                            if waited.get((kind, who), -1) >= v:
                                continue
                            waited[(kind, who)] = v
                            eng.wait_ge(sem, v)
                        ins = o.fn(eng)
                        if o.slot is not None:
                            ins.then_inc(dsem[o.slot], 16)
                        elif o.signal:
                            ins.then_inc(esem[e], 1)
                    if e == "sp":
                        for s in final_slots:
                            eng.wait_ge(dsem[s], 16 * prog.slot_count[s])
                return body

            block.tensor(make("pe"))
            block.scalar(make("act"))
            block.vector(make("dve"))
            block.gpsimd(make("pool"))
            block.sync(make("sp"))


S = 2048
D = 1024
TB = 512
NTB = 4
KC = 8
NL = 2
FH = 2816
NJ = 22
EPS = 1e-6
IDX_SCALE = (8 ** -0.5) * (64 ** -0.5)
NEG = -30000.0
NBIS = 15

C_POOL, C_Q, C_K, C_V, C_QI, C_KI, C_WI, C_S5, C_G = 0, 512, 1536, 1600, 1664, 2176, 2240, 2248, 2760

PF_GMP, PF_GMO, PF_GFP, PF_GFO, PF_PSC = 0, 8, 16, 24, 32
PF_LRS, PF_LIS, PF_LDS = 36, 52, 68
PF_LRX, PF_LIX, PF_LDX, PF_BRX, PF_BIX = 84, 596, 1108, 1620, 2132
NPF = 2644
CF_CMASK, CF_INVC, CF_ONES, CF_PAR = 0, 128, 144, 208
NCF = 212
CB_ID, CB_SEL, CB_BIAS = 0, 128, 640
NCB = 640 + 4096

ARENA = 86 * 1024
RSLOT = 4096
NRING = 4
KV0 = 75 * 1024


def rel_bucket_np(dist):
    dist = np.asarray(dist, np.int32)
    d_f = np.maximum(dist, 1).astype(np.float32)
    large = 16 + (np.log(d_f / np.float32(16)) / np.float32(math.log(128 / 16)) * np.float32(16)).astype(np.int32)
    large = np.minimum(large, 31)
    return np.where(dist < 16, dist, large)


class Builder:
    def __init__(self, nlayers=NL, stop=None, taps=()):
        self.nlayers = nlayers
        self.stop = stop
        self.taps = taps
        self.seg_off = {}
        self.seg_total = 0
        self.ring_i = 0
        self.bank_i = 0
        self.bankset = list(range(8))
        self.tapouts = {}

    def carve(self, off, parts, shape, dt):
        esz = 2 if dt == BF16 else 4
        n = int(np.prod(shape))
        assert off % 4 == 0 and off + n * esz <= ARENA, (off, n, esz)
        ap = self.arena[0:parts, off // 2: off // 2 + n * esz // 2]
        if dt != BF16:
            ap = ap.bitcast(dt)
        if len(shape) == 2:
            return ap.rearrange("p (a b) -> p a b", a=shape[0])
        if len(shape) == 3:
            return ap.rearrange("p (a b c) -> p a b c", a=shape[0], b=shape[1])
        return ap

    def bank(self):
        b = self.bankset[self.bank_i % len(self.bankset)]
        self.bank_i += 1
        return b

    def psb(self, b, parts=128, n=512):
        return self.ps[0:parts, b * 512: b * 512 + n]

    def wload(self, l, name, n):
        assert n <= RSLOT
        if name not in self.seg_off:
            self.seg_off[name] = (self.seg_total, n)
            self.seg_total += n
        off, n0 = self.seg_off[name]
        assert n0 == n
        slot = self.ring_i % NRING
        self.ring_i += 1
        dst = self.ring[:, slot * RSLOT: slot * RSLOT + n]
        src = self.wstream[l, :, off:off + n]
        key = ("ring", slot)
        self.P.op("pool", lambda e: e.dma_start(out=dst, in_=src, max_dma_last_dim=8192), w=[key], slot="ring%d" % slot)
        return dst, key

    def mm(self, out, lhsT, rhs, start, stop, r, w):
        self.P.op("pe", lambda e: e.matmul(out, lhsT, rhs, start=start, stop=stop), r=r, w=w)

    def act(self, out, in_, func, r, w, scale=1.0, bias=0.0):
        self.P.op("act", lambda e: e.activation(out=out, in_=in_, func=func, bias=bias, scale=scale), r=r, w=w)

    def tt(self, out, in0, in1, op, r, w):
        self.P.op("dve", lambda e: e.tensor_tensor(out=out, in0=in0, in1=in1, op=op), r=r, w=w)

    def ts(self, out, in0, s1, s2, op0, op1, r, w, accum_out=None):
        if accum_out is None:
            self.P.op("dve", lambda e: e.tensor_scalar(out=out, in0=in0, scalar1=s1, scalar2=s2, op0=op0, op1=op1), r=r, w=w)
        else:
            self.P.op("dve", lambda e: e.tensor_scalar(out=out, in0=in0, scalar1=s1, scalar2=s2, op0=op0, op1=op1, accum_out=accum_out), r=r, w=w)

    def stt(self, out, in0, scalar, in1, op0, op1, r, w):
        self.P.op("dve", lambda e: e.scalar_tensor_tensor(out=out, in0=in0, scalar=scalar, in1=in1, op0=op0, op1=op1), r=r, w=w)

    def dma(self, eng, out, in_, r, w, slot):
        self.P.op(eng, lambda e: e.dma_start(out=out, in_=in_), r=r, w=w, slot=slot)

    def tap(self, name, ap, keys, shape, parts=128):
        if name not in self.taps:
            return
        t = self.nc.dram_tensor("tap_" + name, [parts] + list(shape), ap.dtype, kind="ExternalOutput").ap()
        self.tapouts[name] = t
        self.P.op("sp", lambda e: e.dma_start(out=t, in_=ap), r=keys, slot="tap_" + name)
        self.final_slots.append("tap_" + name)

    def rms_rstd(self, src_f32, src_keys, sq, tag):
        P = self.P
        for c in range(KC):
            self.act(sq[:, c, :], src_f32[:, c, :], AF.Square, r=[src_keys[c]], w=[("sq", c)])
        b = self.bank()
        for c in range(KC):
            self.mm(self.psb(b), self.ones_bf[:, :], sq[:, c, :], c == 0, c == KC - 1, r=[("sq", c), "ones_bf"], w=[("ps", b)])
        self.act(self.rstd[:, :], self.psb(b), AF.Sqrt, r=[("ps", b), "epsc"], w=["rstd"], scale=1.0 / D, bias=self.epsc[:, 0:1])
        P.op("dve", lambda e: e.reciprocal(out=self.rstd[:, :], in_=self.rstd[:, :]), r=["rstd"], w=["rstd"])

    def build(self):
        nc = bass.Bass("TRN2", target_bir_lowering=False)
        self.nc = nc
        self.final_slots = []
        P = self.P = Prog()
        xin = nc.dram_tensor("x", [128, KC, S], F32, kind="ExternalInput").ap()
        out = nc.dram_tensor("out", [128, KC, S], F32, kind="ExternalOutput").ap()
        xres = nc.dram_tensor("xres", [128, KC, S], F32, kind="Internal").ap()
        self.wstream = nc.dram_tensor("wstream", [NL, 128, self.wtotal], F32, kind="ExternalInput").ap()
        pfd = nc.dram_tensor("pf", [NL, 128, NPF], F32, kind="ExternalInput").ap()
        cfd = nc.dram_tensor("cf", [128, NCF], F32, kind="ExternalInput").ap()
        cbd = nc.dram_tensor("cb", [128, NCB], F32, kind="ExternalInput").ap()
        b31d = nc.dram_tensor("b31", [1, 16 * TB], F32, kind="ExternalInput").ap()
        with contextlib.ExitStack() as st:
            E = st.enter_context
            self.arena = E(nc.sbuf_tensor("arena", [128, ARENA // 2], BF16))
            hT = E(nc.sbuf_tensor("hT", [128, KC, S], BF16))
            merged = E(nc.sbuf_tensor("merged", [128, KC, S], BF16))
            self.ring = E(nc.sbuf_tensor("ring", [128, NRING * RSLOT], BF16))
            pf = E(nc.sbuf_tensor("pfs", [128, NPF], F32))
            self.pf_t = pf
            cf = E(nc.sbuf_tensor("cfs", [128, NCF], F32))
            cb = E(nc.sbuf_tensor("cbs", [128, NCB], BF16))
            self.ones_bf = E(nc.sbuf_tensor("ones_bf", [128, 128], BF16))
            self.rstd = E(nc.sbuf_tensor("rstd", [128, TB], F32))
            self.epsc = E(nc.sbuf_tensor("epsc", [128, 8], F32))
            self.hpi = E(nc.sbuf_tensor("hpi", [128, 8], F32))
            self.ps = E(nc.psum_tensor("ps", [128, 4096], F32))
            ps = self.ps
            ident = cb[:, CB_ID:CB_ID + 128]
            sel4 = cb[:, CB_SEL:CB_SEL + 512].rearrange("p (h q) -> p h q", h=4)
            biasT = cb[:, CB_BIAS:CB_BIAS + 4096].rearrange("p (k h q) -> p k h q", k=2, h=16)
            cmask = cf[:, CF_CMASK:CF_CMASK + 128]
            invc = cf[:, CF_INVC:CF_INVC + 16]
            ones64 = cf[:, CF_ONES:CF_ONES + 64]
            self.cf = cf

            P.op("dve", lambda e: e.memset(self.ones_bf[:, :], 1.0), w=["ones_bf"])
            P.op("dve", lambda e: e.memset(self.epsc[:, :], EPS), w=["epsc"])
            P.op("dve", lambda e: e.memset(self.hpi[:, :], math.pi / 2), w=["hpi"])
            self.dma("sp", cf[:, :], cfd, r=[], w=["cf"], slot="cf")
            P.op("pool", lambda e: e.dma_start(out=cb[:, :], in_=cbd, max_dma_last_dim=8192), w=["cb"], slot="cb")

            for l in range(self.nlayers):
                xsrc = xin if l == 0 else xres
                xdst = out if l == self.nlayers - 1 else xres
                self.layer(l, xsrc, xdst, hT, merged, pf, pfd, ident, sel4, biasT, cmask, invc, ones64, b31d)
                if self.stop is not None:
                    break
            P.barrier()
            P.emit(nc, final_slots=self.final_slots)
        return nc

    def layer(self, l, xsrc, xdst, hT, merged, pf, pfd, ident, sel4, biasT, cmask, invc, ones64, b31d):
        P = self.P
        nc = self.nc
        ps = self.ps
        stop = self.stop
        P.barrier()
        self.bankset = list(range(8))
        self.dma("sp", pf[:, :], pfd[l], r=[], w=["pf"], slot="pf")
        gmp = pf[:, PF_GMP:PF_GMP + 8]
        gmo = pf[:, PF_GMO:PF_GMO + 8]
        gfp = pf[:, PF_GFP:PF_GFP + 8]
        gfo = pf[:, PF_GFO:PF_GFO + 8]
        psc = pf[:, PF_PSC:PF_PSC + 4]

        xblk = self.carve(0, 128, [KC, TB], F32)
        sq = self.carve(16 * 1024, 128, [KC, TB], BF16)
        for n in range(NTB):
            tsl = slice(n * TB, (n + 1) * TB)
            self.dma("sp", xblk[:, :, :], xsrc[:, :, tsl], r=[], w=[("xblk", c) for c in range(KC)], slot="xblk")
            self.rms_rstd(xblk, [("xblk", c) for c in range(KC)], sq, "p1")
            for c in range(KC):
                self.stt(hT[:, c, tsl], xblk[:, c, :], gmp[:, c:c + 1], self.rstd[:, :], ALU.mult, ALU.mult,
                         r=[("xblk", c), "pf", "rstd"], w=[("hT", c, n)])
        self.tap("hT", hT[:, :, :], [("hT", c, n) for c in range(KC) for n in range(NTB)], [KC, S])
        if stop == 1:
            return
        p1done = [("hT", c, NTB - 1) for c in range(KC)]

        kaugT = self.arena[0:65, KV0 // 2: KV0 // 2 + S]
        kiT = self.arena[0:64, KV0 // 2 + S: KV0 // 2 + 2 * S]
        vaug = self.arena[:, KV0 // 2 + 2 * S: KV0 // 2 + 2 * S + 16 * 66].rearrange("p (t d) -> p t d", t=16)
        wi_off = KV0 + 2 * (2 * S + 16 * 66)
        wi_s = self.arena[:, wi_off // 2: wi_off // 2 + 256].bitcast(F32).rearrange("p (t h) -> p t h", t=16)
        assert wi_off + 512 <= ARENA
        uz = self.carve(0, 128, [4, S], BF16)
        upp = [self.carve(16 * 1024, 128, [1, S], F32)[:, 0, :], self.carve(24 * 1024, 128, [1, S], F32)[:, 0, :]]
        dT = self.carve(32 * 1024, 128, [4, S], BF16)
        yp = self.carve(48 * 1024, 128, [4, S], BF16)
        sgt = self.carve(64 * 1024, 128, [1, TB], BF16)[:, 0, :]

        def hkeys(n):
            return [("hT", c, n) for c in range(KC)]

        seg, sk = self.wload(l, "kk", 1024)
        seg = seg.rearrange("p (k m) -> p k m", k=KC)
        P.op("dve", lambda e: e.memset(kaugT[64:65, :], 1.0), w=["kaug1"])
        P.op("dve", lambda e: e.memset(vaug[:, :, 64:65], 1.0), w=["vaug1"])
        for n in range(NTB):
            tsl = slice(n * TB, (n + 1) * TB)
            for half, dst, key in ((0, kaugT, "kT"), (1, kiT, "kiT")):
                b = self.bank()
                for kc in range(KC):
                    self.mm(self.psb(b, 64), seg[:, kc, half * 64:(half + 1) * 64], hT[:, kc, tsl], kc == 0, kc == KC - 1,
                            r=[sk, ("hT", kc, n)], w=[("ps", b)])
                self.act(dst[0:64, tsl], self.psb(b, 64), AF.Copy, r=[("ps", b)], w=[(key, n)])
        seg, sk = self.wload(l, "vw", KC * 72)
        seg = seg.rearrange("p (k m) -> p k m", k=KC)
        for tt_ in range(16):
            n = tt_ // 4
            b = self.bank()
            for kc in range(KC):
                self.mm(ps[:, b * 512: b * 512 + 72], hT[:, kc, tt_ * 128:(tt_ + 1) * 128], seg[:, kc, :], kc == 0, kc == KC - 1,
                        r=[sk, ("hT", kc, n)], w=[("ps", b)])
            self.act(vaug[:, tt_, 0:64], ps[:, b * 512: b * 512 + 64], AF.Copy, r=[("ps", b)], w=[("vaug", tt_)])
            self.act(wi_s[:, tt_, :], ps[:, b * 512 + 64: b * 512 + 72], AF.Copy, r=[("ps", b)], w=[("wi", tt_)], scale=IDX_SCALE)
        for cc in range(4):
            seg, sk = self.wload(l, "su%d" % cc, 1024)
            seg = seg.rearrange("p (k m) -> p k m", k=KC)
            for n in range(NTB):
                tsl = slice(n * TB, (n + 1) * TB)
                b = self.bank()
                for kc in range(KC):
                    self.mm(self.psb(b), seg[:, kc, :], hT[:, kc, tsl], kc == 0, kc == KC - 1, r=[sk, ("hT", kc, n)], w=[("ps", b)])
                self.act(uz[:, cc, tsl], self.psb(b), AF.Copy, r=[("ps", b)] + p1done, w=[("uz", cc, n)])
        for cc in range(4):
            seg, sk = self.wload(l, "pu%d" % cc, 1024)
            seg = seg.rearrange("p (k m) -> p k m", k=KC)
            u0 = upp[0]
            for n in range(NTB):
                tsl = slice(n * TB, (n + 1) * TB)
                b = self.bank()
                for kc in range(KC):
                    self.mm(self.psb(b), seg[:, kc, :], hT[:, kc, tsl], kc == 0, kc == KC - 1, r=[sk, ("hT", kc, n)], w=[("ps", b)])
                self.act(u0[:, tsl], self.psb(b), AF.Copy, r=[("ps", b)] + p1done, w=["up0"])
            wlen = 2 ** (cc + 1)
            sA = upp[1]
            sB = self.carve(66 * 1024, 128, [1, S], F32)[:, 0, :]
            bufs = [(sA, "upA"), (sB, "upB")]
            k = 1
            bi = 0
            src, srck = u0, "up0"
            while k < wlen:
                dstb, dstk = bufs[bi % 2]
                self.tt(dstb[:, k:], src[:, k:], src[:, :S - k], ALU.add, r=[srck], w=[dstk])
                P.op("dve", lambda e, d=dstb, s_=src, k=k: e.tensor_copy(out=d[:, 0:k], in_=s_[:, 0:k]), r=[srck], w=[dstk])
                src, srck = dstb, dstk
                bi += 1
                k *= 2
            self.stt(dT[:, cc, :], src[:, :], 1.0 / wlen, u0[:, :], ALU.mult, ALU.subtract, r=[srck, "up0"], w=[("dT", cc)])
            tmpc = self.carve(64 * 1024 + 1024, 128, [1, 16], F32)[:, 0, :]
            self.tt(tmpc[:, 0:wlen - 1], src[:, 0:wlen - 1], invc[:, 0:wlen - 1], ALU.mult, r=[srck, "cf"], w=["tmpc"])
            self.tt(dT[:, cc, 0:wlen - 1], tmpc[:, 0:wlen - 1], u0[:, 0:wlen - 1], ALU.subtract, r=["tmpc", "up0", ("dT", cc)], w=[("dT", cc)])
        self.tap("dT", dT[:, :, :], [("dT", cc) for cc in range(4)], [4, S])
        self.tap("kT", kaugT[:, :], [("kT", n) for n in range(NTB)] + ["kaug1"], [S], parts=65)
        self.tap("vaug", vaug[:, :, :], [("vaug", t) for t in range(16)] + ["vaug1"], [16, 66])
        self.tap("wi", wi_s[:, :, :], [("wi", t) for t in range(16)], [16, 8])
        if stop == 2:
            return

        seg, sk = self.wload(l, "mix", 512)
        seg = seg.rearrange("p (g m) -> p g m", g=4)
        for cc in range(4):
            for n in range(NTB):
                tsl = slice(n * TB, (n + 1) * TB)
                b = self.bank()
                self.mm(self.psb(b), seg[:, cc, :], dT[:, cc, tsl], True, True, r=[sk, ("dT", cc)], w=[("ps", b)])
                self.act(yp[:, cc, tsl], self.psb(b), AF.Copy, r=[("ps", b), "pf"], w=[("yp", cc, n)], scale=psc[:, cc:cc + 1])
        for c in range(KC):
            segp, skp = self.wload(l, "po%d" % c, 512)
            segp = segp.rearrange("p (k m) -> p k m", k=4)
            segg, skg = self.wload(l, "g0_%d" % c, 1024)
            segg = segg.rearrange("p (k m) -> p k m", k=KC)
            for n in range(NTB):
                tsl = slice(n * TB, (n + 1) * TB)
                by = self.bank()
                for cc in range(4):
                    self.mm(self.psb(by), segp[:, cc, :], yp[:, cc, tsl], cc == 0, cc == 3, r=[skp, ("yp", cc, n)], w=[("ps", by)])
                bg = self.bank()
                for kc in range(KC):
                    self.mm(self.psb(bg), segg[:, kc, :], hT[:, kc, tsl], kc == 0, kc == KC - 1, r=[skg, ("hT", kc, n)], w=[("ps", bg)])
                self.act(sgt[:, :], self.psb(bg), AF.Sigmoid, r=[("ps", bg)], w=["sgt"])
                self.tt(merged[:, c, tsl], sgt[:, :], self.psb(by), ALU.mult, r=["sgt", ("ps", by)], w=[("mg", c, n)])
        self.tap("mg3", merged[:, :, S - 256:S], [("mg", c, n) for c in range(KC) for n in range(NTB)], [KC, 256])
        if stop == 3:
            return
        P.barrier()

        self.phase_s5(l, hT, merged, pf, uz)
        self.tap("mg4", merged[:, :, S - 256:S], [("mg", c, n) for c in range(KC) for n in range(NTB)], [KC, 256])
        if stop == 4:
            return
        P.barrier()

        self.phase_attn(l, hT, merged, kaugT, kiT, vaug, wi_s, ident, sel4, biasT, cmask, ones64, b31d)
        self.tap("mg5", merged[:, :, S - 256:S], [("mg", c, n) for c in range(KC) for n in range(NTB)], [KC, 256])
        if stop == 5:
            return
        P.barrier()

        self.bankset = list(range(8))
        xT = self.carve(0, 128, [KC, S], F32)
        sq = hT[:, 4:6, :].rearrange("p a b -> p (a b)").rearrange("p (c t) -> p c t", c=KC)
        xblk = hT[:, 0:4, :].rearrange("p a b -> p (a b)").bitcast(F32).rearrange("p (c t) -> p c t", c=KC)
        tmpf = self.carve(64 * 1024, 128, [1, TB], F32)[:, 0, :]
        for c in range(KC):
            seg, sk = self.wload(l, "wo%d" % c, 1024)
            seg = seg.rearrange("p (k m) -> p k m", k=KC)
            for n in range(NTB):
                tsl = slice(n * TB, (n + 1) * TB)
                b = self.bank()
                for kc in range(KC):
                    self.mm(self.psb(b), seg[:, kc, :], merged[:, kc, tsl], kc == 0, kc == KC - 1, r=[sk, ("mg", kc, n)], w=[("ps", b)])
                self.act(xT[:, c, tsl], self.psb(b), AF.Copy, r=[("ps", b)], w=[("xT", c, n)])
        for n in range(NTB):
            tsl = slice(n * TB, (n + 1) * TB)
            self.dma("sp", xblk[:, :, :], xsrc[:, :, tsl], r=[], w=[("xblk", c) for c in range(KC)], slot="xblk")
            self.rms_rstd(xT[:, :, tsl], [("xT", c, n) for c in range(KC)], sq, "p6")
            for c in range(KC):
                self.stt(tmpf[:, :], xT[:, c, tsl], gmo[:, c:c + 1], self.rstd[:, :], ALU.mult, ALU.mult,
                         r=[("xT", c, n), "pf", "rstd"], w=["tmpf"])
                self.tt(xT[:, c, tsl], tmpf[:, :], xblk[:, c, :], ALU.add, r=["tmpf", ("xblk", c)], w=[("xT", c, n)])
        self.tap("x6", xT[:, :, S - 256:S], [("xT", c, n) for c in range(KC) for n in range(NTB)], [KC, 256])
        if stop == 6:
            return
        P.barrier()

        fT = self.carve(64 * 1024, 128, [NJ, TB], BF16)
        sq = merged[:, 4:6, :].rearrange("p a b -> p (a b)").rearrange("p (c t) -> p c t", c=KC)
        mbuf = merged[:, 0:4, :].rearrange("p a b -> p (a b)").bitcast(F32).rearrange("p (c t) -> p c t", c=KC)
        sgf = merged[:, 6, 0:TB]
        tmpf = merged[:, 7, 0:2 * TB].bitcast(F32)
        def prenorm(n):
            tsl = slice(n * TB, (n + 1) * TB)
            self.rms_rstd(xT[:, :, tsl], [("xT", c, n) for c in range(KC)], sq, "p7a")
            for c in range(KC):
                self.stt(hT[:, c, tsl], xT[:, c, tsl], gfp[:, c:c + 1], self.rstd[:, :], ALU.mult, ALU.mult,
                         r=[("xT", c, n), "pf", "rstd"], w=[("hT", c, n)])

        prenorm(0)
        for n in range(NTB):
            tsl = slice(n * TB, (n + 1) * TB)
            for j in range(NJ):
                seg, sk = self.wload(l, "f%d" % j, 2048)
                seg = seg.rearrange("p (k m) -> p k m", k=KC)
                bg = self.bank()
                for kc in range(KC):
                    self.mm(self.psb(bg), seg[:, kc, 0:128], hT[:, kc, tsl], kc == 0, kc == KC - 1, r=[sk, ("hT", kc, n)], w=[("ps", bg)])
                bu = self.bank()
                for kc in range(KC):
                    self.mm(self.psb(bu), seg[:, kc, 128:256], hT[:, kc, tsl], kc == 0, kc == KC - 1, r=[sk, ("hT", kc, n)], w=[("ps", bu)])
                self.act(sgf, self.psb(bg), AF.Silu, r=[("ps", bg)], w=["sgf"])
                self.tt(fT[:, j, :], sgf, self.psb(bu), ALU.mult, r=["sgf", ("ps", bu)], w=[("fT", j)])
            if n + 1 < NTB:
                prenorm(n + 1)
            for c in range(KC):
                seg, sk = self.wload(l, "fo%d" % c, NJ * 128)
                seg = seg.rearrange("p (k m) -> p k m", k=NJ)
                b = self.bank()
                for j in range(NJ):
                    self.mm(self.psb(b), seg[:, j, :], fT[:, j, :], j == 0, j == NJ - 1, r=[sk, ("fT", j)], w=[("ps", b)])
                self.act(mbuf[:, c, :], self.psb(b), AF.Copy, r=[("ps", b)], w=[("mbuf", c)])
            self.rms_rstd(mbuf, [("mbuf", c) for c in range(KC)], sq, "p7b")
            for c in range(KC):
                self.stt(tmpf, mbuf[:, c, :], gfo[:, c:c + 1], self.rstd[:, :], ALU.mult, ALU.mult,
                         r=[("mbuf", c), "pf", "rstd"], w=["tmpf"])
                self.tt(mbuf[:, c, :], tmpf, xT[:, c, tsl], ALU.add, r=["tmpf", ("xT", c, n)], w=[("mbuf", c)])
            self.dma("sp", xdst[:, :, tsl], mbuf[:, :, :], r=[("mbuf", c) for c in range(KC)], w=[], slot="xout")
        if "xout" not in self.final_slots:
            self.final_slots.append("xout")

    def phase_s5(self, l, hT, merged, pf, uz):
        P = self.P
        K = 1024
        Ec = self.carve(16 * K, 128, [1, S], F32)[:, 0, :]
        Es = self.carve(24 * K, 128, [1, S], F32)[:, 0, :]
        vre = self.carve(32 * K, 128, [1, S], F32)[:, 0, :]
        vim = self.carve(40 * K, 128, [1, S], F32)[:, 0, :]
        xre = self.carve(48 * K, 128, [1, S], BF16)[:, 0, :]
        xim = self.carve(52 * K, 128, [1, S], BF16)[:, 0, :]
        tmp1 = self.carve(56 * K, 128, [1, TB], F32)[:, 0, :]
        tmp2 = self.carve(58 * K, 128, [1, TB], F32)[:, 0, :]
        Bre = self.carve(60 * K, 128, [2, 4, 128], BF16)
        Bim = self.carve(62 * K, 128, [2, 4, 128], BF16)
        gt = self.carve(71 * K, 128, [1, TB], F32)[:, 0, :]
        sm = self.carve(64 * K, 128, [16, 16], F32)
        s1 = self.carve(66 * K, 128, [1, TB], BF16)[:, 0, :]
        s2 = self.carve(67 * K, 128, [1, TB], BF16)[:, 0, :]
        t3 = self.carve(68 * K, 128, [1, TB], F32)[:, 0, :]
        xw = self.carve(32 * K, 128, [8, 512], F32)
        smi = self.carve(70 * K, 128, [1, 16], I32)[:, 0, :]
        TWO_PI = 2.0 * math.pi

        def pfx(o):
            return pf[:, o:o + 512]

        def dve(fn, r, w):
            P.op("dve", fn, r=r, w=w)

        def zoh(lr, li, ld, wk, n, tag, itile):
            dt_, lrdt, th, kf, q, sn, cs, t0 = wk[:8]
            kk = ["z%s%d" % (tag, i) for i in range(8)]
            kdt, klrdt, kth, kkf, kq, ksn, kcs, kt0 = kk
            ki = "z%si" % tag
            self.act(dt_, ld, AF.Exp, r=["pf"], w=[kdt])
            self.tt(lrdt, lr, dt_, ALU.mult, r=["pf", kdt], w=[klrdt])
            self.tt(th, li, dt_, ALU.mult, r=["pf", kdt], w=[kth])
            self.ts(kf, th, 1.0 / TWO_PI, None, ALU.mult, ALU.bypass, r=[kth], w=[kkf])
            dve(lambda e: e.tensor_copy(out=itile, in_=kf), r=[kkf], w=[ki])
            dve(lambda e: e.tensor_copy(out=kf, in_=itile), r=[ki], w=[kkf])
            self.stt(q, kf, -TWO_PI, th, ALU.mult, ALU.add, r=[kkf, kth], w=[kq])
            self.act(sn, q, AF.Sin, r=[kq], w=[ksn], scale=0.25)
            self.act(cs, q, AF.Sin, r=[kq, "hpi"], w=[kcs], scale=0.25, bias=self.hpi[:, 0:1])
            for it in range(2):
                self.tt(t0, sn, sn, ALU.mult, r=[ksn], w=[kt0])
                self.stt(sn, sn, 2.0, cs, ALU.mult, ALU.mult, r=[ksn, kcs], w=[ksn])
                self.ts(cs, t0, -2.0, 1.0, ALU.mult, ALU.add, r=[kt0], w=[kcs])
            self.act(dt_, lrdt, AF.Exp, r=[klrdt], w=[kdt])
            return dict(mag=dt_, cos=cs, sin=sn, keys=[kdt, kcs, ksn], k=kk)

        wkx = [xw[:, i, :] for i in range(8)]
        smx = self.carve(56 * K, 128, [1, 512], I32)[:, 0, :]
        zx = zoh(pfx(PF_LRX), pfx(PF_LIX), pfx(PF_LDX), wkx, 512, "x", smx)
        K_ = zx["k"]
        mg_, cs_x, sn_x = wkx[0], wkx[6], wkx[5]
        are, aim, den, cre, cim = wkx[1], wkx[2], wkx[3], wkx[4], wkx[7]
        kare, kaim, kden, kcre, kcim = K_[1], K_[2], K_[3], K_[4], K_[7]
        kmg, kcs, ksn = K_[0], K_[6], K_[5]
        lr, li = pfx(PF_LRX), pfx(PF_LIX)
        self.tt(are, mg_, cs_x, ALU.mult, r=[kmg, kcs], w=[kare])
        self.tt(aim, mg_, sn_x, ALU.mult, r=[kmg, ksn], w=[kaim])
        self.ts(are, are, -1.0, None, ALU.add, ALU.bypass, r=[kare], w=[kare])
        t0, kt0 = wkx[0], K_[0]
        t1, kt1, t2, kt2 = wkx[5], K_[5], wkx[6], K_[6]
        self.tt(den, lr, lr, ALU.mult, r=["pf"], w=[kden])
        self.tt(cre, li, li, ALU.mult, r=["pf"], w=[kcre])
        self.tt(den, den, cre, ALU.add, r=[kden, kcre], w=[kden])
        dve(lambda e: e.reciprocal(out=den, in_=den), r=[kden], w=[kden])
        self.tt(cre, are, lr, ALU.mult, r=[kare, "pf"], w=[kcre])
        self.tt(t0, aim, li, ALU.mult, r=[kaim, "pf"], w=[kt0])
        self.tt(cre, cre, t0, ALU.add, r=[kcre, kt0], w=[kcre])
        self.tt(cre, cre, den, ALU.mult, r=[kcre, kden], w=[kcre])
        self.tt(cim, aim, lr, ALU.mult, r=[kaim, "pf"], w=[kcim])
        self.tt(t0, are, li, ALU.mult, r=[kare, "pf"], w=[kt0])
        self.tt(cim, cim, t0, ALU.subtract, r=[kcim, kt0], w=[kcim])
        self.tt(cim, cim, den, ALU.mult, r=[kcim, kden], w=[kcim])
        br, bi = pfx(PF_BRX), pfx(PF_BIX)
        self.tt(t1, cre, br, ALU.mult, r=[kcre, "pf"], w=[kt1])
        self.tt(t2, cim, bi, ALU.mult, r=[kcim, "pf"], w=[kt2])
        self.tt(t1, t1, t2, ALU.subtract, r=[kt1, kt2], w=[kt1])
        for v in range(2):
            self.ts(Bre[:, v, :, :].rearrange("p a b -> p (a b)"), t1, self.cf[:, CF_PAR + v:CF_PAR + v + 1], None, ALU.mult, ALU.bypass,
                    r=[kt1, "cf"], w=["Bre"])
        self.tt(t1, cre, bi, ALU.mult, r=[kcre, "pf"], w=[kt1])
        self.tt(t2, cim, br, ALU.mult, r=[kcim, "pf"], w=[kt2])
        self.tt(t1, t1, t2, ALU.add, r=[kt1, kt2], w=[kt1])
        for v in range(2):
            self.ts(Bim[:, v, :, :].rearrange("p a b -> p (a b)"), t1, self.cf[:, CF_PAR + v:CF_PAR + v + 1], None, ALU.mult, ALU.bypass,
                    r=[kt1, "cf"], w=["Bim"])

        wks = [sm[:, i, :] for i in range(8)]
        zs = zoh(pf[:, PF_LRS:PF_LRS + 16], pf[:, PF_LIS:PF_LIS + 16], pf[:, PF_LDS:PF_LDS + 16], wks, 16, "s", smi)
        mag, cth, sth = zs["mag"], zs["cos"], zs["sin"]
        nsc = sm[:, 8, :]

        segC, skC = self.wload(l, "sC", 4096)
        Cre = segC[:, 0:2048].rearrange("p (j m) -> p j m", j=16)
        Cim = segC[:, 2048:4096].rearrange("p (j m) -> p j m", j=16)
        segD, skD = self.wload(l, "sD", 512)
        Dg = segD.rearrange("p (c m) -> p c m", c=4)

        Ecb = pf[:, PF_LRX:PF_LRX + 1024].rearrange("p (j i) -> p j i", j=16)
        Esb = pf[:, PF_LDX:PF_LDX + 1024].rearrange("p (j i) -> p j i", j=16)
        tA = tmp1[:, 0:512].rearrange("p (j i) -> p j i", j=16)
        tB = tmp2[:, 0:512].rearrange("p (j i) -> p j i", j=16)
        dve(lambda e: e.tensor_copy(out=Ecb[:, :, 0:1], in_=cth.unsqueeze(2)), r=zs["keys"] + ["Bre", "Bim"], w=["Ecb"])
        dve(lambda e: e.tensor_copy(out=Esb[:, :, 0:1], in_=sth.unsqueeze(2)), r=zs["keys"] + ["Bre", "Bim"], w=["Esb"])
        nn = 1
        while nn < 64:
            cb = Ecb[:, :, nn - 1:nn].to_broadcast([128, 16, nn])
            sb_ = Esb[:, :, nn - 1:nn].to_broadcast([128, 16, nn])
            self.tt(tA[:, :, 0:nn], Ecb[:, :, 0:nn], cb, ALU.mult, r=["Ecb"], w=["tA"])
            self.tt(tB[:, :, 0:nn], Esb[:, :, 0:nn], sb_, ALU.mult, r=["Esb"], w=["tB"])
            self.tt(Ecb[:, :, nn:2 * nn], tA[:, :, 0:nn], tB[:, :, 0:nn], ALU.subtract, r=["tA", "tB"], w=["Ecb2"])
            self.tt(tA[:, :, 0:nn], Esb[:, :, 0:nn], cb, ALU.mult, r=["Esb", "Ecb"], w=["tA"])
            self.tt(tB[:, :, 0:nn], Ecb[:, :, 0:nn], sb_, ALU.mult, r=["Ecb", "Esb"], w=["tB"])
            self.tt(Esb[:, :, nn:2 * nn], tA[:, :, 0:nn], tB[:, :, 0:nn], ALU.add, r=["tA", "tB", "Ecb2"], w=["Esb"])
            dve(lambda e: e.engine_nop(), r=["Ecb2", "Esb"], w=["Ecb"])
            nn *= 2

        self.bankset = [0, 1, 2, 3]
        Ec2 = [self.carve((16 + 4 * i) * K, 128, [1, TB], F32)[:, 0, :] for i in range(2)]
        Es2 = [self.carve((18 + 4 * i) * K, 128, [1, TB], F32)[:, 0, :] for i in range(2)]
        vslot_re = [self.carve((32 + 4 * i) * K, 128, [1, TB], F32)[:, 0, :] for i in range(4)]
        vslot_im = [self.carve((34 + 4 * i) * K, 128, [1, TB], F32)[:, 0, :] for i in range(4)]
        pt4 = [self.carve(o * K, 128, [1, TB], F32)[:, 0, :] for o in (73, 24, 27, 29)]
        car = self.carve(26 * K, 128, [1, 16], F32)[:, 0, :]
        vi = 0
        pending = None
        ydefer = []
        for cc in range(4):
            ybanks = [4, 5, 6, 7]
            for jj in range(4):
                j = cc * 4 + jj
                rows = slice(64 * (jj // 2), 64 * (jj // 2) + 64)
                pv = jj % 2
                Ec, Es = Ec2[j % 2], Es2[j % 2]
                ke, ks = ("Ec", j % 2), ("Es", j % 2)
                dve(lambda e, j=j, Ec=Ec: e.tensor_copy(out=Ec[:, 0:64], in_=Ecb[:, j, :]), r=["Ecb"], w=[ke])
                dve(lambda e, j=j, Es=Es: e.tensor_copy(out=Es[:, 0:64], in_=Esb[:, j, :]), r=["Esb"], w=[ks])
                nn = 64
                lev = 6
                while nn < TB:
                    cs_ = Ec[:, nn - 1:nn]
                    ss_ = Es[:, nn - 1:nn]
                    ns_ = nsc[:, lev:lev + 1]
                    self.ts(ns_, ss_, -1.0, None, ALU.mult, ALU.bypass, r=[ks], w=["nsc"])
                    self.ts(Ec[:, nn:2 * nn], Ec[:, 0:nn], cs_, None, ALU.mult, ALU.bypass, r=[ke], w=[ke])
                    self.stt(Ec[:, nn:2 * nn], Es[:, 0:nn], ns_, Ec[:, nn:2 * nn], ALU.mult, ALU.add, r=[ks, "nsc", ke], w=[ke])
                    self.ts(Es[:, nn:2 * nn], Es[:, 0:nn], cs_, None, ALU.mult, ALU.bypass, r=[ks, ke], w=[ks])
                    self.stt(Es[:, nn:2 * nn], Ec[:, 0:nn], ss_, Es[:, nn:2 * nn], ALU.mult, ALU.add, r=[ke, ks], w=[ks])
                    nn *= 2
                    lev += 1
                cl, sl_, nsl = Ec[:, TB - 1:TB], Es[:, TB - 1:TB], nsc[:, 12:13]
                self.ts(nsl, sl_, -1.0, None, ALU.mult, ALU.bypass, r=[ks], w=["nsl"])
                for n in range(NTB):
                    tsl = slice(n * TB, (n + 1) * TB)
                    vre, vim = vslot_re[vi % 4], vslot_im[vi % 4]
                    kvr, kvi = ("vre", vi % 4), ("vim", vi % 4)
                    vi += 1
                    b1 = self.bank()
                    self.mm(self.psb(b1), Bre[rows, pv, cc, :], uz[rows, cc, tsl], True, True, r=["Bre", ("uz", cc, n)], w=[("ps", b1)])
                    b2 = self.bank()
                    self.mm(self.psb(b2), Bim[rows, pv, cc, :], uz[rows, cc, tsl], True, True, r=["Bim", ("uz", cc, n)], w=[("ps", b2)])
                    self.tt(vre, Ec, self.psb(b1), ALU.mult, r=[ke, ks, ("ps", b1)], w=[kvr])
                    self.tt(tmp1, Es, self.psb(b2), ALU.mult, r=[ks, ("ps", b2)], w=["tmp1"])
                    self.tt(vre, vre, tmp1, ALU.add, r=[kvr, "tmp1"], w=[kvr])
                    self.tt(vim, Ec, self.psb(b2), ALU.mult, r=[ke, ("ps", b2)], w=[kvi])
                    self.tt(tmp2, Es, self.psb(b1), ALU.mult, r=[ks, ("ps", b1)], w=["tmp2"])
                    self.tt(vim, vim, tmp2, ALU.subtract, r=[kvi, "tmp2"], w=[kvi])
                    if n == 0:
                        dve(lambda e, j=j, vre=vre: e.tensor_tensor_scan(out=vre, data0=mag[:, j:j + 1].to_broadcast([128, TB]), data1=vre,
                                                                        initial=0.0, op0=ALU.mult, op1=ALU.add), r=[kvr] + zs["keys"], w=[kvr])
                        dve(lambda e, j=j, vim=vim: e.tensor_tensor_scan(out=vim, data0=mag[:, j:j + 1].to_broadcast([128, TB]), data1=vim,
                                                                        initial=0.0, op0=ALU.mult, op1=ALU.add), r=[kvi] + zs["keys"], w=[kvi])
                    else:
                        dve(lambda e, j=j, vre=vre: e.tensor_tensor_scan(out=vre, data0=mag[:, j:j + 1].to_broadcast([128, TB]), data1=vre,
                                                                        initial=car[:, 0:1], op0=ALU.mult, op1=ALU.add), r=[kvr, "car"] + zs["keys"], w=[kvr])
                        dve(lambda e, j=j, vim=vim: e.tensor_tensor_scan(out=vim, data0=mag[:, j:j + 1].to_broadcast([128, TB]), data1=vim,
                                                                        initial=car[:, 1:2], op0=ALU.mult, op1=ALU.add), r=[kvi, "car"] + zs["keys"], w=[kvi])
                    if n < NTB - 1:
                        wr_l, wi_l = vre[:, TB - 1:TB], vim[:, TB - 1:TB]
                        self.ts(car[:, 2:3], wr_l, cl, None, ALU.mult, ALU.bypass, r=[kvr, ke], w=["car2"])
                        self.ts(car[:, 3:4], wr_l, sl_, None, ALU.mult, ALU.bypass, r=[kvr, ks], w=["car3"])
                        self.stt(car[:, 0:1], wi_l, nsl, car[:, 2:3], ALU.mult, ALU.add, r=[kvi, "nsl", "car2"], w=["car"])
                        self.stt(car[:, 1:2], wi_l, cl, car[:, 3:4], ALU.mult, ALU.add, r=[kvi, ke, "car3", "car"], w=["car"])
                    def ptt(out, in0, in1, op, r, w):
                        P.op("pool", lambda e: e.tensor_tensor(out=out, in0=in0, in1=in1, op=op), r=r, w=w)
                    yb = ybanks[n]
                    ptt(pt4[0], Ec, vre, ALU.mult, [ke, kvr], ["pt0"])
                    ptt(pt4[1], Es, vim, ALU.mult, [ks, kvi], ["pt1"])
                    if pending is not None:
                        pending()
                        pending = None
                    ptt(pt4[2], Es, vre, ALU.mult, [ks, kvr], ["pt2"])
                    ptt(pt4[3], Ec, vim, ALU.mult, [ke, kvi], ["pt3"])
                    ptt(xre[:, tsl], pt4[0], pt4[1], ALU.subtract, ["pt0", "pt1"], [("xre", n)])
                    ptt(pt4[2], pt4[2], pt4[3], ALU.add, ["pt2", "pt3"], ["pt2"])

                    def ymm(j=j, jj=jj, cc=cc, n=n, tsl=tsl, yb=yb):
                        self.mm(self.psb(yb), Cre[:, j, :], xre[:, tsl], jj == 0, False, r=[skC, ("xre", n)], w=[("ps", yb)])
                        self.mm(self.psb(yb), Cim[:, j, :], xim[:, tsl], False, False, r=[skC, ("xim", n)], w=[("ps", yb)])
                        if jj == 3:
                            self.mm(self.psb(yb), Dg[:, cc, :], uz[:, cc, tsl], False, True, r=[skD, ("uz", cc, n)], w=[("ps", yb)])

                    def fin(n=n, tsl=tsl):
                        xo = xim[:, tsl]
                        P.op("pool", lambda e: e.tensor_scalar(out=xo, in0=pt4[2], scalar1=-1.0, scalar2=0.0, op0=ALU.mult, op1=ALU.add),
                             r=["pt2"], w=[("xim", n)])
                    pending = fin
                    ydefer.append(ymm)
                    while len(ydefer) > 3:
                        ydefer.pop(0)()
                pending()
                pending = None
            while ydefer:
                ydefer.pop(0)()
            for n in range(NTB):
                tsl = slice(n * TB, (n + 1) * TB)
                yb = ybanks[n]
                self.act(gt, self.psb(yb), AF.Square, r=[("ps", yb)], w=["gt"])
                self.ts(gt, gt, 0.044715, 1.0, ALU.mult, ALU.add, r=["gt"], w=["gt"])
                self.tt(gt, gt, self.psb(yb), ALU.mult, r=["gt", ("ps", yb)], w=["gt"])
                self.act(t3, gt, AF.Sigmoid, r=["gt"], w=["t3"], scale=1.5957691216057308)
                self.tt(uz[:, cc, tsl], t3, self.psb(yb), ALU.mult, r=["t3", ("ps", yb)], w=[("uz", cc, n)])
        self.tap("zT", uz[:, :, :], [("uz", cc, n) for cc in range(4) for n in range(NTB)], [4, S])
        self.bankset = list(range(8))
        for c in range(KC):
            segl, skl = self.wload(l, "glu%d" % c, 1024)
            segl = segl.rearrange("p (a k m) -> p a k m", a=2, k=4)
            segg, skg = self.wload(l, "g2_%d" % c, 1024)
            segg = segg.rearrange("p (k m) -> p k m", k=KC)
            for n in range(NTB):
                tsl = slice(n * TB, (n + 1) * TB)
                ba = self.bank()
                for cc in range(4):
                    self.mm(self.psb(ba), segl[:, 0, cc, :], uz[:, cc, tsl], cc == 0, cc == 3, r=[skl, ("uz", cc, n)], w=[("ps", ba)])
                bb = self.bank()
                for cc in range(4):
                    self.mm(self.psb(bb), segl[:, 1, cc, :], uz[:, cc, tsl], cc == 0, cc == 3, r=[skl, ("uz", cc, n)], w=[("ps", bb)])
                bg = self.bank()
                for kc in range(KC):
                    self.mm(self.psb(bg), segg[:, kc, :], hT[:, kc, tsl], kc == 0, kc == KC - 1, r=[skg, ("hT", kc, n)], w=[("ps", bg)])
                self.act(s1, self.psb(bb), AF.Sigmoid, r=[("ps", bb)], w=["s1"])
                self.act(s2, self.psb(bg), AF.Sigmoid, r=[("ps", bg)], w=["s2"])
                self.tt(t3, s1, self.psb(ba), ALU.mult, r=["s1", ("ps", ba)], w=["t3"])
                self.tt(t3, t3, s2, ALU.mult, r=["t3", "s2"], w=["t3"])
                self.tt(merged[:, c, tsl], merged[:, c, tsl], t3, ALU.add, r=[("mg", c, n), "t3"], w=[("mg", c, n)])

    def phase_attn(self, l, hT, merged, kaugT, kiT, vaug, wi_s, ident, sel4, biasT, cmask, ones64, b31d):
        P = self.P
        K = 1024
        ps = self.ps
        qT = self.arena[0:65, 0: 16 * TB].rearrange("p (h q) -> p h q", h=16)
        qiT = self.arena[0:64, 8 * K: 8 * K + 8 * TB].rearrange("p (h q) -> p h q", h=8)
        OTn = self.arena[0:64, 12 * K: 12 * K + 16 * TB].rearrange("p (h q) -> p h q", h=16)
        score2 = [self.carve(40 * K, 128, [1, S], F32)[:, 0, :], self.pf_t[:, PF_LRX:PF_LRX + S]]
        rl = [self.carve(48 * K, 128, [1, TB], F32)[:, 0, :], self.carve(50 * K, 128, [1, TB], F32)[:, 0, :]]
        nm3 = [self.carve((52 + 4 * i) * K, 128, [1, S], BF16)[:, 0, :] for i in range(3)]
        self.idx_i = 0
        PT = [self.carve(64 * K, 128, [1, 1024], BF16)[:, 0, :], self.carve(66 * K, 128, [1, 1024], BF16)[:, 0, :]]
        ot = self.carve(68 * K, 64, [1, 1024], F32)[:, 0, :]
        aotmp = self.carve(60 * K, 128, [1, TB], F32)[:, 0, :]
        bis = self.carve(72 * K, 128, [1, 16], F32)[:, 0, :]
        sgt = self.carve(73 * K, 128, [1, TB], BF16)[:, 0, :]
        lnr = self.arena[64:65, 12 * K: 12 * K + 2048].bitcast(F32)
        rrow = self.arena[64:65, 14 * K: 14 * K + 1024]
        P.op("pool", lambda e: e.dma_start(out=self.arena[64:65, 0:16 * TB], in_=b31d, max_dma_last_dim=8192), w=["qT64"], slot="b31")

        def emit_ao(nn):
            tsl = slice(nn * TB, (nn + 1) * TB)
            self.bankset = [0, 1, 2, 3, 4, 5]
            for c in range(KC):
                sego, sko = self.wload(l, "ao%d" % c, 2048)
                sego = sego.rearrange("p (h m) -> p h m", h=16)
                segg, skg = self.wload(l, "g1_%d" % c, 1024)
                segg = segg.rearrange("p (k m) -> p k m", k=KC)
                by = self.bank()
                for h in range(16):
                    self.mm(self.psb(by), sego[0:64, h, :], OTn[:, h, :], h == 0, h == 15, r=[sko] + [("OTn", i) for i in range(4)], w=[("ps", by)])
                bg = self.bank()
                for kc in range(KC):
                    self.mm(self.psb(bg), segg[:, kc, :], hT[:, kc, tsl], kc == 0, kc == KC - 1, r=[skg, ("hT", kc, nn)], w=[("ps", bg)])
                self.act(sgt, self.psb(bg), AF.Sigmoid, r=[("ps", bg)], w=["sgt"])
                t3 = aotmp
                self.tt(t3, sgt, self.psb(by), ALU.mult, r=["sgt", ("ps", by)], w=["aotmp"])
                self.tt(merged[:, c, tsl], merged[:, c, tsl], t3, ALU.add, r=[("mg", c, nn), "aotmp"], w=[("mg", c, nn)])
                yield

        for n in range(NTB):
            tsl = slice(n * TB, (n + 1) * TB)
            self.bankset = list(range(8))
            for hp in range(8):
                seg, sk = self.wload(l, "q%d" % hp, 1024)
                seg = seg.rearrange("p (k m) -> p k m", k=KC)
                for half in range(2):
                    h = 2 * hp + half
                    b = self.bank()
                    for kc in range(KC):
                        self.mm(self.psb(b, 64), seg[:, kc, half * 64:(half + 1) * 64], hT[:, kc, tsl], kc == 0, kc == KC - 1,
                                r=[sk, ("hT", kc, n)], w=[("ps", b)])
                    self.act(qT[0:64, h, :], self.psb(b, 64), AF.Copy, r=[("ps", b)], w=[("qT", h)], scale=0.125)
            for hp in range(4):
                seg, sk = self.wload(l, "qi%d" % hp, 1024)
                seg = seg.rearrange("p (k m) -> p k m", k=KC)
                for half in range(2):
                    h = 2 * hp + half
                    b = self.bank()
                    for kc in range(KC):
                        self.mm(self.psb(b, 64), seg[:, kc, half * 64:(half + 1) * 64], hT[:, kc, tsl], kc == 0, kc == KC - 1,
                                r=[sk, ("hT", kc, n)], w=[("ps", b)])
                    self.act(qiT[0:64, h, :], self.psb(b, 64), AF.Copy, r=[("ps", b)], w=[("qiT", h)])
            if n == 0:
                self.tap("qT", qT[:, :, :], [("qT", h) for h in range(16)] + ["qT64"], [16, TB], parts=65)

            def genA(qq):
                qb = 4 * n + qq
                qsl = slice(qq * 128, (qq + 1) * 128)
                L = (qb + 1) * 128
                sc = score2[qb % 2]
                ngr = (L + 511) // 512
                for kg in range(ngr):
                    k0 = kg * 512
                    nk = min(512, L - k0)
                    sk_ = ("sc", qb % 2, kg)
                    for h in range(8):
                        b = 6 + (self.idx_i % 2)
                        r_ = rl[self.idx_i % 2]
                        rk = ("rl", self.idx_i % 2)
                        self.idx_i += 1
                        self.mm(self.psb(b, 128, nk), qiT[0:64, h, qsl], kiT[0:64, k0:k0 + nk], True, True,
                                r=[("qiT", h)] + [("kiT", i) for i in range(NTB)], w=[("ps", b)])
                        self.act(r_[:, 0:nk], self.psb(b, 128, nk), AF.Relu, r=[("ps", b)], w=[rk])
                        wcol = wi_s[:, qb, h:h + 1]
                        wk = [("wi", qb)]
                        if h == 0:
                            ndiag = nk - 128 if (k0 + nk == L) else nk
                            if ndiag > 0:
                                self.ts(sc[:, k0:k0 + ndiag], r_[:, 0:ndiag], wcol, None, ALU.mult, ALU.bypass, r=[rk] + wk, w=[sk_])
                            if k0 + nk == L:
                                self.stt(sc[:, L - 128:L], r_[:, nk - 128:nk], wcol, cmask, ALU.mult, ALU.add, r=[rk, "cf"] + wk, w=[sk_])
                        else:
                            self.stt(sc[:, k0:k0 + nk], r_[:, 0:nk], wcol, sc[:, k0:k0 + nk], ALU.mult, ALU.add,
                                     r=[rk, sk_] + wk, w=[sk_])
                        yield

            def genB(qq):
                qb = 4 * n + qq
                L = (qb + 1) * 128
                sc = score2[qb % 2]
                nmb = nm3[qb % 2]
                nmk = ("nm", qb % 2)
                ngr = (L + 511) // 512
                sck = [("sc", qb % 2, kg) for kg in range(ngr)]
                if qb >= 2:
                    o = 8 * (qb % 2)
                    cA, cB, cnt, tmpb, thr = bis[:, o:o + 1], bis[:, o + 1:o + 2], bis[:, o + 2:o + 3], bis[:, o + 3:o + 4], bis[:, o + 4:o + 5]
                    kp = "b%d" % (qb % 2)
                    P.op("dve", lambda e, cA=cA: e.memset(cA, 0.0), w=[kp + "c0"])
                    cur, nxt = cA, cB
                    curk, nxtk = kp + "c0", kp + "c1"
                    step = 4.0
                    for it in range(NBIS):
                        self.ts(nmb[:, 0:L], sc[:, 0:L], cur, None, ALU.is_ge, ALU.add, r=sck + [curk], w=[nmk, kp + "cnt"], accum_out=cnt)
                        self.ts(tmpb, cnt, 256.0, 2.0 * step, ALU.is_ge, ALU.mult, r=[kp + "cnt"], w=[kp + "tmpb"])
                        self.ts(nxt, tmpb, -step, cur, ALU.add, ALU.add, r=[kp + "tmpb", curk], w=[nxtk])
                        cur, nxt = nxt, cur
                        curk, nxtk = nxtk, curk
                        step *= 0.5
                        yield
                    self.ts(thr, cur, -2.0 * step - 1e-5, None, ALU.add, ALU.bypass, r=[curk], w=[kp + "thr"])
                    self.ts(nmb[:, 0:L], sc[:, 0:L], thr, NEG, ALU.is_lt, ALU.mult, r=sck + [kp + "thr"], w=[nmk])
                else:
                    self.ts(nmb[:, 0:L], sc[:, 0:L], -16.0, NEG, ALU.is_lt, ALU.mult, r=sck, w=[nmk])
                if qb == 5:
                    self.tap("score5", sc[:, 0:L], sck, [L])
                    self.tap("nm5", nmb[:, 0:L], [nmk], [L])
                yield

            def n_units_A(qq):
                qb = 4 * n + qq
                return 8 * (((qb + 1) * 128 + 511) // 512)

            def step_gen(g):
                if g is None:
                    return None
                try:
                    next(g)
                    return g
                except StopIteration:
                    return None

            def drain(g):
                while g is not None:
                    g = step_gen(g)

            def emit_attn(qq, gB, nB, gA, nA):
                qb = 4 * n + qq
                qsl = slice(qq * 128, (qq + 1) * 128)
                nmb = nm3[qb % 2]
                nmk = ("nm", qb % 2)
                niter = 2 * (qb + 1)
                perB = -(-nB // niter)
                perA = -(-nA // niter)
                for hh in range(2):
                    def emit_S(kb):
                        sb = kb % 2
                        near = (qb - kb) < 2
                        kk = 64 if near else 65
                        ksl = slice(kb * 128, (kb + 1) * 128)
                        for bk in range(2):
                            bnk = 2 * sb + bk
                            hs = slice(hh * 8 + bk * 4, hh * 8 + bk * 4 + 4)
                            outp = self.psb(bnk).rearrange("p (h q) -> p h q", h=4)
                            qk = [("qT", h) for h in range(hh * 8 + bk * 4, hh * 8 + bk * 4 + 4)] + ["qT64"]
                            self.mm(outp, kaugT[0:kk, ksl], qT[0:kk, hs, qsl], True, False,
                                    r=qk + [("kT", kb // 4), "kaug1"], w=[("ps", bnk)])
                            self.mm(outp, nmb[:, ksl], sel4, False, not near, r=[nmk, "cb"], w=[("ps", bnk)])
                            if near:
                                self.mm(outp, ident, biasT[:, qb - kb, hs, :], False, True, r=["cb"], w=[("ps", bnk)])
                        self.act(PT[sb][:, :], ps[:, 2 * sb * 512: 2 * sb * 512 + 1024], AF.Exp,
                                 r=[("ps", 2 * sb), ("ps", 2 * sb + 1)], w=[("PT", sb)])

                    def emit_PV(kb):
                        sb = kb % 2
                        for bk in range(2):
                            self.mm(self.psb(4 + bk, 65), vaug[:, kb, 0:65], PT[sb][:, bk * 512:(bk + 1) * 512], kb == 0, kb == qb,
                                    r=[("PT", sb), ("vaug", kb), "vaug1"], w=[("ps", 4 + bk)])

                    for kb in range(qb + 1):
                        emit_S(kb)
                        if kb > 0:
                            emit_PV(kb - 1)
                        for _ in range(perB):
                            gB = step_gen(gB)
                        for _ in range(perA):
                            gA = step_gen(gA)
                    emit_PV(qb)
                    if hh == 1:
                        drain(gB)
                        drain(gA)
                        gB = gA = None
                    self.act(lnr, ps[64:65, 2048:3072], AF.Ln, r=[("ps", 4), ("ps", 5)], w=["lnr"])
                    self.act(rrow, lnr, AF.Exp, r=["lnr"], w=["rrow"], scale=-1.0)
                    for bk in range(2):
                        self.mm(self.psb(6 + bk, 64), self.ones_bf[64:65, 0:64], rrow[:, bk * 512:(bk + 1) * 512], True, True,
                                r=["ones_bf", "rrow"], w=[("ps", 6 + bk)])
                    self.act(ot[:, :], ps[0:64, 2048:3072], AF.Copy, r=[("ps", 4), ("ps", 5)], w=["ot"])
                    for bk in range(2):
                        hs = slice(hh * 8 + bk * 4, hh * 8 + bk * 4 + 4)
                        self.tt(OTn[:, hs, qsl], ot[:, bk * 512:(bk + 1) * 512].rearrange("p (h q) -> p h q", h=4),
                                self.psb(6 + bk, 64).rearrange("p (h q) -> p h q", h=4), ALU.mult,
                                r=["ot", ("ps", 6 + bk)], w=[("OTn", hh * 2 + bk)])

            drain(genA(0))
            gB0, gA1 = genB(0), genA(1)
            gAO = emit_ao(n - 1) if n > 0 else None
            rnd = 0
            while gB0 is not None or gA1 is not None or gAO is not None:
                gB0 = step_gen(gB0)
                gA1 = step_gen(step_gen(gA1))
                if rnd % 2 == 1:
                    gAO = step_gen(gAO)
                rnd += 1
            for qq in range(4):
                gB = genB(qq + 1) if qq + 1 < 4 else None
                gA = genA(qq + 2) if qq + 2 < 4 else None
                emit_attn(qq, gB, NBIS + 1, gA, n_units_A(qq + 2) if qq + 2 < 4 else 0)
            if n == 0:
                self.tap("OTn", OTn[:, :, :], [("OTn", i) for i in range(4)], [16, TB], parts=64)
        for _ in emit_ao(NTB - 1):
            pass


def km(w):
    k, m = w.shape
    return np.ascontiguousarray(w.reshape(k // 128, 128, m).transpose(1, 0, 2)).reshape(128, (k // 128) * m)


def host_segments(inp, l):
    w = inp["w_in"][l]
    segs = {}
    segs["kk"] = km(np.concatenate([w[:, C_K:C_K + 64], w[:, C_KI:C_KI + 64]], axis=1))
    segs["vw"] = km(np.concatenate([w[:, C_V:C_V + 64], w[:, C_WI:C_WI + 8]], axis=1))
    for cc in range(4):
        segs["pu%d" % cc] = km(w[:, C_POOL + cc * 128: C_POOL + (cc + 1) * 128])
        segs["su%d" % cc] = km(w[:, C_S5 + cc * 128: C_S5 + (cc + 1) * 128])
    segs["mix"] = np.ascontiguousarray(inp["pool_mix_w"][l].transpose(1, 0, 2)).reshape(128, 512)
    for c in range(8):
        cs = slice(c * 128, (c + 1) * 128)
        segs["po%d" % c] = km(inp["pool_out_w"][l][:, cs])
        for b in range(3):
            segs["g%d_%d" % (b, c)] = km(w[:, C_G + b * 1024 + c * 128: C_G + b * 1024 + (c + 1) * 128])
        glu = inp["s5_glu_w"][l]
        segs["glu%d" % c] = np.concatenate([km(glu[:, cs]), km(glu[:, 1024 + c * 128: 1024 + (c + 1) * 128])], axis=1)
        segs["q%d" % c] = km(w[:, C_Q + c * 128: C_Q + (c + 1) * 128])
        ao = inp["attn_out_w"][l][:, cs].reshape(16, 64, 128).transpose(1, 0, 2).reshape(64, 2048)
        segs["ao%d" % c] = np.concatenate([ao, np.zeros((64, 2048), np.float32)], axis=0)
        segs["wo%d" % c] = km(inp["w_out"][l][:, cs])
        segs["fo%d" % c] = km(inp["ffn_w_out"][l][:, cs])
    for hp in range(4):
        segs["qi%d" % hp] = km(w[:, C_QI + hp * 128: C_QI + (hp + 1) * 128])
    fw = inp["ffn_w_in"][l]
    for j in range(NJ):
        segs["f%d" % j] = km(np.concatenate([fw[:, j * 128:(j + 1) * 128], fw[:, FH + j * 128: FH + (j + 1) * 128]], axis=1))
    cre = inp["s5_c_re"][l]
    cim = inp["s5_c_im"][l]
    Cre = np.zeros((128, 16, 128), np.float32)
    Cim = np.zeros((128, 16, 128), np.float32)
    for g in range(32):
        j, g2 = g // 2, g % 2
        jj = j % 4
        m0 = 32 * jj + 16 * g2
        Cre[g2 * 64:(g2 + 1) * 64, j, m0:m0 + 16] = cre[g].T
        Cim[g2 * 64:(g2 + 1) * 64, j, m0:m0 + 16] = cim[g].T
    segs["sC"] = np.concatenate([Cre.reshape(128, 2048), Cim.reshape(128, 2048)], axis=1)
    Dg = np.zeros((128, 4, 128), np.float32)
    d = inp["s5_d"][l]
    for cc in range(4):
        Dg[np.arange(128), cc, np.arange(128)] = d[cc * 128:(cc + 1) * 128]
    segs["sD"] = Dg.reshape(128, 512)
    return segs


def host_pf(inp, l):
    pf = np.zeros((128, NPF), np.float32)
    for off, name in ((PF_GMP, "norm_mix_pre"), (PF_GMO, "norm_mix_post"), (PF_GFP, "norm_ffn_pre"), (PF_GFO, "norm_ffn_post")):
        pf[:, off:off + 8] = inp[name][l].reshape(8, 128).T
    pf[:, PF_PSC:PF_PSC + 4] = inp["pool_scale"][l].reshape(4, 128).T
    lr, li, ld = inp["s5_lambda_re"][l], inp["s5_lambda_im"][l], inp["s5_log_dt"][l]
    for j in range(16):
        for g2 in range(2):
            g = 2 * j + g2
            pf[g2 * 64:(g2 + 1) * 64, PF_LRS + j] = lr[g]
            pf[g2 * 64:(g2 + 1) * 64, PF_LIS + j] = li[g]
            pf[g2 * 64:(g2 + 1) * 64, PF_LDS + j] = ld[g]
    br, bi = inp["s5_b_re"][l], inp["s5_b_im"][l]
    X = np.zeros((5, 128, 4, 128), np.float32)
    for cc in range(4):
        for p in range(128):
            g = cc * 8 + p // 16
            i = p % 16
            X[0, p, cc, :] = np.tile(lr[g], 2)
            X[1, p, cc, :] = np.tile(li[g], 2)
            X[2, p, cc, :] = ld[g]
            g2 = g % 2
            X[3, p, cc, g2 * 64:(g2 + 1) * 64] = br[g, :, i]
            X[4, p, cc, g2 * 64:(g2 + 1) * 64] = bi[g, :, i]
    for k, off in enumerate((PF_LRX, PF_LIX, PF_LDX, PF_BRX, PF_BIX)):
        pf[:, off:off + 512] = X[k].reshape(128, 512)
    return pf


def host_consts(inp):
    cf = np.zeros((128, NCF), np.float32)
    q = np.arange(128)[:, None]
    s = np.arange(128)[None, :]
    cf[:, CF_CMASK:CF_CMASK + 128] = np.where(s > q, np.float32(-1e4), np.float32(0.0))
    cf[:, CF_INVC:CF_INVC + 16] = (1.0 / np.arange(1, 17, dtype=np.float32))[None, :]
    cf[:, CF_ONES:CF_ONES + 64] = 1.0
    par = ((np.arange(128) // 32) % 2).astype(np.float32)
    cf[:, CF_PAR] = 1.0 - par
    cf[:, CF_PAR + 1] = par
    cb = np.zeros((128, NCB), np.float32)
    cb[:, CB_ID:CB_ID + 128] = np.eye(128, dtype=np.float32)
    cb[:, CB_SEL:CB_SEL + 512] = np.tile(np.eye(128, dtype=np.float32), (1, 4))
    rb = inp["rel_bias"]
    sl = np.arange(128)[:, None]
    ql = np.arange(128)[None, :]
    bt = np.zeros((128, 2, 16, 128), np.float32)
    for kind in range(2):
        dist = ql - sl + 128 * kind
        idx = rel_bucket_np(np.maximum(dist, 0))
        bt[:, kind, :, :] = rb[idx].transpose(0, 2, 1)
    cb[:, CB_BIAS:CB_BIAS + 4096] = bt.reshape(128, 4096)
    b31 = np.ascontiguousarray(np.repeat(rb[31][:, None], TB, axis=1)).reshape(1, 16 * TB).astype(np.float32)
    return cf, cb, b31


_CACHE = {}


def get_program(nlayers=NL, stop=None, taps=()):
    key = (nlayers, stop, tuple(taps))
    if key not in _CACHE:
        b0 = Builder(nlayers, stop, taps)
        b0.wtotal = 1 << 20
        b0.build()
        b = Builder(nlayers, stop, taps)
        b.wtotal = max(b0.seg_total, 16)
        nc = b.build()
        _CACHE[key] = (nc, b)
    return _CACHE[key]


def run(inputs, nlayers=NL, stop=None, taps=()):
    nc, b = get_program(nlayers, stop, taps)
    inp = {k: np.asarray(v, dtype=np.float32) for k, v in inputs.items()}
    ws = np.zeros((NL, 128, b.wtotal), np.float32)
    pfs = np.zeros((NL, 128, NPF), np.float32)
    for l in range(NL):
        segs = host_segments(inp, l)
        for name, (off, n) in b.seg_off.items():
            a = segs[name]
            assert a.shape == (128, n), (name, a.shape, n)
            ws[l, :, off:off + n] = a
        pfs[l] = host_pf(inp, l)
    cf, cb, b31 = host_consts(inp)
    x = inp["x"]
    in_maps = []
    for c in range(8):
        xt = np.ascontiguousarray(x[c].T.reshape(KC, 128, S).transpose(1, 0, 2))
        in_maps.append({"x": xt, "wstream": ws, "pf": pfs, "cf": cf, "cb": cb, "b31": b31})
    res = run_bass_kernel_spmd(nc, in_maps, core_ids=list(range(8)))
    return res, b


def kernel(**inputs):
    res, b = run(inputs)
    outs = []
    for c in range(8):
        o = res.results[c]["out"]
        outs.append(np.ascontiguousarray(o.transpose(1, 0, 2).reshape(D, S).T))
    return np.stack(outs, axis=0).astype(np.float32)
```
